# Optimizing a Trainium2 kernel written in Bass

```python
import math
import jax
import jax.numpy as jnp
from jax import lax
import numpy as np

D_MODEL = 1024
BATCH = 8
SEQ = 4096
DEPTH = 1

CTX_LEN = 256
GRID_W = 64

MIX_W = 2 * D_MODEL
SSD_HEADDIM = 64
SSD_W = 3 * MIX_W // 4
SSD_HEADS = SSD_W // SSD_HEADDIM
SSD_GROUPS = 4
SSD_HPG = SSD_HEADS // SSD_GROUPS
SSD_STATE = 128
SSD_CONV = 5
SSD_CHUNK = 128
CONV_CH = SSD_W + 2 * SSD_GROUPS * SSD_STATE
XBCDT_COLS = CONV_CH + 2 * SSD_HEADS
POOL_W = MIX_W - SSD_W
POOL_WINDOWS = (2, 4, 8, 16)
POOL_GROUPS = len(POOL_WINDOWS)
POOL_GW = POOL_W // POOL_GROUPS
IN_COLS = XBCDT_COLS + SSD_W + POOL_W
N_EXPERTS = 32
TOP_K = 4
D_FF = D_MODEL
SWIGLU_ALPHA = 1.702
SWIGLU_LIMIT = 7.0
MOE_BLOCK = 128
RMS_EPS = 1e-6

kernel_name = "hybrid_ssd_pool_moe_prefix_dit"


def rmsnorm(h, w):
    hf = h.astype(jnp.float32)
    hf = hf * lax.rsqrt(jnp.mean(hf * hf, axis=-1, keepdims=True) + RMS_EPS)
    return (hf * w.astype(jnp.float32)).astype(h.dtype)


def modulate(h, shift, scale):
    return h * (1 + scale) + shift


def dwconv_centred(u, w, b):
    ch = u.shape[-1]
    y = lax.conv_general_dilated(
        u, w[:, None, :].astype(u.dtype), window_strides=(1,),
        padding=[(SSD_CONV // 2, SSD_CONV // 2)],
        dimension_numbers=("NWC", "WIO", "NWC"), feature_group_count=ch)
    return y + b.astype(u.dtype)


def segsum(a):
    t = a.shape[-1]
    cs = jnp.cumsum(a, axis=-1)
    diff = cs[..., :, None] - cs[..., None, :]
    return jnp.where(np.tril(np.ones((t, t), dtype=bool)), diff, -jnp.inf)


def ssd_direction_inputs(xbc, dt_raw, dt_bias, a_log, d):
    dtr = dt_raw[..., d, :]
    if d == 1:
        xbc = xbc[:, ::-1]
        dtr = dtr[:, ::-1]
    b, L, _ = xbc.shape
    gn = SSD_GROUPS * SSD_STATE
    xh = xbc[..., :SSD_W].reshape(b, L, SSD_GROUPS, SSD_HPG, SSD_HEADDIM).astype(jnp.float32)
    bm = xbc[..., SSD_W:SSD_W + gn].reshape(b, L, SSD_GROUPS, SSD_STATE).astype(jnp.float32)
    cm = xbc[..., SSD_W + gn:].reshape(b, L, SSD_GROUPS, SSD_STATE).astype(jnp.float32)
    dt = jax.nn.softplus(dtr.astype(jnp.float32) + dt_bias[d].astype(jnp.float32))
    dt = dt.reshape(b, L, SSD_GROUPS, SSD_HPG)
    log_a = -jnp.exp(a_log[d].astype(jnp.float32)).reshape(SSD_GROUPS, SSD_HPG) * dt
    return xh * dt[..., None], log_a, bm, cm


def ssd_chunked(xs, log_a, bm, cm, init_state):
    b, L, G, E, P = xs.shape
    n = bm.shape[-1]
    nc = L // SSD_CHUNK
    xs = xs.reshape(b, nc, SSD_CHUNK, G, E, P)
    bm = bm.reshape(b, nc, SSD_CHUNK, G, n)
    cm = cm.reshape(b, nc, SSD_CHUNK, G, n)
    a = jnp.moveaxis(log_a.reshape(b, nc, SSD_CHUNK, G, E), 2, -1)
    a_cs = jnp.cumsum(a, axis=-1)
    lmat = jnp.exp(segsum(a))
    cb = jnp.einsum("bclgn,bcsgn->bcgls", cm, bm)
    y_diag = jnp.einsum("bcgls,bcgels,bcsgep->bclgep", cb, lmat, xs)
    decay_states = jnp.exp(a_cs[..., -1:] - a_cs)
    states = jnp.einsum("bclgn,bcgel,bclgep->bcgepn", bm, decay_states, xs)
    states = jnp.concatenate([init_state[:, None], states], axis=1)
    chunk_end = jnp.pad(a_cs[..., -1], ((0, 0), (1, 0), (0, 0), (0, 0)))
    decay_chunk = jnp.exp(segsum(jnp.moveaxis(chunk_end, 1, -1)))
    new_states = jnp.einsum("bgezy,bygepn->bzgepn", decay_chunk, states)
    prev_states, final_state = new_states[:, :-1], new_states[:, -1]
    y_off = jnp.einsum("bclgn,bcgepn,bcgel->bclgep", cm, prev_states, jnp.exp(a_cs))
    return (y_diag + y_off).reshape(b, L, G, E, P), final_state


def ssd_final_state(xbc, dt_raw, dt_bias, a_log, d):
    xs, log_a, bm, _ = ssd_direction_inputs(xbc, dt_raw, dt_bias, a_log, d)
    a_cs = jnp.cumsum(log_a, axis=1)
    w = jnp.exp(a_cs[:, -1:] - a_cs)
    return jnp.einsum("blgn,blge,blgep->bgepn", bm, w, xs)


def ssd_bidir(xbc, dt_raw, dt_bias, a_log, d_skip, init_states):
    b, L, _ = xbc.shape
    xs_f, la_f, bm_f, cm_f = ssd_direction_inputs(xbc, dt_raw, dt_bias, a_log, 0)
    y_f, fin_f = ssd_chunked(xs_f, la_f, bm_f, cm_f, init_states[0])
    xs_b, la_b, bm_b, cm_b = ssd_direction_inputs(xbc, dt_raw, dt_bias, a_log, 1)
    y_b, fin_b = ssd_chunked(xs_b, la_b, bm_b, cm_b, init_states[1])
    x_heads = xbc[..., :SSD_W].reshape(b, L, SSD_HEADS, SSD_HEADDIM).astype(jnp.float32)
    y = (y_f + y_b[:, ::-1]).reshape(b, L, SSD_W) \
        + (x_heads * d_skip.astype(jnp.float32)[:, None]).reshape(b, L, SSD_W)
    return y.astype(xbc.dtype), fin_f, fin_b


def centred_mean_minus_self(u, window):
    wl = u.shape[1]
    cs = jnp.pad(jnp.cumsum(u.astype(jnp.float32), axis=1), ((0, 0), (1, 0), (0, 0)))
    t = np.arange(wl)
    lo = np.clip(t - window // 2, 0, wl)
    hi = np.clip(t + window - window // 2, 0, wl)
    cnt = (hi - lo).astype(np.float32)
    mean = (cs[:, hi] - cs[:, lo]) / cnt[None, :, None]
    return mean.astype(u.dtype) - u


def pool_mixer(u, pool_w, pool_scale):
    n, wl, _ = u.shape
    ug = u.reshape(n, wl, POOL_GROUPS, POOL_GW)
    pooled = jnp.stack([centred_mean_minus_self(ug[:, :, g], w)
                        for g, w in enumerate(POOL_WINDOWS)], axis=2)
    y = jnp.einsum("nwgi,gio->nwgo", pooled, pool_w)
    return y.reshape(n, wl, POOL_W) * pool_scale


def mixer_out(y_ssd, z, y_pool, ssd_norm_w, w_out):
    y_ssd = rmsnorm(y_ssd * jax.nn.silu(z), ssd_norm_w)
    return jnp.concatenate([y_ssd, y_pool], axis=-1) @ w_out


def moe_ffn(h, router_w, router_b, w1, b1, w2, b2):
    shp = h.shape
    xt = h.reshape(-1, shp[-1])
    t = xt.shape[0]
    logits = (xt @ router_w + router_b).astype(jnp.float32)
    top_val, top_idx = lax.top_k(logits, TOP_K)
    gates = jax.nn.softmax(top_val, axis=-1)
    e_flat = top_idx.reshape(-1).astype(jnp.int32)
    tok_flat = jnp.repeat(jnp.arange(t, dtype=jnp.int32), TOP_K)
    g_flat = gates.reshape(-1)
    n_assign = t * TOP_K
    order = jnp.argsort(e_flat)
    e_sorted = e_flat[order]
    counts = jnp.bincount(e_flat, length=N_EXPERTS).astype(jnp.int32)
    padded = (counts + MOE_BLOCK - 1) // MOE_BLOCK * MOE_BLOCK
    pad_end = jnp.cumsum(padded)
    pad_start = pad_end - padded
    grp_start = jnp.cumsum(counts) - counts
    rank = jnp.arange(n_assign, dtype=jnp.int32) - grp_start[e_sorted]
    dest = pad_start[e_sorted] + rank
    n_blocks = -(-n_assign // MOE_BLOCK) + N_EXPERTS
    n_slots = n_blocks * MOE_BLOCK
    slot_tok = jnp.full((n_slots,), t, jnp.int32).at[dest].set(tok_flat[order])
    slot_gate = jnp.zeros((n_slots,), jnp.float32).at[dest].set(g_flat[order])
    block_start = jnp.arange(n_blocks, dtype=jnp.int32) * MOE_BLOCK
    block_exp = jnp.minimum(jnp.searchsorted(pad_end, block_start, side="right"),
                            N_EXPERTS - 1).astype(jnp.int32)
    x_pad = jnp.concatenate([xt, jnp.zeros((1, shp[-1]), xt.dtype)], axis=0)

    def run_block(args):
        toks, e = args
        hb = x_pad[toks] @ w1[e] + b1[e]
        g_part = jnp.minimum(hb[..., ::2], SWIGLU_LIMIT)
        u_part = jnp.clip(hb[..., 1::2], -SWIGLU_LIMIT, SWIGLU_LIMIT)
        act = g_part * jax.nn.sigmoid(SWIGLU_ALPHA * g_part) * (u_part + 1)
        return act @ w2[e] + b2[e]

    y_slots = lax.map(run_block, (slot_tok.reshape(n_blocks, MOE_BLOCK), block_exp))
    y_slots = y_slots.reshape(n_slots, shp[-1]) * slot_gate[:, None].astype(xt.dtype)
    out = jnp.zeros((t + 1, shp[-1]), xt.dtype).at[slot_tok].add(y_slots)[:t]
    return out.reshape(shp)


def setup_inputs(seed: int = 0) -> dict:
    key = jax.random.key(seed)
    ks = jax.random.split(key, 25)
    f32 = jnp.float32

    def nrm(k, shape, s):
        return jax.random.normal(k, shape, f32) * s

    dt0 = jnp.exp(jax.random.uniform(ks[10], (DEPTH, 2, SSD_HEADS), f32,
                                     math.log(1e-3), math.log(1e-1)))
    return {
        "x": nrm(ks[0], (BATCH, SEQ, D_MODEL), 1.0),
        "c": nrm(ks[1], (BATCH, D_MODEL), 1.0),
        "ctx": nrm(ks[2], (BATCH, CTX_LEN, D_MODEL), 1.0),
        "c_ctx": nrm(ks[3], (D_MODEL,), 1.0),
        "w_mod": nrm(ks[4], (DEPTH, D_MODEL, 6 * D_MODEL), 0.5 * D_MODEL ** -0.5),
        "b_mod": nrm(ks[5], (DEPTH, 6 * D_MODEL), 0.01),
        "norm1_w": 1.0 + nrm(ks[6], (DEPTH, D_MODEL), 0.02),
        "norm2_w": 1.0 + nrm(ks[7], (DEPTH, D_MODEL), 0.02),
        "w_in": nrm(ks[8], (DEPTH, D_MODEL, IN_COLS), D_MODEL ** -0.5),
        "conv_w": nrm(ks[9], (DEPTH, SSD_CONV, CONV_CH), SSD_CONV ** -0.5),
        "conv_b": nrm(ks[11], (DEPTH, CONV_CH), 0.01),
        "dt_bias": dt0 + jnp.log(-jnp.expm1(-dt0)),
        "a_log": jnp.log(jax.random.uniform(ks[12], (DEPTH, 2, SSD_HEADS), f32, 1.0, 16.0)),
        "d_skip": 1.0 + nrm(ks[13], (DEPTH, SSD_HEADS), 0.1),
        "ssd_norm_w": 1.0 + nrm(ks[14], (DEPTH, SSD_W), 0.02),
        "pool_w": nrm(ks[15], (DEPTH, POOL_GROUPS, POOL_GW, POOL_GW), POOL_GW ** -0.5),
        "pool_scale": 1.0 + nrm(ks[16], (DEPTH, POOL_W), 0.1),
        "w_out": nrm(ks[17], (DEPTH, MIX_W, D_MODEL), MIX_W ** -0.5),
        "router_w": nrm(ks[18], (DEPTH, D_MODEL, N_EXPERTS), D_MODEL ** -0.5),
        "router_b": nrm(ks[19], (DEPTH, N_EXPERTS), 0.01),
        "w1": nrm(ks[20], (DEPTH, N_EXPERTS, D_MODEL, 2 * D_FF), D_MODEL ** -0.5),
        "b1": nrm(ks[21], (DEPTH, N_EXPERTS, 2 * D_FF), 0.01),
        "w2": nrm(ks[22], (DEPTH, N_EXPERTS, D_FF, D_MODEL), D_FF ** -0.5),
        "b2": nrm(ks[23], (DEPTH, N_EXPERTS, D_MODEL), 0.01),
        "final_norm_w": 1.0 + nrm(ks[24], (D_MODEL,), 0.02),
    }


def reference(x, c, ctx, c_ctx, w_mod, b_mod, norm1_w, norm2_w, w_in, conv_w, conv_b,
              dt_bias, a_log, d_skip, ssd_norm_w, pool_w, pool_scale, w_out,
              router_w, router_b, w1, b1, w2, b2, final_norm_w):
    b, L, _ = x.shape
    rows = L // GRID_W
    zero_state = jnp.zeros((b, SSD_GROUPS, SSD_HPG, SSD_HEADDIM, SSD_STATE), jnp.float32)
    for l in range(DEPTH):
        last = l == DEPTH - 1
        mod = (jax.nn.silu(c) @ w_mod[l] + b_mod[l])[:, None, :]
        mod_c = jax.nn.silu(c_ctx) @ w_mod[l] + b_mod[l]
        sh1, sc1, g1, sh2, sc2, g2 = jnp.split(mod, 6, axis=-1)
        csh1, csc1, cg1, csh2, csc2, cg2 = jnp.split(mod_c, 6, axis=-1)

        hc = modulate(rmsnorm(ctx, norm1_w[l]), csh1, csc1)
        pc = hc @ w_in[l][:, :(XBCDT_COLS if last else IN_COLS)]
        xbc_c = jax.nn.silu(dwconv_centred(pc[..., :CONV_CH], conv_w[l], conv_b[l]))
        dt_c = pc[..., CONV_CH:XBCDT_COLS].reshape(b, -1, 2, SSD_HEADS)
        if last:
            init_states = (ssd_final_state(xbc_c, dt_c, dt_bias[l], a_log[l], 0),
                           ssd_final_state(xbc_c, dt_c, dt_bias[l], a_log[l], 1))
            ctx_next = ctx
        else:
            y_c, fin_cf, fin_cb = ssd_bidir(xbc_c, dt_c, dt_bias[l], a_log[l], d_skip[l],
                                            (zero_state, zero_state))
            init_states = (fin_cf, fin_cb)
            yp_c = pool_mixer(pc[..., XBCDT_COLS + SSD_W:], pool_w[l], pool_scale[l])
            ctx_next = ctx + cg1 * mixer_out(y_c, pc[..., XBCDT_COLS:XBCDT_COLS + SSD_W],
                                             yp_c, ssd_norm_w[l], w_out[l])
            ctx_next = ctx_next + cg2 * moe_ffn(
                modulate(rmsnorm(ctx_next, norm2_w[l]), csh2, csc2),
                router_w[l], router_b[l], w1[l], b1[l], w2[l], b2[l])

        hx = modulate(rmsnorm(x, norm1_w[l]), sh1, sc1)
        p = hx @ w_in[l]
        xbc = jax.nn.silu(dwconv_centred(p[..., :CONV_CH], conv_w[l], conv_b[l]))
        dt_x = p[..., CONV_CH:XBCDT_COLS].reshape(b, L, 2, SSD_HEADS)
        y_x, _, _ = ssd_bidir(xbc, dt_x, dt_bias[l], a_log[l], d_skip[l], init_states)
        u_pool = p[..., XBCDT_COLS + SSD_W:].reshape(b * rows, GRID_W, POOL_W)
        y_pool = pool_mixer(u_pool, pool_w[l], pool_scale[l]).reshape(b, L, POOL_W)
        x = x + g1 * mixer_out(y_x, p[..., XBCDT_COLS:XBCDT_COLS + SSD_W], y_pool,
                               ssd_norm_w[l], w_out[l])
        x = x + g2 * moe_ffn(modulate(rmsnorm(x, norm2_w[l]), sh2, sc2),
                             router_w[l], router_b[l], w1[l], b1[l], w2[l], b2[l])
        ctx = ctx_next
    return rmsnorm(x, final_norm_w)
```

```python
import numpy as np
from contextlib import ExitStack
import concourse.bass as bass
import concourse.mybir as mybir
from concourse.bass_utils import run_bass_kernel_spmd

F32 = mybir.dt.float32; BF16 = mybir.dt.bfloat16; I32 = mybir.dt.int32
AF = mybir.ActivationFunctionType; ALU = mybir.AluOpType; AX = mybir.AxisListType

D = 1024; L = 4096; CTXL = 256; NT = 32; NCH = 34
INC = 4656; CONVCH = 2560; SSDW = 1536
RAWW = 4360
NEG = -30000.0
NE = 32
BS = 256
NBLK = 16384 // BS + NE
NSLOT = NBLK * BS
EPS = 1e-6


class Sched:
    STRICT = True

    def __init__(self, nc, es):
        self.nc = nc; self.es = es
        self.eng = {'pe': nc.tensor, 'act': nc.scalar, 'dve': nc.vector, 'pool': nc.gpsimd, 'sp': nc.sync}
        self.sem = {}; self.cnt = {}
        for e in self.eng:
            self.sem[e] = es.enter_context(nc.semaphore("s_" + e)); self.cnt[e] = 0
        self.seen = {e: {} for e in self.eng}
        self.w = {}; self.r = {}
        self.dsem = {}; self.dcnt = {}; self.free_sems = []; self.nsem = 0; self.bregs = {}

    def _dsem(self, name):
        if name not in self.dsem:
            if self.free_sems:
                h, c = self.free_sems.pop()
                self.dsem[name] = h; self.dcnt[name] = c
            else:
                self.nsem += 1
                self.dsem[name] = self.es.enter_context(self.nc.semaphore("d_%d" % self.nsem)); self.dcnt[name] = 0
        return self.dsem[name]

    def _wait(self, e, tok):
        if tok is None:
            return
        kind, name, val = tok
        if kind == 'e' and name == e and (e == 'pe' or not self.STRICT):
            return
        skey = (kind, name)
        if self.seen[e].get(skey, 0) >= val:
            return
        self.seen[e][skey] = val
        s = self.sem[name] if kind == 'e' else self.dsem[name]
        self.eng[e].wait_ge(s, val)

    def deps(self, e, reads, writes):
        for k in reads:
            self._wait(e, self.w.get(k))
        for k in writes:
            self._wait(e, self.w.get(k))
            for tok in list(self.r.get(k, {}).values()):
                self._wait(e, tok)

    def record(self, tok, reads, writes):
        for k in reads:
            self.r.setdefault(k, {})[(tok[0], tok[1])] = tok
        for k in writes:
            self.w[k] = tok; self.r[k] = {}

    def op(self, e, fn, reads=(), writes=(), inc=True):
        self.deps(e, reads, writes)
        ins = fn()
        tok = ('e', e, self.cnt[e] + 1)
        self.record(tok, reads, writes)
        if inc:
            ins.then_inc(self.sem[e], 1); self.cnt[e] += 1
        return ins

    def dma(self, q, out, in_, reads=(), writes=(), sem=None):
        if sem is None:
            sem = ('L_' + writes[0]) if writes else ('S_' + reads[0])
        s = self._dsem(sem)
        self.deps(q, reads, writes)
        ins = self.eng[q].dma_start(out=out, in_=in_)
        ins.then_inc(s, 16); self.dcnt[sem] += 16
        tok = ('d', sem, self.dcnt[sem])
        self.record(tok, reads, writes)
        return ins

    def idma(self, out, in_, idx, scatter, bound, reads=(), writes=(), sem=None):
        s = self._dsem(sem)
        self.deps('pool', reads, writes)
        if bound not in self.bregs:
            r = self.nc.gpsimd.alloc_register("bc%d" % bound)
            self.nc.gpsimd.reg_mov(r, bound)
            self.bregs[bound] = r
        bound = self.bregs[bound]
        off = bass.IndirectOffsetOnAxis(ap=idx, axis=0)
        if scatter:
            ins = self.nc.gpsimd.indirect_dma_start(out=out, out_offset=off, in_=in_, in_offset=None, bounds_check=bound, oob_is_err=False)
        else:
            ins = self.nc.gpsimd.indirect_dma_start(out=out, out_offset=None, in_=in_, in_offset=off, bounds_check=bound, oob_is_err=False)
        ins.then_inc(s, 16); self.dcnt[sem] += 16
        tok = ('d', sem, self.dcnt[sem])
        self.record(tok, reads, writes)
        return ins

    def regroup(self, sem, keys):
        for k in keys:
            self.w[k] = ('d', sem, self.dcnt[sem])

    def barrier(self):
        for e in self.eng:
            for f in self.eng:
                if f != e and self.cnt[f] > 0:
                    self._wait(e, ('e', f, self.cnt[f]))
            for name in self.dsem:
                if self.dcnt[name] > 0:
                    self._wait(e, ('d', name, self.dcnt[name]))
        for name in list(self.dsem):
            self.free_sems.append((self.dsem[name], self.dcnt[name]))
            for e in self.eng:
                self.seen[e].pop(('d', name), None)
        self.dsem = {}; self.dcnt = {}
        for k in list(self.w):
            if self.w[k][0] == 'd':
                del self.w[k]
        for k in list(self.r):
            for kk in [kk for kk in self.r[k] if kk[0] == 'd']:
                del self.r[k][kk]


class Ring:
    def __init__(self, name, n):
        self.name = name; self.n = n; self.i = -1

    def next(self):
        self.i += 1
        return self.i % self.n


class _Stop(Exception):
    pass


def build(dbg=False, upto='ALL'):
    try:
        return _build(dbg, upto)
    except _Stop as e:
        return e.args[0]


def _build(dbg=False, upto='ALL'):
    nc = bass.Bass("TRN2", target_bir_lowering=False)
    es0 = ExitStack()
    S = Sched(nc, es0)
    V = nc.vector; A = nc.scalar; G = nc.gpsimd; PE = nc.tensor

    def din(name, shape, dt=F32):
        return nc.dram_tensor(name, list(shape), dt, kind="ExternalInput").ap()

    def dscr(name, shape, dt):
        return nc.dram_tensor(name, list(shape), dt, kind=("ExternalOutput" if dbg else "Internal")).ap()

    x_d = din("x", [L, D]); ctx_d = din("ctx", [CTXL, D]); cvec_d = din("cvec", [128, 8, 2])
    wmod_d = din("w_mod", [D, 6 * D]); bmodT_d = din("bmodT", [128, 48]); bmodr_d = din("bmodr", [1, 6 * D])
    n1T_d = din("n1T", [128, 8]); n2T_d = din("n2T", [128, 8]); fnw_d = din("fnw", [1, D])
    win_d = din("w_in", [D, INC]); convw_d = din("convw", [128, 20, 5]); convb_d = din("convb", [128, 20])
    dtb_d = din("dtb", [1, 48]); alog_d = din("alog", [1, 48]); dvec_d = din("dvec", [1, SSDW])
    ynw_d = din("ynw", [128, 12]); poolw_d = din("poolw", [128, 4, 128]); pscale_d = din("pscale", [128, 4])
    wout_d = din("w_out", [2 * D, D]); rw_d = din("rw", [128, 8, NE]); rb_d = din("rb", [1, NE])
    W1G_d = din("W1G", [NE * 128, 8 * D]); W1U_d = din("W1U", [NE * 128, 8 * D]); W2_d = din("W2", [NE * 128, 8 * D])
    B1_d = din("B1", [NE * 128, 16]); b2_d = din("b2", [NE, D]); n2r_d = din("n2r", [1, D])
    cSU_d = din("cSU", [128, 128]); cIota_d = din("cIota", [1, NE]); cJv_d = din("cJv", [1, NBLK]); cPidx_d = din("cPidx", [128, 1])
    cI_d = din("cI", [128, 128]); cU_d = din("cU", [128, 128]); cLo_d = din("cLo", [128, 128])
    cMask_d = din("cMask", [128, 2, 128]); cSel_d = din("cSel", [6, 6, 128]); cAT_d = din("cAT", [128, 4, 128])
    out_d = nc.dram_tensor("out", [L, D], F32, kind="ExternalOutput").ap()
    rawT_d = dscr("rawT", [CONVCH, RAWW], BF16); zt_d = dscr("zt", [L, SSDW], BF16)
    ypT_d = dscr("ypT", [512, L], BF16); yzT_d = dscr("yzT", [SSDW, L], BF16)
    x1_d = dscr("x1", [L, D], F32); h2tok_d = dscr("h2tok", [L, D], BF16)
    Xg_d = nc.dram_tensor("Xg", [NSLOT, D], BF16, kind="Internal").ap(); Y_d = nc.dram_tensor("Y", [NSLOT, D], F32, kind="Internal").ap()

    def stop(tag):
        if upto == tag:
            S.barrier()
            raise _Stop(nc)

    def T(es, name, shape, dt=F32):
        return es.enter_context(nc.sbuf_tensor("t_" + name, list(shape), dt))

    def PS(es, name, shape, dt=F32):
        return es.enter_context(nc.psum_tensor("p_" + name, list(shape), dt))

    def mm(out, lhsT, rhs, start, stop, reads, writes, inc=True, sgc=False):
        return S.op('pe', lambda: PE.matmul(out, lhsT=lhsT, rhs=rhs, start=start, stop=stop, skip_group_check=sgc), reads, writes, inc)

    def tr(out, in_, ident, reads, writes, inc=True):
        return S.op('pe', lambda: PE.transpose(out=out, in_=in_, identity=ident), reads, writes, inc)

    def act(out, in_, func, reads, writes, bias=None, scale=None):
        kw = {}
        if bias is not None:
            kw['bias'] = bias
        if scale is not None:
            kw['scale'] = scale
        return S.op('act', lambda: A.activation(out=out, in_=in_, func=func, **kw), reads, writes)

    def tt(e, out, in0, in1, op, reads, writes):
        eng = V if e == 'dve' else G
        return S.op(e, lambda: eng.tensor_tensor(out=out, in0=in0, in1=in1, op=op), reads, writes)

    def ts(e, out, in0, s1, op0, reads, writes, s2=None, op1=None):
        eng = V if e == 'dve' else G
        if op1 is None:
            return S.op(e, lambda: eng.tensor_scalar(out=out, in0=in0, scalar1=s1, scalar2=None, op0=op0), reads, writes)
        return S.op(e, lambda: eng.tensor_scalar(out=out, in0=in0, scalar1=s1, scalar2=s2, op0=op0, op1=op1), reads, writes)

    def cp(e, out, in_, reads, writes):
        if e == 'act':
            return S.op('act', lambda: A.activation(out=out, in_=in_, func=AF.Copy), reads, writes)
        eng = V if e == 'dve' else G
        return S.op(e, lambda: eng.tensor_copy(out=out, in_=in_), reads, writes)

    P0 = es0
    identf = T(P0, "identf", [128, 128]); identb = T(P0, "identb", [128, 128], BF16)
    g_bc = T(P0, "g_bc", [128, 2, D]); fnw_bc = T(P0, "fnw_bc", [128, D])
    s1 = T(P0, "s1", [128, 8]); sh1 = T(P0, "sh1", [128, 8]); cs1 = T(P0, "cs1", [128, 8]); csh1 = T(P0, "csh1", [128, 8])
    s2 = T(P0, "s2", [128, 8]); sh2 = T(P0, "sh2", [128, 8])
    G_all = T(P0, "G_all", [128, NT, NE]); ssq = T(P0, "ssq", [128, 4, NT])
    modrow_d = nc.dram_tensor("modrow", [2, 128, D], F32, kind="Internal").ap()
    S.dma('sp', identf[:], cI_d[:, :], writes=['identf'], sem='c0')
    S.dma('sp', fnw_bc[:], fnw_d.partition_broadcast(128), writes=['fnw_bc'], sem='c0')
    S.regroup('c0', ['identf', 'fnw_bc'])
    cp('dve', identb[:], identf[:], ['identf'], ['identb'])

    with ExitStack() as es:
        silu_c = T(es, "silu_c", [128, 8, 2]); cbc = T(es, "cbc", [128, 8, 128]); ones = T(es, "ones1", [128, 128])
        wm = T(es, "wm", [128, 8, D]); modT = T(es, "modT", [128, 48, 2]); bmodT = T(es, "bmodT_s", [128, 48])
        bm_bc = T(es, "bm_bc", [128, D]); n1T = T(es, "n1T_s", [128, 8]); n2T = T(es, "n2T_s", [128, 8])
        tmp8 = T(es, "tmp8", [128, 8]); n2r_bc = T(es, "n2r_bc", [128, D])
        s2_bc = T(es, "s2_bc1", [128, D]); sh2_bc = T(es, "sh2_bc1", [128, D])
        S.dma('sp', n2r_bc[:], n2r_d.partition_broadcast(128), writes=['n2r_bc'])
        pm = PS(es, "pm", [128, 512]); pg = [PS(es, "pg0", [128, 512]), PS(es, "pg1", [128, 512])]
        S.dma('sp', silu_c[:], cvec_d[:, :, :], writes=['silu_c'], sem='c1')
        S.dma('sp', bmodT[:], bmodT_d[:, :], writes=['bmodT'], sem='c1')
        S.dma('sp', n1T[:], n1T_d[:, :], writes=['n1T'], sem='c1')
        S.dma('sp', n2T[:], n2T_d[:, :], writes=['n2T'], sem='c1')
        S.regroup('c1', ['silu_c', 'bmodT', 'n1T', 'n2T'])
        act(silu_c[:], silu_c[:], AF.Silu, ['silu_c'], ['silu_c'])
        S.op('dve', lambda: V.memset(ones[:], 1.0), writes=['ones1'])
        for k in range(8):
            ts('dve', cbc[:, k, :], ones[:], silu_c[:, k, 0:1], ALU.mult, ['ones1', 'silu_c'], ['cbc'])
        wm_v = wmod_d.rearrange("(k p) c -> p k c", p=128)
        for s in range(6):
            S.dma('sp', wm[:], wm_v[:, :, s * D:(s + 1) * D], writes=['wm'], sem='wm')
            if s in (2, 3, 4, 5):
                S.dma('sp', bm_bc[:], bmodr_d[:, s * D:(s + 1) * D].partition_broadcast(128), writes=['bm_bc'])
                for half in range(2):
                    hc = slice(half * 512, (half + 1) * 512)
                    for k in range(8):
                        mm(pg[half][:, :], cbc[:, k, :], wm[:, k, hc], k == 0, k == 7,
                           ['cbc', 'wm'], ['pg%d' % half], inc=(k == 7))
                    dst = {2: g_bc[:, 0, hc], 5: g_bc[:, 1, hc], 3: sh2_bc[:, hc], 4: s2_bc[:, hc]}[s]
                    dk = 'g_bc' if s in (2, 5) else 's2_bc'
                    tt('dve', dst, pg[half][:, :], bm_bc[:, hc], ALU.add, ['pg%d' % half, 'bm_bc'], [dk])
                    if s == 4:
                        ts('dve', dst, dst, 1.0, ALU.add, [dk], [dk])
                        tt('dve', dst, dst, n2r_bc[:, hc], ALU.mult, [dk, 'n2r_bc'], [dk])
            if s not in (2, 5):
                for j in range(8):
                    for k in range(8):
                        mm(pm[:, j * 2:j * 2 + 2], wm[:, k, j * 128:(j + 1) * 128], silu_c[:, k, :], k == 0, k == 7,
                           ['wm', 'silu_c'], ['pm'], inc=(k == 7))
                tt('dve', modT[:, s * 8:(s + 1) * 8, :], pm[:, 0:16].rearrange("p (j t) -> p j t", t=2),
                   bmodT[:, s * 8:(s + 1) * 8].unsqueeze(2).to_broadcast([128, 8, 2]), ALU.add, ['pm', 'bmodT'], ['modT'])
        for (dst, nT, sc_lo, col) in ((s1, n1T, 8, 0), (cs1, n1T, 8, 1), (s2, n2T, 32, 0)):
            ts('dve', tmp8[:], modT[:, sc_lo:sc_lo + 8, col], 1.0, ALU.add, ['modT'], ['tmp8'])
            tt('dve', dst[:], tmp8[:], nT[:], ALU.mult, ['tmp8', 'n1T', 'n2T'], ['modv'])
        cp('dve', sh1[:], modT[:, 0:8, 0], ['modT'], ['modv'])
        cp('dve', csh1[:], modT[:, 0:8, 1], ['modT'], ['modv'])
        cp('dve', sh2[:], modT[:, 24:32, 0], ['modT'], ['modv'])
        S.dma('sp', modrow_d[0], s2_bc[:], reads=['s2_bc'])
        S.dma('sp', modrow_d[1], sh2_bc[:], reads=['s2_bc'], sem='S_s2_bc')
        S.barrier()

    if upto == '1':
        es0.close(); return nc
    esAB = ExitStack()
    dtr_all = T(esAB, "dtr_all", [128, NCH, 48])

    def rms_to_hT(es_tiles, src_ap, s_vec, sh_vec, dst3, ps_T, names):
        xt, sq, ss, xn, tmp = es_tiles
        (kx, ksq, kss, kxn, ktmp, kps, kdst) = names
        S.dma('sp', xt, src_ap, writes=[kx], sem=kx)
        act(sq, xt, AF.Square, [kx], [ksq])
        S.op('dve', lambda: V.reduce_sum(out=ss[:, 0:1], in_=sq, axis=AX.X), [ksq], [kss])
        act(ss[:, 1:2], ss[:, 0:1], AF.Ln, [kss], [kss], bias=EPS, scale=1.0 / D)
        act(ss[:, 2:3], ss[:, 1:2], AF.Exp, [kss], [kss], scale=-0.5)
        ts('dve', xn, xt, ss[:, 2:3], ALU.mult, [kx, kss], [kxn])
        for k in range(8):
            tr(ps_T[:, k * 128:(k + 1) * 128], xn[:, k * 128:(k + 1) * 128], identf[:], [kxn, 'identf'], [kps], inc=(k == 7))
        tt('dve', tmp, ps_T[:, :].rearrange("p (k t) -> p k t", t=128), s_vec[:, :].unsqueeze(2).to_broadcast([128, 8, 128]),
           ALU.mult, [kps, 'modv'], [ktmp])
        tt('pool', dst3, tmp, sh_vec[:, :].unsqueeze(2).to_broadcast([128, 8, 128]), ALU.add, [ktmp, 'modv'], [kdst])

    with ExitStack() as es:
        win = T(es, "win", [128, 8, INC], BF16)
        for k in range(8):
            S.dma('pool', win[:, k, :], win_d[k * 128:(k + 1) * 128, :], writes=['win'], sem='win')
        dtb_bc = T(es, "dtb_bc", [128, 48]); AT = T(es, "AT", [128, 4, 128], BF16); ATf = T(es, "ATf", [128, 4, 128])
        poolw = T(es, "poolw", [128, 4, 128], BF16); poolwf = T(es, "poolwf", [128, 4, 128]); pscale = T(es, "pscale", [128, 4])
        zpad = T(es, "zpad", [128, 20, 4], BF16)
        S.dma('sp', dtb_bc[:], dtb_d.partition_broadcast(128), writes=['dtb_bc'], sem='c2')
        S.dma('sp', ATf[:], cAT_d[:, :, :], writes=['ATf'], sem='c2')
        S.dma('sp', poolwf[:], poolw_d[:, :, :], writes=['poolwf'], sem='c2')
        S.dma('sp', pscale[:], pscale_d[:, :], writes=['pscale'], sem='c2')
        S.regroup('c2', ['dtb_bc', 'ATf', 'poolwf', 'pscale'])
        cp('dve', AT[:], ATf[:], ['ATf'], ['AT']); cp('dve', poolw[:], poolwf[:], ['poolwf'], ['poolw'])
        S.op('dve', lambda: V.memset(zpad[:], 0.0), writes=['zpad'])
        zrow = T(es, "zrow", [128, 4, D], BF16)
        S.op('pool', lambda: G.memset(zrow[:], 0.0), writes=['zrow'])
        Xg_v0 = Xg_d.rearrange("(b j p) d -> b p j d", p=128, j=4)
        for blk in range(NSLOT // 512):
            S.dma('act', Xg_v0[blk], zrow[:], reads=['zrow'], sem='xgz')
        raw_v = rawT_d.rearrange("(t p) w -> p t w", p=128)
        S.dma('sp', raw_v[:, :, 0:2], zpad[:, :, 0:2], reads=['zpad'])
        S.dma('sp', raw_v[:, :, 4098:4102], zpad[:, :, 0:4], reads=['zpad'])
        S.dma('sp', raw_v[:, :, 4358:4360], zpad[:, :, 0:2], reads=['zpad'])
        xt_ = [T(es, "xt%d" % i, [128, D]) for i in range(2)]; sq_ = T(es, "sq", [128, D]); ss_ = [T(es, "ss%d" % i, [128, 4]) for i in range(2)]
        xn_ = [T(es, "xn%d" % i, [128, D]) for i in range(2)]; tmpm = T(es, "tmpm", [128, 8, 128])
        hT = [T(es, "hT%d" % i, [128, 8, 512], BF16) for i in range(2)]
        rawblk = [T(es, "rawblk0", [128, 20, 512], BF16)] * 2
        zsb = [T(es, "zsb%d" % i, [128, SSDW], BF16) for i in range(2)]
        usb = [T(es, "usb%d" % i, [128, 512], BF16) for i in range(2)]
        plsb = [T(es, "plsb%d" % i, [128, 4, 128], BF16) for i in range(2)]
        ypsb = [T(es, "ypsb%d" % i, [128, 4, 128], BF16) for i in range(2)]
        pT = PS(es, "pT", [128, 1024]); pfm = [PS(es, "pfm%d" % i, [128, 512]) for i in range(2)]
        ptm = [PS(es, "ptm%d" % i, [128, 512]) for i in range(2)]; ppl = PS(es, "ppl", [128, 512]); pyp = PS(es, "pyp", [128, 512])
        rx = Ring('x', 2); rfm = Ring('fm', 2); rtm = Ring('tm', 2); rz = Ring('z', 2)
        ypT_v = ypT_d.rearrange("(g o) t -> o g t", o=128)
        def RMS_(blk):
            ntile = 4 if blk < 8 else 2
            hs = blk % 2
            for j in range(ntile):
                i = rx.next()
                src = x_d[blk * 512 + j * 128: blk * 512 + (j + 1) * 128, :] if blk < 8 else ctx_d[j * 128:(j + 1) * 128, :]
                rms_to_hT((xt_[i][:], sq_[:], ss_[i], xn_[i][:], tmpm[:]), src,
                          s1 if blk < 8 else cs1, sh1 if blk < 8 else csh1, hT[hs][:, :, j * 128:(j + 1) * 128], pT,
                          ('xt%d' % i, 'sq', 'ss%d' % i, 'xn%d' % i, 'tmpm', 'pT', 'hT%d' % hs))

        RMS_(0)
        for blk in range(9):
            ntile = 4 if blk < 8 else 2
            ntok = ntile * 128
            hs = blk % 2
            if blk + 1 < 9:
                RMS_(blk + 1)
            for t in range(20):
                b = rfm.next()
                for k in range(8):
                    mm(pfm[b][:, 0:ntok], win[:, k, t * 128:(t + 1) * 128], hT[hs][:, k, 0:ntok], k == 0, k == 7,
                       ['win', 'hT%d' % hs], ['pfm%d' % b], inc=(k == 7))
                cp('act' if t % 2 == 0 else 'dve', rawblk[hs][:, t, 0:ntok], pfm[b][:, 0:ntok], ['pfm%d' % b], ['rawblk0'])
            off = 2 + 512 * blk if blk < 8 else 4102
            S.dma('sp', raw_v[:, :, off:off + ntok], rawblk[hs][:, :, 0:ntok], reads=['rawblk0'])
            for j in range(ntile):
                chunk = blk * 4 + j
                lt = hT[hs][:, :, j * 128:(j + 1) * 128]
                b = rtm.next()
                for k in range(8):
                    mm(ptm[b][:, 0:48], lt[:, k, :], win[:, k, CONVCH:CONVCH + 48], k == 0, k == 7, ['win', 'hT%d' % hs], ['ptm%d' % b], inc=(k == 7))
                tt('dve', dtr_all[:, chunk, :], ptm[b][:, 0:48], dtb_bc[:], ALU.add, ['ptm%d' % b, 'dtb_bc'], ['dtr_all'])
                if blk == 8:
                    continue
                zi = rz.next()
                for q in range(3):
                    b = rtm.next()
                    c0 = 2608 + q * 512
                    for k in range(8):
                        mm(ptm[b][:, :], lt[:, k, :], win[:, k, c0:c0 + 512], k == 0, k == 7, ['win', 'hT%d' % hs], ['ptm%d' % b], inc=(k == 7))
                    act(zsb[zi][:, q * 512:(q + 1) * 512], ptm[b][:, :], AF.Silu, ['ptm%d' % b], ['zsb%d' % zi])
                S.dma('sp', zt_d[chunk * 128:(chunk + 1) * 128, :], zsb[zi][:], reads=['zsb%d' % zi])
                b = rtm.next()
                for k in range(8):
                    mm(ptm[b][:, :], lt[:, k, :], win[:, k, 4144:4656], k == 0, k == 7, ['win', 'hT%d' % hs], ['ptm%d' % b], inc=(k == 7))
                cp('act', usb[zi][:], ptm[b][:, :], ['ptm%d' % b], ['usb%d' % zi])
                for g in range(4):
                    mm(ppl[:, g * 128:(g + 1) * 128], usb[zi][:, g * 128:(g + 1) * 128], AT[:, g, :], True, True, ['usb%d' % zi, 'AT'], ['ppl'], inc=(g == 3))
                cp('dve', plsb[zi][:], ppl[:, :].rearrange("p (g t) -> p g t", t=128), ['ppl'], ['plsb%d' % zi])
                for g in range(4):
                    mm(pyp[:, g * 128:(g + 1) * 128], poolw[:, g, :], plsb[zi][:, g, :], True, True, ['poolw', 'plsb%d' % zi], ['pyp'], inc=(g == 3))
                tt('dve', ypsb[zi][:], pyp[:, :].rearrange("p (g t) -> p g t", t=128), pscale[:, :].unsqueeze(2).to_broadcast([128, 4, 128]),
                   ALU.mult, ['pyp', 'pscale'], ['ypsb%d' % zi])
                S.dma('sp', ypT_v[:, :, chunk * 128:(chunk + 1) * 128], ypsb[zi][:], reads=['ypsb%d' % zi])
        S.barrier()

    if upto == 'A':
        esAB.close(); es0.close(); return nc
    with ExitStack() as es:
        convw = T(es, "convw", [128, 20, 5]); convb = T(es, "convb", [128, 20]); Dvec = T(es, "Dvec", [128, SSDW])
        alog_bc = T(es, "alog_bc", [128, 48]); negA = T(es, "negA", [128, 48]); ynw = T(es, "ynw", [128, 12])
        cU = T(es, "cU", [128, 128]); cLo = T(es, "cLo", [128, 128]); ones = T(es, "ones2", [128, 128])
        cMask = T(es, "cMask", [128, 2, 128]); cSel = T(es, "cSel", [6, 6, 128])
        S.dma('sp', convw[:], convw_d[:, :, :], writes=['convw'], sem='c3')
        S.dma('sp', convb[:], convb_d[:, :], writes=['convb'], sem='c3')
        S.dma('sp', Dvec[:], dvec_d.partition_broadcast(128), writes=['Dvec'], sem='c3')
        S.dma('sp', alog_bc[:], alog_d.partition_broadcast(128), writes=['alog'], sem='c3')
        S.dma('sp', ynw[:], ynw_d[:, :], writes=['ynw'], sem='c3')
        S.dma('sp', cU[:], cU_d[:, :], writes=['cU'], sem='c3')
        S.dma('sp', cLo[:], cLo_d[:, :], writes=['cLo'], sem='c3')
        S.dma('sp', cMask[:], cMask_d[:, :, :], writes=['cMask'], sem='c3')
        S.dma('sp', cSel[:], cSel_d[:, :, :], writes=['cSel'], sem='c3')
        S.regroup('c3', ['convw', 'convb', 'Dvec', 'alog', 'ynw', 'cU', 'cLo', 'cMask', 'cSel'])
        S.op('dve', lambda: V.memset(ones[:], 1.0), writes=['ones2'])
        cMaskb = T(es, "cMaskb", [128, 2, 128], BF16); cSelb = T(es, "cSelb", [6, 6, 128], BF16)
        cp('dve', cMaskb[:], cMask[:], ['cMask'], ['cMaskb']); cp('dve', cSelb[:], cSel[:], ['cSel'], ['cSelb'])
        act(negA[:], alog_bc[:], AF.Exp, ['alog'], ['negA'])
        ts('dve', negA[:], negA[:], -1.0, ALU.mult, ['negA'], ['negA'])
        rawg = T(es, "rawg", [128, 5, RAWW], BF16)
        BT = T(es, "BT", [128, NCH * 128], BF16); CT = T(es, "CT", [128, NCH * 128], BF16)
        x_tok = T(es, "x_tok", [128, NCH, 384], BF16); B_tok = T(es, "B_tok", [128, NCH, 128], BF16)
        Sb_all = T(es, "Sb_all", [128, NT, 384], BF16)
        dg = [T(es, "dg%d" % i, [128, 5, 128], BF16) for i in range(2)]
        xc = [T(es, "xc%d" % i, [128, 512], BF16) for i in range(2)]
        dtg = T(es, "dtg", [128, NCH, 12]); av = T(es, "av", [128, NCH, 12]); lndt = T(es, "lndt", [128, NCH, 12])
        cs_all = T(es, "cs_all", [128, NCH, 12]); tot_all = T(es, "tot_all", [128, NCH, 12]); nb = T(es, "nb", [128, NCH, 12])
        eoff = T(es, "eoff", [128, NCH, 12]); wst = T(es, "wst", [128, NCH, 12]); dch = T(es, "dch", [128, NCH, 12])
        Srun = [T(es, "Srun%d" % i, [128, 384]) for i in range(3)]
        Sfb = [T(es, "Sfb%d" % i, [128, 384], BF16) for i in range(2)]
        xd = [T(es, "xd%d" % i, [128, 384], BF16) for i in range(4)]
        CBt = [T(es, "CBt%d" % i, [128, 128], BF16) for i in range(2)]
        csTh = [T(es, "csTh%d" % i, [6, 2, 128], BF16) for i in range(2)]; csTl = [T(es, "csTl%d" % i, [6, 2, 128], BF16) for i in range(2)]
        Lm = [T(es, "Lm%d" % i, [128, 128], BF16) for i in range(8)]
        Mt = [T(es, "Mt%d" % i, [128, 128], BF16) for i in range(8)]
        t1 = [T(es, "t1_%d" % i, [128, 384]) for i in range(2)]; t2 = T(es, "t2", [128, 384]); t3 = T(es, "t3", [128, 384])
        zg = [T(es, "zg%d" % i, [128, 384], BF16) for i in range(2)]; sz = T(es, "sz", [128, 384]); sqj = T(es, "sqj", [128, 384])
        yzb = [T(es, "yzb%d" % i, [128, 384], BF16) for i in range(2)]; yzTs = [T(es, "yzTs%d" % i, [128, 3, 128], BF16) for i in range(2)]
        pf = [PS(es, "pf%d" % i, [128, 512]) for i in range(7)]; pb = PS(es, "pb", [128, 1024], BF16)
        raw_rows = rawT_d.rearrange("(t p) w -> t p w", p=128)
        yzT_v = yzT_d.rearrange("(i p) t -> p i t", p=128)
        rdg = Ring('dg', 2); rcv = Ring('cv', 2); rxc = Ring('xc', 2)
        for g in range(4):
            tiles = [3 * g, 3 * g + 1, 3 * g + 2, 12 + g, 16 + g]
            for ti, Tt in enumerate(tiles):
                S.dma('sp', rawg[:, ti, :], raw_rows[Tt, :, :], writes=['rawg%d' % ti], sem='rawg')
            S.regroup('rawg', ['rawg%d' % ti for ti in range(5)])
            for ti, Tt in enumerate(tiles):
                di = rdg.next()
                for j in range(5):
                    ts('dve', dg[di][:, j, :], identb[:], convw[:, Tt, j:j + 1], ALU.mult, ['identb', 'convw'], ['dg%d' % di])
                for blk in range(9):
                    n = 512 if blk < 8 else 256
                    off = (2 + 512 * blk) if blk < 8 else 4102
                    tok0 = 512 * blk
                    b = rcv.next()
                    for j in range(5):
                        mm(pf[b][:, 0:n], dg[di][:, j, :], rawg[:, ti, off - 2 + j: off - 2 + j + n], j == 0, j == 4,
                           ['dg%d' % di, 'rawg%d' % ti], ['pf%d' % b], inc=(j == 4))
                    if ti < 3:
                        xi = rxc.next()
                        dst = xc[xi][:, 0:n]; dkey = 'xc%d' % xi
                    elif ti == 3:
                        dst = BT[:, tok0:tok0 + n]; dkey = 'BT'
                    else:
                        dst = CT[:, tok0:tok0 + n]; dkey = 'CT'
                    act(dst, pf[b][:, 0:n], AF.Silu, ['pf%d' % b, 'convb'], [dkey], bias=convb[:, Tt:Tt + 1])
                    if ti <= 3:
                        nj = n // 128
                        for jj in range(nj):
                            tr(pb[:, jj * 128:(jj + 1) * 128], dst[:, jj * 128:(jj + 1) * 128], identb[:], [dkey, 'identb'], ['pb'], inc=(jj == nj - 1))
                        src3 = pb[:, 0:n].rearrange("p (j c) -> p j c", c=128)
                        if ti < 3:
                            cp('dve', x_tok[:, 4 * blk:4 * blk + nj, ti * 128:(ti + 1) * 128], src3, ['pb'], ['x_tok'])
                        else:
                            cp('dve', B_tok[:, 4 * blk:4 * blk + nj, :], src3, ['pb'], ['B_tok'])
            S.barrier()
            stop('B1')
            for d in range(2):
                cp('dve', dtg[:, :, d * 6:(d + 1) * 6], dtr_all[:, :, d * 24 + g * 6: d * 24 + g * 6 + 6], ['dtr_all'], ['dtg'])
            act(dtg[:], dtg[:], AF.Exp, ['dtg'], ['dtg'])
            act(dtg[:], dtg[:], AF.Ln, ['dtg'], ['dtg'], bias=1.0, scale=1.0)
            act(lndt[:], dtg[:], AF.Ln, ['dtg'], ['lndt'])
            for d in range(2):
                tt('dve', av[:, :, d * 6:(d + 1) * 6], dtg[:, :, d * 6:(d + 1) * 6],
                   negA[:, d * 24 + g * 6: d * 24 + g * 6 + 6].unsqueeze(1).to_broadcast([128, NCH, 6]), ALU.mult, ['dtg', 'negA'], ['av'])
            for c in range(NCH):
                last = (c == NCH - 1)
                mm(pf[4][:, c * 12:c * 12 + 6], cU[:], av[:, c, 0:6], True, True, ['cU', 'av'], ['pf4'], inc=False)
                mm(pf[4][:, c * 12 + 6:c * 12 + 12], cLo[:], av[:, c, 6:12], True, True, ['cLo', 'av'], ['pf4'], inc=False)
                mm(pf[5][:, c * 12:c * 12 + 12], ones[:], av[:, c, :], True, True, ['ones2', 'av'], ['pf5'], inc=last)
            cp('dve', cs_all[:], pf[4][:, 0:NCH * 12].rearrange("p (c h) -> p c h", h=12), ['pf4'], ['cs_all'])
            cp('dve', tot_all[:], pf[5][:, 0:NCH * 12].rearrange("p (c h) -> p c h", h=12), ['pf5'], ['tot_all'])
            tt('dve', nb[:], lndt[:], cs_all[:], ALU.subtract, ['lndt', 'cs_all'], ['nb'])
            act(eoff[:], cs_all[:], AF.Exp, ['cs_all'], ['eoff'])
            tt('dve', wst[:], tot_all[:], nb[:], ALU.add, ['tot_all', 'nb'], ['wst'])
            act(wst[:], wst[:], AF.Exp, ['wst'], ['wst'])
            act(dch[:], tot_all[:], AF.Exp, ['tot_all'], ['dch'])

            stop('B2')
            rxd = Ring('xd', 4)

            def chunk_state(c, d, bank=6):
                i = rxd.next()
                tt('dve', xd[i][:].rearrange("p (h q) -> p h q", q=64), x_tok[:, c, :].rearrange("p (h q) -> p h q", q=64),
                   wst[:, c, d * 6:(d + 1) * 6].unsqueeze(2).to_broadcast([128, 6, 64]), ALU.mult, ['x_tok', 'wst'], ['xd%d' % i])
                mm(pf[bank][:, 0:384], B_tok[:, c, :], xd[i][:], True, True, ['B_tok', 'xd%d' % i], ['pf%d' % bank])

            def dec_bc(c, d):
                return dch[:, c, d * 6:(d + 1) * 6].unsqueeze(2).to_broadcast([128, 6, 64])

            def v3(ap):
                return ap.rearrange("p (h q) -> p h q", q=64)

            for d, (ca, cb_) in enumerate(((32, 33), (33, 32))):
                chunk_state(ca, d)
                cp('dve', Srun[d][:], pf[6][:, 0:384], ['pf6'], ['Srun%d' % d])
                tt('dve', v3(Srun[d][:]), v3(Srun[d][:]), dec_bc(cb_, d), ALU.mult, ['Srun%d' % d, 'dch'], ['Srun%d' % d])
                chunk_state(cb_, d)
                tt('dve', Srun[d][:], Srun[d][:], pf[6][:, 0:384], ALU.add, ['Srun%d' % d, 'pf6'], ['Srun%d' % d])
            stop('B3')
            order = list(range(NT - 1, -1, -1))
            banks = [3, 4, 5, 6]
            AHEAD = 3
            for n in range(min(AHEAD, NT)):
                chunk_state(order[n], 1, banks[n % 4])
            cur = 1
            for n, c in enumerate(order):
                if n + AHEAD < NT:
                    chunk_state(order[n + AHEAD], 1, banks[(n + AHEAD) % 4])
                nxt = 2 if cur == 1 else 1
                cp('pool', Sb_all[:, c, :], Srun[cur][:], ['Srun%d' % cur], ['Sb_all%d' % c])
                tt('dve', v3(Srun[nxt][:]), v3(Srun[cur][:]), dec_bc(c, 1), ALU.mult, ['Srun%d' % cur, 'dch'], ['Srun%d' % nxt])
                bk = banks[n % 4]
                tt('dve', Srun[nxt][:], Srun[nxt][:], pf[bk][:, 0:384], ALU.add, ['Srun%d' % nxt, 'pf%d' % bk], ['Srun%d' % nxt])
                cur = nxt
            S.barrier()
            stop('B4')
            rL = Ring('L', 2); rq = Ring('q', 8)
            pairs = [(d, h) for d in range(2) for h in range(6)]

            def H_(c, ci):
                tk = slice(c * 128, (c + 1) * 128)
                cp('pool', Sfb[ci][:], Srun[0][:], ['Srun0'], ['Sfb%d' % ci])
                mm(pf[0][:, 0:128], BT[:, tk], CT[:, tk], True, True, ['BT', 'CT'], ['pf0'], inc=False)
                mm(pf[0][0:6, 128:256], av[:, c, 0:6], cU[:], True, True, ['av', 'cU'], ['pf0'], inc=False)
                mm(pf[0][0:6, 256:384], av[:, c, 6:12], cLo[:], True, True, ['av', 'cLo'], ['pf0'])
                cp('dve', CBt[ci][:], pf[0][:, 0:128], ['pf0'], ['CBt%d' % ci])
                src = pf[0][0:6, 128:384].rearrange("p (d l) -> p d l", l=128)
                cp('dve', csTh[ci][:], src, ['pf0'], ['csTh%d' % ci])
                tt('dve', csTl[ci][:], src, csTh[ci][:], ALU.subtract, ['pf0', 'csTh%d' % ci], ['csTl%d' % ci])

            def L_(c, ci, bt):
                lb = 1 + rL.next()
                bk = 'pf%d' % lb
                for q in range(4):
                    d, h = pairs[bt * 4 + q]
                    reg = pf[lb][:, q * 128:(q + 1) * 128]
                    mm(reg, cSelb[0:6, h, :], csTh[ci][0:6, d, :], True, False, ['cSelb', 'csTh%d' % ci], [bk], inc=False)
                    mm(reg, cSelb[0:6, h, :], csTl[ci][0:6, d, :], False, False, ['cSelb', 'csTl%d' % ci], [bk], inc=False)
                    mm(reg, identb[:], cMaskb[:, d, :], False, True, ['identb', 'cMaskb'], [bk], inc=(q == 3))
                return lb

            def E_(c, ci, bt, lb):
                bk = 'pf%d' % lb
                qis = []
                for q in range(4):
                    d, h = pairs[bt * 4 + q]
                    reg = pf[lb][:, q * 128:(q + 1) * 128]
                    qi = rq.next()
                    act(Lm[qi][:], reg, AF.Exp, [bk, 'nb'], ['Lm%d' % qi], bias=nb[:, c, d * 6 + h: d * 6 + h + 1])
                    tt('dve', Mt[qi][:], Lm[qi][:], CBt[ci][:], ALU.mult, ['Lm%d' % qi, 'CBt%d' % ci], ['Mt%d' % qi])
                    qis.append(qi)
                return qis

            def Y_(c, bt, qis):
                for q in range(4):
                    d, h = pairs[bt * 4 + q]
                    qi = qis[q]
                    mm(pf[3][:, h * 64:(h + 1) * 64], Mt[qi][:], x_tok[:, c, h * 64:(h + 1) * 64], (bt == 0 and q == 0), (bt == 2 and q == 3),
                       ['Mt%d' % qi, 'x_tok'], ['pf3'], inc=(q == 3), sgc=True)

            def O_(c, ci):
                tk = slice(c * 128, (c + 1) * 128)
                mm(pf[4][:, 0:384], CT[:, tk], Sfb[ci][:], True, True, ['CT', 'Sfb%d' % ci], ['pf4'])
                mm(pf[5][:, 0:384], CT[:, tk], Sb_all[:, c, :], True, True, ['CT', 'Sb_all%d' % c], ['pf5'])

            def U_(c):
                chunk_state(c, 0)
                tt('dve', v3(Srun[0][:]), v3(Srun[0][:]), dec_bc(c, 0), ALU.mult, ['Srun0', 'dch'], ['Srun0'])
                tt('dve', Srun[0][:], Srun[0][:], pf[6][:, 0:384], ALU.add, ['Srun0', 'pf6'], ['Srun0'])

            def F_(c, ci):
                k1 = 't1_%d' % ci
                S.dma('sp', zg[ci][:], zt_d[c * 128:(c + 1) * 128, g * 384:(g + 1) * 384], writes=['zg%d' % ci], sem='zg%d' % ci)
                tt('dve', v3(t1[ci][:]), v3(pf[4][:, 0:384]), eoff[:, c, 0:6].unsqueeze(2).to_broadcast([128, 6, 64]), ALU.mult, ['pf4', 'eoff'], [k1])
                tt('dve', v3(t2[:]), v3(pf[5][:, 0:384]), eoff[:, c, 6:12].unsqueeze(2).to_broadcast([128, 6, 64]), ALU.mult, ['pf5', 'eoff'], ['t2'])
                tt('dve', t1[ci][:], pf[3][:, 0:384], t1[ci][:], ALU.add, ['pf3', k1], [k1])
                tt('pool', t3[:], x_tok[:, c, :], Dvec[:, g * 384:(g + 1) * 384], ALU.mult, ['x_tok', 'Dvec'], ['t3'])
                tt('pool', t2[:], t2[:], t3[:], ALU.add, ['t2', 't3'], ['t2'])
                tt('pool', t1[ci][:], t1[ci][:], t2[:], ALU.add, [k1, 't2'], [k1])
                tt('dve', t1[ci][:], t1[ci][:], zg[ci][:], ALU.mult, [k1, 'zg%d' % ci], [k1])
                tt('pool', sqj[:], t1[ci][:], t1[ci][:], ALU.mult, [k1], ['sqj'])
                S.op('dve', lambda: V.reduce_sum(out=ssq[:, g, c:c + 1], in_=sqj[:], axis=AX.X), ['sqj'], ['ssq'])
                cp('pool', yzb[ci][:], t1[ci][:], [k1], ['yzb%d' % ci])

            def T_(c, ci):
                tk = slice(c * 128, (c + 1) * 128)
                for i3 in range(3):
                    tr(pb[:, 512 + i3 * 128: 512 + (i3 + 1) * 128], yzb[ci][:, i3 * 128:(i3 + 1) * 128], identb[:], ['yzb%d' % ci, 'identb'], ['pbz'], inc=(i3 == 2))
                tt('dve', yzTs[ci][:], pb[:, 512:896].rearrange("p (i t) -> p i t", t=128),
                   ynw[:, g * 3:(g + 1) * 3].unsqueeze(2).to_broadcast([128, 3, 128]), ALU.mult, ['pbz', 'ynw'], ['yzTs%d' % ci])
                S.dma('sp', yzT_v[:, g * 3:(g + 1) * 3, tk], yzTs[ci][:], reads=['yzTs%d' % ci])

            for c in range(NT):
                ci = c % 2
                H_(c, ci)
                lb0 = L_(c, ci, 0); q0 = E_(c, ci, 0, lb0)
                lb1 = L_(c, ci, 1); q1 = E_(c, ci, 1, lb1)
                Y_(c, 0, q0)
                lb2 = L_(c, ci, 2); q2 = E_(c, ci, 2, lb2)
                Y_(c, 1, q1)
                if c > 0:
                    T_(c - 1, (c - 1) % 2)
                Y_(c, 2, q2)
                O_(c, ci)
                U_(c)
                F_(c, ci)
            T_(NT - 1, (NT - 1) % 2)
            S.barrier()
    esAB.close()
    if upto == 'B':
        es0.close(); return nc

    mask_all = T(P0, "mask_all", [128, NT, NE]); gates_all = T(P0, "gates_all", [128, NT, 4])
    idx_all = T(P0, "idx_all", [128, NT * 4], I32); widx = T(P0, "widx", [128, NBLK], I32)
    esC = ExitStack()
    s2_bc = T(esC, "s2_bc", [128, D]); sh2_bc = T(esC, "sh2_bc", [128, D])
    S.dma('sp', s2_bc[:], modrow_d[0], writes=['s2_bc'], sem='L_s2bc')
    S.dma('sp', sh2_bc[:], modrow_d[1], writes=['s2_bc'], sem='L_s2bc')
    with ExitStack() as es:
        wout = T(es, "wout", [128, 16, D], BF16)
        for k in range(16):
            S.dma('pool', wout[:, k, :], wout_d[k * 128:(k + 1) * 128, :], writes=['wout'], sem='wout')
        rw = T(es, "rw", [128, 8, NE]); rb_bc = T(es, "rb_bc", [128, NE])
        S.dma('sp', rw[:], rw_d[:, :, :], writes=['rw'], sem='c4')
        S.dma('sp', rb_bc[:], rb_d.partition_broadcast(128), writes=['rb_bc'], sem='c4')
        S.regroup('c4', ['rw', 'rb_bc'])
        rs_ssd = T(es, "rs_ssd", [128, NT]); tq = T(es, "tq", [128, NT])
        tt('dve', tq[:], ssq[:, 0, :], ssq[:, 1, :], ALU.add, ['ssq'], ['tq'])
        tt('dve', tq[:], tq[:], ssq[:, 2, :], ALU.add, ['tq', 'ssq'], ['tq'])
        tt('dve', tq[:], tq[:], ssq[:, 3, :], ALU.add, ['tq', 'ssq'], ['tq'])
        act(tq[:], tq[:], AF.Ln, ['tq'], ['tq'], bias=EPS, scale=1.0 / SSDW)
        act(rs_ssd[:], tq[:], AF.Exp, ['tq'], ['rs_ssd'], scale=-0.5)
        yzt = [T(es, "yzt%d" % i, [128, 12, 128], BF16) for i in range(2)]; ypt = [T(es, "ypt%d" % i, [128, 4, 128], BF16) for i in range(2)]
        xt_ = [T(es, "cxt%d" % i, [128, D]) for i in range(2)]; m_ = T(es, "cm", [128, D]); x1s = [T(es, "x1s%d" % i, [128, D]) for i in range(2)]
        sq_ = T(es, "csq", [128, D]); ss_ = [T(es, "css%d" % i, [128, 4]) for i in range(2)]; xn_ = [T(es, "cxn%d" % i, [128, D]) for i in range(2)]
        tmpm = T(es, "ctmpm", [128, 8, 128]); h2f = T(es, "h2f", [128, 8, 128]); htk = T(es, "htk", [128, D]); htb = [T(es, "htb%d" % i, [128, D], BF16) for i in range(2)]
        lg = T(es, "lg", [128, NE]); m8 = T(es, "m8", [128, 8]); ex = T(es, "ex", [128, NE]); sm = T(es, "sm", [128, 4])
        ps_s = [PS(es, "ps_s%d" % i, [128, 512]) for i in range(2)]; ps_p = [PS(es, "ps_p%d" % i, [128, 512]) for i in range(2)]
        pT = PS(es, "pT2", [128, 1024]); pr = PS(es, "pr", [128, 512])
        yzT_v = yzT_d.rearrange("(i p) t -> p i t", p=128); ypT_v = ypT_d.rearrange("(g o) t -> o g t", o=128)
        def CX_(j):
            i = j % 2
            tk = slice(j * 128, (j + 1) * 128)
            S.dma('sp', yzt[i][:], yzT_v[:, :, tk], writes=['yzt%d' % i], sem='yzt%d' % i)
            S.dma('sp', ypt[i][:], ypT_v[:, :, tk], writes=['ypt%d' % i], sem='ypt%d' % i)
            S.dma('sp', xt_[i][:], x_d[tk, :], writes=['cxt%d' % i], sem='cxt%d' % i)
            for half in range(2):
                hc = slice(half * 512, (half + 1) * 512)
                for k in range(12):
                    mm(ps_s[half][:, :], yzt[i][:, k, :], wout[:, k, hc], k == 0, k == 11, ['yzt%d' % i, 'wout'], ['ps_s%d' % half], inc=(k == 11))
                for k in range(4):
                    mm(ps_p[half][:, :], ypt[i][:, k, :], wout[:, 12 + k, hc], k == 0, k == 3, ['ypt%d' % i, 'wout'], ['ps_p%d' % half], inc=(k == 3))
                ts('dve', m_[:, hc], ps_s[half][:, :], rs_ssd[:, j:j + 1], ALU.mult, ['ps_s%d' % half, 'rs_ssd'], ['cm%d' % half])
                tt('dve', m_[:, hc], m_[:, hc], ps_p[half][:, :], ALU.add, ['cm%d' % half, 'ps_p%d' % half], ['cm%d' % half])
                tt('pool', m_[:, hc], m_[:, hc], g_bc[:, 0, hc], ALU.mult, ['cm%d' % half, 'g_bc'], ['cm%d' % half])
                tt('pool', x1s[i][:, hc], m_[:, hc], xt_[i][:, hc], ALU.add, ['cm%d' % half, 'cxt%d' % i], ['x1s%d' % i])
            S.dma('sp', x1_d[tk, :], x1s[i][:], reads=['x1s%d' % i])
            xt = x1s[i][:]
            act(sq_[:], xt, AF.Square, ['x1s%d' % i], ['csq'])
            S.op('dve', lambda: V.reduce_sum(out=ss_[i][:, 0:1], in_=sq_[:], axis=AX.X), ['csq'], ['css%d' % i])
            act(ss_[i][:, 1:2], ss_[i][:, 0:1], AF.Ln, ['css%d' % i], ['css%d' % i], bias=EPS, scale=1.0 / D)
            act(ss_[i][:, 2:3], ss_[i][:, 1:2], AF.Exp, ['css%d' % i], ['css%d' % i], scale=-0.5)
            ts('dve', xn_[i][:], xt, ss_[i][:, 2:3], ALU.mult, ['x1s%d' % i, 'css%d' % i], ['cxn%d' % i])
        def CY_(j):
            i = j % 2
            tk = slice(j * 128, (j + 1) * 128)
            for k in range(8):
                tr(pT[:, k * 128:(k + 1) * 128], xn_[i][:, k * 128:(k + 1) * 128], identf[:], ['cxn%d' % i, 'identf'], ['pT2'], inc=(k == 7))
            tt('dve', tmpm[:], pT[:, :].rearrange("p (k t) -> p k t", t=128), s2[:, :].unsqueeze(2).to_broadcast([128, 8, 128]),
               ALU.mult, ['pT2', 'modv'], ['ctmpm'])
            tt('pool', h2f[:], tmpm[:], sh2[:, :].unsqueeze(2).to_broadcast([128, 8, 128]), ALU.add, ['ctmpm', 'modv'], ['h2f'])
            tt('pool', htk[:], xn_[i][:], s2_bc[:], ALU.mult, ['cxn%d' % i, 's2_bc'], ['htk'])
            tt('pool', htb[i][:], htk[:], sh2_bc[:], ALU.add, ['htk', 's2_bc'], ['htb%d' % i])
            S.dma('sp', h2tok_d[tk, :], htb[i][:], reads=['htb%d' % i])
            for k in range(8):
                mm(pr[:, 0:NE], h2f[:, k, :], rw[:, k, :], k == 0, k == 7, ['h2f', 'rw'], ['pr'], inc=(k == 7))
            tt('dve', lg[:], pr[:, 0:NE], rb_bc[:], ALU.add, ['pr', 'rb_bc'], ['lg'])
            S.op('dve', lambda: V.max(out=m8[:], in_=lg[:]), ['lg'], ['m8'])
            ts('dve', mask_all[:, j, :], lg[:], m8[:, 3:4], ALU.is_ge, ['lg', 'm8'], ['mask_all'])
            ts('dve', sm[:, 0:1], m8[:, 0:1], -1.0, ALU.mult, ['m8'], ['sm'])
            act(ex[:], lg[:], AF.Exp, ['lg', 'sm'], ['ex'], bias=sm[:, 0:1])
            tt('dve', ex[:], ex[:], mask_all[:, j, :], ALU.mult, ['ex', 'mask_all'], ['ex'])
            S.op('dve', lambda: V.reduce_sum(out=sm[:, 1:2], in_=ex[:], axis=AX.X), ['ex'], ['sm'])
            S.op('dve', lambda: V.reciprocal(out=sm[:, 2:3], in_=sm[:, 1:2]), ['sm'], ['sm'])
            ts('dve', G_all[:, j, :], ex[:], sm[:, 2:3], ALU.mult, ['ex', 'sm'], ['G_all'])
        CX_(0)
        for j in range(NT):
            if j + 1 < NT:
                CX_(j + 1)
            CY_(j)
        S.barrier()
    esC.close()
    stop('C')

    with ExitStack() as es:
        SU = T(es, "SU", [128, 128], BF16); SUf = T(es, "SUf", [128, 128]); onesb = T(es, "onesb", [128, 128], BF16)
        mask_bf = T(es, "mask_bf", [128, NT, NE], BF16); P_all = T(es, "P_all", [128, NT + 1, NE], BF16)
        rank_all = T(es, "rank_all", [128, NT, NE]); counts = T(es, "counts", [128, NE]); padded = T(es, "padded", [128, NE])
        tmpc = T(es, "tmpc", [128, NE]); pad_end = T(es, "pad_end", [128, NE]); pad_start = T(es, "pad_start", [128, NE])
        onesf = T(es, "onesf", [128, NE]); Dm = T(es, "Dm", [128, NT, NE]); Em = T(es, "Em", [128, NT, NE])
        iota1 = T(es, "iota1", [128, NE]); jv = T(es, "jv", [128, NBLK]); pidx = T(es, "pidx", [128, 1])
        d8 = T(es, "d8", [128, 8]); e8 = T(es, "e8", [128, 8]); d4 = T(es, "d4", [128, NT, 4]); oh = T(es, "oh", [128, NE])
        cmp3 = T(es, "cmp3", [128, NBLK, NE]); bexp = T(es, "bexp", [128, NBLK])
        rows = [T(es, "rows%d" % i, [128, D], BF16) for i in range(2)]
        pk = [PS(es, "pk%d" % i, [128, 512]) for i in range(2)]; pc_ = PS(es, "pc_", [128, 512])
        S.dma('sp', SUf[:], cSU_d[:, :], writes=['SUf'], sem='c6')
        S.dma('sp', iota1[:], cIota_d.partition_broadcast(128), writes=['iota1'], sem='c6')
        S.dma('sp', jv[:], cJv_d.partition_broadcast(128), writes=['jv'], sem='c6')
        S.dma('sp', pidx[:], cPidx_d[:, :], writes=['pidx'], sem='c6')
        S.regroup('c6', ['SUf', 'iota1', 'jv', 'pidx'])
        cp('dve', SU[:], SUf[:], ['SUf'], ['SU'])
        S.op('dve', lambda: V.memset(onesb[:], 1.0), writes=['onesb'])
        S.op('dve', lambda: V.memset(onesf[:], 1.0), writes=['onesf'])
        cp('dve', mask_bf[:], mask_all[:], ['mask_all'], ['mask_bf'])
        S.op('dve', lambda: V.memset(P_all[:, 0, :], 0.0), writes=['P_all'])
        for j in range(NT):
            tt('dve', P_all[:, j + 1, :], P_all[:, j, :], mask_all[:, j, :], ALU.add, ['P_all', 'mask_all'], ['P_all'])
        for j in range(NT):
            b = j // 16
            reg = pk[b][:, (j % 16) * NE:(j % 16 + 1) * NE]
            mm(reg, SU[:], mask_bf[:, j, :], True, False, ['SU', 'mask_bf'], ['pk%d' % b], inc=False)
            mm(reg, onesb[:], P_all[:, j, :], False, True, ['onesb', 'P_all'], ['pk%d' % b], inc=(j % 16 == 15))
        for b in range(2):
            cp('dve', rank_all[:, b * 16:(b + 1) * 16, :], pk[b][:, :].rearrange("p (j e) -> p j e", e=NE), ['pk%d' % b], ['rank_all'])
        mm(pc_[:, 0:NE], onesb[:], P_all[:, NT, :], True, True, ['onesb', 'P_all'], ['pc_'])
        cp('dve', counts[:], pc_[:, 0:NE], ['pc_'], ['counts'])
        S.op('dve', lambda: V.memset(padded[:], 0.0), writes=['padded'])
        for m in range(4096 // BS):
            ts('dve', tmpc[:], counts[:], float(BS * m), ALU.is_gt, ['counts'], ['tmpc'], s2=float(BS), op1=ALU.mult)
            tt('dve', padded[:], padded[:], tmpc[:], ALU.add, ['padded', 'tmpc'], ['padded'])
        S.op('dve', lambda: V.tensor_tensor_scan(out=pad_end[:], data0=onesf[:], data1=padded[:], initial=0.0, op0=ALU.mult, op1=ALU.add),
             ['onesf', 'padded'], ['pad_end'])
        tt('dve', pad_start[:], pad_end[:], padded[:], ALU.subtract, ['pad_end', 'padded'], ['pad_start'])
        tt('dve', Dm[:], rank_all[:], pad_start[:, :].unsqueeze(1).to_broadcast([128, NT, NE]), ALU.add, ['rank_all', 'pad_start'], ['Dm'])
        ts('dve', Dm[:], Dm[:], 1.0, ALU.add, ['Dm'], ['Dm'])
        tt('dve', Dm[:], Dm[:], mask_all[:], ALU.mult, ['Dm', 'mask_all'], ['Dm'])
        tt('dve', Em[:], mask_all[:], iota1[:, :].unsqueeze(1).to_broadcast([128, NT, NE]), ALU.mult, ['mask_all', 'iota1'], ['Em'])
        for j in range(NT):
            S.op('dve', lambda: V.max(out=d8[:], in_=Dm[:, j, :]), ['Dm'], ['d8'])
            ts('dve', d4[:, j, :], d8[:, 0:4], -1.0, ALU.add, ['d8'], ['d4'])
        cp('dve', idx_all[:].rearrange("p (j k) -> p j k", k=4), d4[:], ['d4'], ['idx_all'])
        tt('dve', cmp3[:], pad_end[:, :].unsqueeze(1).to_broadcast([128, NBLK, NE]), jv[:, :].unsqueeze(2).to_broadcast([128, NBLK, NE]),
           ALU.is_le, ['pad_end', 'jv'], ['cmp3'])
        S.op('dve', lambda: V.reduce_sum(out=bexp[:], in_=cmp3[:], axis=AX.X), ['cmp3'], ['bexp'])
        ts('dve', bexp[:], bexp[:], float(NE - 1), ALU.min, ['bexp'], ['bexp'], s2=128.0, op1=ALU.mult)
        skp = T(es, "skp", [128, NBLK])
        S.op('dve', lambda: V.memset(skp[:], 0.0), writes=['skp'])
        tt('dve', skp[:, 2:NBLK], bexp[:, 2:NBLK], bexp[:, 0:NBLK - 2], ALU.is_equal, ['bexp', 'skp'], ['skp'])
        ts('dve', skp[:], skp[:], 1.0e6, ALU.mult, ['skp'], ['skp'])
        ts('dve', bexp[:], bexp[:], pidx[:, 0:1], ALU.add, ['bexp', 'pidx'], ['bexp'])
        tt('dve', bexp[:], bexp[:], skp[:], ALU.add, ['bexp', 'skp'], ['bexp'])
        cp('dve', widx[:], bexp[:], ['bexp'], ['widx'])
        S.barrier()
        stop('C2')
        for j in range(NT):
            i = j % 2
            S.dma('sp', rows[i][:], h2tok_d[j * 128:(j + 1) * 128, :], writes=['rows%d' % i], sem='rows%d' % i)
            for k in range(4):
                S.idma(out=Xg_d[:, :], in_=rows[i][:, :], idx=idx_all[:, j * 4 + k:j * 4 + k + 1], scatter=True, bound=NSLOT - 1,
                       reads=['rows%d' % i, 'idx_all'], sem='sc%d' % i)
        for j in range(NT):
            S.op('dve', lambda: V.max(out=e8[:], in_=Em[:, j, :]), ['Em'], ['e8'])
            for k in range(4):
                ts('dve', oh[:], iota1[:], e8[:, k:k + 1], ALU.is_equal, ['iota1', 'e8'], ['oh'])
                tt('dve', oh[:], oh[:], G_all[:, j, :], ALU.mult, ['oh', 'G_all'], ['oh'])
                S.op('dve', lambda: V.reduce_sum(out=gates_all[:, j, k:k + 1], in_=oh[:], axis=AX.X), ['oh'], ['gates_all'])
        S.barrier()
    stop('C3')

    with ExitStack() as es:
        wg_ = [T(es, "wg%d" % i, [128, 8, D], BF16) for i in range(2)]; wu_ = [T(es, "wu%d" % i, [128, 8, D], BF16) for i in range(2)]
        w2_ = [T(es, "w2_%d" % i, [128, 8, D], BF16) for i in range(2)]; b1t = [T(es, "b1t%d" % i, [128, 16]) for i in range(2)]
        NJ = BS // 128
        b1p = [T(es, "b1p%d" % i, [128, 8]) for i in range(2)]
        xgs = [T(es, "xgs%d" % i, [128, NJ, D], BF16) for i in range(2)]; xgT = [T(es, "xgT%d" % i, [128, 8, BS], BF16) for i in range(2)]
        actT = [T(es, "actT%d" % i, [128, 8, BS], BF16) for i in range(2)]
        gt3 = [T(es, "gt3_%d" % i, [128, BS]) for i in range(3)]; sg3 = [T(es, "sg3_%d" % i, [128, BS], BF16) for i in range(3)]
        ut3 = [T(es, "ut3_%d" % i, [128, BS]) for i in range(3)]
        ysb = [T(es, "ysb%d" % i, [128, NJ, D]) for i in range(2)]
        pg_ = [PS(es, "mpg%d" % i, [128, 512]) for i in range(2)]; pu_ = [PS(es, "mpu%d" % i, [128, 512]) for i in range(2)]
        py_ = [PS(es, "mpy%d" % i, [128, 512]) for i in range(2)]; pb_ = [PS(es, "mpb%d" % i, [128, 1024], BF16) for i in range(2)]
        Xg_v = Xg_d.rearrange("(b j p) d -> b p j d", p=128, j=NJ); Y_v = Y_d.rearrange("(b j p) o -> b p j o", p=128, j=NJ)

        def wload(blk, i):
            ix = widx[:, blk:blk + 1]
            for (dst, src, nm) in ((wg_[i], W1G_d, 'wg%d' % i), (wu_[i], W1U_d, 'wu%d' % i), (w2_[i], W2_d, 'w2_%d' % i)):
                S.idma(out=dst[:].rearrange("p k c -> p (k c)"), in_=src[:, :], idx=ix, scatter=False, bound=NE * 128 - 1,
                       reads=['widx'], writes=[nm], sem=nm)
            S.idma(out=b1t[i][:, :], in_=B1_d[:, :], idx=ix, scatter=False, bound=NE * 128 - 1, reads=['widx'], writes=['b1t%d' % i], sem='b1t%d' % i)

        rgu = Ring('gu', 2); ry = Ring('y', 2); rpb = Ring('pb', 2); r3 = Ring('r3', 3)
        KB = 1024 // BS

        def TR_(blk):
            wi = blk % 2
            S.dma('sp', xgs[wi][:], Xg_v[blk], writes=['xgs%d' % wi], sem='xgs%d' % wi)
            for k0 in range(0, 8, KB):
                pi = rpb.next()
                for kk in range(KB):
                    k = k0 + kk
                    for jj in range(NJ):
                        tr(pb_[pi][:, kk * BS + jj * 128: kk * BS + (jj + 1) * 128], xgs[wi][:, jj, k * 128:(k + 1) * 128], identb[:],
                           ['xgs%d' % wi, 'identb'], ['mpb%d' % pi], inc=(kk == KB - 1 and jj == NJ - 1))
                cp('act' if (k0 // KB) % 2 == 0 else 'dve', xgT[wi][:, k0:k0 + KB, :], pb_[pi][:, :].rearrange("p (k s) -> p k s", s=BS),
                   ['mpb%d' % pi], ['xgT%d' % wi])

        def FL_(blk):
            wi = blk % 2
            ts('pool', b1p[wi][:], b1t[wi][:, 8:16], 1.0, ALU.add, ['b1t%d' % wi], ['b1p%d' % wi])
            prev = None

            def fin(pv):
                ri, fp = pv
                S.op('dve', lambda: V.scalar_tensor_tensor(out=actT[wi][:, fp, :], in0=ut3[ri][:], scalar=-6.0, in1=gt3[ri][:], op0=ALU.max, op1=ALU.mult),
                     ['ut3_%d' % ri, 'gt3_%d' % ri], ['actT%d' % wi])
            for f in range(8):
                b = rgu.next(); ri = r3.next()
                fs = slice(f * 128, (f + 1) * 128)
                for k in range(8):
                    mm(pg_[b][:, 0:BS], wg_[wi][:, k, fs], xgT[wi][:, k, :], k == 0, k == 7, ['wg%d' % wi, 'xgT%d' % wi], ['mpg%d' % b], inc=(k == 7))
                for k in range(8):
                    mm(pu_[b][:, 0:BS], wu_[wi][:, k, fs], xgT[wi][:, k, :], k == 0, k == 7, ['wu%d' % wi, 'xgT%d' % wi], ['mpu%d' % b], inc=(k == 7))
                ts('dve', gt3[ri][:], pg_[b][:, 0:BS], b1t[wi][:, f:f + 1], ALU.add, ['mpg%d' % b, 'b1t%d' % wi], ['gt3_%d' % ri], s2=7.0, op1=ALU.min)
                act(sg3[ri][:], gt3[ri][:], AF.Sigmoid, ['gt3_%d' % ri], ['sg3_%d' % ri], scale=1.702)
                ts('dve', ut3[ri][:], pu_[b][:, 0:BS], b1p[wi][:, f:f + 1], ALU.add, ['mpu%d' % b, 'b1p%d' % wi], ['ut3_%d' % ri], s2=8.0, op1=ALU.min)
                tt('pool', gt3[ri][:], gt3[ri][:], sg3[ri][:], ALU.mult, ['gt3_%d' % ri, 'sg3_%d' % ri], ['gt3_%d' % ri])
                if prev is not None:
                    fin(prev)
                prev = (ri, f)
            fin(prev)

        def W2_(blk):
            wi = blk % 2
            for jj in range(NJ):
                for half in range(2):
                    b = ry.next()
                    for k in range(8):
                        mm(py_[b][:, :], actT[wi][:, k, jj * 128:(jj + 1) * 128], w2_[wi][:, k, half * 512:(half + 1) * 512], k == 0, k == 7,
                           ['actT%d' % wi, 'w2_%d' % wi], ['mpy%d' % b], inc=(k == 7))
                    cp('act' if half == 0 else 'dve', ysb[wi][:, jj, half * 512:(half + 1) * 512], py_[b][:, :], ['mpy%d' % b], ['ysb%d' % wi])
            S.dma('sp', Y_v[blk], ysb[wi][:], reads=['ysb%d' % wi])

        wload(0, 0)
        TR_(0)
        for blk in range(NBLK):
            if blk + 1 < NBLK:
                wload(blk + 1, (blk + 1) % 2)
            FL_(blk)
            if blk + 1 < NBLK:
                TR_(blk + 1)
            W2_(blk)
        S.barrier()
    stop('D')

    with ExitStack() as es:
        b2s = T(es, "b2s", [NE, D]); GT = T(es, "GT", [NE, 128])
        S.dma('sp', b2s[:], b2_d[:, :], writes=['b2s'], sem='b2s')
        yk = [T(es, "yk%d" % i, [128, D]) for i in range(8)]; acc = [T(es, "acc%d" % i, [128, D]) for i in range(2)]
        x1t = [T(es, "x1t%d" % i, [128, D]) for i in range(2)]; sq_ = T(es, "dsq", [128, D]); ss_ = [T(es, "dss%d" % i, [128, 4]) for i in range(2)]
        pgt = PS(es, "pgt", [128, 512]); pa = [PS(es, "pa%d" % i, [128, 512]) for i in range(2)]
        def EG_(j):
            i = j % 2
            tk = slice(j * 128, (j + 1) * 128)
            S.dma('sp', x1t[i][:], x1_d[tk, :], writes=['x1t%d' % i], sem='x1t%d' % i)
            for k in range(4):
                kk = i * 4 + k
                S.idma(out=yk[kk][:, :], in_=Y_d[:, :], idx=idx_all[:, j * 4 + k:j * 4 + k + 1], scatter=False, bound=NSLOT - 1,
                       reads=['idx_all'], writes=['yk%d' % kk], sem='yk%d' % kk)

        EG_(0)
        for j in range(NT):
            i = j % 2
            tk = slice(j * 128, (j + 1) * 128)
            if j + 1 < NT:
                EG_(j + 1)
            tr(pgt[0:NE, 0:128], G_all[:, j, :], identf[:], ['G_all', 'identf'], ['pgt'])
            cp('act', GT[:], pgt[0:NE, 0:128], ['pgt'], ['GT'])
            for half in range(2):
                hc = slice(half * 512, (half + 1) * 512)
                mm(pa[half][:, :], GT[:, :], b2s[:, hc], True, True, ['GT', 'b2s'], ['pa%d' % half])
                S.op('dve', lambda: V.scalar_tensor_tensor(out=acc[i][:, hc], in0=yk[i * 4][:, hc], scalar=gates_all[:, j, 0:1], in1=pa[half][:, :],
                                                            op0=ALU.mult, op1=ALU.add), ['yk%d' % (i * 4), 'gates_all', 'pa%d' % half], ['acc%d_%d' % (i, half)])
                for k in range(1, 4):
                    S.op('dve', lambda: V.scalar_tensor_tensor(out=acc[i][:, hc], in0=yk[i * 4 + k][:, hc], scalar=gates_all[:, j, k:k + 1], in1=acc[i][:, hc],
                                                                op0=ALU.mult, op1=ALU.add), ['yk%d' % (i * 4 + k), 'gates_all', 'acc%d_%d' % (i, half)], ['acc%d_%d' % (i, half)])
            ak = ['acc%d_0' % i, 'acc%d_1' % i]
            tt('pool', acc[i][:], acc[i][:], g_bc[:, 1, :], ALU.mult, ak + ['g_bc'], ak)
            tt('pool', x1t[i][:], x1t[i][:], acc[i][:], ALU.add, ['x1t%d' % i] + ak, ['x1t%d' % i])
            act(sq_[:], x1t[i][:], AF.Square, ['x1t%d' % i], ['dsq'])
            S.op('dve', lambda: V.reduce_sum(out=ss_[i][:, 0:1], in_=sq_[:], axis=AX.X), ['dsq'], ['dss%d' % i])
            act(ss_[i][:, 1:2], ss_[i][:, 0:1], AF.Ln, ['dss%d' % i], ['dss%d' % i], bias=EPS, scale=1.0 / D)
            act(ss_[i][:, 2:3], ss_[i][:, 1:2], AF.Exp, ['dss%d' % i], ['dss%d' % i], scale=-0.5)
            ts('dve', x1t[i][:], x1t[i][:], ss_[i][:, 2:3], ALU.mult, ['x1t%d' % i, 'dss%d' % i], ['x1t%d' % i])
            tt('pool', x1t[i][:], x1t[i][:], fnw_bc[:], ALU.mult, ['x1t%d' % i, 'fnw_bc'], ['x1t%d' % i])
            S.dma('sp', out_d[tk, :], x1t[i][:], reads=['x1t%d' % i])
        S.barrier()
    es0.close()
    return nc


def _consts():
    I = np.eye(128, dtype=np.float32)
    k = np.arange(128)
    U = (k[:, None] <= k[None, :]).astype(np.float32)
    Lo = (k[:, None] >= k[None, :]).astype(np.float32)
    mask = np.zeros((128, 2, 128), np.float32)
    mask[:, 0, :] = np.where(k[None, :] >= k[:, None], 0.0, NEG)
    mask[:, 1, :] = np.where(k[None, :] <= k[:, None], 0.0, NEG)
    sel = np.zeros((6, 6, 128), np.float32)
    for h in range(6):
        sel[h, h, :] = 1.0
    AT = np.zeros((128, 4, 128), np.float32)
    t = np.arange(64)
    for g, w in enumerate((2, 4, 8, 16)):
        lo = np.clip(t - w // 2, 0, 64); hi = np.clip(t + w - w // 2, 0, 64)
        Am = np.zeros((64, 64), np.float32)
        for ti in range(64):
            Am[ti, lo[ti]:hi[ti]] = 1.0 / float(hi[ti] - lo[ti])
        Am -= np.eye(64, dtype=np.float32)
        for r in range(2):
            AT[r * 64:(r + 1) * 64, g, r * 64:(r + 1) * 64] = Am.T
    SU = (k[:, None] < k[None, :]).astype(np.float32)
    iota1 = (np.arange(NE, dtype=np.float32) + 1.0).reshape(1, NE)
    jv = (np.arange(NBLK, dtype=np.float32) * float(BS)).reshape(1, NBLK)
    pidx = np.arange(128, dtype=np.float32).reshape(128, 1)
    return dict(cI=I, cU=U, cLo=Lo, cMask=mask, cSel=sel, cAT=AT, cSU=SU, cIota=iota1, cJv=jv, cPidx=pidx)


_NC_CACHE = {}


def kernel(x, c, ctx, c_ctx, w_mod, b_mod, norm1_w, norm2_w, w_in, conv_w, conv_b, dt_bias, a_log, d_skip,
           ssd_norm_w, pool_w, pool_scale, w_out, router_w, router_b, w1, b1, w2, b2, final_norm_w, _dbg=False, _upto='ALL', _cores=8):
    f = lambda a: np.ascontiguousarray(np.asarray(a, dtype=np.float32))
    x = f(x); c = f(c); ctx = f(ctx); c_ctx = f(c_ctx)
    shared = dict(_consts())
    shared["w_mod"] = f(w_mod[0])
    shared["bmodT"] = f(b_mod[0].reshape(48, 128).T)
    shared["bmodr"] = f(b_mod[0].reshape(1, -1))
    shared["n1T"] = f(norm1_w[0].reshape(8, 128).T); shared["n2T"] = f(norm2_w[0].reshape(8, 128).T)
    shared["fnw"] = f(final_norm_w.reshape(1, -1))
    shared["w_in"] = f(w_in[0])
    shared["convw"] = f(np.asarray(conv_w[0]).T.reshape(20, 128, 5).transpose(1, 0, 2))
    shared["convb"] = f(np.asarray(conv_b[0]).reshape(20, 128).T)
    shared["dtb"] = f(np.asarray(dt_bias[0]).reshape(1, 48)); shared["alog"] = f(np.asarray(a_log[0]).reshape(1, 48))
    shared["dvec"] = f(np.repeat(np.asarray(d_skip[0]), 64).reshape(1, -1))
    shared["ynw"] = f(np.asarray(ssd_norm_w[0]).reshape(12, 128).T)
    shared["poolw"] = f(np.asarray(pool_w[0]).transpose(1, 0, 2))
    shared["pscale"] = f(np.asarray(pool_scale[0]).reshape(4, 128).T)
    shared["w_out"] = f(w_out[0])
    shared["rw"] = f(np.asarray(router_w[0]).reshape(8, 128, NE).transpose(1, 0, 2))
    shared["rb"] = f(np.asarray(router_b[0]).reshape(1, NE))
    w1a = np.asarray(w1[0])

    def ptile(w):
        return f(w.reshape(NE, 8, 128, -1).transpose(0, 2, 1, 3).reshape(NE * 128, -1))
    shared["W1G"] = ptile(w1a[:, :, 0::2]); shared["W1U"] = ptile(w1a[:, :, 1::2])
    shared["W2"] = ptile(np.asarray(w2[0]))
    b1a = np.asarray(b1[0])
    b1g_ = b1a[:, 0::2].reshape(NE, 8, 128).transpose(0, 2, 1)
    b1u_ = b1a[:, 1::2].reshape(NE, 8, 128).transpose(0, 2, 1)
    shared["B1"] = f(np.concatenate([b1g_, b1u_], axis=2).reshape(NE * 128, 16))
    shared["b2"] = f(b2[0])
    shared["n2r"] = f(norm2_w[0].reshape(1, -1))
    if (_dbg, _upto) not in _NC_CACHE:
        _NC_CACHE[(_dbg, _upto)] = build(dbg=_dbg, upto=_upto)
    nc = _NC_CACHE[(_dbg, _upto)]
    in_maps = []
    for b in range(_cores):
        m = dict(shared)
        m["x"] = x[b]; m["ctx"] = ctx[b]
        cv = np.stack([c[b], c_ctx], axis=-1)
        m["cvec"] = f(cv.reshape(8, 128, 2).transpose(1, 0, 2))
        in_maps.append(m)
    res = run_bass_kernel_spmd(nc, in_maps, core_ids=list(range(_cores)))
    if _dbg:
        return res
    return np.stack([r["out"] for r in res.results], axis=0).astype(np.float32)
```

```python
import numpy as np
from contextlib import ExitStack
import concourse.bass as bass
import concourse.mybir as mybir
from concourse.bass_utils import run_bass_kernel_spmd

F32 = mybir.dt.float32; BF16 = mybir.dt.bfloat16; I32 = mybir.dt.int32
AF = mybir.ActivationFunctionType; ALU = mybir.AluOpType; AX = mybir.AxisListType

D = 1024; L = 4096; CTXL = 256; NT = 32; NCH = 34
INC = 4656; CONVCH = 2560; SSDW = 1536
RAWW = 4360
NEG = -30000.0
NE = 32
BS = 256
NBLK = 16384 // BS + NE
NSLOT = NBLK * BS
EPS = 1e-6


class Sched:
    STRICT = True

    def __init__(self, nc, es):
        self.nc = nc; self.es = es
        self.eng = {'pe': nc.tensor, 'act': nc.scalar, 'dve': nc.vector, 'pool': nc.gpsimd, 'sp': nc.sync}
        self.sem = {}; self.cnt = {}
        for e in self.eng:
            self.sem[e] = es.enter_context(nc.semaphore("s_" + e)); self.cnt[e] = 0
        self.seen = {e: {} for e in self.eng}
        self.w = {}; self.r = {}
        self.dsem = {}; self.dcnt = {}; self.free_sems = []; self.free_sw = []; self.dsw = {}; self.nsem = 0; self.bregs = {}

    def _dsem(self, name, sw=False):
        if name not in self.dsem:
            pool = self.free_sw if sw else self.free_sems
            if pool:
                h, c = pool.pop()
                self.dsem[name] = h; self.dcnt[name] = c
            else:
                self.nsem += 1
                self.dsem[name] = self.es.enter_context(self.nc.semaphore("d_%d" % self.nsem)); self.dcnt[name] = 0
            self.dsw[name] = sw
        assert self.dsw[name] == sw, name
        return self.dsem[name]

    def _wait(self, e, tok):
        if tok is None:
            return
        kind, name, val = tok
        if kind == 'e' and name == e and (e == 'pe' or not self.STRICT):
            return
        skey = (kind, name)
        if self.seen[e].get(skey, 0) >= val:
            return
        self.seen[e][skey] = val
        s = self.sem[name] if kind == 'e' else self.dsem[name]
        self.eng[e].wait_ge(s, val)

    def deps(self, e, reads, writes):
        for k in reads:
            self._wait(e, self.w.get(k))
        for k in writes:
            self._wait(e, self.w.get(k))
            for tok in list(self.r.get(k, {}).values()):
                self._wait(e, tok)

    def record(self, tok, reads, writes):
        for k in reads:
            self.r.setdefault(k, {})[(tok[0], tok[1])] = tok
        for k in writes:
            self.w[k] = tok; self.r[k] = {}

    def op(self, e, fn, reads=(), writes=(), inc=True):
        self.deps(e, reads, writes)
        ins = fn()
        tok = ('e', e, self.cnt[e] + 1)
        self.record(tok, reads, writes)
        if inc:
            ins.then_inc(self.sem[e], 1); self.cnt[e] += 1
        return ins

    def dma(self, q, out, in_, reads=(), writes=(), sem=None):
        if sem is None:
            sem = ('L_' + writes[0]) if writes else ('S_' + reads[0])
        if q == 'pool':
            sem = 'sw_' + sem
        s = self._dsem(sem, sw=(q == 'pool'))
        self.deps(q, reads, writes)
        ins = self.eng[q].dma_start(out=out, in_=in_)
        ins.then_inc(s, 16); self.dcnt[sem] += 16
        tok = ('d', sem, self.dcnt[sem])
        self.record(tok, reads, writes)
        return ins

    def idma(self, out, in_, idx, scatter, bound, reads=(), writes=(), sem=None):
        sem = 'sw_' + sem
        s = self._dsem(sem, sw=True)
        self.deps('pool', reads, writes)
        if bound not in self.bregs:
            r = self.nc.gpsimd.alloc_register("bc%d" % bound)
            self.nc.gpsimd.reg_mov(r, bound)
            self.bregs[bound] = r
        bound = self.bregs[bound]
        off = bass.IndirectOffsetOnAxis(ap=idx, axis=0)
        if scatter:
            ins = self.nc.gpsimd.indirect_dma_start(out=out, out_offset=off, in_=in_, in_offset=None, bounds_check=bound, oob_is_err=False)
        else:
            ins = self.nc.gpsimd.indirect_dma_start(out=out, out_offset=None, in_=in_, in_offset=off, bounds_check=bound, oob_is_err=False)
        ins.then_inc(s, 16); self.dcnt[sem] += 16
        tok = ('d', sem, self.dcnt[sem])
        self.record(tok, reads, writes)
        return ins

    def regroup(self, sem, keys):
        for k in keys:
            self.w[k] = ('d', sem, self.dcnt[sem])

    def barrier(self):
        for e in self.eng:
            for f in self.eng:
                if f != e and self.cnt[f] > 0:
                    self._wait(e, ('e', f, self.cnt[f]))
            for name in self.dsem:
                if self.dcnt[name] > 0:
                    self._wait(e, ('d', name, self.dcnt[name]))
        for name in list(self.dsem):
            (self.free_sw if self.dsw[name] else self.free_sems).append((self.dsem[name], self.dcnt[name]))
            for e in self.eng:
                self.seen[e].pop(('d', name), None)
        self.dsem = {}; self.dcnt = {}; self.dsw = {}
        for k in list(self.w):
            if self.w[k][0] == 'd':
                del self.w[k]
        for k in list(self.r):
            for kk in [kk for kk in self.r[k] if kk[0] == 'd']:
                del self.r[k][kk]


class Ring:
    def __init__(self, name, n):
        self.name = name; self.n = n; self.i = -1

    def next(self):
        self.i += 1
        return self.i % self.n


class _Stop(Exception):
    pass


def build(dbg=False, upto='ALL'):
    try:
        return _build(dbg, upto)
    except _Stop as e:
        return e.args[0]


def _build(dbg=False, upto='ALL'):
    nc = bass.Bass("TRN2", target_bir_lowering=False)
    es0 = ExitStack()
    S = Sched(nc, es0)
    V = nc.vector; A = nc.scalar; G = nc.gpsimd; PE = nc.tensor

    def din(name, shape, dt=F32):
        return nc.dram_tensor(name, list(shape), dt, kind="ExternalInput").ap()

    def dscr(name, shape, dt):
        return nc.dram_tensor(name, list(shape), dt, kind=("ExternalOutput" if dbg else "Internal")).ap()

    x_d = din("x", [L, D]); ctx_d = din("ctx", [CTXL, D]); cvec_d = din("cvec", [128, 8, 2])
    wmod_d = din("w_mod", [D, 6 * D]); bmodT_d = din("bmodT", [128, 48]); bmodr_d = din("bmodr", [1, 6 * D])
    n1T_d = din("n1T", [128, 8]); n2T_d = din("n2T", [128, 8]); fnw_d = din("fnw", [1, D])
    win_d = din("w_in", [D, INC]); convw_d = din("convw", [128, 20, 5]); convb_d = din("convb", [128, 20])
    dtb_d = din("dtb", [1, 48]); alog_d = din("alog", [1, 48]); dvec_d = din("dvec", [1, SSDW])
    ynw_d = din("ynw", [128, 12]); poolw_d = din("poolw", [128, 4, 128]); pscale_d = din("pscale", [128, 4])
    wout_d = din("w_out", [2 * D, D]); rw_d = din("rw", [128, 8, NE]); rb_d = din("rb", [1, NE])
    W1G_d = din("W1G", [NE * 128, 8 * D]); W1U_d = din("W1U", [NE * 128, 8 * D]); W2_d = din("W2", [NE * 128, 8 * D])
    B1_d = din("B1", [NE * 128, 16]); b2_d = din("b2", [NE, D]); n2r_d = din("n2r", [1, D])
    cSU_d = din("cSU", [128, 128]); cIota_d = din("cIota", [1, NE]); cJv_d = din("cJv", [1, NBLK]); cPidx_d = din("cPidx", [128, 1])
    cI_d = din("cI", [128, 128]); cU_d = din("cU", [128, 128]); cLo_d = din("cLo", [128, 128])
    cMask_d = din("cMask", [128, 2, 128]); cSel_d = din("cSel", [6, 6, 128]); cAT_d = din("cAT", [128, 4, 128])
    out_d = nc.dram_tensor("out", [L, D], F32, kind="ExternalOutput").ap()
    rawT_d = dscr("rawT", [CONVCH, RAWW], BF16); zt_d = dscr("zt", [L, SSDW], BF16)
    ypT_d = dscr("ypT", [512, L], BF16); yzT_d = dscr("yzT", [SSDW, L], BF16)
    x1_d = dscr("x1", [L, D], F32); h2tok_d = dscr("h2tok", [L, D], BF16)
    Xg_d = nc.dram_tensor("Xg", [NSLOT, D], BF16, kind="Internal").ap(); Y_d = nc.dram_tensor("Y", [NSLOT, D], F32, kind="Internal").ap()

    def stop(tag):
        if upto == tag:
            S.barrier()
            raise _Stop(nc)

    def T(es, name, shape, dt=F32):
        return es.enter_context(nc.sbuf_tensor("t_" + name, list(shape), dt))

    def PS(es, name, shape, dt=F32):
        return es.enter_context(nc.psum_tensor("p_" + name, list(shape), dt))

    def mm(out, lhsT, rhs, start, stop, reads, writes, inc=True, sgc=False):
        return S.op('pe', lambda: PE.matmul(out, lhsT=lhsT, rhs=rhs, start=start, stop=stop, skip_group_check=sgc), reads, writes, inc)

    def tr(out, in_, ident, reads, writes, inc=True):
        return S.op('pe', lambda: PE.transpose(out=out, in_=in_, identity=ident), reads, writes, inc)

    def act(out, in_, func, reads, writes, bias=None, scale=None):
        kw = {}
        if bias is not None:
            kw['bias'] = bias
        if scale is not None:
            kw['scale'] = scale
        return S.op('act', lambda: A.activation(out=out, in_=in_, func=func, **kw), reads, writes)

    def tt(e, out, in0, in1, op, reads, writes):
        eng = V if e == 'dve' else G
        return S.op(e, lambda: eng.tensor_tensor(out=out, in0=in0, in1=in1, op=op), reads, writes)

    def ts(e, out, in0, s1, op0, reads, writes, s2=None, op1=None):
        eng = V if e == 'dve' else G
        if op1 is None:
            return S.op(e, lambda: eng.tensor_scalar(out=out, in0=in0, scalar1=s1, scalar2=None, op0=op0), reads, writes)
        return S.op(e, lambda: eng.tensor_scalar(out=out, in0=in0, scalar1=s1, scalar2=s2, op0=op0, op1=op1), reads, writes)

    def cp(e, out, in_, reads, writes):
        if e == 'act':
            return S.op('act', lambda: A.activation(out=out, in_=in_, func=AF.Copy), reads, writes)
        eng = V if e == 'dve' else G
        return S.op(e, lambda: eng.tensor_copy(out=out, in_=in_), reads, writes)

    P0 = es0
    identf = T(P0, "identf", [128, 128]); identb = T(P0, "identb", [128, 128], BF16)
    g_bc = T(P0, "g_bc", [128, 2, D]); fnw_bc = T(P0, "fnw_bc", [128, D])
    s1 = T(P0, "s1", [128, 8]); sh1 = T(P0, "sh1", [128, 8]); cs1 = T(P0, "cs1", [128, 8]); csh1 = T(P0, "csh1", [128, 8])
    s2 = T(P0, "s2", [128, 8]); sh2 = T(P0, "sh2", [128, 8])
    G_all = T(P0, "G_all", [128, NT, NE]); ssq = T(P0, "ssq", [128, 4, NT])
    modrow_d = nc.dram_tensor("modrow", [2, 128, D], F32, kind="Internal").ap()
    S.dma('sp', identf[:], cI_d[:, :], writes=['identf'], sem='c0')
    S.dma('sp', fnw_bc[:], fnw_d.partition_broadcast(128), writes=['fnw_bc'], sem='c0')
    S.regroup('c0', ['identf', 'fnw_bc'])
    cp('dve', identb[:], identf[:], ['identf'], ['identb'])

    esAB = ExitStack()
    dtr_all = T(esAB, "dtr_all", [128, NCH, 48])
    esW = ExitStack()
    win = T(esW, "win", [128, 8, INC], BF16)
    for k in range(8):
        S.dma('pool', win[:, k, :], win_d[k * 128:(k + 1) * 128, :], writes=['win'], sem='win')
    with ExitStack() as es:
        silu_c = T(es, "silu_c", [128, 8, 2]); cbc = T(es, "cbc", [128, 8, 128]); ones = T(es, "ones1", [128, 128])
        wm2 = [T(es, "wm%d" % i, [128, 8, D]) for i in range(2)]; modT = T(es, "modT", [128, 48, 2]); bmodT = T(es, "bmodT_s", [128, 48])
        bm_bc = T(es, "bm_bc", [128, D]); n1T = T(es, "n1T_s", [128, 8]); n2T = T(es, "n2T_s", [128, 8])
        tmp8 = T(es, "tmp8", [128, 8]); n2r_bc = T(es, "n2r_bc", [128, D])
        s2_bc = T(es, "s2_bc1", [128, D]); sh2_bc = T(es, "sh2_bc1", [128, D])
        S.dma('sp', n2r_bc[:], n2r_d.partition_broadcast(128), writes=['n2r_bc'])
        pm = PS(es, "pm", [128, 512]); pg = [PS(es, "pg0", [128, 512]), PS(es, "pg1", [128, 512])]
        S.dma('sp', silu_c[:], cvec_d[:, :, :], writes=['silu_c'], sem='c1')
        S.dma('sp', bmodT[:], bmodT_d[:, :], writes=['bmodT'], sem='c1')
        S.dma('sp', n1T[:], n1T_d[:, :], writes=['n1T'], sem='c1')
        S.dma('sp', n2T[:], n2T_d[:, :], writes=['n2T'], sem='c1')
        S.regroup('c1', ['silu_c', 'bmodT', 'n1T', 'n2T'])
        act(silu_c[:], silu_c[:], AF.Silu, ['silu_c'], ['silu_c'])
        S.op('dve', lambda: V.memset(ones[:], 1.0), writes=['ones1'])
        for k in range(8):
            ts('dve', cbc[:, k, :], ones[:], silu_c[:, k, 0:1], ALU.mult, ['ones1', 'silu_c'], ['cbc'])
        wm_v = wmod_d.rearrange("(k p) c -> p k c", p=128)
        S.dma('sp', wm2[0][:], wm_v[:, :, 0:D], writes=['wm0'], sem='wm0')
        for s in range(6):
            wm = wm2[s % 2]; wk = 'wm%d' % (s % 2)
            if s + 1 < 6:
                S.dma('sp', wm2[(s + 1) % 2][:], wm_v[:, :, (s + 1) * D:(s + 2) * D], writes=['wm%d' % ((s + 1) % 2)], sem='wm%d' % ((s + 1) % 2))
            if s in (2, 3, 4, 5):
                S.dma('sp', bm_bc[:], bmodr_d[:, s * D:(s + 1) * D].partition_broadcast(128), writes=['bm_bc'])
                for half in range(2):
                    hc = slice(half * 512, (half + 1) * 512)
                    for k in range(8):
                        mm(pg[half][:, :], cbc[:, k, :], wm[:, k, hc], k == 0, k == 7,
                           ['cbc', wk], ['pg%d' % half], inc=(k == 7))
                    dst = {2: g_bc[:, 0, hc], 5: g_bc[:, 1, hc], 3: sh2_bc[:, hc], 4: s2_bc[:, hc]}[s]
                    dk = 'g_bc' if s in (2, 5) else 's2_bc'
                    tt('dve', dst, pg[half][:, :], bm_bc[:, hc], ALU.add, ['pg%d' % half, 'bm_bc'], [dk])
                    if s == 4:
                        ts('dve', dst, dst, 1.0, ALU.add, [dk], [dk])
                        tt('dve', dst, dst, n2r_bc[:, hc], ALU.mult, [dk, 'n2r_bc'], [dk])
            if s not in (2, 5):
                for j in range(8):
                    for k in range(8):
                        mm(pm[:, j * 2:j * 2 + 2], wm[:, k, j * 128:(j + 1) * 128], silu_c[:, k, :], k == 0, k == 7,
                           [wk, 'silu_c'], ['pm'], inc=(k == 7))
                tt('dve', modT[:, s * 8:(s + 1) * 8, :], pm[:, 0:16].rearrange("p (j t) -> p j t", t=2),
                   bmodT[:, s * 8:(s + 1) * 8].unsqueeze(2).to_broadcast([128, 8, 2]), ALU.add, ['pm', 'bmodT'], ['modT'])
        for (dst, nT, sc_lo, col) in ((s1, n1T, 8, 0), (cs1, n1T, 8, 1), (s2, n2T, 32, 0)):
            ts('dve', tmp8[:], modT[:, sc_lo:sc_lo + 8, col], 1.0, ALU.add, ['modT'], ['tmp8'])
            tt('dve', dst[:], tmp8[:], nT[:], ALU.mult, ['tmp8', 'n1T', 'n2T'], ['modv'])
        cp('dve', sh1[:], modT[:, 0:8, 0], ['modT'], ['modv'])
        cp('dve', csh1[:], modT[:, 0:8, 1], ['modT'], ['modv'])
        cp('dve', sh2[:], modT[:, 24:32, 0], ['modT'], ['modv'])
        S.dma('sp', modrow_d[0], s2_bc[:], reads=['s2_bc'])
        S.dma('sp', modrow_d[1], sh2_bc[:], reads=['s2_bc'], sem='S_s2_bc')
        S.barrier()

    if upto == '1':
        esW.close(); esAB.close(); es0.close(); return nc

    def rms_to_hT(es_tiles, src_ap, s_vec, sh_vec, dst3, ps_T, names):
        xt, sq, ss, xn, tmp = es_tiles
        (kx, ksq, kss, kxn, ktmp, kps, kdst) = names
        S.dma('sp', xt, src_ap, writes=[kx], sem=kx)
        act(sq, xt, AF.Square, [kx], [ksq])
        S.op('dve', lambda: V.reduce_sum(out=ss[:, 0:1], in_=sq, axis=AX.X), [ksq], [kss])
        act(ss[:, 1:2], ss[:, 0:1], AF.Ln, [kss], [kss], bias=EPS, scale=1.0 / D)
        act(ss[:, 2:3], ss[:, 1:2], AF.Exp, [kss], [kss], scale=-0.5)
        ts('dve', xn, xt, ss[:, 2:3], ALU.mult, [kx, kss], [kxn])
        for k in range(8):
            tr(ps_T[:, k * 128:(k + 1) * 128], xn[:, k * 128:(k + 1) * 128], identf[:], [kxn, 'identf'], [kps], inc=(k == 7))
        tt('dve', tmp, ps_T[:, :].rearrange("p (k t) -> p k t", t=128), s_vec[:, :].unsqueeze(2).to_broadcast([128, 8, 128]),
           ALU.mult, [kps, 'modv'], [ktmp])
        tt('pool', dst3, tmp, sh_vec[:, :].unsqueeze(2).to_broadcast([128, 8, 128]), ALU.add, [ktmp, 'modv'], [kdst])

    with ExitStack() as es:
        dtb_bc = T(es, "dtb_bc", [128, 48]); AT = T(es, "AT", [128, 4, 128], BF16); ATf = T(es, "ATf", [128, 4, 128])
        poolw = T(es, "poolw", [128, 4, 128], BF16); poolwf = T(es, "poolwf", [128, 4, 128]); pscale = T(es, "pscale", [128, 4])
        zpad = T(es, "zpad", [128, 20, 4], BF16)
        S.dma('sp', dtb_bc[:], dtb_d.partition_broadcast(128), writes=['dtb_bc'], sem='c2')
        S.dma('sp', ATf[:], cAT_d[:, :, :], writes=['ATf'], sem='c2')
        S.dma('sp', poolwf[:], poolw_d[:, :, :], writes=['poolwf'], sem='c2')
        S.dma('sp', pscale[:], pscale_d[:, :], writes=['pscale'], sem='c2')
        S.regroup('c2', ['dtb_bc', 'ATf', 'poolwf', 'pscale'])
        cp('dve', AT[:], ATf[:], ['ATf'], ['AT']); cp('dve', poolw[:], poolwf[:], ['poolwf'], ['poolw'])
        S.op('dve', lambda: V.memset(zpad[:], 0.0), writes=['zpad'])
        zrow = T(es, "zrow", [128, 4, D], BF16)
        S.op('pool', lambda: G.memset(zrow[:], 0.0), writes=['zrow'])
        Xg_v0 = Xg_d.rearrange("(b j p) d -> b p j d", p=128, j=4)
        for blk in range(NSLOT // 512):
            S.dma('act', Xg_v0[blk], zrow[:], reads=['zrow'], sem='xgz')
        raw_v = rawT_d.rearrange("(t p) w -> p t w", p=128)
        S.dma('sp', raw_v[:, :, 0:2], zpad[:, :, 0:2], reads=['zpad'])
        S.dma('sp', raw_v[:, :, 4098:4102], zpad[:, :, 0:4], reads=['zpad'])
        S.dma('sp', raw_v[:, :, 4358:4360], zpad[:, :, 0:2], reads=['zpad'])
        xt_ = [T(es, "xt%d" % i, [128, D]) for i in range(2)]; sq_ = T(es, "sq", [128, D]); ss_ = [T(es, "ss%d" % i, [128, 4]) for i in range(2)]
        xn_ = [T(es, "xn%d" % i, [128, D]) for i in range(2)]; tmpm = T(es, "tmpm", [128, 8, 128])
        hT = [T(es, "hT%d" % i, [128, 8, 512], BF16) for i in range(2)]
        rawblk = [T(es, "rawblk0", [128, 20, 512], BF16)] * 2
        zsb = [T(es, "zsb%d" % i, [128, SSDW], BF16) for i in range(2)]
        usb = [T(es, "usb%d" % i, [128, 512], BF16) for i in range(2)]
        plsb = [T(es, "plsb%d" % i, [128, 4, 128], BF16) for i in range(2)]
        ypsb = [T(es, "ypsb%d" % i, [128, 4, 128], BF16) for i in range(2)]
        pT = PS(es, "pT", [128, 1024]); pfm = [PS(es, "pfm%d" % i, [128, 512]) for i in range(2)]
        ptm = [PS(es, "ptm%d" % i, [128, 512]) for i in range(2)]; ppl = PS(es, "ppl", [128, 512]); pyp = PS(es, "pyp", [128, 512])
        rx = Ring('x', 2); rfm = Ring('fm', 2); rtm = Ring('tm', 2); rz = Ring('z', 2)
        ypT_v = ypT_d.rearrange("(g o) t -> o g t", o=128)
        def RMS_(blk):
            ntile = 4 if blk < 8 else 2
            hs = blk % 2
            for j in range(ntile):
                i = rx.next()
                src = x_d[blk * 512 + j * 128: blk * 512 + (j + 1) * 128, :] if blk < 8 else ctx_d[j * 128:(j + 1) * 128, :]
                rms_to_hT((xt_[i][:], sq_[:], ss_[i], xn_[i][:], tmpm[:]), src,
                          s1 if blk < 8 else cs1, sh1 if blk < 8 else csh1, hT[hs][:, :, j * 128:(j + 1) * 128], pT,
                          ('xt%d' % i, 'sq', 'ss%d' % i, 'xn%d' % i, 'tmpm', 'pT', 'hT%d' % hs))

        RMS_(0)
        for blk in range(9):
            ntile = 4 if blk < 8 else 2
            ntok = ntile * 128
            hs = blk % 2
            if blk + 1 < 9:
                RMS_(blk + 1)
            for t in range(20):
                b = rfm.next()
                for k in range(8):
                    mm(pfm[b][:, 0:ntok], win[:, k, t * 128:(t + 1) * 128], hT[hs][:, k, 0:ntok], k == 0, k == 7,
                       ['win', 'hT%d' % hs], ['pfm%d' % b], inc=(k == 7))
                cp('act' if t % 2 == 0 else 'dve', rawblk[hs][:, t, 0:ntok], pfm[b][:, 0:ntok], ['pfm%d' % b], ['rawblk0'])
            off = 2 + 512 * blk if blk < 8 else 4102
            S.dma('sp', raw_v[:, :, off:off + ntok], rawblk[hs][:, :, 0:ntok], reads=['rawblk0'])
            for j in range(ntile):
                chunk = blk * 4 + j
                lt = hT[hs][:, :, j * 128:(j + 1) * 128]
                b = rtm.next()
                for k in range(8):
                    mm(ptm[b][:, 0:48], lt[:, k, :], win[:, k, CONVCH:CONVCH + 48], k == 0, k == 7, ['win', 'hT%d' % hs], ['ptm%d' % b], inc=(k == 7))
                tt('dve', dtr_all[:, chunk, :], ptm[b][:, 0:48], dtb_bc[:], ALU.add, ['ptm%d' % b, 'dtb_bc'], ['dtr_all'])
                if blk == 8:
                    continue
                zi = rz.next()
                for q in range(3):
                    b = rtm.next()
                    c0 = 2608 + q * 512
                    for k in range(8):
                        mm(ptm[b][:, :], lt[:, k, :], win[:, k, c0:c0 + 512], k == 0, k == 7, ['win', 'hT%d' % hs], ['ptm%d' % b], inc=(k == 7))
                    act(zsb[zi][:, q * 512:(q + 1) * 512], ptm[b][:, :], AF.Silu, ['ptm%d' % b], ['zsb%d' % zi])
                S.dma('sp', zt_d[chunk * 128:(chunk + 1) * 128, :], zsb[zi][:], reads=['zsb%d' % zi])
                b = rtm.next()
                for k in range(8):
                    mm(ptm[b][:, :], lt[:, k, :], win[:, k, 4144:4656], k == 0, k == 7, ['win', 'hT%d' % hs], ['ptm%d' % b], inc=(k == 7))
                cp('act', usb[zi][:], ptm[b][:, :], ['ptm%d' % b], ['usb%d' % zi])
                for g in range(4):
                    mm(ppl[:, g * 128:(g + 1) * 128], usb[zi][:, g * 128:(g + 1) * 128], AT[:, g, :], True, True, ['usb%d' % zi, 'AT'], ['ppl'], inc=(g == 3))
                cp('dve', plsb[zi][:], ppl[:, :].rearrange("p (g t) -> p g t", t=128), ['ppl'], ['plsb%d' % zi])
                for g in range(4):
                    mm(pyp[:, g * 128:(g + 1) * 128], poolw[:, g, :], plsb[zi][:, g, :], True, True, ['poolw', 'plsb%d' % zi], ['pyp'], inc=(g == 3))
                tt('dve', ypsb[zi][:], pyp[:, :].rearrange("p (g t) -> p g t", t=128), pscale[:, :].unsqueeze(2).to_broadcast([128, 4, 128]),
                   ALU.mult, ['pyp', 'pscale'], ['ypsb%d' % zi])
                S.dma('sp', ypT_v[:, :, chunk * 128:(chunk + 1) * 128], ypsb[zi][:], reads=['ypsb%d' % zi])
        S.barrier()

    esW.close()
    if upto == 'A':
        esAB.close(); es0.close(); return nc
    with ExitStack() as es:
        convw = T(es, "convw", [128, 20, 5]); convb = T(es, "convb", [128, 20]); Dvec = T(es, "Dvec", [128, SSDW])
        alog_bc = T(es, "alog_bc", [128, 48]); negA = T(es, "negA", [128, 48]); ynw = T(es, "ynw", [128, 12])
        cU = T(es, "cU", [128, 128]); cLo = T(es, "cLo", [128, 128]); ones = T(es, "ones2", [128, 128])
        cMask = T(es, "cMask", [128, 2, 128]); cSel = T(es, "cSel", [6, 6, 128])
        S.dma('sp', convw[:], convw_d[:, :, :], writes=['convw'], sem='c3')
        S.dma('sp', convb[:], convb_d[:, :], writes=['convb'], sem='c3')
        S.dma('sp', Dvec[:], dvec_d.partition_broadcast(128), writes=['Dvec'], sem='c3')
        S.dma('sp', alog_bc[:], alog_d.partition_broadcast(128), writes=['alog'], sem='c3')
        S.dma('sp', ynw[:], ynw_d[:, :], writes=['ynw'], sem='c3')
        S.dma('sp', cU[:], cU_d[:, :], writes=['cU'], sem='c3')
        S.dma('sp', cLo[:], cLo_d[:, :], writes=['cLo'], sem='c3')
        S.dma('sp', cMask[:], cMask_d[:, :, :], writes=['cMask'], sem='c3')
        S.dma('sp', cSel[:], cSel_d[:, :, :], writes=['cSel'], sem='c3')
        S.regroup('c3', ['convw', 'convb', 'Dvec', 'alog', 'ynw', 'cU', 'cLo', 'cMask', 'cSel'])
        S.op('dve', lambda: V.memset(ones[:], 1.0), writes=['ones2'])
        cMaskb = T(es, "cMaskb", [128, 2, 128], BF16); cSelb = T(es, "cSelb", [6, 6, 128], BF16)
        cp('dve', cMaskb[:], cMask[:], ['cMask'], ['cMaskb']); cp('dve', cSelb[:], cSel[:], ['cSel'], ['cSelb'])
        act(negA[:], alog_bc[:], AF.Exp, ['alog'], ['negA'])
        ts('dve', negA[:], negA[:], -1.0, ALU.mult, ['negA'], ['negA'])
        rawg = T(es, "rawg", [128, 5, RAWW], BF16)
        BT = T(es, "BT", [128, NCH * 128], BF16); CT = T(es, "CT", [128, NCH * 128], BF16)
        x_tok = T(es, "x_tok", [128, NCH, 384], BF16); B_tok = T(es, "B_tok", [128, NCH, 128], BF16)
        Sb_all = T(es, "Sb_all", [128, NT, 384], BF16)
        dg = [T(es, "dg%d" % i, [128, 5, 128], BF16) for i in range(2)]
        xc = [T(es, "xc%d" % i, [128, 512], BF16) for i in range(2)]
        dtg = T(es, "dtg", [128, NCH, 12]); av = T(es, "av", [128, NCH, 12]); lndt = T(es, "lndt", [128, NCH, 12])
        cs_all = T(es, "cs_all", [128, NCH, 12]); tot_all = T(es, "tot_all", [128, NCH, 12]); nb = T(es, "nb", [128, NCH, 12])
        eoff = T(es, "eoff", [128, NCH, 12]); wst = T(es, "wst", [128, NCH, 12]); dch = T(es, "dch", [128, NCH, 12])
        Srun = [T(es, "Srun%d" % i, [128, 384]) for i in range(3)]
        Sfb = [T(es, "Sfb%d" % i, [128, 384], BF16) for i in range(2)]
        xd = [T(es, "xd%d" % i, [128, 384], BF16) for i in range(4)]
        CBt = [T(es, "CBt%d" % i, [128, 128], BF16) for i in range(2)]
        csTh = [T(es, "csTh%d" % i, [6, 2, 128], BF16) for i in range(2)]; csTl = [T(es, "csTl%d" % i, [6, 2, 128], BF16) for i in range(2)]
        Lm = [T(es, "Lm%d" % i, [128, 128], BF16) for i in range(8)]
        Mt = [T(es, "Mt%d" % i, [128, 128], BF16) for i in range(8)]
        t1 = [T(es, "t1_%d" % i, [128, 384]) for i in range(2)]; t2 = T(es, "t2", [128, 384]); t3 = T(es, "t3", [128, 384])
        zg = [T(es, "zg%d" % i, [128, 384], BF16) for i in range(2)]; sz = T(es, "sz", [128, 384]); sqj = T(es, "sqj", [128, 384])
        yzb = [T(es, "yzb%d" % i, [128, 384], BF16) for i in range(2)]; yzTs = [T(es, "yzTs%d" % i, [128, 3, 128], BF16) for i in range(2)]
        pf = [PS(es, "pf%d" % i, [128, 512]) for i in range(7)]; pb = PS(es, "pb", [128, 1024], BF16)
        raw_rows = rawT_d.rearrange("(t p) w -> t p w", p=128)
        yzT_v = yzT_d.rearrange("(i p) t -> p i t", p=128)
        rdg = Ring('dg', 2); rcv = Ring('cv', 2); rxc = Ring('xc', 2)
        for g in range(4):
            tiles = [3 * g, 3 * g + 1, 3 * g + 2, 12 + g, 16 + g]
            for ti, Tt in enumerate(tiles):
                S.dma('sp', rawg[:, ti, :], raw_rows[Tt, :, :], writes=['rawg%d' % ti], sem='rawg')
            S.regroup('rawg', ['rawg%d' % ti for ti in range(5)])
            for ti, Tt in enumerate(tiles):
                di = rdg.next()
                for j in range(5):
                    ts('dve', dg[di][:, j, :], identb[:], convw[:, Tt, j:j + 1], ALU.mult, ['identb', 'convw'], ['dg%d' % di])
                for blk in range(9):
                    n = 512 if blk < 8 else 256
                    off = (2 + 512 * blk) if blk < 8 else 4102
                    tok0 = 512 * blk
                    b = rcv.next()
                    for j in range(5):
                        mm(pf[b][:, 0:n], dg[di][:, j, :], rawg[:, ti, off - 2 + j: off - 2 + j + n], j == 0, j == 4,
                           ['dg%d' % di, 'rawg%d' % ti], ['pf%d' % b], inc=(j == 4))
                    if ti < 3:
                        xi = rxc.next()
                        dst = xc[xi][:, 0:n]; dkey = 'xc%d' % xi
                    elif ti == 3:
                        dst = BT[:, tok0:tok0 + n]; dkey = 'BT'
                    else:
                        dst = CT[:, tok0:tok0 + n]; dkey = 'CT'
                    act(dst, pf[b][:, 0:n], AF.Silu, ['pf%d' % b, 'convb'], [dkey], bias=convb[:, Tt:Tt + 1])
                    if ti <= 3:
                        nj = n // 128
                        for jj in range(nj):
                            tr(pb[:, jj * 128:(jj + 1) * 128], dst[:, jj * 128:(jj + 1) * 128], identb[:], [dkey, 'identb'], ['pb'], inc=(jj == nj - 1))
                        src3 = pb[:, 0:n].rearrange("p (j c) -> p j c", c=128)
                        if ti < 3:
                            cp('dve', x_tok[:, 4 * blk:4 * blk + nj, ti * 128:(ti + 1) * 128], src3, ['pb'], ['x_tok'])
                        else:
                            cp('dve', B_tok[:, 4 * blk:4 * blk + nj, :], src3, ['pb'], ['B_tok'])
            S.barrier()
            stop('B1')
            for d in range(2):
                cp('dve', dtg[:, :, d * 6:(d + 1) * 6], dtr_all[:, :, d * 24 + g * 6: d * 24 + g * 6 + 6], ['dtr_all'], ['dtg'])
            act(dtg[:], dtg[:], AF.Exp, ['dtg'], ['dtg'])
            act(dtg[:], dtg[:], AF.Ln, ['dtg'], ['dtg'], bias=1.0, scale=1.0)
            act(lndt[:], dtg[:], AF.Ln, ['dtg'], ['lndt'])
            for d in range(2):
                tt('dve', av[:, :, d * 6:(d + 1) * 6], dtg[:, :, d * 6:(d + 1) * 6],
                   negA[:, d * 24 + g * 6: d * 24 + g * 6 + 6].unsqueeze(1).to_broadcast([128, NCH, 6]), ALU.mult, ['dtg', 'negA'], ['av'])
            for c in range(NCH):
                last = (c == NCH - 1)
                mm(pf[4][:, c * 12:c * 12 + 6], cU[:], av[:, c, 0:6], True, True, ['cU', 'av'], ['pf4'], inc=False)
                mm(pf[4][:, c * 12 + 6:c * 12 + 12], cLo[:], av[:, c, 6:12], True, True, ['cLo', 'av'], ['pf4'], inc=False)
                mm(pf[5][:, c * 12:c * 12 + 12], ones[:], av[:, c, :], True, True, ['ones2', 'av'], ['pf5'], inc=last)
            cp('dve', cs_all[:], pf[4][:, 0:NCH * 12].rearrange("p (c h) -> p c h", h=12), ['pf4'], ['cs_all'])
            cp('dve', tot_all[:], pf[5][:, 0:NCH * 12].rearrange("p (c h) -> p c h", h=12), ['pf5'], ['tot_all'])
            tt('dve', nb[:], lndt[:], cs_all[:], ALU.subtract, ['lndt', 'cs_all'], ['nb'])
            act(eoff[:], cs_all[:], AF.Exp, ['cs_all'], ['eoff'])
            tt('dve', wst[:], tot_all[:], nb[:], ALU.add, ['tot_all', 'nb'], ['wst'])
            act(wst[:], wst[:], AF.Exp, ['wst'], ['wst'])
            act(dch[:], tot_all[:], AF.Exp, ['tot_all'], ['dch'])

            stop('B2')
            rxd = Ring('xd', 4)

            def chunk_state(c, d, bank=6):
                i = rxd.next()
                tt('dve', xd[i][:].rearrange("p (h q) -> p h q", q=64), x_tok[:, c, :].rearrange("p (h q) -> p h q", q=64),
                   wst[:, c, d * 6:(d + 1) * 6].unsqueeze(2).to_broadcast([128, 6, 64]), ALU.mult, ['x_tok', 'wst'], ['xd%d' % i])
                mm(pf[bank][:, 0:384], B_tok[:, c, :], xd[i][:], True, True, ['B_tok', 'xd%d' % i], ['pf%d' % bank])

            def dec_bc(c, d):
                return dch[:, c, d * 6:(d + 1) * 6].unsqueeze(2).to_broadcast([128, 6, 64])

            def v3(ap):
                return ap.rearrange("p (h q) -> p h q", q=64)

            for d, (ca, cb_) in enumerate(((32, 33), (33, 32))):
                chunk_state(ca, d)
                cp('dve', Srun[d][:], pf[6][:, 0:384], ['pf6'], ['Srun%d' % d])
                tt('dve', v3(Srun[d][:]), v3(Srun[d][:]), dec_bc(cb_, d), ALU.mult, ['Srun%d' % d, 'dch'], ['Srun%d' % d])
                chunk_state(cb_, d)
                tt('dve', Srun[d][:], Srun[d][:], pf[6][:, 0:384], ALU.add, ['Srun%d' % d, 'pf6'], ['Srun%d' % d])
            stop('B3')
            order = list(range(NT - 1, -1, -1))
            banks = [3, 4, 5, 6]
            AHEAD = 3
            for n in range(min(AHEAD, NT)):
                chunk_state(order[n], 1, banks[n % 4])
            cur = 1
            for n, c in enumerate(order):
                if n + AHEAD < NT:
                    chunk_state(order[n + AHEAD], 1, banks[(n + AHEAD) % 4])
                nxt = 2 if cur == 1 else 1
                cp('act', Sb_all[:, c, :], Srun[cur][:], ['Srun%d' % cur], ['Sb_all%d' % c])
                tt('dve', v3(Srun[nxt][:]), v3(Srun[cur][:]), dec_bc(c, 1), ALU.mult, ['Srun%d' % cur, 'dch'], ['Srun%d' % nxt])
                bk = banks[n % 4]
                tt('dve', Srun[nxt][:], Srun[nxt][:], pf[bk][:, 0:384], ALU.add, ['Srun%d' % nxt, 'pf%d' % bk], ['Srun%d' % nxt])
                cur = nxt
            S.barrier()
            stop('B4')
            rL = Ring('L', 2); rq = Ring('q', 8)
            pairs = [(d, h) for d in range(2) for h in range(6)]

            def H_(c, ci):
                tk = slice(c * 128, (c + 1) * 128)
                cp('act', Sfb[ci][:], Srun[0][:], ['Srun0'], ['Sfb%d' % ci])
                mm(pf[0][:, 0:128], BT[:, tk], CT[:, tk], True, True, ['BT', 'CT'], ['pf0'], inc=False)
                mm(pf[0][0:6, 128:256], av[:, c, 0:6], cU[:], True, True, ['av', 'cU'], ['pf0'], inc=False)
                mm(pf[0][0:6, 256:384], av[:, c, 6:12], cLo[:], True, True, ['av', 'cLo'], ['pf0'])
                cp('dve', CBt[ci][:], pf[0][:, 0:128], ['pf0'], ['CBt%d' % ci])
                src = pf[0][0:6, 128:384].rearrange("p (d l) -> p d l", l=128)
                cp('dve', csTh[ci][:], src, ['pf0'], ['csTh%d' % ci])
                tt('dve', csTl[ci][:], src, csTh[ci][:], ALU.subtract, ['pf0', 'csTh%d' % ci], ['csTl%d' % ci])

            def L_(c, ci, bt):
                lb = 1 + rL.next()
                bk = 'pf%d' % lb
                for q in range(4):
                    d, h = pairs[bt * 4 + q]
                    reg = pf[lb][:, q * 128:(q + 1) * 128]
                    mm(reg, cSelb[0:6, h, :], csTh[ci][0:6, d, :], True, False, ['cSelb', 'csTh%d' % ci], [bk], inc=False)
                    mm(reg, cSelb[0:6, h, :], csTl[ci][0:6, d, :], False, False, ['cSelb', 'csTl%d' % ci], [bk], inc=False)
                    mm(reg, identb[:], cMaskb[:, d, :], False, True, ['identb', 'cMaskb'], [bk], inc=(q == 3))
                return lb

            def E_(c, ci, bt, lb):
                bk = 'pf%d' % lb
                qis = []
                for q in range(4):
                    d, h = pairs[bt * 4 + q]
                    reg = pf[lb][:, q * 128:(q + 1) * 128]
                    qi = rq.next()
                    act(Lm[qi][:], reg, AF.Exp, [bk, 'nb'], ['Lm%d' % qi], bias=nb[:, c, d * 6 + h: d * 6 + h + 1])
                    tt('dve', Mt[qi][:], Lm[qi][:], CBt[ci][:], ALU.mult, ['Lm%d' % qi, 'CBt%d' % ci], ['Mt%d' % qi])
                    qis.append(qi)
                return qis

            def Y_(c, bt, qis):
                for q in range(4):
                    d, h = pairs[bt * 4 + q]
                    qi = qis[q]
                    mm(pf[3][:, h * 64:(h + 1) * 64], Mt[qi][:], x_tok[:, c, h * 64:(h + 1) * 64], (bt == 0 and q == 0), (bt == 2 and q == 3),
                       ['Mt%d' % qi, 'x_tok'], ['pf3'], inc=(q == 3), sgc=True)

            def O_(c, ci):
                tk = slice(c * 128, (c + 1) * 128)
                mm(pf[4][:, 0:384], CT[:, tk], Sfb[ci][:], True, True, ['CT', 'Sfb%d' % ci], ['pf4'])
                mm(pf[5][:, 0:384], CT[:, tk], Sb_all[:, c, :], True, True, ['CT', 'Sb_all%d' % c], ['pf5'])

            def U_(c):
                chunk_state(c, 0)
                tt('dve', v3(Srun[0][:]), v3(Srun[0][:]), dec_bc(c, 0), ALU.mult, ['Srun0', 'dch'], ['Srun0'])
                tt('dve', Srun[0][:], Srun[0][:], pf[6][:, 0:384], ALU.add, ['Srun0', 'pf6'], ['Srun0'])

            def F_(c, ci):
                k1 = 't1_%d' % ci
                S.dma('sp', zg[ci][:], zt_d[c * 128:(c + 1) * 128, g * 384:(g + 1) * 384], writes=['zg%d' % ci], sem='zg%d' % ci)
                tt('dve', v3(t1[ci][:]), v3(pf[4][:, 0:384]), eoff[:, c, 0:6].unsqueeze(2).to_broadcast([128, 6, 64]), ALU.mult, ['pf4', 'eoff'], [k1])
                tt('dve', v3(t2[:]), v3(pf[5][:, 0:384]), eoff[:, c, 6:12].unsqueeze(2).to_broadcast([128, 6, 64]), ALU.mult, ['pf5', 'eoff'], ['t2'])
                tt('dve', t1[ci][:], pf[3][:, 0:384], t1[ci][:], ALU.add, ['pf3', k1], [k1])
                tt('pool', t3[:], x_tok[:, c, :], Dvec[:, g * 384:(g + 1) * 384], ALU.mult, ['x_tok', 'Dvec'], ['t3'])
                tt('pool', t2[:], t2[:], t3[:], ALU.add, ['t2', 't3'], ['t2'])
                tt('pool', t1[ci][:], t1[ci][:], t2[:], ALU.add, [k1, 't2'], [k1])
                tt('dve', t1[ci][:], t1[ci][:], zg[ci][:], ALU.mult, [k1, 'zg%d' % ci], [k1])
                tt('pool', sqj[:], t1[ci][:], t1[ci][:], ALU.mult, [k1], ['sqj'])
                S.op('dve', lambda: V.reduce_sum(out=ssq[:, g, c:c + 1], in_=sqj[:], axis=AX.X), ['sqj'], ['ssq'])
                cp('act', yzb[ci][:], t1[ci][:], [k1], ['yzb%d' % ci])

            def T_(c, ci):
                tk = slice(c * 128, (c + 1) * 128)
                for i3 in range(3):
                    tr(pb[:, 512 + i3 * 128: 512 + (i3 + 1) * 128], yzb[ci][:, i3 * 128:(i3 + 1) * 128], identb[:], ['yzb%d' % ci, 'identb'], ['pbz'], inc=(i3 == 2))
                tt('dve', yzTs[ci][:], pb[:, 512:896].rearrange("p (i t) -> p i t", t=128),
                   ynw[:, g * 3:(g + 1) * 3].unsqueeze(2).to_broadcast([128, 3, 128]), ALU.mult, ['pbz', 'ynw'], ['yzTs%d' % ci])
                S.dma('sp', yzT_v[:, g * 3:(g + 1) * 3, tk], yzTs[ci][:], reads=['yzTs%d' % ci])

            for c in range(NT):
                ci = c % 2
                H_(c, ci)
                lb0 = L_(c, ci, 0); q0 = E_(c, ci, 0, lb0)
                lb1 = L_(c, ci, 1); q1 = E_(c, ci, 1, lb1)
                Y_(c, 0, q0)
                lb2 = L_(c, ci, 2); q2 = E_(c, ci, 2, lb2)
                Y_(c, 1, q1)
                if c > 0:
                    T_(c - 1, (c - 1) % 2)
                Y_(c, 2, q2)
                O_(c, ci)
                U_(c)
                F_(c, ci)
            T_(NT - 1, (NT - 1) % 2)
            S.barrier()
    esAB.close()
    if upto == 'B':
        es0.close(); return nc

    mask_all = T(P0, "mask_all", [128, NT, NE]); gates_all = T(P0, "gates_all", [128, NT, 4])
    idx_all = T(P0, "idx_all", [128, NT * 4], I32); widx = T(P0, "widx", [128, NBLK], I32)
    esC = ExitStack()
    s2_bc = T(esC, "s2_bc", [128, D]); sh2_bc = T(esC, "sh2_bc", [128, D])
    S.dma('sp', s2_bc[:], modrow_d[0], writes=['s2_bc'], sem='L_s2bc')
    S.dma('sp', sh2_bc[:], modrow_d[1], writes=['s2_bc'], sem='L_s2bc')
    with ExitStack() as es:
        wout = T(es, "wout", [128, 16, D], BF16)
        for k in range(16):
            S.dma('pool', wout[:, k, :], wout_d[k * 128:(k + 1) * 128, :], writes=['wout'], sem='wout')
        rw = T(es, "rw", [128, 8, NE]); rb_bc = T(es, "rb_bc", [128, NE])
        S.dma('sp', rw[:], rw_d[:, :, :], writes=['rw'], sem='c4')
        S.dma('sp', rb_bc[:], rb_d.partition_broadcast(128), writes=['rb_bc'], sem='c4')
        S.regroup('c4', ['rw', 'rb_bc'])
        rs_ssd = T(es, "rs_ssd", [128, NT]); tq = T(es, "tq", [128, NT])
        tt('dve', tq[:], ssq[:, 0, :], ssq[:, 1, :], ALU.add, ['ssq'], ['tq'])
        tt('dve', tq[:], tq[:], ssq[:, 2, :], ALU.add, ['tq', 'ssq'], ['tq'])
        tt('dve', tq[:], tq[:], ssq[:, 3, :], ALU.add, ['tq', 'ssq'], ['tq'])
        act(tq[:], tq[:], AF.Ln, ['tq'], ['tq'], bias=EPS, scale=1.0 / SSDW)
        act(rs_ssd[:], tq[:], AF.Exp, ['tq'], ['rs_ssd'], scale=-0.5)
        yzt = [T(es, "yzt%d" % i, [128, 12, 128], BF16) for i in range(2)]; ypt = [T(es, "ypt%d" % i, [128, 4, 128], BF16) for i in range(2)]
        xt_ = [T(es, "cxt%d" % i, [128, D]) for i in range(2)]; m_ = T(es, "cm", [128, D]); x1s = [T(es, "x1s%d" % i, [128, D]) for i in range(2)]
        sq_ = T(es, "csq", [128, D]); ss_ = [T(es, "css%d" % i, [128, 4]) for i in range(2)]; xn_ = [T(es, "cxn%d" % i, [128, D]) for i in range(2)]
        tmpm = T(es, "ctmpm", [128, 8, 128]); h2f = T(es, "h2f", [128, 8, 128]); htk = T(es, "htk", [128, D]); htb = [T(es, "htb%d" % i, [128, D], BF16) for i in range(2)]
        lg = T(es, "lg", [128, NE]); m8 = T(es, "m8", [128, 8]); ex = T(es, "ex", [128, NE]); sm = T(es, "sm", [128, 4])
        ps_s = [PS(es, "ps_s%d" % i, [128, 512]) for i in range(2)]; ps_p = [PS(es, "ps_p%d" % i, [128, 512]) for i in range(2)]
        pT = PS(es, "pT2", [128, 1024]); pr = PS(es, "pr", [128, 512])
        yzT_v = yzT_d.rearrange("(i p) t -> p i t", p=128); ypT_v = ypT_d.rearrange("(g o) t -> o g t", o=128)
        def CX_(j):
            i = j % 2
            tk = slice(j * 128, (j + 1) * 128)
            S.dma('sp', yzt[i][:], yzT_v[:, :, tk], writes=['yzt%d' % i], sem='yzt%d' % i)
            S.dma('sp', ypt[i][:], ypT_v[:, :, tk], writes=['ypt%d' % i], sem='ypt%d' % i)
            S.dma('sp', xt_[i][:], x_d[tk, :], writes=['cxt%d' % i], sem='cxt%d' % i)
            for half in range(2):
                hc = slice(half * 512, (half + 1) * 512)
                for k in range(12):
                    mm(ps_s[half][:, :], yzt[i][:, k, :], wout[:, k, hc], k == 0, k == 11, ['yzt%d' % i, 'wout'], ['ps_s%d' % half], inc=(k == 11))
                for k in range(4):
                    mm(ps_p[half][:, :], ypt[i][:, k, :], wout[:, 12 + k, hc], k == 0, k == 3, ['ypt%d' % i, 'wout'], ['ps_p%d' % half], inc=(k == 3))
                ts('dve', m_[:, hc], ps_s[half][:, :], rs_ssd[:, j:j + 1], ALU.mult, ['ps_s%d' % half, 'rs_ssd'], ['cm%d' % half])
                tt('dve', m_[:, hc], m_[:, hc], ps_p[half][:, :], ALU.add, ['cm%d' % half, 'ps_p%d' % half], ['cm%d' % half])
                tt('pool', m_[:, hc], m_[:, hc], g_bc[:, 0, hc], ALU.mult, ['cm%d' % half, 'g_bc'], ['cm%d' % half])
                tt('pool', x1s[i][:, hc], m_[:, hc], xt_[i][:, hc], ALU.add, ['cm%d' % half, 'cxt%d' % i], ['x1s%d' % i])
            S.dma('sp', x1_d[tk, :], x1s[i][:], reads=['x1s%d' % i])
            xt = x1s[i][:]
            act(sq_[:], xt, AF.Square, ['x1s%d' % i], ['csq'])
            S.op('dve', lambda: V.reduce_sum(out=ss_[i][:, 0:1], in_=sq_[:], axis=AX.X), ['csq'], ['css%d' % i])
            act(ss_[i][:, 1:2], ss_[i][:, 0:1], AF.Ln, ['css%d' % i], ['css%d' % i], bias=EPS, scale=1.0 / D)
            act(ss_[i][:, 2:3], ss_[i][:, 1:2], AF.Exp, ['css%d' % i], ['css%d' % i], scale=-0.5)
            ts('dve', xn_[i][:], xt, ss_[i][:, 2:3], ALU.mult, ['x1s%d' % i, 'css%d' % i], ['cxn%d' % i])
        def CY_(j):
            i = j % 2
            tk = slice(j * 128, (j + 1) * 128)
            for k in range(8):
                tr(pT[:, k * 128:(k + 1) * 128], xn_[i][:, k * 128:(k + 1) * 128], identf[:], ['cxn%d' % i, 'identf'], ['pT2'], inc=(k == 7))
            tt('dve', tmpm[:], pT[:, :].rearrange("p (k t) -> p k t", t=128), s2[:, :].unsqueeze(2).to_broadcast([128, 8, 128]),
               ALU.mult, ['pT2', 'modv'], ['ctmpm'])
            tt('pool', h2f[:], tmpm[:], sh2[:, :].unsqueeze(2).to_broadcast([128, 8, 128]), ALU.add, ['ctmpm', 'modv'], ['h2f'])
            tt('pool', htk[:], xn_[i][:], s2_bc[:], ALU.mult, ['cxn%d' % i, 's2_bc'], ['htk'])
            tt('pool', htb[i][:], htk[:], sh2_bc[:], ALU.add, ['htk', 's2_bc'], ['htb%d' % i])
            S.dma('sp', h2tok_d[tk, :], htb[i][:], reads=['htb%d' % i])
            for k in range(8):
                mm(pr[:, 0:NE], h2f[:, k, :], rw[:, k, :], k == 0, k == 7, ['h2f', 'rw'], ['pr'], inc=(k == 7))
            tt('dve', lg[:], pr[:, 0:NE], rb_bc[:], ALU.add, ['pr', 'rb_bc'], ['lg'])
            S.op('dve', lambda: V.max(out=m8[:], in_=lg[:]), ['lg'], ['m8'])
            ts('dve', mask_all[:, j, :], lg[:], m8[:, 3:4], ALU.is_ge, ['lg', 'm8'], ['mask_all'])
            ts('dve', sm[:, 0:1], m8[:, 0:1], -1.0, ALU.mult, ['m8'], ['sm'])
            act(ex[:], lg[:], AF.Exp, ['lg', 'sm'], ['ex'], bias=sm[:, 0:1])
            tt('dve', ex[:], ex[:], mask_all[:, j, :], ALU.mult, ['ex', 'mask_all'], ['ex'])
            S.op('dve', lambda: V.reduce_sum(out=sm[:, 1:2], in_=ex[:], axis=AX.X), ['ex'], ['sm'])
            S.op('dve', lambda: V.reciprocal(out=sm[:, 2:3], in_=sm[:, 1:2]), ['sm'], ['sm'])
            ts('dve', G_all[:, j, :], ex[:], sm[:, 2:3], ALU.mult, ['ex', 'sm'], ['G_all'])
        CX_(0)
        for j in range(NT):
            if j + 1 < NT:
                CX_(j + 1)
            CY_(j)
        S.barrier()
    esC.close()
    stop('C')

    with ExitStack() as es:
        SU = T(es, "SU", [128, 128], BF16); SUf = T(es, "SUf", [128, 128]); onesb = T(es, "onesb", [128, 128], BF16)
        mask_bf = T(es, "mask_bf", [128, NT, NE], BF16); P_all = T(es, "P_all", [128, NT + 1, NE], BF16)
        rank_all = T(es, "rank_all", [128, NT, NE]); counts = T(es, "counts", [128, NE]); padded = T(es, "padded", [128, NE])
        tmpc = T(es, "tmpc", [128, NE]); pad_end = T(es, "pad_end", [128, NE]); pad_start = T(es, "pad_start", [128, NE])
        onesf = T(es, "onesf", [128, NE]); Dm = T(es, "Dm", [128, NT, NE]); Em = T(es, "Em", [128, NT, NE])
        iota1 = T(es, "iota1", [128, NE]); jv = T(es, "jv", [128, NBLK]); pidx = T(es, "pidx", [128, 1])
        d8 = T(es, "d8", [128, 8]); e8 = T(es, "e8", [128, 8]); d4 = T(es, "d4", [128, NT, 4]); oh = T(es, "oh", [128, NE])
        cmp3 = T(es, "cmp3", [128, NBLK, NE]); bexp = T(es, "bexp", [128, NBLK])
        rows = [T(es, "rows%d" % i, [128, D], BF16) for i in range(2)]
        pk = [PS(es, "pk%d" % i, [128, 512]) for i in range(2)]; pc_ = PS(es, "pc_", [128, 512])
        S.dma('sp', SUf[:], cSU_d[:, :], writes=['SUf'], sem='c6')
        S.dma('sp', iota1[:], cIota_d.partition_broadcast(128), writes=['iota1'], sem='c6')
        S.dma('sp', jv[:], cJv_d.partition_broadcast(128), writes=['jv'], sem='c6')
        S.dma('sp', pidx[:], cPidx_d[:, :], writes=['pidx'], sem='c6')
        S.regroup('c6', ['SUf', 'iota1', 'jv', 'pidx'])
        cp('dve', SU[:], SUf[:], ['SUf'], ['SU'])
        S.op('dve', lambda: V.memset(onesb[:], 1.0), writes=['onesb'])
        S.op('dve', lambda: V.memset(onesf[:], 1.0), writes=['onesf'])
        cp('dve', mask_bf[:], mask_all[:], ['mask_all'], ['mask_bf'])
        S.op('dve', lambda: V.memset(P_all[:, 0, :], 0.0), writes=['P_all'])
        for j in range(NT):
            tt('dve', P_all[:, j + 1, :], P_all[:, j, :], mask_all[:, j, :], ALU.add, ['P_all', 'mask_all'], ['P_all'])
        for j in range(NT):
            b = j // 16
            reg = pk[b][:, (j % 16) * NE:(j % 16 + 1) * NE]
            mm(reg, SU[:], mask_bf[:, j, :], True, False, ['SU', 'mask_bf'], ['pk%d' % b], inc=False)
            mm(reg, onesb[:], P_all[:, j, :], False, True, ['onesb', 'P_all'], ['pk%d' % b], inc=(j % 16 == 15))
        for b in range(2):
            cp('dve', rank_all[:, b * 16:(b + 1) * 16, :], pk[b][:, :].rearrange("p (j e) -> p j e", e=NE), ['pk%d' % b], ['rank_all'])
        mm(pc_[:, 0:NE], onesb[:], P_all[:, NT, :], True, True, ['onesb', 'P_all'], ['pc_'])
        cp('dve', counts[:], pc_[:, 0:NE], ['pc_'], ['counts'])
        S.op('dve', lambda: V.memset(padded[:], 0.0), writes=['padded'])
        for m in range(4096 // BS):
            ts('dve', tmpc[:], counts[:], float(BS * m), ALU.is_gt, ['counts'], ['tmpc'], s2=float(BS), op1=ALU.mult)
            tt('dve', padded[:], padded[:], tmpc[:], ALU.add, ['padded', 'tmpc'], ['padded'])
        S.op('dve', lambda: V.tensor_tensor_scan(out=pad_end[:], data0=onesf[:], data1=padded[:], initial=0.0, op0=ALU.mult, op1=ALU.add),
             ['onesf', 'padded'], ['pad_end'])
        tt('dve', pad_start[:], pad_end[:], padded[:], ALU.subtract, ['pad_end', 'padded'], ['pad_start'])
        tt('dve', Dm[:], rank_all[:], pad_start[:, :].unsqueeze(1).to_broadcast([128, NT, NE]), ALU.add, ['rank_all', 'pad_start'], ['Dm'])
        ts('dve', Dm[:], Dm[:], 1.0, ALU.add, ['Dm'], ['Dm'])
        tt('dve', Dm[:], Dm[:], mask_all[:], ALU.mult, ['Dm', 'mask_all'], ['Dm'])
        tt('dve', Em[:], mask_all[:], iota1[:, :].unsqueeze(1).to_broadcast([128, NT, NE]), ALU.mult, ['mask_all', 'iota1'], ['Em'])
        for j in range(NT):
            S.op('dve', lambda: V.max(out=d8[:], in_=Dm[:, j, :]), ['Dm'], ['d8'])
            ts('dve', d4[:, j, :], d8[:, 0:4], -1.0, ALU.add, ['d8'], ['d4'])
        cp('dve', idx_all[:].rearrange("p (j k) -> p j k", k=4), d4[:], ['d4'], ['idx_all'])
        tt('dve', cmp3[:], pad_end[:, :].unsqueeze(1).to_broadcast([128, NBLK, NE]), jv[:, :].unsqueeze(2).to_broadcast([128, NBLK, NE]),
           ALU.is_le, ['pad_end', 'jv'], ['cmp3'])
        S.op('dve', lambda: V.reduce_sum(out=bexp[:], in_=cmp3[:], axis=AX.X), ['cmp3'], ['bexp'])
        ts('dve', bexp[:], bexp[:], float(NE - 1), ALU.min, ['bexp'], ['bexp'], s2=128.0, op1=ALU.mult)
        skp = T(es, "skp", [128, NBLK])
        S.op('dve', lambda: V.memset(skp[:], 0.0), writes=['skp'])
        tt('dve', skp[:, 2:NBLK], bexp[:, 2:NBLK], bexp[:, 0:NBLK - 2], ALU.is_equal, ['bexp', 'skp'], ['skp'])
        ts('dve', skp[:], skp[:], 1.0e6, ALU.mult, ['skp'], ['skp'])
        ts('dve', bexp[:], bexp[:], pidx[:, 0:1], ALU.add, ['bexp', 'pidx'], ['bexp'])
        tt('dve', bexp[:], bexp[:], skp[:], ALU.add, ['bexp', 'skp'], ['bexp'])
        cp('dve', widx[:], bexp[:], ['bexp'], ['widx'])
        S.barrier()
        stop('C2')
        for j in range(NT):
            i = j % 2
            S.dma('sp', rows[i][:], h2tok_d[j * 128:(j + 1) * 128, :], writes=['rows%d' % i], sem='rows%d' % i)
            for k in range(4):
                S.idma(out=Xg_d[:, :], in_=rows[i][:, :], idx=idx_all[:, j * 4 + k:j * 4 + k + 1], scatter=True, bound=NSLOT - 1,
                       reads=['rows%d' % i, 'idx_all'], sem='sc%d' % i)
        for j in range(NT):
            S.op('dve', lambda: V.max(out=e8[:], in_=Em[:, j, :]), ['Em'], ['e8'])
            for k in range(4):
                ts('dve', oh[:], iota1[:], e8[:, k:k + 1], ALU.is_equal, ['iota1', 'e8'], ['oh'])
                tt('dve', oh[:], oh[:], G_all[:, j, :], ALU.mult, ['oh', 'G_all'], ['oh'])
                S.op('dve', lambda: V.reduce_sum(out=gates_all[:, j, k:k + 1], in_=oh[:], axis=AX.X), ['oh'], ['gates_all'])
        S.barrier()
    stop('C3')

    with ExitStack() as es:
        wg_ = [T(es, "wg%d" % i, [128, 8, D], BF16) for i in range(2)]; wu_ = [T(es, "wu%d" % i, [128, 8, D], BF16) for i in range(2)]
        w2_ = [T(es, "w2_%d" % i, [128, 8, D], BF16) for i in range(2)]; b1t = [T(es, "b1t%d" % i, [128, 16]) for i in range(2)]
        NJ = BS // 128
        b1p = [T(es, "b1p%d" % i, [128, 8]) for i in range(2)]
        xgs = [T(es, "xgs%d" % i, [128, NJ, D], BF16) for i in range(2)]; xgT = [T(es, "xgT%d" % i, [128, 8, BS], BF16) for i in range(2)]
        actT = [T(es, "actT%d" % i, [128, 8, BS], BF16) for i in range(2)]
        gt3 = [T(es, "gt3_%d" % i, [128, BS]) for i in range(3)]; sg3 = [T(es, "sg3_%d" % i, [128, BS], BF16) for i in range(3)]
        ut3 = [T(es, "ut3_%d" % i, [128, BS]) for i in range(3)]
        ysb = [T(es, "ysb%d" % i, [128, NJ, D]) for i in range(2)]
        pg_ = [PS(es, "mpg%d" % i, [128, 512]) for i in range(2)]; pu_ = [PS(es, "mpu%d" % i, [128, 512]) for i in range(2)]
        py_ = [PS(es, "mpy%d" % i, [128, 512]) for i in range(2)]; pb_ = [PS(es, "mpb%d" % i, [128, 1024], BF16) for i in range(2)]
        Xg_v = Xg_d.rearrange("(b j p) d -> b p j d", p=128, j=NJ); Y_v = Y_d.rearrange("(b j p) o -> b p j o", p=128, j=NJ)

        def wload(blk, i):
            ix = widx[:, blk:blk + 1]
            for (dst, src, nm) in ((wg_[i], W1G_d, 'wg%d' % i), (wu_[i], W1U_d, 'wu%d' % i), (w2_[i], W2_d, 'w2_%d' % i)):
                S.idma(out=dst[:].rearrange("p k c -> p (k c)"), in_=src[:, :], idx=ix, scatter=False, bound=NE * 128 - 1,
                       reads=['widx'], writes=[nm], sem=nm)
            S.idma(out=b1t[i][:, :], in_=B1_d[:, :], idx=ix, scatter=False, bound=NE * 128 - 1, reads=['widx'], writes=['b1t%d' % i], sem='b1t%d' % i)

        rgu = Ring('gu', 2); ry = Ring('y', 2); rpb = Ring('pb', 2); r3 = Ring('r3', 3)
        KB = 1024 // BS

        def TR_(blk):
            wi = blk % 2
            S.dma('sp', xgs[wi][:], Xg_v[blk], writes=['xgs%d' % wi], sem='xgs%d' % wi)
            for k0 in range(0, 8, KB):
                pi = rpb.next()
                for kk in range(KB):
                    k = k0 + kk
                    for jj in range(NJ):
                        tr(pb_[pi][:, kk * BS + jj * 128: kk * BS + (jj + 1) * 128], xgs[wi][:, jj, k * 128:(k + 1) * 128], identb[:],
                           ['xgs%d' % wi, 'identb'], ['mpb%d' % pi], inc=(kk == KB - 1 and jj == NJ - 1))
                cp('act' if (k0 // KB) % 2 == 0 else 'dve', xgT[wi][:, k0:k0 + KB, :], pb_[pi][:, :].rearrange("p (k s) -> p k s", s=BS),
                   ['mpb%d' % pi], ['xgT%d' % wi])

        def FL_(blk):
            wi = blk % 2
            ts('pool', b1p[wi][:], b1t[wi][:, 8:16], 1.0, ALU.add, ['b1t%d' % wi], ['b1p%d' % wi])
            prev = None

            def fin(pv):
                ri, fp = pv
                S.op('dve', lambda: V.scalar_tensor_tensor(out=actT[wi][:, fp, :], in0=ut3[ri][:], scalar=-6.0, in1=gt3[ri][:], op0=ALU.max, op1=ALU.mult),
                     ['ut3_%d' % ri, 'gt3_%d' % ri], ['actT%d' % wi])
            for f in range(8):
                b = rgu.next(); ri = r3.next()
                fs = slice(f * 128, (f + 1) * 128)
                for k in range(8):
                    mm(pg_[b][:, 0:BS], wg_[wi][:, k, fs], xgT[wi][:, k, :], k == 0, k == 7, ['wg%d' % wi, 'xgT%d' % wi], ['mpg%d' % b], inc=(k == 7))
                for k in range(8):
                    mm(pu_[b][:, 0:BS], wu_[wi][:, k, fs], xgT[wi][:, k, :], k == 0, k == 7, ['wu%d' % wi, 'xgT%d' % wi], ['mpu%d' % b], inc=(k == 7))
                ts('dve', gt3[ri][:], pg_[b][:, 0:BS], b1t[wi][:, f:f + 1], ALU.add, ['mpg%d' % b, 'b1t%d' % wi], ['gt3_%d' % ri], s2=7.0, op1=ALU.min)
                act(sg3[ri][:], gt3[ri][:], AF.Sigmoid, ['gt3_%d' % ri], ['sg3_%d' % ri], scale=1.702)
                ts('dve', ut3[ri][:], pu_[b][:, 0:BS], b1p[wi][:, f:f + 1], ALU.add, ['mpu%d' % b, 'b1p%d' % wi], ['ut3_%d' % ri], s2=8.0, op1=ALU.min)
                tt('pool', gt3[ri][:], gt3[ri][:], sg3[ri][:], ALU.mult, ['gt3_%d' % ri, 'sg3_%d' % ri], ['gt3_%d' % ri])
                if prev is not None:
                    fin(prev)
                prev = (ri, f)
            fin(prev)

        def W2_(blk):
            wi = blk % 2
            for jj in range(NJ):
                for half in range(2):
                    b = ry.next()
                    for k in range(8):
                        mm(py_[b][:, :], actT[wi][:, k, jj * 128:(jj + 1) * 128], w2_[wi][:, k, half * 512:(half + 1) * 512], k == 0, k == 7,
                           ['actT%d' % wi, 'w2_%d' % wi], ['mpy%d' % b], inc=(k == 7))
                    cp('act' if half == 0 else 'dve', ysb[wi][:, jj, half * 512:(half + 1) * 512], py_[b][:, :], ['mpy%d' % b], ['ysb%d' % wi])
            S.dma('sp', Y_v[blk], ysb[wi][:], reads=['ysb%d' % wi])

        wload(0, 0)
        TR_(0)
        for blk in range(NBLK):
            if blk + 1 < NBLK:
                wload(blk + 1, (blk + 1) % 2)
            FL_(blk)
            if blk + 1 < NBLK:
                TR_(blk + 1)
            W2_(blk)
        S.barrier()
    stop('D')

    with ExitStack() as es:
        b2s = T(es, "b2s", [NE, D]); GT = T(es, "GT", [NE, 128])
        S.dma('sp', b2s[:], b2_d[:, :], writes=['b2s'], sem='b2s')
        yk = [T(es, "yk%d" % i, [128, D]) for i in range(8)]; acc = [T(es, "acc%d" % i, [128, D]) for i in range(2)]
        x1t = [T(es, "x1t%d" % i, [128, D]) for i in range(2)]; sq_ = T(es, "dsq", [128, D]); ss_ = [T(es, "dss%d" % i, [128, 4]) for i in range(2)]
        pgt = PS(es, "pgt", [128, 512]); pa = [PS(es, "pa%d" % i, [128, 512]) for i in range(2)]
        def EG_(j):
            i = j % 2
            tk = slice(j * 128, (j + 1) * 128)
            S.dma('sp', x1t[i][:], x1_d[tk, :], writes=['x1t%d' % i], sem='x1t%d' % i)
            for k in range(4):
                kk = i * 4 + k
                S.idma(out=yk[kk][:, :], in_=Y_d[:, :], idx=idx_all[:, j * 4 + k:j * 4 + k + 1], scatter=False, bound=NSLOT - 1,
                       reads=['idx_all'], writes=['yk%d' % kk], sem='yk%d' % kk)

        EG_(0)
        for j in range(NT):
            i = j % 2
            tk = slice(j * 128, (j + 1) * 128)
            if j + 1 < NT:
                EG_(j + 1)
            tr(pgt[0:NE, 0:128], G_all[:, j, :], identf[:], ['G_all', 'identf'], ['pgt'])
            cp('act', GT[:], pgt[0:NE, 0:128], ['pgt'], ['GT'])
            for half in range(2):
                hc = slice(half * 512, (half + 1) * 512)
                mm(pa[half][:, :], GT[:, :], b2s[:, hc], True, True, ['GT', 'b2s'], ['pa%d' % half])
                S.op('dve', lambda: V.scalar_tensor_tensor(out=acc[i][:, hc], in0=yk[i * 4][:, hc], scalar=gates_all[:, j, 0:1], in1=pa[half][:, :],
                                                            op0=ALU.mult, op1=ALU.add), ['yk%d' % (i * 4), 'gates_all', 'pa%d' % half], ['acc%d_%d' % (i, half)])
                for k in range(1, 4):
                    S.op('dve', lambda: V.scalar_tensor_tensor(out=acc[i][:, hc], in0=yk[i * 4 + k][:, hc], scalar=gates_all[:, j, k:k + 1], in1=acc[i][:, hc],
                                                                op0=ALU.mult, op1=ALU.add), ['yk%d' % (i * 4 + k), 'gates_all', 'acc%d_%d' % (i, half)], ['acc%d_%d' % (i, half)])
            ak = ['acc%d_0' % i, 'acc%d_1' % i]
            tt('pool', acc[i][:], acc[i][:], g_bc[:, 1, :], ALU.mult, ak + ['g_bc'], ak)
            tt('pool', x1t[i][:], x1t[i][:], acc[i][:], ALU.add, ['x1t%d' % i] + ak, ['x1t%d' % i])
            act(sq_[:], x1t[i][:], AF.Square, ['x1t%d' % i], ['dsq'])
            S.op('dve', lambda: V.reduce_sum(out=ss_[i][:, 0:1], in_=sq_[:], axis=AX.X), ['dsq'], ['dss%d' % i])
            act(ss_[i][:, 1:2], ss_[i][:, 0:1], AF.Ln, ['dss%d' % i], ['dss%d' % i], bias=EPS, scale=1.0 / D)
            act(ss_[i][:, 2:3], ss_[i][:, 1:2], AF.Exp, ['dss%d' % i], ['dss%d' % i], scale=-0.5)
            ts('dve', x1t[i][:], x1t[i][:], ss_[i][:, 2:3], ALU.mult, ['x1t%d' % i, 'dss%d' % i], ['x1t%d' % i])
            tt('pool', x1t[i][:], x1t[i][:], fnw_bc[:], ALU.mult, ['x1t%d' % i, 'fnw_bc'], ['x1t%d' % i])
            S.dma('sp', out_d[tk, :], x1t[i][:], reads=['x1t%d' % i])
        S.barrier()
    es0.close()
    return nc


def _consts():
    I = np.eye(128, dtype=np.float32)
    k = np.arange(128)
    U = (k[:, None] <= k[None, :]).astype(np.float32)
    Lo = (k[:, None] >= k[None, :]).astype(np.float32)
    mask = np.zeros((128, 2, 128), np.float32)
    mask[:, 0, :] = np.where(k[None, :] >= k[:, None], 0.0, NEG)
    mask[:, 1, :] = np.where(k[None, :] <= k[:, None], 0.0, NEG)
    sel = np.zeros((6, 6, 128), np.float32)
    for h in range(6):
        sel[h, h, :] = 1.0
    AT = np.zeros((128, 4, 128), np.float32)
    t = np.arange(64)
    for g, w in enumerate((2, 4, 8, 16)):
        lo = np.clip(t - w // 2, 0, 64); hi = np.clip(t + w - w // 2, 0, 64)
        Am = np.zeros((64, 64), np.float32)
        for ti in range(64):
            Am[ti, lo[ti]:hi[ti]] = 1.0 / float(hi[ti] - lo[ti])
        Am -= np.eye(64, dtype=np.float32)
        for r in range(2):
            AT[r * 64:(r + 1) * 64, g, r * 64:(r + 1) * 64] = Am.T
    SU = (k[:, None] < k[None, :]).astype(np.float32)
    iota1 = (np.arange(NE, dtype=np.float32) + 1.0).reshape(1, NE)
    jv = (np.arange(NBLK, dtype=np.float32) * float(BS)).reshape(1, NBLK)
    pidx = np.arange(128, dtype=np.float32).reshape(128, 1)
    return dict(cI=I, cU=U, cLo=Lo, cMask=mask, cSel=sel, cAT=AT, cSU=SU, cIota=iota1, cJv=jv, cPidx=pidx)


_NC_CACHE = {}


def kernel(x, c, ctx, c_ctx, w_mod, b_mod, norm1_w, norm2_w, w_in, conv_w, conv_b, dt_bias, a_log, d_skip,
           ssd_norm_w, pool_w, pool_scale, w_out, router_w, router_b, w1, b1, w2, b2, final_norm_w, _dbg=False, _upto='ALL', _cores=8):
    f = lambda a: np.ascontiguousarray(np.asarray(a, dtype=np.float32))
    x = f(x); c = f(c); ctx = f(ctx); c_ctx = f(c_ctx)
    shared = dict(_consts())
    shared["w_mod"] = f(w_mod[0])
    shared["bmodT"] = f(b_mod[0].reshape(48, 128).T)
    shared["bmodr"] = f(b_mod[0].reshape(1, -1))
    shared["n1T"] = f(norm1_w[0].reshape(8, 128).T); shared["n2T"] = f(norm2_w[0].reshape(8, 128).T)
    shared["fnw"] = f(final_norm_w.reshape(1, -1))
    shared["w_in"] = f(w_in[0])
    shared["convw"] = f(np.asarray(conv_w[0]).T.reshape(20, 128, 5).transpose(1, 0, 2))
    shared["convb"] = f(np.asarray(conv_b[0]).reshape(20, 128).T)
    shared["dtb"] = f(np.asarray(dt_bias[0]).reshape(1, 48)); shared["alog"] = f(np.asarray(a_log[0]).reshape(1, 48))
    shared["dvec"] = f(np.repeat(np.asarray(d_skip[0]), 64).reshape(1, -1))
    shared["ynw"] = f(np.asarray(ssd_norm_w[0]).reshape(12, 128).T)
    shared["poolw"] = f(np.asarray(pool_w[0]).transpose(1, 0, 2))
    shared["pscale"] = f(np.asarray(pool_scale[0]).reshape(4, 128).T)
    shared["w_out"] = f(w_out[0])
    shared["rw"] = f(np.asarray(router_w[0]).reshape(8, 128, NE).transpose(1, 0, 2))
    shared["rb"] = f(np.asarray(router_b[0]).reshape(1, NE))
    w1a = np.asarray(w1[0])

    def ptile(w):
        return f(w.reshape(NE, 8, 128, -1).transpose(0, 2, 1, 3).reshape(NE * 128, -1))
    shared["W1G"] = ptile(w1a[:, :, 0::2]); shared["W1U"] = ptile(w1a[:, :, 1::2])
    shared["W2"] = ptile(np.asarray(w2[0]))
    b1a = np.asarray(b1[0])
    b1g_ = b1a[:, 0::2].reshape(NE, 8, 128).transpose(0, 2, 1)
    b1u_ = b1a[:, 1::2].reshape(NE, 8, 128).transpose(0, 2, 1)
    shared["B1"] = f(np.concatenate([b1g_, b1u_], axis=2).reshape(NE * 128, 16))
    shared["b2"] = f(b2[0])
    shared["n2r"] = f(norm2_w[0].reshape(1, -1))
    if (_dbg, _upto) not in _NC_CACHE:
        _NC_CACHE[(_dbg, _upto)] = build(dbg=_dbg, upto=_upto)
    nc = _NC_CACHE[(_dbg, _upto)]
    in_maps = []
    for b in range(_cores):
        m = dict(shared)
        m["x"] = x[b]; m["ctx"] = ctx[b]
        cv = np.stack([c[b], c_ctx], axis=-1)
        m["cvec"] = f(cv.reshape(8, 128, 2).transpose(1, 0, 2))
        in_maps.append(m)
    res = run_bass_kernel_spmd(nc, in_maps, core_ids=list(range(_cores)))
    if _dbg:
        return res
    return np.stack([r["out"] for r in res.results], axis=0).astype(np.float32)
```

```python
import numpy as np
from contextlib import ExitStack
import concourse.bass as bass
import concourse.mybir as mybir
from concourse.bass_utils import run_bass_kernel_spmd

F32 = mybir.dt.float32; BF16 = mybir.dt.bfloat16; I32 = mybir.dt.int32
AF = mybir.ActivationFunctionType; ALU = mybir.AluOpType; AX = mybir.AxisListType

D = 1024; L = 4096; CTXL = 256; NT = 32; NCH = 34
INC = 4656; CONVCH = 2560; SSDW = 1536
RAWW = 4360
NEG = -30000.0
NE = 32
BS = 256
NBLK = 16384 // BS + NE
NSLOT = NBLK * BS
EPS = 1e-6


class Sched:
    STRICT = True

    def __init__(self, nc, es):
        self.nc = nc; self.es = es
        self.eng = {'pe': nc.tensor, 'act': nc.scalar, 'dve': nc.vector, 'pool': nc.gpsimd, 'sp': nc.sync}
        self.sem = {}; self.cnt = {}
        for e in self.eng:
            self.sem[e] = es.enter_context(nc.semaphore("s_" + e)); self.cnt[e] = 0
        self.seen = {e: {} for e in self.eng}
        self.w = {}; self.r = {}
        self.dsem = {}; self.dcnt = {}; self.free_sems = []; self.free_sw = []; self.dsw = {}; self.nsem = 0; self.bregs = {}

    def _dsem(self, name, sw=False):
        if name not in self.dsem:
            pool = self.free_sw if sw else self.free_sems
            if pool:
                h, c = pool.pop()
                self.dsem[name] = h; self.dcnt[name] = c
            else:
                self.nsem += 1
                self.dsem[name] = self.es.enter_context(self.nc.semaphore("d_%d" % self.nsem)); self.dcnt[name] = 0
            self.dsw[name] = sw
        assert self.dsw[name] == sw, name
        return self.dsem[name]

    def _wait(self, e, tok):
        if tok is None:
            return
        kind, name, val = tok
        if kind == 'e' and name == e and (e == 'pe' or not self.STRICT):
            return
        skey = (kind, name)
        if self.seen[e].get(skey, 0) >= val:
            return
        self.seen[e][skey] = val
        s = self.sem[name] if kind == 'e' else self.dsem[name]
        self.eng[e].wait_ge(s, val)

    def deps(self, e, reads, writes):
        for k in reads:
            self._wait(e, self.w.get(k))
        for k in writes:
            self._wait(e, self.w.get(k))
            for tok in list(self.r.get(k, {}).values()):
                self._wait(e, tok)

    def record(self, tok, reads, writes):
        for k in reads:
            self.r.setdefault(k, {})[(tok[0], tok[1])] = tok
        for k in writes:
            self.w[k] = tok; self.r[k] = {}

    def op(self, e, fn, reads=(), writes=(), inc=True):
        self.deps(e, reads, writes)
        ins = fn()
        tok = ('e', e, self.cnt[e] + 1)
        self.record(tok, reads, writes)
        if inc:
            ins.then_inc(self.sem[e], 1); self.cnt[e] += 1
        return ins

    def dma(self, q, out, in_, reads=(), writes=(), sem=None):
        if sem is None:
            sem = ('L_' + writes[0]) if writes else ('S_' + reads[0])
        if q == 'pool':
            sem = 'sw_' + sem
        s = self._dsem(sem, sw=(q == 'pool'))
        self.deps(q, reads, writes)
        ins = self.eng[q].dma_start(out=out, in_=in_)
        ins.then_inc(s, 16); self.dcnt[sem] += 16
        tok = ('d', sem, self.dcnt[sem])
        self.record(tok, reads, writes)
        return ins

    def idma(self, out, in_, idx, scatter, bound, reads=(), writes=(), sem=None):
        sem = 'sw_' + sem
        s = self._dsem(sem, sw=True)
        self.deps('pool', reads, writes)
        if bound not in self.bregs:
            r = self.nc.gpsimd.alloc_register("bc%d" % bound)
            self.nc.gpsimd.reg_mov(r, bound)
            self.bregs[bound] = r
        bound = self.bregs[bound]
        off = bass.IndirectOffsetOnAxis(ap=idx, axis=0)
        if scatter:
            ins = self.nc.gpsimd.indirect_dma_start(out=out, out_offset=off, in_=in_, in_offset=None, bounds_check=bound, oob_is_err=False)
        else:
            ins = self.nc.gpsimd.indirect_dma_start(out=out, out_offset=None, in_=in_, in_offset=off, bounds_check=bound, oob_is_err=False)
        ins.then_inc(s, 16); self.dcnt[sem] += 16
        tok = ('d', sem, self.dcnt[sem])
        self.record(tok, reads, writes)
        return ins

    def regroup(self, sem, keys):
        for k in keys:
            self.w[k] = ('d', sem, self.dcnt[sem])

    def barrier(self):
        for e in self.eng:
            for f in self.eng:
                if f != e and self.cnt[f] > 0:
                    self._wait(e, ('e', f, self.cnt[f]))
            for name in self.dsem:
                if self.dcnt[name] > 0:
                    self._wait(e, ('d', name, self.dcnt[name]))
        for name in list(self.dsem):
            (self.free_sw if self.dsw[name] else self.free_sems).append((self.dsem[name], self.dcnt[name]))
            for e in self.eng:
                self.seen[e].pop(('d', name), None)
        self.dsem = {}; self.dcnt = {}; self.dsw = {}
        for k in list(self.w):
            if self.w[k][0] == 'd':
                del self.w[k]
        for k in list(self.r):
            for kk in [kk for kk in self.r[k] if kk[0] == 'd']:
                del self.r[k][kk]


class Ring:
    def __init__(self, name, n):
        self.name = name; self.n = n; self.i = -1

    def next(self):
        self.i += 1
        return self.i % self.n


class _Stop(Exception):
    pass


def build(dbg=False, upto='ALL'):
    try:
        return _build(dbg, upto)
    except _Stop as e:
        return e.args[0]


def _build(dbg=False, upto='ALL'):
    nc = bass.Bass("TRN2", target_bir_lowering=False)
    es0 = ExitStack()
    S = Sched(nc, es0)
    V = nc.vector; A = nc.scalar; G = nc.gpsimd; PE = nc.tensor

    def din(name, shape, dt=F32):
        return nc.dram_tensor(name, list(shape), dt, kind="ExternalInput").ap()

    def dscr(name, shape, dt):
        return nc.dram_tensor(name, list(shape), dt, kind=("ExternalOutput" if dbg else "Internal")).ap()

    x_d = din("x", [L, D]); ctx_d = din("ctx", [CTXL, D]); cvec_d = din("cvec", [128, 8, 2])
    wmod_d = din("w_mod", [D, 6 * D]); bmodT_d = din("bmodT", [128, 48]); bmodr_d = din("bmodr", [1, 6 * D])
    n1T_d = din("n1T", [128, 8]); n2T_d = din("n2T", [128, 8]); fnw_d = din("fnw", [1, D])
    win_d = din("w_in", [D, INC]); convw_d = din("convw", [128, 20, 5]); convb_d = din("convb", [128, 20])
    dtb_d = din("dtb", [1, 48]); alog_d = din("alog", [1, 48]); dvec_d = din("dvec", [1, SSDW])
    ynw_d = din("ynw", [128, 12]); poolw_d = din("poolw", [128, 4, 128]); pscale_d = din("pscale", [128, 4])
    wout_d = din("w_out", [2 * D, D]); rw_d = din("rw", [128, 8, NE]); rb_d = din("rb", [1, NE])
    W1G_d = din("W1G", [NE * 128, 8 * D]); W1U_d = din("W1U", [NE * 128, 8 * D]); W2_d = din("W2", [NE * 128, 8 * D])
    B1_d = din("B1", [NE * 128, 16]); b2_d = din("b2", [NE, D]); n2r_d = din("n2r", [1, D])
    cSU_d = din("cSU", [128, 128]); cIota_d = din("cIota", [1, NE]); cJv_d = din("cJv", [1, NBLK]); cPidx_d = din("cPidx", [128, 1])
    cI_d = din("cI", [128, 128]); cU_d = din("cU", [128, 128]); cLo_d = din("cLo", [128, 128])
    cMask_d = din("cMask", [128, 2, 128]); cSel_d = din("cSel", [6, 6, 128]); cAT_d = din("cAT", [128, 4, 128])
    out_d = nc.dram_tensor("out", [L, D], F32, kind="ExternalOutput").ap()
    rawT_d = dscr("rawT", [CONVCH, RAWW], BF16); zt_d = dscr("zt", [L, SSDW], BF16)
    ypT_d = dscr("ypT", [512, L], BF16); yzT_d = dscr("yzT", [SSDW, L], BF16)
    x1_d = dscr("x1", [L, D], F32); h2tok_d = dscr("h2tok", [L, D], BF16)
    Xg_d = nc.dram_tensor("Xg", [NSLOT, D], BF16, kind="Internal").ap(); Y_d = nc.dram_tensor("Y", [NSLOT, D], F32, kind="Internal").ap()

    def stop(tag):
        if upto == tag:
            S.barrier()
            raise _Stop(nc)

    def T(es, name, shape, dt=F32):
        return es.enter_context(nc.sbuf_tensor("t_" + name, list(shape), dt))

    def PS(es, name, shape, dt=F32):
        return es.enter_context(nc.psum_tensor("p_" + name, list(shape), dt))

    def mm(out, lhsT, rhs, start, stop, reads, writes, inc=True, sgc=False):
        return S.op('pe', lambda: PE.matmul(out, lhsT=lhsT, rhs=rhs, start=start, stop=stop, skip_group_check=sgc), reads, writes, inc)

    def tr(out, in_, ident, reads, writes, inc=True):
        return S.op('pe', lambda: PE.transpose(out=out, in_=in_, identity=ident), reads, writes, inc)

    def act(out, in_, func, reads, writes, bias=None, scale=None):
        kw = {}
        if bias is not None:
            kw['bias'] = bias
        if scale is not None:
            kw['scale'] = scale
        return S.op('act', lambda: A.activation(out=out, in_=in_, func=func, **kw), reads, writes)

    def tt(e, out, in0, in1, op, reads, writes):
        eng = V if e == 'dve' else G
        return S.op(e, lambda: eng.tensor_tensor(out=out, in0=in0, in1=in1, op=op), reads, writes)

    def ts(e, out, in0, s1, op0, reads, writes, s2=None, op1=None):
        eng = V if e == 'dve' else G
        if op1 is None:
            return S.op(e, lambda: eng.tensor_scalar(out=out, in0=in0, scalar1=s1, scalar2=None, op0=op0), reads, writes)
        return S.op(e, lambda: eng.tensor_scalar(out=out, in0=in0, scalar1=s1, scalar2=s2, op0=op0, op1=op1), reads, writes)

    def cp(e, out, in_, reads, writes):
        if e == 'act':
            return S.op('act', lambda: A.activation(out=out, in_=in_, func=AF.Copy), reads, writes)
        eng = V if e == 'dve' else G
        return S.op(e, lambda: eng.tensor_copy(out=out, in_=in_), reads, writes)

    P0 = es0
    identf = T(P0, "identf", [128, 128]); identb = T(P0, "identb", [128, 128], BF16)
    g_bc = T(P0, "g_bc", [128, 2, D]); fnw_bc = T(P0, "fnw_bc", [128, D])
    s1 = T(P0, "s1", [128, 8]); sh1 = T(P0, "sh1", [128, 8]); cs1 = T(P0, "cs1", [128, 8]); csh1 = T(P0, "csh1", [128, 8])
    s2 = T(P0, "s2", [128, 8]); sh2 = T(P0, "sh2", [128, 8])
    G_all = T(P0, "G_all", [128, NT, NE]); ssq = T(P0, "ssq", [128, 4, NT])
    modrow_d = nc.dram_tensor("modrow", [2, 128, D], F32, kind="Internal").ap()
    S.dma('sp', identf[:], cI_d[:, :], writes=['identf'], sem='c0')
    S.dma('sp', fnw_bc[:], fnw_d.partition_broadcast(128), writes=['fnw_bc'], sem='c0')
    S.regroup('c0', ['identf', 'fnw_bc'])
    cp('dve', identb[:], identf[:], ['identf'], ['identb'])

    esAB = ExitStack()
    dtr_all = T(esAB, "dtr_all", [128, NCH, 48])
    esW = ExitStack()
    win = T(esW, "win", [128, 8, INC], BF16)
    for k in range(8):
        S.dma('pool', win[:, k, :], win_d[k * 128:(k + 1) * 128, :], writes=['win'], sem='win')
    with ExitStack() as es:
        silu_c = T(es, "silu_c", [128, 8, 2]); cbc = T(es, "cbc", [128, 8, 128]); ones = T(es, "ones1", [128, 128])
        wm2 = [T(es, "wm%d" % i, [128, 8, D]) for i in range(2)]; modT = T(es, "modT", [128, 48, 2]); bmodT = T(es, "bmodT_s", [128, 48])
        bm_bc = T(es, "bm_bc", [128, D]); n1T = T(es, "n1T_s", [128, 8]); n2T = T(es, "n2T_s", [128, 8])
        tmp8 = T(es, "tmp8", [128, 8]); n2r_bc = T(es, "n2r_bc", [128, D])
        s2_bc = T(es, "s2_bc1", [128, D]); sh2_bc = T(es, "sh2_bc1", [128, D])
        S.dma('sp', n2r_bc[:], n2r_d.partition_broadcast(128), writes=['n2r_bc'])
        pm = PS(es, "pm", [128, 512]); pg = [PS(es, "pg0", [128, 512]), PS(es, "pg1", [128, 512])]
        S.dma('sp', silu_c[:], cvec_d[:, :, :], writes=['silu_c'], sem='c1')
        S.dma('sp', bmodT[:], bmodT_d[:, :], writes=['bmodT'], sem='c1')
        S.dma('sp', n1T[:], n1T_d[:, :], writes=['n1T'], sem='c1')
        S.dma('sp', n2T[:], n2T_d[:, :], writes=['n2T'], sem='c1')
        S.regroup('c1', ['silu_c', 'bmodT', 'n1T', 'n2T'])
        act(silu_c[:], silu_c[:], AF.Silu, ['silu_c'], ['silu_c'])
        S.op('dve', lambda: V.memset(ones[:], 1.0), writes=['ones1'])
        for k in range(8):
            ts('dve', cbc[:, k, :], ones[:], silu_c[:, k, 0:1], ALU.mult, ['ones1', 'silu_c'], ['cbc'])
        wm_v = wmod_d.rearrange("(k p) c -> p k c", p=128)
        S.dma('sp', wm2[0][:], wm_v[:, :, 0:D], writes=['wm0'], sem='wm0')
        for s in range(6):
            wm = wm2[s % 2]; wk = 'wm%d' % (s % 2)
            if s + 1 < 6:
                S.dma('sp', wm2[(s + 1) % 2][:], wm_v[:, :, (s + 1) * D:(s + 2) * D], writes=['wm%d' % ((s + 1) % 2)], sem='wm%d' % ((s + 1) % 2))
            if s in (2, 3, 4, 5):
                S.dma('sp', bm_bc[:], bmodr_d[:, s * D:(s + 1) * D].partition_broadcast(128), writes=['bm_bc'])
                for half in range(2):
                    hc = slice(half * 512, (half + 1) * 512)
                    for k in range(8):
                        mm(pg[half][:, :], cbc[:, k, :], wm[:, k, hc], k == 0, k == 7,
                           ['cbc', wk], ['pg%d' % half], inc=(k == 7))
                    dst = {2: g_bc[:, 0, hc], 5: g_bc[:, 1, hc], 3: sh2_bc[:, hc], 4: s2_bc[:, hc]}[s]
                    dk = 'g_bc' if s in (2, 5) else 's2_bc'
                    tt('dve', dst, pg[half][:, :], bm_bc[:, hc], ALU.add, ['pg%d' % half, 'bm_bc'], [dk])
                    if s == 4:
                        ts('dve', dst, dst, 1.0, ALU.add, [dk], [dk])
                        tt('dve', dst, dst, n2r_bc[:, hc], ALU.mult, [dk, 'n2r_bc'], [dk])
            if s not in (2, 5):
                for j in range(8):
                    for k in range(8):
                        mm(pm[:, j * 2:j * 2 + 2], wm[:, k, j * 128:(j + 1) * 128], silu_c[:, k, :], k == 0, k == 7,
                           [wk, 'silu_c'], ['pm'], inc=(k == 7))
                tt('dve', modT[:, s * 8:(s + 1) * 8, :], pm[:, 0:16].rearrange("p (j t) -> p j t", t=2),
                   bmodT[:, s * 8:(s + 1) * 8].unsqueeze(2).to_broadcast([128, 8, 2]), ALU.add, ['pm', 'bmodT'], ['modT'])
        for (dst, nT, sc_lo, col) in ((s1, n1T, 8, 0), (cs1, n1T, 8, 1), (s2, n2T, 32, 0)):
            ts('dve', tmp8[:], modT[:, sc_lo:sc_lo + 8, col], 1.0, ALU.add, ['modT'], ['tmp8'])
            tt('dve', dst[:], tmp8[:], nT[:], ALU.mult, ['tmp8', 'n1T', 'n2T'], ['modv'])
        cp('dve', sh1[:], modT[:, 0:8, 0], ['modT'], ['modv'])
        cp('dve', csh1[:], modT[:, 0:8, 1], ['modT'], ['modv'])
        cp('dve', sh2[:], modT[:, 24:32, 0], ['modT'], ['modv'])
        S.dma('sp', modrow_d[0], s2_bc[:], reads=['s2_bc'])
        S.dma('sp', modrow_d[1], sh2_bc[:], reads=['s2_bc'], sem='S_s2_bc')
        S.barrier()

    if upto == '1':
        esW.close(); esAB.close(); es0.close(); return nc

    def rms_to_hT(es_tiles, src_ap, s_vec, sh_vec, dst3, ps_T, names):
        xt, sq, ss, xn, tmp = es_tiles
        (kx, ksq, kss, kxn, ktmp, kps, kdst) = names
        S.dma('sp', xt, src_ap, writes=[kx], sem=kx)
        act(sq, xt, AF.Square, [kx], [ksq])
        S.op('dve', lambda: V.reduce_sum(out=ss[:, 0:1], in_=sq, axis=AX.X), [ksq], [kss])
        act(ss[:, 1:2], ss[:, 0:1], AF.Ln, [kss], [kss], bias=EPS, scale=1.0 / D)
        act(ss[:, 2:3], ss[:, 1:2], AF.Exp, [kss], [kss], scale=-0.5)
        ts('dve', xn, xt, ss[:, 2:3], ALU.mult, [kx, kss], [kxn])
        for k in range(8):
            tr(ps_T[:, k * 128:(k + 1) * 128], xn[:, k * 128:(k + 1) * 128], identf[:], [kxn, 'identf'], [kps], inc=(k == 7))
        tt('dve', tmp, ps_T[:, :].rearrange("p (k t) -> p k t", t=128), s_vec[:, :].unsqueeze(2).to_broadcast([128, 8, 128]),
           ALU.mult, [kps, 'modv'], [ktmp])
        tt('pool', dst3, tmp, sh_vec[:, :].unsqueeze(2).to_broadcast([128, 8, 128]), ALU.add, [ktmp, 'modv'], [kdst])

    with ExitStack() as es:
        dtb_bc = T(es, "dtb_bc", [128, 48]); AT = T(es, "AT", [128, 4, 128], BF16); ATf = T(es, "ATf", [128, 4, 128])
        poolw = T(es, "poolw", [128, 4, 128], BF16); poolwf = T(es, "poolwf", [128, 4, 128]); pscale = T(es, "pscale", [128, 4])
        zpad = T(es, "zpad", [128, 20, 4], BF16)
        S.dma('sp', dtb_bc[:], dtb_d.partition_broadcast(128), writes=['dtb_bc'], sem='c2')
        S.dma('sp', ATf[:], cAT_d[:, :, :], writes=['ATf'], sem='c2')
        S.dma('sp', poolwf[:], poolw_d[:, :, :], writes=['poolwf'], sem='c2')
        S.dma('sp', pscale[:], pscale_d[:, :], writes=['pscale'], sem='c2')
        S.regroup('c2', ['dtb_bc', 'ATf', 'poolwf', 'pscale'])
        cp('dve', AT[:], ATf[:], ['ATf'], ['AT']); cp('dve', poolw[:], poolwf[:], ['poolwf'], ['poolw'])
        S.op('dve', lambda: V.memset(zpad[:], 0.0), writes=['zpad'])
        zrow = T(es, "zrow", [128, 4, D], BF16)
        S.op('pool', lambda: G.memset(zrow[:], 0.0), writes=['zrow'])
        Xg_v0 = Xg_d.rearrange("(b j p) d -> b p j d", p=128, j=4)
        for blk in range(NSLOT // 512):
            S.dma('act', Xg_v0[blk], zrow[:], reads=['zrow'], sem='xgz')
        raw_v = rawT_d.rearrange("(t p) w -> p t w", p=128)
        S.dma('sp', raw_v[:, :, 0:2], zpad[:, :, 0:2], reads=['zpad'])
        S.dma('sp', raw_v[:, :, 4098:4102], zpad[:, :, 0:4], reads=['zpad'])
        S.dma('sp', raw_v[:, :, 4358:4360], zpad[:, :, 0:2], reads=['zpad'])
        xt_ = [T(es, "xt%d" % i, [128, D]) for i in range(2)]; sq_ = T(es, "sq", [128, D]); ss_ = [T(es, "ss%d" % i, [128, 4]) for i in range(2)]
        xn_ = [T(es, "xn%d" % i, [128, D]) for i in range(2)]; tmpm = T(es, "tmpm", [128, 8, 128])
        hT = [T(es, "hT%d" % i, [128, 8, 512], BF16) for i in range(2)]
        rawblk = [T(es, "rawblk0", [128, 20, 512], BF16)] * 2
        zsb = [T(es, "zsb%d" % i, [128, SSDW], BF16) for i in range(2)]
        usb = [T(es, "usb%d" % i, [128, 512], BF16) for i in range(2)]
        plsb = [T(es, "plsb%d" % i, [128, 4, 128], BF16) for i in range(2)]
        ypsb = [T(es, "ypsb%d" % i, [128, 4, 128], BF16) for i in range(2)]
        pT = PS(es, "pT", [128, 1024]); pfm = [PS(es, "pfm%d" % i, [128, 512]) for i in range(2)]
        ptm = [PS(es, "ptm%d" % i, [128, 512]) for i in range(2)]; ppl = PS(es, "ppl", [128, 512]); pyp = PS(es, "pyp", [128, 512])
        rx = Ring('x', 2); rfm = Ring('fm', 2); rtm = Ring('tm', 2); rz = Ring('z', 2)
        ypT_v = ypT_d.rearrange("(g o) t -> o g t", o=128)
        def RMS_(blk):
            ntile = 4 if blk < 8 else 2
            hs = blk % 2
            for j in range(ntile):
                i = rx.next()
                src = x_d[blk * 512 + j * 128: blk * 512 + (j + 1) * 128, :] if blk < 8 else ctx_d[j * 128:(j + 1) * 128, :]
                rms_to_hT((xt_[i][:], sq_[:], ss_[i], xn_[i][:], tmpm[:]), src,
                          s1 if blk < 8 else cs1, sh1 if blk < 8 else csh1, hT[hs][:, :, j * 128:(j + 1) * 128], pT,
                          ('xt%d' % i, 'sq', 'ss%d' % i, 'xn%d' % i, 'tmpm', 'pT', 'hT%d' % hs))

        RMS_(0)
        for blk in range(9):
            ntile = 4 if blk < 8 else 2
            ntok = ntile * 128
            hs = blk % 2
            if blk + 1 < 9:
                RMS_(blk + 1)
            for t in range(20):
                b = rfm.next()
                for k in range(8):
                    mm(pfm[b][:, 0:ntok], win[:, k, t * 128:(t + 1) * 128], hT[hs][:, k, 0:ntok], k == 0, k == 7,
                       ['win', 'hT%d' % hs], ['pfm%d' % b], inc=(k == 7))
                cp('act' if t % 2 == 0 else 'dve', rawblk[hs][:, t, 0:ntok], pfm[b][:, 0:ntok], ['pfm%d' % b], ['rawblk0'])
            off = 2 + 512 * blk if blk < 8 else 4102
            S.dma('sp', raw_v[:, :, off:off + ntok], rawblk[hs][:, :, 0:ntok], reads=['rawblk0'])
            for j in range(ntile):
                chunk = blk * 4 + j
                lt = hT[hs][:, :, j * 128:(j + 1) * 128]
                b = rtm.next()
                for k in range(8):
                    mm(ptm[b][:, 0:48], lt[:, k, :], win[:, k, CONVCH:CONVCH + 48], k == 0, k == 7, ['win', 'hT%d' % hs], ['ptm%d' % b], inc=(k == 7))
                tt('dve', dtr_all[:, chunk, :], ptm[b][:, 0:48], dtb_bc[:], ALU.add, ['ptm%d' % b, 'dtb_bc'], ['dtr_all'])
                if blk == 8:
                    continue
                zi = rz.next()
                for q in range(3):
                    b = rtm.next()
                    c0 = 2608 + q * 512
                    for k in range(8):
                        mm(ptm[b][:, :], lt[:, k, :], win[:, k, c0:c0 + 512], k == 0, k == 7, ['win', 'hT%d' % hs], ['ptm%d' % b], inc=(k == 7))
                    act(zsb[zi][:, q * 512:(q + 1) * 512], ptm[b][:, :], AF.Silu, ['ptm%d' % b], ['zsb%d' % zi])
                S.dma('sp', zt_d[chunk * 128:(chunk + 1) * 128, :], zsb[zi][:], reads=['zsb%d' % zi])
                b = rtm.next()
                for k in range(8):
                    mm(ptm[b][:, :], lt[:, k, :], win[:, k, 4144:4656], k == 0, k == 7, ['win', 'hT%d' % hs], ['ptm%d' % b], inc=(k == 7))
                cp('act', usb[zi][:], ptm[b][:, :], ['ptm%d' % b], ['usb%d' % zi])
                for g in range(4):
                    mm(ppl[:, g * 128:(g + 1) * 128], usb[zi][:, g * 128:(g + 1) * 128], AT[:, g, :], True, True, ['usb%d' % zi, 'AT'], ['ppl'], inc=(g == 3))
                cp('dve', plsb[zi][:], ppl[:, :].rearrange("p (g t) -> p g t", t=128), ['ppl'], ['plsb%d' % zi])
                for g in range(4):
                    mm(pyp[:, g * 128:(g + 1) * 128], poolw[:, g, :], plsb[zi][:, g, :], True, True, ['poolw', 'plsb%d' % zi], ['pyp'], inc=(g == 3))
                tt('dve', ypsb[zi][:], pyp[:, :].rearrange("p (g t) -> p g t", t=128), pscale[:, :].unsqueeze(2).to_broadcast([128, 4, 128]),
                   ALU.mult, ['pyp', 'pscale'], ['ypsb%d' % zi])
                S.dma('sp', ypT_v[:, :, chunk * 128:(chunk + 1) * 128], ypsb[zi][:], reads=['ypsb%d' % zi])
        S.barrier()

    esW.close()
    if upto == 'A':
        esAB.close(); es0.close(); return nc
    with ExitStack() as es:
        convw = T(es, "convw", [128, 20, 5]); convb = T(es, "convb", [128, 20]); Dvec = T(es, "Dvec", [128, SSDW])
        alog_bc = T(es, "alog_bc", [128, 48]); negA = T(es, "negA", [128, 48]); ynw = T(es, "ynw", [128, 12])
        cU = T(es, "cU", [128, 128]); cLo = T(es, "cLo", [128, 128]); ones = T(es, "ones2", [128, 128])
        cMask = T(es, "cMask", [128, 2, 128]); cSel = T(es, "cSel", [6, 6, 128])
        S.dma('sp', convw[:], convw_d[:, :, :], writes=['convw'], sem='c3')
        S.dma('sp', convb[:], convb_d[:, :], writes=['convb'], sem='c3')
        S.dma('sp', Dvec[:], dvec_d.partition_broadcast(128), writes=['Dvec'], sem='c3')
        S.dma('sp', alog_bc[:], alog_d.partition_broadcast(128), writes=['alog'], sem='c3')
        S.dma('sp', ynw[:], ynw_d[:, :], writes=['ynw'], sem='c3')
        S.dma('sp', cU[:], cU_d[:, :], writes=['cU'], sem='c3')
        S.dma('sp', cLo[:], cLo_d[:, :], writes=['cLo'], sem='c3')
        S.dma('sp', cMask[:], cMask_d[:, :, :], writes=['cMask'], sem='c3')
        S.dma('sp', cSel[:], cSel_d[:, :, :], writes=['cSel'], sem='c3')
        S.regroup('c3', ['convw', 'convb', 'Dvec', 'alog', 'ynw', 'cU', 'cLo', 'cMask', 'cSel'])
        S.op('dve', lambda: V.memset(ones[:], 1.0), writes=['ones2'])
        cMaskb = T(es, "cMaskb", [128, 2, 128], BF16); cSelb = T(es, "cSelb", [6, 6, 128], BF16)
        cp('dve', cMaskb[:], cMask[:], ['cMask'], ['cMaskb']); cp('dve', cSelb[:], cSel[:], ['cSel'], ['cSelb'])
        act(negA[:], alog_bc[:], AF.Exp, ['alog'], ['negA'])
        ts('dve', negA[:], negA[:], -1.0, ALU.mult, ['negA'], ['negA'])
        rawg = T(es, "rawg", [128, 5, RAWW], BF16)
        BT = T(es, "BT", [128, NCH * 128], BF16); CT = T(es, "CT", [128, NCH * 128], BF16)
        x_tok = T(es, "x_tok", [128, NCH, 384], BF16); B_tok = T(es, "B_tok", [128, NCH, 128], BF16)
        Sb_all = T(es, "Sb_all", [128, NT, 384], BF16)
        dg = [T(es, "dg%d" % i, [128, 5, 128], BF16) for i in range(2)]
        xc = [T(es, "xc%d" % i, [128, 512], BF16) for i in range(2)]
        dtg = T(es, "dtg", [128, NCH, 12]); av = T(es, "av", [128, NCH, 12]); lndt = T(es, "lndt", [128, NCH, 12])
        cs_all = T(es, "cs_all", [128, NCH, 12]); tot_all = T(es, "tot_all", [128, NCH, 12]); nb = T(es, "nb", [128, NCH, 12])
        eoff = T(es, "eoff", [128, NCH, 12]); wst = T(es, "wst", [128, NCH, 12]); dch = T(es, "dch", [128, NCH, 12])
        Srun = [T(es, "Srun%d" % i, [128, 384]) for i in range(3)]
        Sfb = [T(es, "Sfb%d" % i, [128, 384], BF16) for i in range(2)]
        xd = [T(es, "xd%d" % i, [128, 384], BF16) for i in range(4)]
        CBt = [T(es, "CBt%d" % i, [128, 128], BF16) for i in range(2)]
        csTh = [T(es, "csTh%d" % i, [6, 2, 128], BF16) for i in range(2)]; csTl = [T(es, "csTl%d" % i, [6, 2, 128], BF16) for i in range(2)]
        Lm = [T(es, "Lm%d" % i, [128, 128], BF16) for i in range(8)]
        Mt = [T(es, "Mt%d" % i, [128, 128], BF16) for i in range(8)]
        t1 = [T(es, "t1_%d" % i, [128, 384]) for i in range(2)]; t2 = T(es, "t2", [128, 384]); t3 = T(es, "t3", [128, 384])
        zg = [T(es, "zg%d" % i, [128, 384], BF16) for i in range(2)]; sz = T(es, "sz", [128, 384]); sqj = T(es, "sqj", [128, 384])
        yzb = [T(es, "yzb%d" % i, [128, 384], BF16) for i in range(2)]; yzTs = [T(es, "yzTs%d" % i, [128, 3, 128], BF16) for i in range(2)]
        pf = [PS(es, "pf%d" % i, [128, 512]) for i in range(7)]; pb = PS(es, "pb", [128, 1024], BF16)
        raw_rows = rawT_d.rearrange("(t p) w -> t p w", p=128)
        yzT_v = yzT_d.rearrange("(i p) t -> p i t", p=128)
        rdg = Ring('dg', 2); rcv = Ring('cv', 2); rxc = Ring('xc', 2)
        for g in range(4):
            tiles = [3 * g, 3 * g + 1, 3 * g + 2, 12 + g, 16 + g]
            if g == 0:
                for ti, Tt in enumerate(tiles):
                    S.dma('sp', rawg[:, ti, :], raw_rows[Tt, :, :], writes=['rawg%d' % ti], sem='rawg')
                S.regroup('rawg', ['rawg%d' % ti for ti in range(5)])
            units = [(ti, Tt, blk) for ti, Tt in enumerate(tiles) for blk in range(9)]
            ustate = {}

            def BP_(u):
                ti, Tt, blk = units[u]
                if blk == 0:
                    di = rdg.next()
                    for j in range(5):
                        ts('dve', dg[di][:, j, :], identb[:], convw[:, Tt, j:j + 1], ALU.mult, ['identb', 'convw'], ['dg%d' % di])
                    ustate['di'] = di
                di = ustate['di']
                n = 512 if blk < 8 else 256
                off = (2 + 512 * blk) if blk < 8 else 4102
                tok0 = 512 * blk
                b = rcv.next()
                for j in range(5):
                    mm(pf[b][:, 0:n], dg[di][:, j, :], rawg[:, ti, off - 2 + j: off - 2 + j + n], j == 0, j == 4,
                       ['dg%d' % di, 'rawg%d' % ti], ['pf%d' % b], inc=(j == 4))
                if ti < 3:
                    xi = rxc.next()
                    dst = xc[xi][:, 0:n]; dkey = 'xc%d' % xi
                elif ti == 3:
                    dst = BT[:, tok0:tok0 + n]; dkey = 'BT%d' % blk
                else:
                    dst = CT[:, tok0:tok0 + n]; dkey = 'CT%d' % blk
                act(dst, pf[b][:, 0:n], AF.Silu, ['pf%d' % b, 'convb'], [dkey], bias=convb[:, Tt:Tt + 1])
                ustate[u] = (dst, dkey, n)

            def BQ_(u):
                ti, Tt, blk = units[u]
                dst, dkey, n = ustate.pop(u)
                if ti <= 3:
                    nj = n // 128
                    for jj in range(nj):
                        tr(pb[:, jj * 128:(jj + 1) * 128], dst[:, jj * 128:(jj + 1) * 128], identb[:], [dkey, 'identb'], ['pb'], inc=(jj == nj - 1))
                    src3 = pb[:, 0:n].rearrange("p (j c) -> p j c", c=128)
                    if ti < 3:
                        cp('dve', x_tok[:, 4 * blk:4 * blk + nj, ti * 128:(ti + 1) * 128], src3, ['pb'], ['x_tok'])
                    else:
                        cp('dve', B_tok[:, 4 * blk:4 * blk + nj, :], src3, ['pb'], ['B_tok'])

            BP_(0)
            for u in range(len(units)):
                if u + 1 < len(units):
                    BP_(u + 1)
                BQ_(u)
            S.barrier()
            stop('B1')
            if g + 1 < 4:
                for ti, Tt in enumerate([3 * (g + 1), 3 * (g + 1) + 1, 3 * (g + 1) + 2, 12 + g + 1, 16 + g + 1]):
                    S.dma('sp', rawg[:, ti, :], raw_rows[Tt, :, :], writes=['rawg%d' % ti], sem='rawg')
                S.regroup('rawg', ['rawg%d' % ti for ti in range(5)])
            for d in range(2):
                cp('dve', dtg[:, :, d * 6:(d + 1) * 6], dtr_all[:, :, d * 24 + g * 6: d * 24 + g * 6 + 6], ['dtr_all'], ['dtg'])
            act(dtg[:], dtg[:], AF.Exp, ['dtg'], ['dtg'])
            act(dtg[:], dtg[:], AF.Ln, ['dtg'], ['dtg'], bias=1.0, scale=1.0)
            act(lndt[:], dtg[:], AF.Ln, ['dtg'], ['lndt'])
            for d in range(2):
                tt('dve', av[:, :, d * 6:(d + 1) * 6], dtg[:, :, d * 6:(d + 1) * 6],
                   negA[:, d * 24 + g * 6: d * 24 + g * 6 + 6].unsqueeze(1).to_broadcast([128, NCH, 6]), ALU.mult, ['dtg', 'negA'], ['av'])
            for c in range(NCH):
                last = (c == NCH - 1)
                mm(pf[4][:, c * 12:c * 12 + 6], cU[:], av[:, c, 0:6], True, True, ['cU', 'av'], ['pf4'], inc=False)
                mm(pf[4][:, c * 12 + 6:c * 12 + 12], cLo[:], av[:, c, 6:12], True, True, ['cLo', 'av'], ['pf4'], inc=False)
                mm(pf[5][:, c * 12:c * 12 + 12], ones[:], av[:, c, :], True, True, ['ones2', 'av'], ['pf5'], inc=last)
            cp('dve', cs_all[:], pf[4][:, 0:NCH * 12].rearrange("p (c h) -> p c h", h=12), ['pf4'], ['cs_all'])
            cp('dve', tot_all[:], pf[5][:, 0:NCH * 12].rearrange("p (c h) -> p c h", h=12), ['pf5'], ['tot_all'])
            tt('dve', nb[:], lndt[:], cs_all[:], ALU.subtract, ['lndt', 'cs_all'], ['nb'])
            act(eoff[:], cs_all[:], AF.Exp, ['cs_all'], ['eoff'])
            tt('dve', wst[:], tot_all[:], nb[:], ALU.add, ['tot_all', 'nb'], ['wst'])
            act(wst[:], wst[:], AF.Exp, ['wst'], ['wst'])
            act(dch[:], tot_all[:], AF.Exp, ['tot_all'], ['dch'])

            stop('B2')
            rxd = Ring('xd', 4)

            def chunk_state(c, d, bank=6):
                i = rxd.next()
                tt('dve', xd[i][:].rearrange("p (h q) -> p h q", q=64), x_tok[:, c, :].rearrange("p (h q) -> p h q", q=64),
                   wst[:, c, d * 6:(d + 1) * 6].unsqueeze(2).to_broadcast([128, 6, 64]), ALU.mult, ['x_tok', 'wst'], ['xd%d' % i])
                mm(pf[bank][:, 0:384], B_tok[:, c, :], xd[i][:], True, True, ['B_tok', 'xd%d' % i], ['pf%d' % bank])

            def dec_bc(c, d):
                return dch[:, c, d * 6:(d + 1) * 6].unsqueeze(2).to_broadcast([128, 6, 64])

            def v3(ap):
                return ap.rearrange("p (h q) -> p h q", q=64)

            for d, (ca, cb_) in enumerate(((32, 33), (33, 32))):
                chunk_state(ca, d)
                cp('dve', Srun[d][:], pf[6][:, 0:384], ['pf6'], ['Srun%d' % d])
                tt('dve', v3(Srun[d][:]), v3(Srun[d][:]), dec_bc(cb_, d), ALU.mult, ['Srun%d' % d, 'dch'], ['Srun%d' % d])
                chunk_state(cb_, d)
                tt('dve', Srun[d][:], Srun[d][:], pf[6][:, 0:384], ALU.add, ['Srun%d' % d, 'pf6'], ['Srun%d' % d])
            stop('B3')
            order = list(range(NT - 1, -1, -1))
            banks = [3, 4, 5, 6]
            AHEAD = 3
            for n in range(min(AHEAD, NT)):
                chunk_state(order[n], 1, banks[n % 4])
            cur = 1
            for n, c in enumerate(order):
                if n + AHEAD < NT:
                    chunk_state(order[n + AHEAD], 1, banks[(n + AHEAD) % 4])
                nxt = 2 if cur == 1 else 1
                cp('act', Sb_all[:, c, :], Srun[cur][:], ['Srun%d' % cur], ['Sb_all%d' % c])
                tt('dve', v3(Srun[nxt][:]), v3(Srun[cur][:]), dec_bc(c, 1), ALU.mult, ['Srun%d' % cur, 'dch'], ['Srun%d' % nxt])
                bk = banks[n % 4]
                tt('dve', Srun[nxt][:], Srun[nxt][:], pf[bk][:, 0:384], ALU.add, ['Srun%d' % nxt, 'pf%d' % bk], ['Srun%d' % nxt])
                cur = nxt
            S.barrier()
            stop('B4')
            rL = Ring('L', 2); rq = Ring('q', 8)
            pairs = [(d, h) for d in range(2) for h in range(6)]

            def H_(c, ci):
                tk = slice(c * 128, (c + 1) * 128)
                cp('act', Sfb[ci][:], Srun[0][:], ['Srun0'], ['Sfb%d' % ci])
                mm(pf[0][:, 0:128], BT[:, tk], CT[:, tk], True, True, ['BT%d' % (c // 4), 'CT%d' % (c // 4)], ['pf0'], inc=False)
                mm(pf[0][0:6, 128:256], av[:, c, 0:6], cU[:], True, True, ['av', 'cU'], ['pf0'], inc=False)
                mm(pf[0][0:6, 256:384], av[:, c, 6:12], cLo[:], True, True, ['av', 'cLo'], ['pf0'])
                cp('dve', CBt[ci][:], pf[0][:, 0:128], ['pf0'], ['CBt%d' % ci])
                src = pf[0][0:6, 128:384].rearrange("p (d l) -> p d l", l=128)
                cp('dve', csTh[ci][:], src, ['pf0'], ['csTh%d' % ci])
                tt('dve', csTl[ci][:], src, csTh[ci][:], ALU.subtract, ['pf0', 'csTh%d' % ci], ['csTl%d' % ci])

            def L_(c, ci, bt):
                lb = 1 + rL.next()
                bk = 'pf%d' % lb
                for q in range(4):
                    d, h = pairs[bt * 4 + q]
                    reg = pf[lb][:, q * 128:(q + 1) * 128]
                    mm(reg, cSelb[0:6, h, :], csTh[ci][0:6, d, :], True, False, ['cSelb', 'csTh%d' % ci], [bk], inc=False)
                    mm(reg, cSelb[0:6, h, :], csTl[ci][0:6, d, :], False, False, ['cSelb', 'csTl%d' % ci], [bk], inc=False)
                    mm(reg, identb[:], cMaskb[:, d, :], False, True, ['identb', 'cMaskb'], [bk], inc=(q == 3))
                return lb

            def E_(c, ci, bt, lb):
                bk = 'pf%d' % lb
                qis = []
                for q in range(4):
                    d, h = pairs[bt * 4 + q]
                    reg = pf[lb][:, q * 128:(q + 1) * 128]
                    qi = rq.next()
                    act(Lm[qi][:], reg, AF.Exp, [bk, 'nb'], ['Lm%d' % qi], bias=nb[:, c, d * 6 + h: d * 6 + h + 1])
                    tt('dve', Mt[qi][:], Lm[qi][:], CBt[ci][:], ALU.mult, ['Lm%d' % qi, 'CBt%d' % ci], ['Mt%d' % qi])
                    qis.append(qi)
                return qis

            def Y_(c, bt, qis):
                for q in range(4):
                    d, h = pairs[bt * 4 + q]
                    qi = qis[q]
                    mm(pf[3][:, h * 64:(h + 1) * 64], Mt[qi][:], x_tok[:, c, h * 64:(h + 1) * 64], (bt == 0 and q == 0), (bt == 2 and q == 3),
                       ['Mt%d' % qi, 'x_tok'], ['pf3'], inc=(q == 3), sgc=True)

            def O_(c, ci):
                tk = slice(c * 128, (c + 1) * 128)
                mm(pf[4][:, 0:384], CT[:, tk], Sfb[ci][:], True, True, ['CT%d' % (c // 4), 'Sfb%d' % ci], ['pf4'])
                mm(pf[5][:, 0:384], CT[:, tk], Sb_all[:, c, :], True, True, ['CT%d' % (c // 4), 'Sb_all%d' % c], ['pf5'])

            def U_(c):
                chunk_state(c, 0)
                tt('dve', v3(Srun[0][:]), v3(Srun[0][:]), dec_bc(c, 0), ALU.mult, ['Srun0', 'dch'], ['Srun0'])
                tt('dve', Srun[0][:], Srun[0][:], pf[6][:, 0:384], ALU.add, ['Srun0', 'pf6'], ['Srun0'])

            def F1_(c, ci):
                k1 = 't1_%d' % ci
                S.dma('sp', zg[ci][:], zt_d[c * 128:(c + 1) * 128, g * 384:(g + 1) * 384], writes=['zg%d' % ci], sem='zg%d' % ci)
                tt('dve', v3(t1[ci][:]), v3(pf[4][:, 0:384]), eoff[:, c, 0:6].unsqueeze(2).to_broadcast([128, 6, 64]), ALU.mult, ['pf4', 'eoff'], [k1])
                tt('dve', v3(t2[:]), v3(pf[5][:, 0:384]), eoff[:, c, 6:12].unsqueeze(2).to_broadcast([128, 6, 64]), ALU.mult, ['pf5', 'eoff'], ['t2'])
                tt('dve', t1[ci][:], pf[3][:, 0:384], t1[ci][:], ALU.add, ['pf3', k1], [k1])
                tt('pool', t3[:], x_tok[:, c, :], Dvec[:, g * 384:(g + 1) * 384], ALU.mult, ['x_tok', 'Dvec'], ['t3'])
                tt('pool', t2[:], t2[:], t3[:], ALU.add, ['t2', 't3'], ['t2'])
                tt('pool', t1[ci][:], t1[ci][:], t2[:], ALU.add, [k1, 't2'], [k1])

            def F2_(c, ci):
                k1 = 't1_%d' % ci
                tt('dve', t1[ci][:], t1[ci][:], zg[ci][:], ALU.mult, [k1, 'zg%d' % ci], [k1])
                tt('pool', sqj[:], t1[ci][:], t1[ci][:], ALU.mult, [k1], ['sqj'])

            def F3_(c, ci):
                k1 = 't1_%d' % ci
                S.op('dve', lambda: V.reduce_sum(out=ssq[:, g, c:c + 1], in_=sqj[:], axis=AX.X), ['sqj'], ['ssq'])
                cp('act', yzb[ci][:], t1[ci][:], [k1], ['yzb%d' % ci])

            def T_(c, ci):
                tk = slice(c * 128, (c + 1) * 128)
                for i3 in range(3):
                    tr(pb[:, 512 + i3 * 128: 512 + (i3 + 1) * 128], yzb[ci][:, i3 * 128:(i3 + 1) * 128], identb[:], ['yzb%d' % ci, 'identb'], ['pbz'], inc=(i3 == 2))
                tt('dve', yzTs[ci][:], pb[:, 512:896].rearrange("p (i t) -> p i t", t=128),
                   ynw[:, g * 3:(g + 1) * 3].unsqueeze(2).to_broadcast([128, 3, 128]), ALU.mult, ['pbz', 'ynw'], ['yzTs%d' % ci])
                S.dma('sp', yzT_v[:, g * 3:(g + 1) * 3, tk], yzTs[ci][:], reads=['yzTs%d' % ci])

            for c in range(NT):
                ci = c % 2
                H_(c, ci)
                lb0 = L_(c, ci, 0); q0 = E_(c, ci, 0, lb0)
                if c > 0:
                    F2_(c - 1, (c - 1) % 2)
                lb1 = L_(c, ci, 1); q1 = E_(c, ci, 1, lb1)
                if c > 0:
                    F3_(c - 1, (c - 1) % 2)
                Y_(c, 0, q0)
                lb2 = L_(c, ci, 2); q2 = E_(c, ci, 2, lb2)
                Y_(c, 1, q1)
                if c > 0:
                    T_(c - 1, (c - 1) % 2)
                Y_(c, 2, q2)
                O_(c, ci)
                U_(c)
                F1_(c, ci)
            F2_(NT - 1, (NT - 1) % 2)
            F3_(NT - 1, (NT - 1) % 2)
            T_(NT - 1, (NT - 1) % 2)
            S.barrier()
    esAB.close()
    if upto == 'B':
        es0.close(); return nc

    mask_all = T(P0, "mask_all", [128, NT, NE]); gates_all = T(P0, "gates_all", [128, NT, 4])
    idx_all = T(P0, "idx_all", [128, NT * 4], I32); widx = T(P0, "widx", [128, NBLK], I32)
    esC = ExitStack()
    s2_bc = T(esC, "s2_bc", [128, D]); sh2_bc = T(esC, "sh2_bc", [128, D])
    S.dma('sp', s2_bc[:], modrow_d[0], writes=['s2_bc'], sem='L_s2bc')
    S.dma('sp', sh2_bc[:], modrow_d[1], writes=['s2_bc'], sem='L_s2bc')
    with ExitStack() as es:
        wout = T(es, "wout", [128, 16, D], BF16)
        for k in range(16):
            S.dma('pool', wout[:, k, :], wout_d[k * 128:(k + 1) * 128, :], writes=['wout'], sem='wout')
        rw = T(es, "rw", [128, 8, NE]); rb_bc = T(es, "rb_bc", [128, NE])
        S.dma('sp', rw[:], rw_d[:, :, :], writes=['rw'], sem='c4')
        S.dma('sp', rb_bc[:], rb_d.partition_broadcast(128), writes=['rb_bc'], sem='c4')
        S.regroup('c4', ['rw', 'rb_bc'])
        rs_ssd = T(es, "rs_ssd", [128, NT]); tq = T(es, "tq", [128, NT])
        tt('dve', tq[:], ssq[:, 0, :], ssq[:, 1, :], ALU.add, ['ssq'], ['tq'])
        tt('dve', tq[:], tq[:], ssq[:, 2, :], ALU.add, ['tq', 'ssq'], ['tq'])
        tt('dve', tq[:], tq[:], ssq[:, 3, :], ALU.add, ['tq', 'ssq'], ['tq'])
        act(tq[:], tq[:], AF.Ln, ['tq'], ['tq'], bias=EPS, scale=1.0 / SSDW)
        act(rs_ssd[:], tq[:], AF.Exp, ['tq'], ['rs_ssd'], scale=-0.5)
        yzt = [T(es, "yzt%d" % i, [128, 12, 128], BF16) for i in range(2)]; ypt = [T(es, "ypt%d" % i, [128, 4, 128], BF16) for i in range(2)]
        xt_ = [T(es, "cxt%d" % i, [128, D]) for i in range(2)]; m_ = T(es, "cm", [128, D]); x1s = [T(es, "x1s%d" % i, [128, D]) for i in range(2)]
        sq_ = T(es, "csq", [128, D]); ss_ = [T(es, "css%d" % i, [128, 4]) for i in range(2)]; xn_ = [T(es, "cxn%d" % i, [128, D]) for i in range(2)]
        tmpm = T(es, "ctmpm", [128, 8, 128]); h2f = T(es, "h2f", [128, 8, 128]); htk = T(es, "htk", [128, D]); htb = [T(es, "htb%d" % i, [128, D], BF16) for i in range(2)]
        lg = T(es, "lg", [128, NE]); m8 = T(es, "m8", [128, 8]); ex = T(es, "ex", [128, NE]); sm = T(es, "sm", [128, 4])
        ps_s = [PS(es, "ps_s%d" % i, [128, 512]) for i in range(2)]; ps_p = [PS(es, "ps_p%d" % i, [128, 512]) for i in range(2)]
        pT = PS(es, "pT2", [128, 1024]); pr = PS(es, "pr", [128, 512])
        yzT_v = yzT_d.rearrange("(i p) t -> p i t", p=128); ypT_v = ypT_d.rearrange("(g o) t -> o g t", o=128)
        def CX_(j):
            i = j % 2
            tk = slice(j * 128, (j + 1) * 128)
            S.dma('sp', yzt[i][:], yzT_v[:, :, tk], writes=['yzt%d' % i], sem='yzt%d' % i)
            S.dma('sp', ypt[i][:], ypT_v[:, :, tk], writes=['ypt%d' % i], sem='ypt%d' % i)
            S.dma('sp', xt_[i][:], x_d[tk, :], writes=['cxt%d' % i], sem='cxt%d' % i)
            for half in range(2):
                hc = slice(half * 512, (half + 1) * 512)
                for k in range(12):
                    mm(ps_s[half][:, :], yzt[i][:, k, :], wout[:, k, hc], k == 0, k == 11, ['yzt%d' % i, 'wout'], ['ps_s%d' % half], inc=(k == 11))
                for k in range(4):
                    mm(ps_p[half][:, :], ypt[i][:, k, :], wout[:, 12 + k, hc], k == 0, k == 3, ['ypt%d' % i, 'wout'], ['ps_p%d' % half], inc=(k == 3))
                ts('dve', m_[:, hc], ps_s[half][:, :], rs_ssd[:, j:j + 1], ALU.mult, ['ps_s%d' % half, 'rs_ssd'], ['cm%d' % half])
                tt('dve', m_[:, hc], m_[:, hc], ps_p[half][:, :], ALU.add, ['cm%d' % half, 'ps_p%d' % half], ['cm%d' % half])
                tt('pool', m_[:, hc], m_[:, hc], g_bc[:, 0, hc], ALU.mult, ['cm%d' % half, 'g_bc'], ['cm%d' % half])
                tt('pool', x1s[i][:, hc], m_[:, hc], xt_[i][:, hc], ALU.add, ['cm%d' % half, 'cxt%d' % i], ['x1s%d' % i])
            S.dma('sp', x1_d[tk, :], x1s[i][:], reads=['x1s%d' % i])
            xt = x1s[i][:]
            act(sq_[:], xt, AF.Square, ['x1s%d' % i], ['csq'])
            S.op('dve', lambda: V.reduce_sum(out=ss_[i][:, 0:1], in_=sq_[:], axis=AX.X), ['csq'], ['css%d' % i])
            act(ss_[i][:, 1:2], ss_[i][:, 0:1], AF.Ln, ['css%d' % i], ['css%d' % i], bias=EPS, scale=1.0 / D)
            act(ss_[i][:, 2:3], ss_[i][:, 1:2], AF.Exp, ['css%d' % i], ['css%d' % i], scale=-0.5)
            ts('dve', xn_[i][:], xt, ss_[i][:, 2:3], ALU.mult, ['x1s%d' % i, 'css%d' % i], ['cxn%d' % i])
        def CY_(j):
            i = j % 2
            tk = slice(j * 128, (j + 1) * 128)
            for k in range(8):
                tr(pT[:, k * 128:(k + 1) * 128], xn_[i][:, k * 128:(k + 1) * 128], identf[:], ['cxn%d' % i, 'identf'], ['pT2'], inc=(k == 7))
            tt('dve', tmpm[:], pT[:, :].rearrange("p (k t) -> p k t", t=128), s2[:, :].unsqueeze(2).to_broadcast([128, 8, 128]),
               ALU.mult, ['pT2', 'modv'], ['ctmpm'])
            tt('pool', h2f[:], tmpm[:], sh2[:, :].unsqueeze(2).to_broadcast([128, 8, 128]), ALU.add, ['ctmpm', 'modv'], ['h2f'])
            tt('pool', htk[:], xn_[i][:], s2_bc[:], ALU.mult, ['cxn%d' % i, 's2_bc'], ['htk'])
            tt('pool', htb[i][:], htk[:], sh2_bc[:], ALU.add, ['htk', 's2_bc'], ['htb%d' % i])
            S.dma('sp', h2tok_d[tk, :], htb[i][:], reads=['htb%d' % i])
            for k in range(8):
                mm(pr[:, 0:NE], h2f[:, k, :], rw[:, k, :], k == 0, k == 7, ['h2f', 'rw'], ['pr'], inc=(k == 7))
            tt('dve', lg[:], pr[:, 0:NE], rb_bc[:], ALU.add, ['pr', 'rb_bc'], ['lg'])
            S.op('dve', lambda: V.max(out=m8[:], in_=lg[:]), ['lg'], ['m8'])
            ts('dve', mask_all[:, j, :], lg[:], m8[:, 3:4], ALU.is_ge, ['lg', 'm8'], ['mask_all'])
            ts('dve', sm[:, 0:1], m8[:, 0:1], -1.0, ALU.mult, ['m8'], ['sm'])
            act(ex[:], lg[:], AF.Exp, ['lg', 'sm'], ['ex'], bias=sm[:, 0:1])
            tt('dve', ex[:], ex[:], mask_all[:, j, :], ALU.mult, ['ex', 'mask_all'], ['ex'])
            S.op('dve', lambda: V.reduce_sum(out=sm[:, 1:2], in_=ex[:], axis=AX.X), ['ex'], ['sm'])
            S.op('dve', lambda: V.reciprocal(out=sm[:, 2:3], in_=sm[:, 1:2]), ['sm'], ['sm'])
            ts('dve', G_all[:, j, :], ex[:], sm[:, 2:3], ALU.mult, ['ex', 'sm'], ['G_all'])
        CX_(0)
        for j in range(NT):
            if j + 1 < NT:
                CX_(j + 1)
            CY_(j)
        S.barrier()
    esC.close()
    stop('C')

    with ExitStack() as es:
        SU = T(es, "SU", [128, 128], BF16); SUf = T(es, "SUf", [128, 128]); onesb = T(es, "onesb", [128, 128], BF16)
        mask_bf = T(es, "mask_bf", [128, NT, NE], BF16); P_all = T(es, "P_all", [128, NT + 1, NE], BF16)
        rank_all = T(es, "rank_all", [128, NT, NE]); counts = T(es, "counts", [128, NE]); padded = T(es, "padded", [128, NE])
        tmpc = T(es, "tmpc", [128, NE]); pad_end = T(es, "pad_end", [128, NE]); pad_start = T(es, "pad_start", [128, NE])
        onesf = T(es, "onesf", [128, NE]); Dm = T(es, "Dm", [128, NT, NE]); Em = T(es, "Em", [128, NT, NE])
        iota1 = T(es, "iota1", [128, NE]); jv = T(es, "jv", [128, NBLK]); pidx = T(es, "pidx", [128, 1])
        d8 = T(es, "d8", [128, 8]); e8 = T(es, "e8", [128, 8]); d4 = T(es, "d4", [128, NT, 4]); oh = T(es, "oh", [128, NE])
        cmp3 = T(es, "cmp3", [128, NBLK, NE]); bexp = T(es, "bexp", [128, NBLK])
        rows = [T(es, "rows%d" % i, [128, D], BF16) for i in range(2)]
        pk = [PS(es, "pk%d" % i, [128, 512]) for i in range(2)]; pc_ = PS(es, "pc_", [128, 512])
        S.dma('sp', SUf[:], cSU_d[:, :], writes=['SUf'], sem='c6')
        S.dma('sp', iota1[:], cIota_d.partition_broadcast(128), writes=['iota1'], sem='c6')
        S.dma('sp', jv[:], cJv_d.partition_broadcast(128), writes=['jv'], sem='c6')
        S.dma('sp', pidx[:], cPidx_d[:, :], writes=['pidx'], sem='c6')
        S.regroup('c6', ['SUf', 'iota1', 'jv', 'pidx'])
        cp('dve', SU[:], SUf[:], ['SUf'], ['SU'])
        S.op('dve', lambda: V.memset(onesb[:], 1.0), writes=['onesb'])
        S.op('dve', lambda: V.memset(onesf[:], 1.0), writes=['onesf'])
        cp('dve', mask_bf[:], mask_all[:], ['mask_all'], ['mask_bf'])
        S.op('dve', lambda: V.memset(P_all[:, 0, :], 0.0), writes=['P_all'])
        for j in range(NT):
            tt('dve', P_all[:, j + 1, :], P_all[:, j, :], mask_all[:, j, :], ALU.add, ['P_all', 'mask_all'], ['P_all'])
        for j in range(NT):
            b = j // 16
            reg = pk[b][:, (j % 16) * NE:(j % 16 + 1) * NE]
            mm(reg, SU[:], mask_bf[:, j, :], True, False, ['SU', 'mask_bf'], ['pk%d' % b], inc=False)
            mm(reg, onesb[:], P_all[:, j, :], False, True, ['onesb', 'P_all'], ['pk%d' % b], inc=(j % 16 == 15))
        for b in range(2):
            cp('dve', rank_all[:, b * 16:(b + 1) * 16, :], pk[b][:, :].rearrange("p (j e) -> p j e", e=NE), ['pk%d' % b], ['rank_all'])
        mm(pc_[:, 0:NE], onesb[:], P_all[:, NT, :], True, True, ['onesb', 'P_all'], ['pc_'])
        cp('dve', counts[:], pc_[:, 0:NE], ['pc_'], ['counts'])
        S.op('dve', lambda: V.memset(padded[:], 0.0), writes=['padded'])
        for m in range(4096 // BS):
            ts('dve', tmpc[:], counts[:], float(BS * m), ALU.is_gt, ['counts'], ['tmpc'], s2=float(BS), op1=ALU.mult)
            tt('dve', padded[:], padded[:], tmpc[:], ALU.add, ['padded', 'tmpc'], ['padded'])
        S.op('dve', lambda: V.tensor_tensor_scan(out=pad_end[:], data0=onesf[:], data1=padded[:], initial=0.0, op0=ALU.mult, op1=ALU.add),
             ['onesf', 'padded'], ['pad_end'])
        tt('dve', pad_start[:], pad_end[:], padded[:], ALU.subtract, ['pad_end', 'padded'], ['pad_start'])
        tt('dve', Dm[:], rank_all[:], pad_start[:, :].unsqueeze(1).to_broadcast([128, NT, NE]), ALU.add, ['rank_all', 'pad_start'], ['Dm'])
        ts('dve', Dm[:], Dm[:], 1.0, ALU.add, ['Dm'], ['Dm'])
        tt('dve', Dm[:], Dm[:], mask_all[:], ALU.mult, ['Dm', 'mask_all'], ['Dm'])
        tt('dve', Em[:], mask_all[:], iota1[:, :].unsqueeze(1).to_broadcast([128, NT, NE]), ALU.mult, ['mask_all', 'iota1'], ['Em'])
        for j in range(NT):
            S.op('dve', lambda: V.max(out=d8[:], in_=Dm[:, j, :]), ['Dm'], ['d8'])
            ts('dve', d4[:, j, :], d8[:, 0:4], -1.0, ALU.add, ['d8'], ['d4'])
        cp('dve', idx_all[:].rearrange("p (j k) -> p j k", k=4), d4[:], ['d4'], ['idx_all'])
        tt('dve', cmp3[:], pad_end[:, :].unsqueeze(1).to_broadcast([128, NBLK, NE]), jv[:, :].unsqueeze(2).to_broadcast([128, NBLK, NE]),
           ALU.is_le, ['pad_end', 'jv'], ['cmp3'])
        S.op('dve', lambda: V.reduce_sum(out=bexp[:], in_=cmp3[:], axis=AX.X), ['cmp3'], ['bexp'])
        ts('dve', bexp[:], bexp[:], float(NE - 1), ALU.min, ['bexp'], ['bexp'], s2=128.0, op1=ALU.mult)
        skp = T(es, "skp", [128, NBLK])
        S.op('dve', lambda: V.memset(skp[:], 0.0), writes=['skp'])
        tt('dve', skp[:, 2:NBLK], bexp[:, 2:NBLK], bexp[:, 0:NBLK - 2], ALU.is_equal, ['bexp', 'skp'], ['skp'])
        ts('dve', skp[:], skp[:], 1.0e6, ALU.mult, ['skp'], ['skp'])
        ts('dve', bexp[:], bexp[:], pidx[:, 0:1], ALU.add, ['bexp', 'pidx'], ['bexp'])
        tt('dve', bexp[:], bexp[:], skp[:], ALU.add, ['bexp', 'skp'], ['bexp'])
        cp('dve', widx[:], bexp[:], ['bexp'], ['widx'])
        S.barrier()
        stop('C2')
        for j in range(NT):
            i = j % 2
            S.dma('sp', rows[i][:], h2tok_d[j * 128:(j + 1) * 128, :], writes=['rows%d' % i], sem='rows%d' % i)
            for k in range(4):
                S.idma(out=Xg_d[:, :], in_=rows[i][:, :], idx=idx_all[:, j * 4 + k:j * 4 + k + 1], scatter=True, bound=NSLOT - 1,
                       reads=['rows%d' % i, 'idx_all'], sem='sc%d' % i)
        for j in range(NT):
            S.op('dve', lambda: V.max(out=e8[:], in_=Em[:, j, :]), ['Em'], ['e8'])
            for k in range(4):
                ts('dve', oh[:], iota1[:], e8[:, k:k + 1], ALU.is_equal, ['iota1', 'e8'], ['oh'])
                tt('dve', oh[:], oh[:], G_all[:, j, :], ALU.mult, ['oh', 'G_all'], ['oh'])
                S.op('dve', lambda: V.reduce_sum(out=gates_all[:, j, k:k + 1], in_=oh[:], axis=AX.X), ['oh'], ['gates_all'])
        S.barrier()
    stop('C3')

    with ExitStack() as es:
        wg_ = [T(es, "wg%d" % i, [128, 8, D], BF16) for i in range(2)]; wu_ = [T(es, "wu%d" % i, [128, 8, D], BF16) for i in range(2)]
        w2_ = [T(es, "w2_%d" % i, [128, 8, D], BF16) for i in range(2)]; b1t = [T(es, "b1t%d" % i, [128, 16]) for i in range(2)]
        NJ = BS // 128
        b1p = [T(es, "b1p%d" % i, [128, 8]) for i in range(2)]
        xgs = [T(es, "xgs%d" % i, [128, NJ, D], BF16) for i in range(2)]; xgT = [T(es, "xgT%d" % i, [128, 8, BS], BF16) for i in range(2)]
        actT = [T(es, "actT%d" % i, [128, 8, BS], BF16) for i in range(2)]
        gt3 = [T(es, "gt3_%d" % i, [128, BS]) for i in range(3)]; sg3 = [T(es, "sg3_%d" % i, [128, BS], BF16) for i in range(3)]
        ut3 = [T(es, "ut3_%d" % i, [128, BS]) for i in range(3)]
        ysb = [T(es, "ysb%d" % i, [128, NJ, D]) for i in range(2)]
        pg_ = [PS(es, "mpg%d" % i, [128, 512]) for i in range(2)]; pu_ = [PS(es, "mpu%d" % i, [128, 512]) for i in range(2)]
        py_ = [PS(es, "mpy%d" % i, [128, 512]) for i in range(2)]; pb_ = [PS(es, "mpb%d" % i, [128, 1024], BF16) for i in range(2)]
        Xg_v = Xg_d.rearrange("(b j p) d -> b p j d", p=128, j=NJ); Y_v = Y_d.rearrange("(b j p) o -> b p j o", p=128, j=NJ)

        def wload(blk, i):
            ix = widx[:, blk:blk + 1]
            for (dst, src, nm) in ((wg_[i], W1G_d, 'wg%d' % i), (wu_[i], W1U_d, 'wu%d' % i), (w2_[i], W2_d, 'w2_%d' % i)):
                S.idma(out=dst[:].rearrange("p k c -> p (k c)"), in_=src[:, :], idx=ix, scatter=False, bound=NE * 128 - 1,
                       reads=['widx'], writes=[nm], sem=nm)
            S.idma(out=b1t[i][:, :], in_=B1_d[:, :], idx=ix, scatter=False, bound=NE * 128 - 1, reads=['widx'], writes=['b1t%d' % i], sem='b1t%d' % i)

        rgu = Ring('gu', 2); ry = Ring('y', 2); rpb = Ring('pb', 2); r3 = Ring('r3', 3)
        KB = 1024 // BS

        def TR_(blk):
            wi = blk % 2
            S.dma('sp', xgs[wi][:], Xg_v[blk], writes=['xgs%d' % wi], sem='xgs%d' % wi)
            for k0 in range(0, 8, KB):
                pi = rpb.next()
                for kk in range(KB):
                    k = k0 + kk
                    for jj in range(NJ):
                        tr(pb_[pi][:, kk * BS + jj * 128: kk * BS + (jj + 1) * 128], xgs[wi][:, jj, k * 128:(k + 1) * 128], identb[:],
                           ['xgs%d' % wi, 'identb'], ['mpb%d' % pi], inc=(kk == KB - 1 and jj == NJ - 1))
                cp('act' if (k0 // KB) % 2 == 0 else 'dve', xgT[wi][:, k0:k0 + KB, :], pb_[pi][:, :].rearrange("p (k s) -> p k s", s=BS),
                   ['mpb%d' % pi], ['xgT%d' % wi])

        def FL_(blk):
            wi = blk % 2
            ts('pool', b1p[wi][:], b1t[wi][:, 8:16], 1.0, ALU.add, ['b1t%d' % wi], ['b1p%d' % wi])
            prev = None

            def fin(pv):
                ri, fp = pv
                S.op('dve', lambda: V.scalar_tensor_tensor(out=actT[wi][:, fp, :], in0=ut3[ri][:], scalar=-6.0, in1=gt3[ri][:], op0=ALU.max, op1=ALU.mult),
                     ['ut3_%d' % ri, 'gt3_%d' % ri], ['actT%d' % wi])
            for f in range(8):
                b = rgu.next(); ri = r3.next()
                fs = slice(f * 128, (f + 1) * 128)
                for k in range(8):
                    mm(pg_[b][:, 0:BS], wg_[wi][:, k, fs], xgT[wi][:, k, :], k == 0, k == 7, ['wg%d' % wi, 'xgT%d' % wi], ['mpg%d' % b], inc=(k == 7))
                for k in range(8):
                    mm(pu_[b][:, 0:BS], wu_[wi][:, k, fs], xgT[wi][:, k, :], k == 0, k == 7, ['wu%d' % wi, 'xgT%d' % wi], ['mpu%d' % b], inc=(k == 7))
                ts('dve', gt3[ri][:], pg_[b][:, 0:BS], b1t[wi][:, f:f + 1], ALU.add, ['mpg%d' % b, 'b1t%d' % wi], ['gt3_%d' % ri], s2=7.0, op1=ALU.min)
                act(sg3[ri][:], gt3[ri][:], AF.Sigmoid, ['gt3_%d' % ri], ['sg3_%d' % ri], scale=1.702)
                ts('dve', ut3[ri][:], pu_[b][:, 0:BS], b1p[wi][:, f:f + 1], ALU.add, ['mpu%d' % b, 'b1p%d' % wi], ['ut3_%d' % ri], s2=8.0, op1=ALU.min)
                tt('pool', gt3[ri][:], gt3[ri][:], sg3[ri][:], ALU.mult, ['gt3_%d' % ri, 'sg3_%d' % ri], ['gt3_%d' % ri])
                if prev is not None:
                    fin(prev)
                prev = (ri, f)
            fin(prev)

        def W2_(blk):
            wi = blk % 2
            for jj in range(NJ):
                for half in range(2):
                    b = ry.next()
                    for k in range(8):
                        mm(py_[b][:, :], actT[wi][:, k, jj * 128:(jj + 1) * 128], w2_[wi][:, k, half * 512:(half + 1) * 512], k == 0, k == 7,
                           ['actT%d' % wi, 'w2_%d' % wi], ['mpy%d' % b], inc=(k == 7))
                    cp('act' if half == 0 else 'dve', ysb[wi][:, jj, half * 512:(half + 1) * 512], py_[b][:, :], ['mpy%d' % b], ['ysb%d' % wi])
            S.dma('sp', Y_v[blk], ysb[wi][:], reads=['ysb%d' % wi])

        wload(0, 0)
        TR_(0)
        for blk in range(NBLK):
            if blk + 1 < NBLK:
                wload(blk + 1, (blk + 1) % 2)
            FL_(blk)
            if blk + 1 < NBLK:
                TR_(blk + 1)
            W2_(blk)
        S.barrier()
    stop('D')

    with ExitStack() as es:
        b2s = T(es, "b2s", [NE, D]); GT = T(es, "GT", [NE, 128])
        S.dma('sp', b2s[:], b2_d[:, :], writes=['b2s'], sem='b2s')
        yk = [T(es, "yk%d" % i, [128, D]) for i in range(8)]; acc = [T(es, "acc%d" % i, [128, D]) for i in range(2)]
        x1t = [T(es, "x1t%d" % i, [128, D]) for i in range(2)]; sq_ = T(es, "dsq", [128, D]); ss_ = [T(es, "dss%d" % i, [128, 4]) for i in range(2)]
        pgt = PS(es, "pgt", [128, 512]); pa = [PS(es, "pa%d" % i, [128, 512]) for i in range(2)]
        def EG_(j):
            i = j % 2
            tk = slice(j * 128, (j + 1) * 128)
            S.dma('sp', x1t[i][:], x1_d[tk, :], writes=['x1t%d' % i], sem='x1t%d' % i)
            for k in range(4):
                kk = i * 4 + k
                S.idma(out=yk[kk][:, :], in_=Y_d[:, :], idx=idx_all[:, j * 4 + k:j * 4 + k + 1], scatter=False, bound=NSLOT - 1,
                       reads=['idx_all'], writes=['yk%d' % kk], sem='yk%d' % kk)

        EG_(0)
        for j in range(NT):
            i = j % 2
            tk = slice(j * 128, (j + 1) * 128)
            if j + 1 < NT:
                EG_(j + 1)
            tr(pgt[0:NE, 0:128], G_all[:, j, :], identf[:], ['G_all', 'identf'], ['pgt'])
            cp('act', GT[:], pgt[0:NE, 0:128], ['pgt'], ['GT'])
            for half in range(2):
                hc = slice(half * 512, (half + 1) * 512)
                mm(pa[half][:, :], GT[:, :], b2s[:, hc], True, True, ['GT', 'b2s'], ['pa%d' % half])
                S.op('dve', lambda: V.scalar_tensor_tensor(out=acc[i][:, hc], in0=yk[i * 4][:, hc], scalar=gates_all[:, j, 0:1], in1=pa[half][:, :],
                                                            op0=ALU.mult, op1=ALU.add), ['yk%d' % (i * 4), 'gates_all', 'pa%d' % half], ['acc%d_%d' % (i, half)])
                for k in range(1, 4):
                    S.op('dve', lambda: V.scalar_tensor_tensor(out=acc[i][:, hc], in0=yk[i * 4 + k][:, hc], scalar=gates_all[:, j, k:k + 1], in1=acc[i][:, hc],
                                                                op0=ALU.mult, op1=ALU.add), ['yk%d' % (i * 4 + k), 'gates_all', 'acc%d_%d' % (i, half)], ['acc%d_%d' % (i, half)])
            ak = ['acc%d_0' % i, 'acc%d_1' % i]
            tt('pool', acc[i][:], acc[i][:], g_bc[:, 1, :], ALU.mult, ak + ['g_bc'], ak)
            tt('pool', x1t[i][:], x1t[i][:], acc[i][:], ALU.add, ['x1t%d' % i] + ak, ['x1t%d' % i])
            act(sq_[:], x1t[i][:], AF.Square, ['x1t%d' % i], ['dsq'])
            S.op('dve', lambda: V.reduce_sum(out=ss_[i][:, 0:1], in_=sq_[:], axis=AX.X), ['dsq'], ['dss%d' % i])
            act(ss_[i][:, 1:2], ss_[i][:, 0:1], AF.Ln, ['dss%d' % i], ['dss%d' % i], bias=EPS, scale=1.0 / D)
            act(ss_[i][:, 2:3], ss_[i][:, 1:2], AF.Exp, ['dss%d' % i], ['dss%d' % i], scale=-0.5)
            ts('dve', x1t[i][:], x1t[i][:], ss_[i][:, 2:3], ALU.mult, ['x1t%d' % i, 'dss%d' % i], ['x1t%d' % i])
            tt('pool', x1t[i][:], x1t[i][:], fnw_bc[:], ALU.mult, ['x1t%d' % i, 'fnw_bc'], ['x1t%d' % i])
            S.dma('sp', out_d[tk, :], x1t[i][:], reads=['x1t%d' % i])
        S.barrier()
    es0.close()
    return nc


def _consts():
    I = np.eye(128, dtype=np.float32)
    k = np.arange(128)
    U = (k[:, None] <= k[None, :]).astype(np.float32)
    Lo = (k[:, None] >= k[None, :]).astype(np.float32)
    mask = np.zeros((128, 2, 128), np.float32)
    mask[:, 0, :] = np.where(k[None, :] >= k[:, None], 0.0, NEG)
    mask[:, 1, :] = np.where(k[None, :] <= k[:, None], 0.0, NEG)
    sel = np.zeros((6, 6, 128), np.float32)
    for h in range(6):
        sel[h, h, :] = 1.0
    AT = np.zeros((128, 4, 128), np.float32)
    t = np.arange(64)
    for g, w in enumerate((2, 4, 8, 16)):
        lo = np.clip(t - w // 2, 0, 64); hi = np.clip(t + w - w // 2, 0, 64)
        Am = np.zeros((64, 64), np.float32)
        for ti in range(64):
            Am[ti, lo[ti]:hi[ti]] = 1.0 / float(hi[ti] - lo[ti])
        Am -= np.eye(64, dtype=np.float32)
        for r in range(2):
            AT[r * 64:(r + 1) * 64, g, r * 64:(r + 1) * 64] = Am.T
    SU = (k[:, None] < k[None, :]).astype(np.float32)
    iota1 = (np.arange(NE, dtype=np.float32) + 1.0).reshape(1, NE)
    jv = (np.arange(NBLK, dtype=np.float32) * float(BS)).reshape(1, NBLK)
    pidx = np.arange(128, dtype=np.float32).reshape(128, 1)
    return dict(cI=I, cU=U, cLo=Lo, cMask=mask, cSel=sel, cAT=AT, cSU=SU, cIota=iota1, cJv=jv, cPidx=pidx)


_NC_CACHE = {}


def kernel(x, c, ctx, c_ctx, w_mod, b_mod, norm1_w, norm2_w, w_in, conv_w, conv_b, dt_bias, a_log, d_skip,
           ssd_norm_w, pool_w, pool_scale, w_out, router_w, router_b, w1, b1, w2, b2, final_norm_w, _dbg=False, _upto='ALL', _cores=8):
    f = lambda a: np.ascontiguousarray(np.asarray(a, dtype=np.float32))
    x = f(x); c = f(c); ctx = f(ctx); c_ctx = f(c_ctx)
    shared = dict(_consts())
    shared["w_mod"] = f(w_mod[0])
    shared["bmodT"] = f(b_mod[0].reshape(48, 128).T)
    shared["bmodr"] = f(b_mod[0].reshape(1, -1))
    shared["n1T"] = f(norm1_w[0].reshape(8, 128).T); shared["n2T"] = f(norm2_w[0].reshape(8, 128).T)
    shared["fnw"] = f(final_norm_w.reshape(1, -1))
    shared["w_in"] = f(w_in[0])
    shared["convw"] = f(np.asarray(conv_w[0]).T.reshape(20, 128, 5).transpose(1, 0, 2))
    shared["convb"] = f(np.asarray(conv_b[0]).reshape(20, 128).T)
    shared["dtb"] = f(np.asarray(dt_bias[0]).reshape(1, 48)); shared["alog"] = f(np.asarray(a_log[0]).reshape(1, 48))
    shared["dvec"] = f(np.repeat(np.asarray(d_skip[0]), 64).reshape(1, -1))
    shared["ynw"] = f(np.asarray(ssd_norm_w[0]).reshape(12, 128).T)
    shared["poolw"] = f(np.asarray(pool_w[0]).transpose(1, 0, 2))
    shared["pscale"] = f(np.asarray(pool_scale[0]).reshape(4, 128).T)
    shared["w_out"] = f(w_out[0])
    shared["rw"] = f(np.asarray(router_w[0]).reshape(8, 128, NE).transpose(1, 0, 2))
    shared["rb"] = f(np.asarray(router_b[0]).reshape(1, NE))
    w1a = np.asarray(w1[0])

    def ptile(w):
        return f(w.reshape(NE, 8, 128, -1).transpose(0, 2, 1, 3).reshape(NE * 128, -1))
    shared["W1G"] = ptile(w1a[:, :, 0::2]); shared["W1U"] = ptile(w1a[:, :, 1::2])
    shared["W2"] = ptile(np.asarray(w2[0]))
    b1a = np.asarray(b1[0])
    b1g_ = b1a[:, 0::2].reshape(NE, 8, 128).transpose(0, 2, 1)
    b1u_ = b1a[:, 1::2].reshape(NE, 8, 128).transpose(0, 2, 1)
    shared["B1"] = f(np.concatenate([b1g_, b1u_], axis=2).reshape(NE * 128, 16))
    shared["b2"] = f(b2[0])
    shared["n2r"] = f(norm2_w[0].reshape(1, -1))
    if (_dbg, _upto) not in _NC_CACHE:
        _NC_CACHE[(_dbg, _upto)] = build(dbg=_dbg, upto=_upto)
    nc = _NC_CACHE[(_dbg, _upto)]
    in_maps = []
    for b in range(_cores):
        m = dict(shared)
        m["x"] = x[b]; m["ctx"] = ctx[b]
        cv = np.stack([c[b], c_ctx], axis=-1)
        m["cvec"] = f(cv.reshape(8, 128, 2).transpose(1, 0, 2))
        in_maps.append(m)
    res = run_bass_kernel_spmd(nc, in_maps, core_ids=list(range(_cores)))
    if _dbg:
        return res
    return np.stack([r["out"] for r in res.results], axis=0).astype(np.float32)
```

```python
import numpy as np
from contextlib import ExitStack
import concourse.bass as bass
import concourse.mybir as mybir
from concourse.bass_utils import run_bass_kernel_spmd

F32 = mybir.dt.float32; BF16 = mybir.dt.bfloat16; I32 = mybir.dt.int32
AF = mybir.ActivationFunctionType; ALU = mybir.AluOpType; AX = mybir.AxisListType

D = 1024; L = 4096; CTXL = 256; NT = 32; NCH = 34
INC = 4656; CONVCH = 2560; SSDW = 1536
RAWW = 4360
NEG = -30000.0
NE = 32
BS = 256
NBLK = 16384 // BS + NE
NSLOT = NBLK * BS
EPS = 1e-6


class Sched:
    STRICT = True

    def __init__(self, nc, es):
        self.nc = nc; self.es = es
        self.eng = {'pe': nc.tensor, 'act': nc.scalar, 'dve': nc.vector, 'pool': nc.gpsimd, 'sp': nc.sync}
        self.sem = {}; self.cnt = {}
        for e in self.eng:
            self.sem[e] = es.enter_context(nc.semaphore("s_" + e)); self.cnt[e] = 0
        self.seen = {e: {} for e in self.eng}
        self.w = {}; self.r = {}
        self.dsem = {}; self.dcnt = {}; self.free_sems = []; self.free_sw = []; self.dsw = {}; self.nsem = 0; self.bregs = {}

    def _dsem(self, name, sw=False):
        if name not in self.dsem:
            pool = self.free_sw if sw else self.free_sems
            if pool:
                h, c = pool.pop()
                self.dsem[name] = h; self.dcnt[name] = c
            else:
                self.nsem += 1
                self.dsem[name] = self.es.enter_context(self.nc.semaphore("d_%d" % self.nsem)); self.dcnt[name] = 0
            self.dsw[name] = sw
        assert self.dsw[name] == sw, name
        return self.dsem[name]

    def _wait(self, e, tok):
        if tok is None:
            return
        kind, name, val = tok
        if kind == 'e' and name == e and (e == 'pe' or not self.STRICT):
            return
        skey = (kind, name)
        if self.seen[e].get(skey, 0) >= val:
            return
        self.seen[e][skey] = val
        s = self.sem[name] if kind == 'e' else self.dsem[name]
        self.eng[e].wait_ge(s, val)

    def deps(self, e, reads, writes):
        for k in reads:
            self._wait(e, self.w.get(k))
        for k in writes:
            self._wait(e, self.w.get(k))
            for tok in list(self.r.get(k, {}).values()):
                self._wait(e, tok)

    def record(self, tok, reads, writes):
        for k in reads:
            self.r.setdefault(k, {})[(tok[0], tok[1])] = tok
        for k in writes:
            self.w[k] = tok; self.r[k] = {}

    def op(self, e, fn, reads=(), writes=(), inc=True):
        self.deps(e, reads, writes)
        ins = fn()
        tok = ('e', e, self.cnt[e] + 1)
        self.record(tok, reads, writes)
        if inc:
            ins.then_inc(self.sem[e], 1); self.cnt[e] += 1
        return ins

    def dma(self, q, out, in_, reads=(), writes=(), sem=None):
        if sem is None:
            sem = ('L_' + writes[0]) if writes else ('S_' + reads[0])
        if q == 'pool':
            sem = 'sw_' + sem
        s = self._dsem(sem, sw=(q == 'pool'))
        self.deps(q, reads, writes)
        ins = self.eng[q].dma_start(out=out, in_=in_)
        ins.then_inc(s, 16); self.dcnt[sem] += 16
        tok = ('d', sem, self.dcnt[sem])
        self.record(tok, reads, writes)
        return ins

    def idma(self, out, in_, idx, scatter, bound, reads=(), writes=(), sem=None):
        sem = 'sw_' + sem
        s = self._dsem(sem, sw=True)
        self.deps('pool', reads, writes)
        if bound not in self.bregs:
            r = self.nc.gpsimd.alloc_register("bc%d" % bound)
            self.nc.gpsimd.reg_mov(r, bound)
            self.bregs[bound] = r
        bound = self.bregs[bound]
        off = bass.IndirectOffsetOnAxis(ap=idx, axis=0)
        if scatter:
            ins = self.nc.gpsimd.indirect_dma_start(out=out, out_offset=off, in_=in_, in_offset=None, bounds_check=bound, oob_is_err=False)
        else:
            ins = self.nc.gpsimd.indirect_dma_start(out=out, out_offset=None, in_=in_, in_offset=off, bounds_check=bound, oob_is_err=False)
        ins.then_inc(s, 16); self.dcnt[sem] += 16
        tok = ('d', sem, self.dcnt[sem])
        self.record(tok, reads, writes)
        return ins

    def regroup(self, sem, keys):
        for k in keys:
            self.w[k] = ('d', sem, self.dcnt[sem])

    def barrier(self):
        for e in self.eng:
            for f in self.eng:
                if f != e and self.cnt[f] > 0:
                    self._wait(e, ('e', f, self.cnt[f]))
            for name in self.dsem:
                if self.dcnt[name] > 0:
                    self._wait(e, ('d', name, self.dcnt[name]))
        for name in list(self.dsem):
            (self.free_sw if self.dsw[name] else self.free_sems).append((self.dsem[name], self.dcnt[name]))
            for e in self.eng:
                self.seen[e].pop(('d', name), None)
        self.dsem = {}; self.dcnt = {}; self.dsw = {}
        for k in list(self.w):
            if self.w[k][0] == 'd':
                del self.w[k]
        for k in list(self.r):
            for kk in [kk for kk in self.r[k] if kk[0] == 'd']:
                del self.r[k][kk]


class Ring:
    def __init__(self, name, n):
        self.name = name; self.n = n; self.i = -1

    def next(self):
        self.i += 1
        return self.i % self.n


class _Stop(Exception):
    pass


def build(dbg=False, upto='ALL'):
    try:
        return _build(dbg, upto)
    except _Stop as e:
        return e.args[0]


def _build(dbg=False, upto='ALL'):
    nc = bass.Bass("TRN2", target_bir_lowering=False)
    es0 = ExitStack()
    S = Sched(nc, es0)
    V = nc.vector; A = nc.scalar; G = nc.gpsimd; PE = nc.tensor

    def din(name, shape, dt=F32):
        return nc.dram_tensor(name, list(shape), dt, kind="ExternalInput").ap()

    def dscr(name, shape, dt):
        return nc.dram_tensor(name, list(shape), dt, kind=("ExternalOutput" if dbg else "Internal")).ap()

    x_d = din("x", [L, D]); ctx_d = din("ctx", [CTXL, D]); cvec_d = din("cvec", [128, 8, 2])
    wmod_d = din("w_mod", [D, 6 * D]); bmodT_d = din("bmodT", [128, 48]); bmodr_d = din("bmodr", [1, 6 * D])
    n1T_d = din("n1T", [128, 8]); n2T_d = din("n2T", [128, 8]); fnw_d = din("fnw", [1, D])
    win_d = din("w_in", [D, INC]); convw_d = din("convw", [128, 20, 5]); convb_d = din("convb", [128, 20])
    dtb_d = din("dtb", [1, 48]); alog_d = din("alog", [1, 48]); dvec_d = din("dvec", [1, SSDW])
    ynw_d = din("ynw", [128, 12]); poolw_d = din("poolw", [128, 4, 128]); pscale_d = din("pscale", [128, 4])
    wout_d = din("w_out", [2 * D, D]); rw_d = din("rw", [128, 8, NE]); rb_d = din("rb", [1, NE])
    W1G_d = din("W1G", [NE * 128, 8 * D]); W1U_d = din("W1U", [NE * 128, 8 * D]); W2_d = din("W2", [NE * 128, 8 * D])
    B1_d = din("B1", [NE * 128, 16]); b2_d = din("b2", [NE, D]); n2r_d = din("n2r", [1, D])
    cSU_d = din("cSU", [128, 128]); cIota_d = din("cIota", [1, NE]); cJv_d = din("cJv", [1, NBLK]); cPidx_d = din("cPidx", [128, 1])
    cI_d = din("cI", [128, 128]); cU_d = din("cU", [128, 128]); cLo_d = din("cLo", [128, 128])
    cMask_d = din("cMask", [128, 2, 128]); cSel_d = din("cSel", [6, 6, 128]); cAT_d = din("cAT", [128, 4, 128])
    out_d = nc.dram_tensor("out", [L, D], F32, kind="ExternalOutput").ap()
    rawT_d = dscr("rawT", [CONVCH, RAWW], BF16); zt_d = dscr("zt", [L, SSDW], BF16)
    ypT_d = dscr("ypT", [512, L], BF16); yzT_d = dscr("yzT", [SSDW, L], BF16)
    x1_d = dscr("x1", [L, D], F32); h2tok_d = dscr("h2tok", [L, D], BF16)
    Xg_d = nc.dram_tensor("Xg", [NSLOT, D], BF16, kind="Internal").ap(); Y_d = nc.dram_tensor("Y", [NSLOT, D], F32, kind="Internal").ap()

    def stop(tag):
        if upto == tag:
            S.barrier()
            raise _Stop(nc)

    def T(es, name, shape, dt=F32):
        return es.enter_context(nc.sbuf_tensor("t_" + name, list(shape), dt))

    def PS(es, name, shape, dt=F32):
        return es.enter_context(nc.psum_tensor("p_" + name, list(shape), dt))

    def mm(out, lhsT, rhs, start, stop, reads, writes, inc=True, sgc=False):
        return S.op('pe', lambda: PE.matmul(out, lhsT=lhsT, rhs=rhs, start=start, stop=stop, skip_group_check=sgc), reads, writes, inc)

    def tr(out, in_, ident, reads, writes, inc=True):
        return S.op('pe', lambda: PE.transpose(out=out, in_=in_, identity=ident), reads, writes, inc)

    def act(out, in_, func, reads, writes, bias=None, scale=None):
        kw = {}
        if bias is not None:
            kw['bias'] = bias
        if scale is not None:
            kw['scale'] = scale
        return S.op('act', lambda: A.activation(out=out, in_=in_, func=func, **kw), reads, writes)

    def tt(e, out, in0, in1, op, reads, writes):
        eng = V if e == 'dve' else G
        return S.op(e, lambda: eng.tensor_tensor(out=out, in0=in0, in1=in1, op=op), reads, writes)

    def ts(e, out, in0, s1, op0, reads, writes, s2=None, op1=None):
        eng = V if e == 'dve' else G
        if op1 is None:
            return S.op(e, lambda: eng.tensor_scalar(out=out, in0=in0, scalar1=s1, scalar2=None, op0=op0), reads, writes)
        return S.op(e, lambda: eng.tensor_scalar(out=out, in0=in0, scalar1=s1, scalar2=s2, op0=op0, op1=op1), reads, writes)

    def cp(e, out, in_, reads, writes):
        if e == 'act':
            return S.op('act', lambda: A.activation(out=out, in_=in_, func=AF.Copy), reads, writes)
        eng = V if e == 'dve' else G
        return S.op(e, lambda: eng.tensor_copy(out=out, in_=in_), reads, writes)

    P0 = es0
    identf = T(P0, "identf", [128, 128]); identb = T(P0, "identb", [128, 128], BF16)
    g_bc = T(P0, "g_bc", [128, 2, D]); fnw_bc = T(P0, "fnw_bc", [128, D])
    s1 = T(P0, "s1", [128, 8]); sh1 = T(P0, "sh1", [128, 8]); cs1 = T(P0, "cs1", [128, 8]); csh1 = T(P0, "csh1", [128, 8])
    s2 = T(P0, "s2", [128, 8]); sh2 = T(P0, "sh2", [128, 8])
    G_all = T(P0, "G_all", [128, NT, NE]); ssq = T(P0, "ssq", [128, 4, NT])
    modrow_d = nc.dram_tensor("modrow", [2, 128, D], F32, kind="Internal").ap()
    S.dma('sp', identf[:], cI_d[:, :], writes=['identf'], sem='c0')
    S.dma('sp', fnw_bc[:], fnw_d.partition_broadcast(128), writes=['fnw_bc'], sem='c0')
    S.regroup('c0', ['identf', 'fnw_bc'])
    cp('dve', identb[:], identf[:], ['identf'], ['identb'])

    esAB = ExitStack()
    dtr_all = T(esAB, "dtr_all", [128, NCH, 48])
    esW = ExitStack()
    win = T(esW, "win", [128, 8, INC], BF16)
    for k in range(8):
        S.dma('pool', win[:, k, :], win_d[k * 128:(k + 1) * 128, :], writes=['win'], sem='win')
    with ExitStack() as es:
        silu_c = T(es, "silu_c", [128, 8, 2]); cbc = T(es, "cbc", [128, 8, 128]); ones = T(es, "ones1", [128, 128])
        wm2 = [T(es, "wm%d" % i, [128, 8, D]) for i in range(2)]; modT = T(es, "modT", [128, 48, 2]); bmodT = T(es, "bmodT_s", [128, 48])
        bm_bc = T(es, "bm_bc", [128, D]); n1T = T(es, "n1T_s", [128, 8]); n2T = T(es, "n2T_s", [128, 8])
        tmp8 = T(es, "tmp8", [128, 8]); n2r_bc = T(es, "n2r_bc", [128, D])
        s2_bc = T(es, "s2_bc1", [128, D]); sh2_bc = T(es, "sh2_bc1", [128, D])
        S.dma('sp', n2r_bc[:], n2r_d.partition_broadcast(128), writes=['n2r_bc'])
        pm = PS(es, "pm", [128, 512]); pg = [PS(es, "pg0", [128, 512]), PS(es, "pg1", [128, 512])]
        S.dma('sp', silu_c[:], cvec_d[:, :, :], writes=['silu_c'], sem='c1')
        S.dma('sp', bmodT[:], bmodT_d[:, :], writes=['bmodT'], sem='c1')
        S.dma('sp', n1T[:], n1T_d[:, :], writes=['n1T'], sem='c1')
        S.dma('sp', n2T[:], n2T_d[:, :], writes=['n2T'], sem='c1')
        S.regroup('c1', ['silu_c', 'bmodT', 'n1T', 'n2T'])
        act(silu_c[:], silu_c[:], AF.Silu, ['silu_c'], ['silu_c'])
        S.op('dve', lambda: V.memset(ones[:], 1.0), writes=['ones1'])
        for k in range(8):
            ts('dve', cbc[:, k, :], ones[:], silu_c[:, k, 0:1], ALU.mult, ['ones1', 'silu_c'], ['cbc'])
        wm_v = wmod_d.rearrange("(k p) c -> p k c", p=128)
        S.dma('sp', wm2[0][:], wm_v[:, :, 0:D], writes=['wm0'], sem='wm0')
        for s in range(6):
            wm = wm2[s % 2]; wk = 'wm%d' % (s % 2)
            if s + 1 < 6:
                S.dma('sp', wm2[(s + 1) % 2][:], wm_v[:, :, (s + 1) * D:(s + 2) * D], writes=['wm%d' % ((s + 1) % 2)], sem='wm%d' % ((s + 1) % 2))
            if s in (2, 3, 4, 5):
                S.dma('sp', bm_bc[:], bmodr_d[:, s * D:(s + 1) * D].partition_broadcast(128), writes=['bm_bc'])
                for half in range(2):
                    hc = slice(half * 512, (half + 1) * 512)
                    for k in range(8):
                        mm(pg[half][:, :], cbc[:, k, :], wm[:, k, hc], k == 0, k == 7,
                           ['cbc', wk], ['pg%d' % half], inc=(k == 7))
                    dst = {2: g_bc[:, 0, hc], 5: g_bc[:, 1, hc], 3: sh2_bc[:, hc], 4: s2_bc[:, hc]}[s]
                    dk = 'g_bc' if s in (2, 5) else 's2_bc'
                    tt('dve', dst, pg[half][:, :], bm_bc[:, hc], ALU.add, ['pg%d' % half, 'bm_bc'], [dk])
                    if s == 4:
                        ts('dve', dst, dst, 1.0, ALU.add, [dk], [dk])
                        tt('dve', dst, dst, n2r_bc[:, hc], ALU.mult, [dk, 'n2r_bc'], [dk])
            if s not in (2, 5):
                for j in range(8):
                    for k in range(8):
                        mm(pm[:, j * 2:j * 2 + 2], wm[:, k, j * 128:(j + 1) * 128], silu_c[:, k, :], k == 0, k == 7,
                           [wk, 'silu_c'], ['pm'], inc=(k == 7))
                tt('dve', modT[:, s * 8:(s + 1) * 8, :], pm[:, 0:16].rearrange("p (j t) -> p j t", t=2),
                   bmodT[:, s * 8:(s + 1) * 8].unsqueeze(2).to_broadcast([128, 8, 2]), ALU.add, ['pm', 'bmodT'], ['modT'])
        for (dst, nT, sc_lo, col) in ((s1, n1T, 8, 0), (cs1, n1T, 8, 1), (s2, n2T, 32, 0)):
            ts('dve', tmp8[:], modT[:, sc_lo:sc_lo + 8, col], 1.0, ALU.add, ['modT'], ['tmp8'])
            tt('dve', dst[:], tmp8[:], nT[:], ALU.mult, ['tmp8', 'n1T', 'n2T'], ['modv'])
        cp('dve', sh1[:], modT[:, 0:8, 0], ['modT'], ['modv'])
        cp('dve', csh1[:], modT[:, 0:8, 1], ['modT'], ['modv'])
        cp('dve', sh2[:], modT[:, 24:32, 0], ['modT'], ['modv'])
        S.dma('sp', modrow_d[0], s2_bc[:], reads=['s2_bc'])
        S.dma('sp', modrow_d[1], sh2_bc[:], reads=['s2_bc'], sem='S_s2_bc')
        S.barrier()

    if upto == '1':
        esW.close(); esAB.close(); es0.close(); return nc

    def rms_to_hT(es_tiles, src_ap, s_vec, sh_vec, dst3, ps_T, names):
        xt, sq, ss, xn, tmp = es_tiles
        (kx, ksq, kss, kxn, ktmp, kps, kdst) = names
        S.dma('sp', xt, src_ap, writes=[kx], sem=kx)
        act(sq, xt, AF.Square, [kx], [ksq])
        S.op('dve', lambda: V.reduce_sum(out=ss[:, 0:1], in_=sq, axis=AX.X), [ksq], [kss])
        act(ss[:, 1:2], ss[:, 0:1], AF.Ln, [kss], [kss], bias=EPS, scale=1.0 / D)
        act(ss[:, 2:3], ss[:, 1:2], AF.Exp, [kss], [kss], scale=-0.5)
        ts('dve', xn, xt, ss[:, 2:3], ALU.mult, [kx, kss], [kxn])
        for k in range(8):
            tr(ps_T[:, k * 128:(k + 1) * 128], xn[:, k * 128:(k + 1) * 128], identf[:], [kxn, 'identf'], [kps], inc=(k == 7))
        tt('dve', tmp, ps_T[:, :].rearrange("p (k t) -> p k t", t=128), s_vec[:, :].unsqueeze(2).to_broadcast([128, 8, 128]),
           ALU.mult, [kps, 'modv'], [ktmp])
        tt('pool', dst3, tmp, sh_vec[:, :].unsqueeze(2).to_broadcast([128, 8, 128]), ALU.add, [ktmp, 'modv'], [kdst])

    with ExitStack() as es:
        dtb_bc = T(es, "dtb_bc", [128, 48]); AT = T(es, "AT", [128, 4, 128], BF16); ATf = T(es, "ATf", [128, 4, 128])
        poolw = T(es, "poolw", [128, 4, 128], BF16); poolwf = T(es, "poolwf", [128, 4, 128]); pscale = T(es, "pscale", [128, 4])
        zpad = T(es, "zpad", [128, 20, 4], BF16)
        S.dma('sp', dtb_bc[:], dtb_d.partition_broadcast(128), writes=['dtb_bc'], sem='c2')
        S.dma('sp', ATf[:], cAT_d[:, :, :], writes=['ATf'], sem='c2')
        S.dma('sp', poolwf[:], poolw_d[:, :, :], writes=['poolwf'], sem='c2')
        S.dma('sp', pscale[:], pscale_d[:, :], writes=['pscale'], sem='c2')
        S.regroup('c2', ['dtb_bc', 'ATf', 'poolwf', 'pscale'])
        cp('dve', AT[:], ATf[:], ['ATf'], ['AT']); cp('dve', poolw[:], poolwf[:], ['poolwf'], ['poolw'])
        S.op('dve', lambda: V.memset(zpad[:], 0.0), writes=['zpad'])
        zrow = T(es, "zrow", [128, 4, D], BF16)
        S.op('pool', lambda: G.memset(zrow[:], 0.0), writes=['zrow'])
        Xg_v0 = Xg_d.rearrange("(b j p) d -> b p j d", p=128, j=4)
        for blk in range(NSLOT // 512):
            S.dma('act', Xg_v0[blk], zrow[:], reads=['zrow'], sem='xgz')
        raw_v = rawT_d.rearrange("(t p) w -> p t w", p=128)
        S.dma('sp', raw_v[:, :, 0:2], zpad[:, :, 0:2], reads=['zpad'])
        S.dma('sp', raw_v[:, :, 4098:4102], zpad[:, :, 0:4], reads=['zpad'])
        S.dma('sp', raw_v[:, :, 4358:4360], zpad[:, :, 0:2], reads=['zpad'])
        xt_ = [T(es, "xt%d" % i, [128, D]) for i in range(2)]; sq_ = T(es, "sq", [128, D]); ss_ = [T(es, "ss%d" % i, [128, 4]) for i in range(2)]
        xn_ = [T(es, "xn%d" % i, [128, D]) for i in range(2)]; tmpm = T(es, "tmpm", [128, 8, 128])
        hT = [T(es, "hT%d" % i, [128, 8, 512], BF16) for i in range(2)]
        rawblk = [T(es, "rawblk0", [128, 20, 512], BF16)] * 2
        zsb = [T(es, "zsb%d" % i, [128, SSDW], BF16) for i in range(2)]
        usb = [T(es, "usb%d" % i, [128, 512], BF16) for i in range(2)]
        plsb = [T(es, "plsb%d" % i, [128, 4, 128], BF16) for i in range(2)]
        ypsb = [T(es, "ypsb%d" % i, [128, 4, 128], BF16) for i in range(2)]
        pT = PS(es, "pT", [128, 1024]); pfm = [PS(es, "pfm%d" % i, [128, 512]) for i in range(2)]
        ptm = [PS(es, "ptm%d" % i, [128, 512]) for i in range(2)]; ppl = PS(es, "ppl", [128, 512]); pyp = PS(es, "pyp", [128, 512])
        rx = Ring('x', 2); rfm = Ring('fm', 2); rtm = Ring('tm', 2); rz = Ring('z', 2)
        ypT_v = ypT_d.rearrange("(g o) t -> o g t", o=128)
        def RMS_(blk):
            ntile = 4 if blk < 8 else 2
            hs = blk % 2
            for j in range(ntile):
                i = rx.next()
                src = x_d[blk * 512 + j * 128: blk * 512 + (j + 1) * 128, :] if blk < 8 else ctx_d[j * 128:(j + 1) * 128, :]
                rms_to_hT((xt_[i][:], sq_[:], ss_[i], xn_[i][:], tmpm[:]), src,
                          s1 if blk < 8 else cs1, sh1 if blk < 8 else csh1, hT[hs][:, :, j * 128:(j + 1) * 128], pT,
                          ('xt%d' % i, 'sq', 'ss%d' % i, 'xn%d' % i, 'tmpm', 'pT', 'hT%d' % hs))

        RMS_(0)
        for blk in range(9):
            ntile = 4 if blk < 8 else 2
            ntok = ntile * 128
            hs = blk % 2
            if blk + 1 < 9:
                RMS_(blk + 1)
            for t in range(20):
                b = rfm.next()
                for k in range(8):
                    mm(pfm[b][:, 0:ntok], win[:, k, t * 128:(t + 1) * 128], hT[hs][:, k, 0:ntok], k == 0, k == 7,
                       ['win', 'hT%d' % hs], ['pfm%d' % b], inc=(k == 7))
                cp('act' if t % 2 == 0 else 'dve', rawblk[hs][:, t, 0:ntok], pfm[b][:, 0:ntok], ['pfm%d' % b], ['rawblk0'])
            off = 2 + 512 * blk if blk < 8 else 4102
            S.dma('sp', raw_v[:, :, off:off + ntok], rawblk[hs][:, :, 0:ntok], reads=['rawblk0'])
            for j in range(ntile):
                chunk = blk * 4 + j
                lt = hT[hs][:, :, j * 128:(j + 1) * 128]
                b = rtm.next()
                for k in range(8):
                    mm(ptm[b][:, 0:48], lt[:, k, :], win[:, k, CONVCH:CONVCH + 48], k == 0, k == 7, ['win', 'hT%d' % hs], ['ptm%d' % b], inc=(k == 7))
                tt('dve', dtr_all[:, chunk, :], ptm[b][:, 0:48], dtb_bc[:], ALU.add, ['ptm%d' % b, 'dtb_bc'], ['dtr_all'])
                if blk == 8:
                    continue
                zi = rz.next()
                for q in range(3):
                    b = rtm.next()
                    c0 = 2608 + q * 512
                    for k in range(8):
                        mm(ptm[b][:, :], lt[:, k, :], win[:, k, c0:c0 + 512], k == 0, k == 7, ['win', 'hT%d' % hs], ['ptm%d' % b], inc=(k == 7))
                    act(zsb[zi][:, q * 512:(q + 1) * 512], ptm[b][:, :], AF.Silu, ['ptm%d' % b], ['zsb%d' % zi])
                S.dma('sp', zt_d[chunk * 128:(chunk + 1) * 128, :], zsb[zi][:], reads=['zsb%d' % zi])
                b = rtm.next()
                for k in range(8):
                    mm(ptm[b][:, :], lt[:, k, :], win[:, k, 4144:4656], k == 0, k == 7, ['win', 'hT%d' % hs], ['ptm%d' % b], inc=(k == 7))
                cp('act', usb[zi][:], ptm[b][:, :], ['ptm%d' % b], ['usb%d' % zi])
                for g in range(4):
                    mm(ppl[:, g * 128:(g + 1) * 128], usb[zi][:, g * 128:(g + 1) * 128], AT[:, g, :], True, True, ['usb%d' % zi, 'AT'], ['ppl'], inc=(g == 3))
                cp('dve', plsb[zi][:], ppl[:, :].rearrange("p (g t) -> p g t", t=128), ['ppl'], ['plsb%d' % zi])
                for g in range(4):
                    mm(pyp[:, g * 128:(g + 1) * 128], poolw[:, g, :], plsb[zi][:, g, :], True, True, ['poolw', 'plsb%d' % zi], ['pyp'], inc=(g == 3))
                tt('dve', ypsb[zi][:], pyp[:, :].rearrange("p (g t) -> p g t", t=128), pscale[:, :].unsqueeze(2).to_broadcast([128, 4, 128]),
                   ALU.mult, ['pyp', 'pscale'], ['ypsb%d' % zi])
                S.dma('sp', ypT_v[:, :, chunk * 128:(chunk + 1) * 128], ypsb[zi][:], reads=['ypsb%d' % zi])
        S.barrier()

    esW.close()
    if upto == 'A':
        esAB.close(); es0.close(); return nc
    with ExitStack() as es:
        convw = T(es, "convw", [128, 20, 5]); convb = T(es, "convb", [128, 20]); Dvec = T(es, "Dvec", [128, SSDW])
        alog_bc = T(es, "alog_bc", [128, 48]); negA = T(es, "negA", [128, 48]); ynw = T(es, "ynw", [128, 12])
        cU = T(es, "cU", [128, 128]); cLo = T(es, "cLo", [128, 128]); ones = T(es, "ones2", [128, 128])
        cMask = T(es, "cMask", [128, 2, 128]); cSel = T(es, "cSel", [6, 6, 128])
        S.dma('sp', convw[:], convw_d[:, :, :], writes=['convw'], sem='c3')
        S.dma('sp', convb[:], convb_d[:, :], writes=['convb'], sem='c3')
        S.dma('sp', Dvec[:], dvec_d.partition_broadcast(128), writes=['Dvec'], sem='c3')
        S.dma('sp', alog_bc[:], alog_d.partition_broadcast(128), writes=['alog'], sem='c3')
        S.dma('sp', ynw[:], ynw_d[:, :], writes=['ynw'], sem='c3')
        S.dma('sp', cU[:], cU_d[:, :], writes=['cU'], sem='c3')
        S.dma('sp', cLo[:], cLo_d[:, :], writes=['cLo'], sem='c3')
        S.dma('sp', cMask[:], cMask_d[:, :, :], writes=['cMask'], sem='c3')
        S.dma('sp', cSel[:], cSel_d[:, :, :], writes=['cSel'], sem='c3')
        S.regroup('c3', ['convw', 'convb', 'Dvec', 'alog', 'ynw', 'cU', 'cLo', 'cMask', 'cSel'])
        S.op('dve', lambda: V.memset(ones[:], 1.0), writes=['ones2'])
        cMaskb = T(es, "cMaskb", [128, 2, 128], BF16); cSelb = T(es, "cSelb", [6, 6, 128], BF16)
        cp('dve', cMaskb[:], cMask[:], ['cMask'], ['cMaskb']); cp('dve', cSelb[:], cSel[:], ['cSel'], ['cSelb'])
        act(negA[:], alog_bc[:], AF.Exp, ['alog'], ['negA'])
        ts('dve', negA[:], negA[:], -1.0, ALU.mult, ['negA'], ['negA'])
        rawg = T(es, "rawg", [128, 5, RAWW], BF16)
        BT = T(es, "BT", [128, NCH * 128], BF16); CT = T(es, "CT", [128, NCH * 128], BF16)
        x_tok = T(es, "x_tok", [128, NCH, 384], BF16); B_tok = T(es, "B_tok", [128, NCH, 128], BF16)
        Sb_all = T(es, "Sb_all", [128, NT, 384], BF16)
        dg = [T(es, "dg%d" % i, [128, 5, 128], BF16) for i in range(2)]
        xc = [T(es, "xc%d" % i, [128, 512], BF16) for i in range(2)]
        dtg = T(es, "dtg", [128, NCH, 12]); av = T(es, "av", [128, NCH, 12]); lndt = T(es, "lndt", [128, NCH, 12])
        cs_all = T(es, "cs_all", [128, NCH, 12]); tot_all = T(es, "tot_all", [128, NCH, 12]); nb = T(es, "nb", [128, NCH, 12])
        eoff = T(es, "eoff", [128, NCH, 12]); wst = T(es, "wst", [128, NCH, 12]); dch = T(es, "dch", [128, NCH, 12])
        Srun = [T(es, "Srun%d" % i, [128, 384]) for i in range(3)]
        Sfb = [T(es, "Sfb%d" % i, [128, 384], BF16) for i in range(2)]
        xd = [T(es, "xd%d" % i, [128, 384], BF16) for i in range(4)]
        CBt = [T(es, "CBt%d" % i, [128, 128], BF16) for i in range(2)]
        csTh = [T(es, "csTh%d" % i, [6, 2, 128], BF16) for i in range(2)]; csTl = [T(es, "csTl%d" % i, [6, 2, 128], BF16) for i in range(2)]
        Lm = [T(es, "Lm%d" % i, [128, 128], BF16) for i in range(8)]
        Mt = [T(es, "Mt%d" % i, [128, 128], BF16) for i in range(8)]
        t1 = [T(es, "t1_%d" % i, [128, 384]) for i in range(2)]; t2 = T(es, "t2", [128, 384]); t3 = T(es, "t3", [128, 384])
        zg = [T(es, "zg%d" % i, [128, 384], BF16) for i in range(2)]; sz = T(es, "sz", [128, 384]); sqj = T(es, "sqj", [128, 384])
        yzb = [T(es, "yzb%d" % i, [128, 384], BF16) for i in range(2)]; yzTs = [T(es, "yzTs%d" % i, [128, 3, 128], BF16) for i in range(2)]
        pf = [PS(es, "pf%d" % i, [128, 512]) for i in range(7)]; pb = PS(es, "pb", [128, 1024], BF16)
        raw_rows = rawT_d.rearrange("(t p) w -> t p w", p=128)
        yzT_v = yzT_d.rearrange("(i p) t -> p i t", p=128)
        rdg = Ring('dg', 2); rcv = Ring('cv', 2); rxc = Ring('xc', 2)
        for g in range(4):
            tiles = [3 * g, 3 * g + 1, 3 * g + 2, 12 + g, 16 + g]
            if g == 0:
                for ti, Tt in enumerate(tiles):
                    S.dma('sp', rawg[:, ti, :], raw_rows[Tt, :, :], writes=['rawg%d' % ti], sem='rawg')
                S.regroup('rawg', ['rawg%d' % ti for ti in range(5)])
            units = [(ti, Tt, blk) for ti, Tt in enumerate(tiles) for blk in range(9)]
            ustate = {}

            def BP_(u):
                ti, Tt, blk = units[u]
                if blk == 0:
                    di = rdg.next()
                    for j in range(5):
                        ts('dve', dg[di][:, j, :], identb[:], convw[:, Tt, j:j + 1], ALU.mult, ['identb', 'convw'], ['dg%d' % di])
                    ustate['di'] = di
                di = ustate['di']
                n = 512 if blk < 8 else 256
                off = (2 + 512 * blk) if blk < 8 else 4102
                tok0 = 512 * blk
                b = rcv.next()
                for j in range(5):
                    mm(pf[b][:, 0:n], dg[di][:, j, :], rawg[:, ti, off - 2 + j: off - 2 + j + n], j == 0, j == 4,
                       ['dg%d' % di, 'rawg%d' % ti], ['pf%d' % b], inc=(j == 4))
                if ti < 3:
                    xi = rxc.next()
                    dst = xc[xi][:, 0:n]; dkey = 'xc%d' % xi
                elif ti == 3:
                    dst = BT[:, tok0:tok0 + n]; dkey = 'BT%d' % blk
                else:
                    dst = CT[:, tok0:tok0 + n]; dkey = 'CT%d' % blk
                act(dst, pf[b][:, 0:n], AF.Silu, ['pf%d' % b, 'convb'], [dkey], bias=convb[:, Tt:Tt + 1])
                ustate[u] = (dst, dkey, n)

            def BQ_(u):
                ti, Tt, blk = units[u]
                dst, dkey, n = ustate.pop(u)
                if ti <= 3:
                    nj = n // 128
                    for jj in range(nj):
                        tr(pb[:, jj * 128:(jj + 1) * 128], dst[:, jj * 128:(jj + 1) * 128], identb[:], [dkey, 'identb'], ['pb'], inc=(jj == nj - 1))
                    src3 = pb[:, 0:n].rearrange("p (j c) -> p j c", c=128)
                    if ti < 3:
                        cp('dve', x_tok[:, 4 * blk:4 * blk + nj, ti * 128:(ti + 1) * 128], src3, ['pb'], ['x_tok'])
                    else:
                        cp('dve', B_tok[:, 4 * blk:4 * blk + nj, :], src3, ['pb'], ['B_tok'])

            BP_(0)
            for u in range(len(units)):
                if u + 1 < len(units):
                    BP_(u + 1)
                BQ_(u)
            S.barrier()
            stop('B1')
            if g + 1 < 4:
                for ti, Tt in enumerate([3 * (g + 1), 3 * (g + 1) + 1, 3 * (g + 1) + 2, 12 + g + 1, 16 + g + 1]):
                    S.dma('sp', rawg[:, ti, :], raw_rows[Tt, :, :], writes=['rawg%d' % ti], sem='rawg')
                S.regroup('rawg', ['rawg%d' % ti for ti in range(5)])
            for d in range(2):
                cp('dve', dtg[:, :, d * 6:(d + 1) * 6], dtr_all[:, :, d * 24 + g * 6: d * 24 + g * 6 + 6], ['dtr_all'], ['dtg'])
            act(dtg[:], dtg[:], AF.Exp, ['dtg'], ['dtg'])
            act(dtg[:], dtg[:], AF.Ln, ['dtg'], ['dtg'], bias=1.0, scale=1.0)
            act(lndt[:], dtg[:], AF.Ln, ['dtg'], ['lndt'])
            for d in range(2):
                tt('dve', av[:, :, d * 6:(d + 1) * 6], dtg[:, :, d * 6:(d + 1) * 6],
                   negA[:, d * 24 + g * 6: d * 24 + g * 6 + 6].unsqueeze(1).to_broadcast([128, NCH, 6]), ALU.mult, ['dtg', 'negA'], ['av'])
            for c in range(NCH):
                last = (c == NCH - 1)
                mm(pf[4][:, c * 12:c * 12 + 6], cU[:], av[:, c, 0:6], True, True, ['cU', 'av'], ['pf4'], inc=False)
                mm(pf[4][:, c * 12 + 6:c * 12 + 12], cLo[:], av[:, c, 6:12], True, True, ['cLo', 'av'], ['pf4'], inc=False)
                mm(pf[5][:, c * 12:c * 12 + 12], ones[:], av[:, c, :], True, True, ['ones2', 'av'], ['pf5'], inc=last)
            cp('dve', cs_all[:], pf[4][:, 0:NCH * 12].rearrange("p (c h) -> p c h", h=12), ['pf4'], ['cs_all'])
            cp('dve', tot_all[:], pf[5][:, 0:NCH * 12].rearrange("p (c h) -> p c h", h=12), ['pf5'], ['tot_all'])
            tt('dve', nb[:], lndt[:], cs_all[:], ALU.subtract, ['lndt', 'cs_all'], ['nb'])
            act(eoff[:], cs_all[:], AF.Exp, ['cs_all'], ['eoff'])
            tt('dve', wst[:], tot_all[:], nb[:], ALU.add, ['tot_all', 'nb'], ['wst'])
            act(wst[:], wst[:], AF.Exp, ['wst'], ['wst'])
            act(dch[:], tot_all[:], AF.Exp, ['tot_all'], ['dch'])

            stop('B2')
            rxd = Ring('xd', 4)

            def chunk_state(c, d, bank=6):
                i = rxd.next()
                tt('dve', xd[i][:].rearrange("p (h q) -> p h q", q=64), x_tok[:, c, :].rearrange("p (h q) -> p h q", q=64),
                   wst[:, c, d * 6:(d + 1) * 6].unsqueeze(2).to_broadcast([128, 6, 64]), ALU.mult, ['x_tok', 'wst'], ['xd%d' % i])
                mm(pf[bank][:, 0:384], B_tok[:, c, :], xd[i][:], True, True, ['B_tok', 'xd%d' % i], ['pf%d' % bank])

            def dec_bc(c, d):
                return dch[:, c, d * 6:(d + 1) * 6].unsqueeze(2).to_broadcast([128, 6, 64])

            def v3(ap):
                return ap.rearrange("p (h q) -> p h q", q=64)

            for d, (ca, cb_) in enumerate(((32, 33), (33, 32))):
                chunk_state(ca, d)
                cp('dve', Srun[d][:], pf[6][:, 0:384], ['pf6'], ['Srun%d' % d])
                tt('dve', v3(Srun[d][:]), v3(Srun[d][:]), dec_bc(cb_, d), ALU.mult, ['Srun%d' % d, 'dch'], ['Srun%d' % d])
                chunk_state(cb_, d)
                tt('dve', Srun[d][:], Srun[d][:], pf[6][:, 0:384], ALU.add, ['Srun%d' % d, 'pf6'], ['Srun%d' % d])
            stop('B3')
            order = list(range(NT - 1, -1, -1))
            banks = [3, 4, 5, 6]
            AHEAD = 3
            for n in range(min(AHEAD, NT)):
                chunk_state(order[n], 1, banks[n % 4])
            cur = 1
            for n, c in enumerate(order):
                if n + AHEAD < NT:
                    chunk_state(order[n + AHEAD], 1, banks[(n + AHEAD) % 4])
                nxt = 2 if cur == 1 else 1
                cp('act', Sb_all[:, c, :], Srun[cur][:], ['Srun%d' % cur], ['Sb_all%d' % c])
                tt('dve', v3(Srun[nxt][:]), v3(Srun[cur][:]), dec_bc(c, 1), ALU.mult, ['Srun%d' % cur, 'dch'], ['Srun%d' % nxt])
                bk = banks[n % 4]
                tt('dve', Srun[nxt][:], Srun[nxt][:], pf[bk][:, 0:384], ALU.add, ['Srun%d' % nxt, 'pf%d' % bk], ['Srun%d' % nxt])
                cur = nxt
            S.barrier()
            stop('B4')
            rL = Ring('L', 2); rq = Ring('q', 8)
            pairs = [(d, h) for d in range(2) for h in range(6)]

            def H_(c, ci):
                tk = slice(c * 128, (c + 1) * 128)
                cp('act', Sfb[ci][:], Srun[0][:], ['Srun0'], ['Sfb%d' % ci])
                mm(pf[0][:, 0:128], BT[:, tk], CT[:, tk], True, True, ['BT%d' % (c // 4), 'CT%d' % (c // 4)], ['pf0'], inc=False)
                mm(pf[0][0:6, 128:256], av[:, c, 0:6], cU[:], True, True, ['av', 'cU'], ['pf0'], inc=False)
                mm(pf[0][0:6, 256:384], av[:, c, 6:12], cLo[:], True, True, ['av', 'cLo'], ['pf0'])
                cp('dve', CBt[ci][:], pf[0][:, 0:128], ['pf0'], ['CBt%d' % ci])
                src = pf[0][0:6, 128:384].rearrange("p (d l) -> p d l", l=128)
                cp('dve', csTh[ci][:], src, ['pf0'], ['csTh%d' % ci])
                tt('dve', csTl[ci][:], src, csTh[ci][:], ALU.subtract, ['pf0', 'csTh%d' % ci], ['csTl%d' % ci])

            def L_(c, ci, bt):
                lb = 1 + rL.next()
                bk = 'pf%d' % lb
                for q in range(4):
                    d, h = pairs[bt * 4 + q]
                    reg = pf[lb][:, q * 128:(q + 1) * 128]
                    mm(reg, cSelb[0:6, h, :], csTh[ci][0:6, d, :], True, False, ['cSelb', 'csTh%d' % ci], [bk], inc=False)
                    mm(reg, cSelb[0:6, h, :], csTl[ci][0:6, d, :], False, False, ['cSelb', 'csTl%d' % ci], [bk], inc=False)
                    mm(reg, identb[:], cMaskb[:, d, :], False, True, ['identb', 'cMaskb'], [bk], inc=(q == 3))
                return lb

            def E_(c, ci, bt, lb):
                bk = 'pf%d' % lb
                qis = []
                for q in range(4):
                    d, h = pairs[bt * 4 + q]
                    reg = pf[lb][:, q * 128:(q + 1) * 128]
                    qi = rq.next()
                    act(Lm[qi][:], reg, AF.Exp, [bk, 'nb'], ['Lm%d' % qi], bias=nb[:, c, d * 6 + h: d * 6 + h + 1])
                    tt('dve', Mt[qi][:], Lm[qi][:], CBt[ci][:], ALU.mult, ['Lm%d' % qi, 'CBt%d' % ci], ['Mt%d' % qi])
                    qis.append(qi)
                return qis

            def Y_(c, bt, qis):
                for q in range(4):
                    d, h = pairs[bt * 4 + q]
                    qi = qis[q]
                    mm(pf[3][:, h * 64:(h + 1) * 64], Mt[qi][:], x_tok[:, c, h * 64:(h + 1) * 64], (bt == 0 and q == 0), (bt == 2 and q == 3),
                       ['Mt%d' % qi, 'x_tok'], ['pf3'], inc=(q == 3), sgc=True)

            def O_(c, ci):
                tk = slice(c * 128, (c + 1) * 128)
                mm(pf[4][:, 0:384], CT[:, tk], Sfb[ci][:], True, True, ['CT%d' % (c // 4), 'Sfb%d' % ci], ['pf4'])
                mm(pf[5][:, 0:384], CT[:, tk], Sb_all[:, c, :], True, True, ['CT%d' % (c // 4), 'Sb_all%d' % c], ['pf5'])

            def U_(c):
                chunk_state(c, 0)
                tt('dve', v3(Srun[0][:]), v3(Srun[0][:]), dec_bc(c, 0), ALU.mult, ['Srun0', 'dch'], ['Srun0'])
                tt('dve', Srun[0][:], Srun[0][:], pf[6][:, 0:384], ALU.add, ['Srun0', 'pf6'], ['Srun0'])

            def F1_(c, ci):
                k1 = 't1_%d' % ci
                S.dma('sp', zg[ci][:], zt_d[c * 128:(c + 1) * 128, g * 384:(g + 1) * 384], writes=['zg%d' % ci], sem='zg%d' % ci)
                tt('dve', v3(t1[ci][:]), v3(pf[4][:, 0:384]), eoff[:, c, 0:6].unsqueeze(2).to_broadcast([128, 6, 64]), ALU.mult, ['pf4', 'eoff'], [k1])
                tt('dve', v3(t2[:]), v3(pf[5][:, 0:384]), eoff[:, c, 6:12].unsqueeze(2).to_broadcast([128, 6, 64]), ALU.mult, ['pf5', 'eoff'], ['t2'])
                tt('dve', t1[ci][:], pf[3][:, 0:384], t1[ci][:], ALU.add, ['pf3', k1], [k1])
                tt('pool', t3[:], x_tok[:, c, :], Dvec[:, g * 384:(g + 1) * 384], ALU.mult, ['x_tok', 'Dvec'], ['t3'])
                tt('pool', t2[:], t2[:], t3[:], ALU.add, ['t2', 't3'], ['t2'])
                tt('pool', t1[ci][:], t1[ci][:], t2[:], ALU.add, [k1, 't2'], [k1])

            def F2_(c, ci):
                k1 = 't1_%d' % ci
                tt('dve', t1[ci][:], t1[ci][:], zg[ci][:], ALU.mult, [k1, 'zg%d' % ci], [k1])
                tt('pool', sqj[:], t1[ci][:], t1[ci][:], ALU.mult, [k1], ['sqj'])

            def F3_(c, ci):
                k1 = 't1_%d' % ci
                S.op('dve', lambda: V.reduce_sum(out=ssq[:, g, c:c + 1], in_=sqj[:], axis=AX.X), ['sqj'], ['ssq'])
                cp('act', yzb[ci][:], t1[ci][:], [k1], ['yzb%d' % ci])

            def T_(c, ci):
                tk = slice(c * 128, (c + 1) * 128)
                for i3 in range(3):
                    tr(pb[:, 512 + i3 * 128: 512 + (i3 + 1) * 128], yzb[ci][:, i3 * 128:(i3 + 1) * 128], identb[:], ['yzb%d' % ci, 'identb'], ['pbz'], inc=(i3 == 2))
                tt('dve', yzTs[ci][:], pb[:, 512:896].rearrange("p (i t) -> p i t", t=128),
                   ynw[:, g * 3:(g + 1) * 3].unsqueeze(2).to_broadcast([128, 3, 128]), ALU.mult, ['pbz', 'ynw'], ['yzTs%d' % ci])
                S.dma('sp', yzT_v[:, g * 3:(g + 1) * 3, tk], yzTs[ci][:], reads=['yzTs%d' % ci])

            for c in range(NT):
                ci = c % 2
                H_(c, ci)
                lb0 = L_(c, ci, 0); q0 = E_(c, ci, 0, lb0)
                if c > 0:
                    F2_(c - 1, (c - 1) % 2)
                lb1 = L_(c, ci, 1); q1 = E_(c, ci, 1, lb1)
                if c > 0:
                    F3_(c - 1, (c - 1) % 2)
                Y_(c, 0, q0)
                lb2 = L_(c, ci, 2); q2 = E_(c, ci, 2, lb2)
                Y_(c, 1, q1)
                if c > 0:
                    T_(c - 1, (c - 1) % 2)
                Y_(c, 2, q2)
                O_(c, ci)
                U_(c)
                F1_(c, ci)
            F2_(NT - 1, (NT - 1) % 2)
            F3_(NT - 1, (NT - 1) % 2)
            T_(NT - 1, (NT - 1) % 2)
            S.barrier()
    esAB.close()
    if upto == 'B':
        es0.close(); return nc

    mask_all = T(P0, "mask_all", [128, NT, NE]); gates_all = T(P0, "gates_all", [128, NT, 4])
    idx_all = T(P0, "idx_all", [128, NT * 4], I32); widx = T(P0, "widx", [128, NBLK], I32)
    esC = ExitStack()
    s2_bc = T(esC, "s2_bc", [128, D]); sh2_bc = T(esC, "sh2_bc", [128, D])
    S.dma('sp', s2_bc[:], modrow_d[0], writes=['s2_bc'], sem='L_s2bc')
    S.dma('sp', sh2_bc[:], modrow_d[1], writes=['s2_bc'], sem='L_s2bc')
    with ExitStack() as es:
        wout = T(es, "wout", [128, 16, D], BF16)
        for k in range(16):
            S.dma('pool', wout[:, k, :], wout_d[k * 128:(k + 1) * 128, :], writes=['wout'], sem='wout')
        rw = T(es, "rw", [128, 8, NE]); rb_bc = T(es, "rb_bc", [128, NE])
        S.dma('sp', rw[:], rw_d[:, :, :], writes=['rw'], sem='c4')
        S.dma('sp', rb_bc[:], rb_d.partition_broadcast(128), writes=['rb_bc'], sem='c4')
        S.regroup('c4', ['rw', 'rb_bc'])
        rs_ssd = T(es, "rs_ssd", [128, NT]); tq = T(es, "tq", [128, NT])
        tt('dve', tq[:], ssq[:, 0, :], ssq[:, 1, :], ALU.add, ['ssq'], ['tq'])
        tt('dve', tq[:], tq[:], ssq[:, 2, :], ALU.add, ['tq', 'ssq'], ['tq'])
        tt('dve', tq[:], tq[:], ssq[:, 3, :], ALU.add, ['tq', 'ssq'], ['tq'])
        act(tq[:], tq[:], AF.Ln, ['tq'], ['tq'], bias=EPS, scale=1.0 / SSDW)
        act(rs_ssd[:], tq[:], AF.Exp, ['tq'], ['rs_ssd'], scale=-0.5)
        yzt = [T(es, "yzt%d" % i, [128, 12, 128], BF16) for i in range(2)]; ypt = [T(es, "ypt%d" % i, [128, 4, 128], BF16) for i in range(2)]
        xt_ = [T(es, "cxt%d" % i, [128, D]) for i in range(2)]; m_ = T(es, "cm", [128, D]); x1s = [T(es, "x1s%d" % i, [128, D]) for i in range(2)]
        sq_ = T(es, "csq", [128, D]); ss_ = [T(es, "css%d" % i, [128, 4]) for i in range(2)]; xn_ = [T(es, "cxn%d" % i, [128, D]) for i in range(2)]
        tmpm = T(es, "ctmpm", [128, 8, 128]); h2f = T(es, "h2f", [128, 8, 128]); htk = T(es, "htk", [128, D]); htb = [T(es, "htb%d" % i, [128, D], BF16) for i in range(2)]
        lg = T(es, "lg", [128, NE]); m8 = T(es, "m8", [128, 8]); ex = T(es, "ex", [128, NE]); sm = T(es, "sm", [128, 4])
        ps_s = [PS(es, "ps_s%d" % i, [128, 512]) for i in range(2)]; ps_p = [PS(es, "ps_p%d" % i, [128, 512]) for i in range(2)]
        pT = PS(es, "pT2", [128, 1024]); pr = PS(es, "pr", [128, 512])
        yzT_v = yzT_d.rearrange("(i p) t -> p i t", p=128); ypT_v = ypT_d.rearrange("(g o) t -> o g t", o=128)
        def CX_(j):
            i = j % 2
            tk = slice(j * 128, (j + 1) * 128)
            S.dma('sp', yzt[i][:], yzT_v[:, :, tk], writes=['yzt%d' % i], sem='yzt%d' % i)
            S.dma('sp', ypt[i][:], ypT_v[:, :, tk], writes=['ypt%d' % i], sem='ypt%d' % i)
            S.dma('sp', xt_[i][:], x_d[tk, :], writes=['cxt%d' % i], sem='cxt%d' % i)
            for half in range(2):
                hc = slice(half * 512, (half + 1) * 512)
                for k in range(12):
                    mm(ps_s[half][:, :], yzt[i][:, k, :], wout[:, k, hc], k == 0, k == 11, ['yzt%d' % i, 'wout'], ['ps_s%d' % half], inc=(k == 11))
                for k in range(4):
                    mm(ps_p[half][:, :], ypt[i][:, k, :], wout[:, 12 + k, hc], k == 0, k == 3, ['ypt%d' % i, 'wout'], ['ps_p%d' % half], inc=(k == 3))
                ts('dve', m_[:, hc], ps_s[half][:, :], rs_ssd[:, j:j + 1], ALU.mult, ['ps_s%d' % half, 'rs_ssd'], ['cm%d' % half])
                tt('dve', m_[:, hc], m_[:, hc], ps_p[half][:, :], ALU.add, ['cm%d' % half, 'ps_p%d' % half], ['cm%d' % half])
                tt('pool', m_[:, hc], m_[:, hc], g_bc[:, 0, hc], ALU.mult, ['cm%d' % half, 'g_bc'], ['cm%d' % half])
                tt('pool', x1s[i][:, hc], m_[:, hc], xt_[i][:, hc], ALU.add, ['cm%d' % half, 'cxt%d' % i], ['x1s%d' % i])
            S.dma('sp', x1_d[tk, :], x1s[i][:], reads=['x1s%d' % i])
            xt = x1s[i][:]
            act(sq_[:], xt, AF.Square, ['x1s%d' % i], ['csq'])
            S.op('dve', lambda: V.reduce_sum(out=ss_[i][:, 0:1], in_=sq_[:], axis=AX.X), ['csq'], ['css%d' % i])
            act(ss_[i][:, 1:2], ss_[i][:, 0:1], AF.Ln, ['css%d' % i], ['css%d' % i], bias=EPS, scale=1.0 / D)
            act(ss_[i][:, 2:3], ss_[i][:, 1:2], AF.Exp, ['css%d' % i], ['css%d' % i], scale=-0.5)
            ts('dve', xn_[i][:], xt, ss_[i][:, 2:3], ALU.mult, ['x1s%d' % i, 'css%d' % i], ['cxn%d' % i])
        def CY_(j):
            i = j % 2
            tk = slice(j * 128, (j + 1) * 128)
            for k in range(8):
                tr(pT[:, k * 128:(k + 1) * 128], xn_[i][:, k * 128:(k + 1) * 128], identf[:], ['cxn%d' % i, 'identf'], ['pT2'], inc=(k == 7))
            tt('dve', tmpm[:], pT[:, :].rearrange("p (k t) -> p k t", t=128), s2[:, :].unsqueeze(2).to_broadcast([128, 8, 128]),
               ALU.mult, ['pT2', 'modv'], ['ctmpm'])
            tt('pool', h2f[:], tmpm[:], sh2[:, :].unsqueeze(2).to_broadcast([128, 8, 128]), ALU.add, ['ctmpm', 'modv'], ['h2f'])
            tt('pool', htk[:], xn_[i][:], s2_bc[:], ALU.mult, ['cxn%d' % i, 's2_bc'], ['htk'])
            tt('pool', htb[i][:], htk[:], sh2_bc[:], ALU.add, ['htk', 's2_bc'], ['htb%d' % i])
            S.dma('sp', h2tok_d[tk, :], htb[i][:], reads=['htb%d' % i])
            for k in range(8):
                mm(pr[:, 0:NE], h2f[:, k, :], rw[:, k, :], k == 0, k == 7, ['h2f', 'rw'], ['pr'], inc=(k == 7))
            tt('dve', lg[:], pr[:, 0:NE], rb_bc[:], ALU.add, ['pr', 'rb_bc'], ['lg'])
            S.op('dve', lambda: V.max(out=m8[:], in_=lg[:]), ['lg'], ['m8'])
            ts('dve', mask_all[:, j, :], lg[:], m8[:, 3:4], ALU.is_ge, ['lg', 'm8'], ['mask_all'])
            ts('dve', sm[:, 0:1], m8[:, 0:1], -1.0, ALU.mult, ['m8'], ['sm'])
            act(ex[:], lg[:], AF.Exp, ['lg', 'sm'], ['ex'], bias=sm[:, 0:1])
            tt('dve', ex[:], ex[:], mask_all[:, j, :], ALU.mult, ['ex', 'mask_all'], ['ex'])
            S.op('dve', lambda: V.reduce_sum(out=sm[:, 1:2], in_=ex[:], axis=AX.X), ['ex'], ['sm'])
            S.op('dve', lambda: V.reciprocal(out=sm[:, 2:3], in_=sm[:, 1:2]), ['sm'], ['sm'])
            ts('dve', G_all[:, j, :], ex[:], sm[:, 2:3], ALU.mult, ['ex', 'sm'], ['G_all'])
        CX_(0)
        for j in range(NT):
            if j + 1 < NT:
                CX_(j + 1)
            CY_(j)
        S.barrier()
    esC.close()
    stop('C')

    with ExitStack() as es:
        SU = T(es, "SU", [128, 128], BF16); SUf = T(es, "SUf", [128, 128]); onesb = T(es, "onesb", [128, 128], BF16)
        mask_bf = T(es, "mask_bf", [128, NT, NE], BF16); P_all = T(es, "P_all", [128, NT + 1, NE], BF16)
        rank_all = T(es, "rank_all", [128, NT, NE]); counts = T(es, "counts", [128, NE]); padded = T(es, "padded", [128, NE])
        tmpc = T(es, "tmpc", [128, NE]); pad_end = T(es, "pad_end", [128, NE]); pad_start = T(es, "pad_start", [128, NE])
        onesf = T(es, "onesf", [128, NE]); Dm = T(es, "Dm", [128, NT, NE]); Em = T(es, "Em", [128, NT, NE])
        iota1 = T(es, "iota1", [128, NE]); jv = T(es, "jv", [128, NBLK]); pidx = T(es, "pidx", [128, 1])
        d8 = T(es, "d8", [128, 8]); e8 = T(es, "e8", [128, 8]); d4 = T(es, "d4", [128, NT, 4]); oh = T(es, "oh", [128, NE])
        cmp3 = T(es, "cmp3", [128, NBLK, NE]); bexp = T(es, "bexp", [128, NBLK])
        rows = [T(es, "rows%d" % i, [128, D], BF16) for i in range(2)]
        pk = [PS(es, "pk%d" % i, [128, 512]) for i in range(2)]; pc_ = PS(es, "pc_", [128, 512])
        S.dma('sp', SUf[:], cSU_d[:, :], writes=['SUf'], sem='c6')
        S.dma('sp', iota1[:], cIota_d.partition_broadcast(128), writes=['iota1'], sem='c6')
        S.dma('sp', jv[:], cJv_d.partition_broadcast(128), writes=['jv'], sem='c6')
        S.dma('sp', pidx[:], cPidx_d[:, :], writes=['pidx'], sem='c6')
        S.regroup('c6', ['SUf', 'iota1', 'jv', 'pidx'])
        cp('dve', SU[:], SUf[:], ['SUf'], ['SU'])
        S.op('dve', lambda: V.memset(onesb[:], 1.0), writes=['onesb'])
        S.op('dve', lambda: V.memset(onesf[:], 1.0), writes=['onesf'])
        cp('dve', mask_bf[:], mask_all[:], ['mask_all'], ['mask_bf'])
        S.op('dve', lambda: V.memset(P_all[:, 0, :], 0.0), writes=['P_all'])
        for j in range(NT):
            tt('dve', P_all[:, j + 1, :], P_all[:, j, :], mask_all[:, j, :], ALU.add, ['P_all', 'mask_all'], ['P_all'])
        for j in range(NT):
            b = j // 16
            reg = pk[b][:, (j % 16) * NE:(j % 16 + 1) * NE]
            mm(reg, SU[:], mask_bf[:, j, :], True, False, ['SU', 'mask_bf'], ['pk%d' % b], inc=False)
            mm(reg, onesb[:], P_all[:, j, :], False, True, ['onesb', 'P_all'], ['pk%d' % b], inc=(j % 16 == 15))
        for b in range(2):
            cp('dve', rank_all[:, b * 16:(b + 1) * 16, :], pk[b][:, :].rearrange("p (j e) -> p j e", e=NE), ['pk%d' % b], ['rank_all'])
        mm(pc_[:, 0:NE], onesb[:], P_all[:, NT, :], True, True, ['onesb', 'P_all'], ['pc_'])
        cp('dve', counts[:], pc_[:, 0:NE], ['pc_'], ['counts'])
        S.op('dve', lambda: V.memset(padded[:], 0.0), writes=['padded'])
        for m in range(4096 // BS):
            ts('dve', tmpc[:], counts[:], float(BS * m), ALU.is_gt, ['counts'], ['tmpc'], s2=float(BS), op1=ALU.mult)
            tt('dve', padded[:], padded[:], tmpc[:], ALU.add, ['padded', 'tmpc'], ['padded'])
        S.op('dve', lambda: V.tensor_tensor_scan(out=pad_end[:], data0=onesf[:], data1=padded[:], initial=0.0, op0=ALU.mult, op1=ALU.add),
             ['onesf', 'padded'], ['pad_end'])
        tt('dve', pad_start[:], pad_end[:], padded[:], ALU.subtract, ['pad_end', 'padded'], ['pad_start'])
        tt('dve', Dm[:], rank_all[:], pad_start[:, :].unsqueeze(1).to_broadcast([128, NT, NE]), ALU.add, ['rank_all', 'pad_start'], ['Dm'])
        ts('dve', Dm[:], Dm[:], 1.0, ALU.add, ['Dm'], ['Dm'])
        tt('dve', Dm[:], Dm[:], mask_all[:], ALU.mult, ['Dm', 'mask_all'], ['Dm'])
        tt('dve', Em[:], mask_all[:], iota1[:, :].unsqueeze(1).to_broadcast([128, NT, NE]), ALU.mult, ['mask_all', 'iota1'], ['Em'])
        for j in range(NT):
            S.op('dve', lambda: V.max(out=d8[:], in_=Dm[:, j, :]), ['Dm'], ['d8'])
            ts('dve', d4[:, j, :], d8[:, 0:4], -1.0, ALU.add, ['d8'], ['d4'])
        cp('dve', idx_all[:].rearrange("p (j k) -> p j k", k=4), d4[:], ['d4'], ['idx_all'])
        tt('dve', cmp3[:], pad_end[:, :].unsqueeze(1).to_broadcast([128, NBLK, NE]), jv[:, :].unsqueeze(2).to_broadcast([128, NBLK, NE]),
           ALU.is_le, ['pad_end', 'jv'], ['cmp3'])
        S.op('dve', lambda: V.reduce_sum(out=bexp[:], in_=cmp3[:], axis=AX.X), ['cmp3'], ['bexp'])
        ts('dve', bexp[:], bexp[:], float(NE - 1), ALU.min, ['bexp'], ['bexp'], s2=128.0, op1=ALU.mult)
        skp = T(es, "skp", [128, NBLK])
        S.op('dve', lambda: V.memset(skp[:], 0.0), writes=['skp'])
        tt('dve', skp[:, 2:NBLK], bexp[:, 2:NBLK], bexp[:, 0:NBLK - 2], ALU.is_equal, ['bexp', 'skp'], ['skp'])
        ts('dve', skp[:], skp[:], 1.0e6, ALU.mult, ['skp'], ['skp'])
        ts('dve', bexp[:], bexp[:], pidx[:, 0:1], ALU.add, ['bexp', 'pidx'], ['bexp'])
        tt('dve', bexp[:], bexp[:], skp[:], ALU.add, ['bexp', 'skp'], ['bexp'])
        cp('dve', widx[:], bexp[:], ['bexp'], ['widx'])
        S.barrier()
        stop('C2')
        for j in range(NT):
            i = j % 2
            S.dma('sp', rows[i][:], h2tok_d[j * 128:(j + 1) * 128, :], writes=['rows%d' % i], sem='rows%d' % i)
            for k in range(4):
                S.idma(out=Xg_d[:, :], in_=rows[i][:, :], idx=idx_all[:, j * 4 + k:j * 4 + k + 1], scatter=True, bound=NSLOT - 1,
                       reads=['rows%d' % i, 'idx_all'], sem='sc%d' % i)
        for j in range(NT):
            S.op('dve', lambda: V.max(out=e8[:], in_=Em[:, j, :]), ['Em'], ['e8'])
            for k in range(4):
                ts('dve', oh[:], iota1[:], e8[:, k:k + 1], ALU.is_equal, ['iota1', 'e8'], ['oh'])
                tt('dve', oh[:], oh[:], G_all[:, j, :], ALU.mult, ['oh', 'G_all'], ['oh'])
                S.op('dve', lambda: V.reduce_sum(out=gates_all[:, j, k:k + 1], in_=oh[:], axis=AX.X), ['oh'], ['gates_all'])
        S.barrier()
    stop('C3')

    with ExitStack() as es:
        wg_ = [T(es, "wg%d" % i, [128, 8, D], BF16) for i in range(2)]; wu_ = [T(es, "wu%d" % i, [128, 8, D], BF16) for i in range(2)]
        w2_ = [T(es, "w2_%d" % i, [128, 8, D], BF16) for i in range(2)]; b1t = [T(es, "b1t%d" % i, [128, 16]) for i in range(2)]
        NJ = BS // 128
        b1p = [T(es, "b1p%d" % i, [128, 8]) for i in range(2)]
        xgs = [T(es, "xgs%d" % i, [128, NJ, D], BF16) for i in range(2)]; xgT = [T(es, "xgT%d" % i, [128, 8, BS], BF16) for i in range(2)]
        actT = [T(es, "actT%d" % i, [128, 8, BS], BF16) for i in range(2)]
        gt3 = [T(es, "gt3_%d" % i, [128, BS]) for i in range(3)]; sg3 = [T(es, "sg3_%d" % i, [128, BS], BF16) for i in range(3)]
        ut3 = [T(es, "ut3_%d" % i, [128, BS]) for i in range(3)]
        ysb = [T(es, "ysb%d" % i, [128, NJ, D]) for i in range(2)]
        pg_ = [PS(es, "mpg%d" % i, [128, 512]) for i in range(2)]; pu_ = [PS(es, "mpu%d" % i, [128, 512]) for i in range(2)]
        py_ = [PS(es, "mpy%d" % i, [128, 512]) for i in range(2)]; pb_ = [PS(es, "mpb%d" % i, [128, 1024], BF16) for i in range(2)]
        Xg_v = Xg_d.rearrange("(b j p) d -> b p j d", p=128, j=NJ); Y_v = Y_d.rearrange("(b j p) o -> b p j o", p=128, j=NJ)

        def wload(blk, i):
            ix = widx[:, blk:blk + 1]
            for (dst, src, nm) in ((wg_[i], W1G_d, 'wg%d' % i), (wu_[i], W1U_d, 'wu%d' % i), (w2_[i], W2_d, 'w2_%d' % i)):
                S.idma(out=dst[:].rearrange("p k c -> p (k c)"), in_=src[:, :], idx=ix, scatter=False, bound=NE * 128 - 1,
                       reads=['widx'], writes=[nm], sem=nm)
            S.idma(out=b1t[i][:, :], in_=B1_d[:, :], idx=ix, scatter=False, bound=NE * 128 - 1, reads=['widx'], writes=['b1t%d' % i], sem='b1t%d' % i)

        rgu = Ring('gu', 2); ry = Ring('y', 2); rpb = Ring('pb', 2); r3 = Ring('r3', 3)
        KB = 1024 // BS

        def TR_(blk):
            wi = blk % 2
            S.dma('sp', xgs[wi][:], Xg_v[blk], writes=['xgs%d' % wi], sem='xgs%d' % wi)
            for k0 in range(0, 8, KB):
                pi = rpb.next()
                for kk in range(KB):
                    k = k0 + kk
                    for jj in range(NJ):
                        tr(pb_[pi][:, kk * BS + jj * 128: kk * BS + (jj + 1) * 128], xgs[wi][:, jj, k * 128:(k + 1) * 128], identb[:],
                           ['xgs%d' % wi, 'identb'], ['mpb%d' % pi], inc=(kk == KB - 1 and jj == NJ - 1))
                cp('act' if (k0 // KB) % 2 == 0 else 'dve', xgT[wi][:, k0:k0 + KB, :], pb_[pi][:, :].rearrange("p (k s) -> p k s", s=BS),
                   ['mpb%d' % pi], ['xgT%d' % wi])

        def FL_(blk):
            wi = blk % 2
            ts('pool', b1p[wi][:], b1t[wi][:, 8:16], 1.0, ALU.add, ['b1t%d' % wi], ['b1p%d' % wi])
            prev = None

            def fin(pv):
                ri, fp = pv
                S.op('dve', lambda: V.scalar_tensor_tensor(out=actT[wi][:, fp, :], in0=ut3[ri][:], scalar=-6.0, in1=gt3[ri][:], op0=ALU.max, op1=ALU.mult),
                     ['ut3_%d' % ri, 'gt3_%d' % ri], ['actT%d' % wi])
            for f in range(8):
                b = rgu.next(); ri = r3.next()
                fs = slice(f * 128, (f + 1) * 128)
                for k in range(8):
                    mm(pg_[b][:, 0:BS], wg_[wi][:, k, fs], xgT[wi][:, k, :], k == 0, k == 7, ['wg%d' % wi, 'xgT%d' % wi], ['mpg%d' % b], inc=(k == 7))
                for k in range(8):
                    mm(pu_[b][:, 0:BS], wu_[wi][:, k, fs], xgT[wi][:, k, :], k == 0, k == 7, ['wu%d' % wi, 'xgT%d' % wi], ['mpu%d' % b], inc=(k == 7))
                ts('dve', gt3[ri][:], pg_[b][:, 0:BS], b1t[wi][:, f:f + 1], ALU.add, ['mpg%d' % b, 'b1t%d' % wi], ['gt3_%d' % ri], s2=7.0, op1=ALU.min)
                act(sg3[ri][:], gt3[ri][:], AF.Sigmoid, ['gt3_%d' % ri], ['sg3_%d' % ri], scale=1.702)
                ts('dve', ut3[ri][:], pu_[b][:, 0:BS], b1p[wi][:, f:f + 1], ALU.add, ['mpu%d' % b, 'b1p%d' % wi], ['ut3_%d' % ri], s2=8.0, op1=ALU.min)
                tt('pool', gt3[ri][:], gt3[ri][:], sg3[ri][:], ALU.mult, ['gt3_%d' % ri, 'sg3_%d' % ri], ['gt3_%d' % ri])
                if prev is not None:
                    fin(prev)
                prev = (ri, f)
            fin(prev)

        def W2_(blk):
            wi = blk % 2
            for jj in range(NJ):
                for half in range(2):
                    b = ry.next()
                    for k in range(8):
                        mm(py_[b][:, :], actT[wi][:, k, jj * 128:(jj + 1) * 128], w2_[wi][:, k, half * 512:(half + 1) * 512], k == 0, k == 7,
                           ['actT%d' % wi, 'w2_%d' % wi], ['mpy%d' % b], inc=(k == 7))
                    cp('act' if half == 0 else 'dve', ysb[wi][:, jj, half * 512:(half + 1) * 512], py_[b][:, :], ['mpy%d' % b], ['ysb%d' % wi])
            S.dma('sp', Y_v[blk], ysb[wi][:], reads=['ysb%d' % wi])

        wload(0, 0)
        TR_(0)
        for blk in range(NBLK):
            if blk + 1 < NBLK:
                wload(blk + 1, (blk + 1) % 2)
            FL_(blk)
            if blk + 1 < NBLK:
                TR_(blk + 1)
            W2_(blk)
        S.barrier()
    stop('D')

    with ExitStack() as es:
        b2s = T(es, "b2s", [NE, D]); GT = T(es, "GT", [NE, 128])
        S.dma('sp', b2s[:], b2_d[:, :], writes=['b2s'], sem='b2s')
        yk = [T(es, "yk%d" % i, [128, D]) for i in range(8)]; acc = [T(es, "acc%d" % i, [128, D]) for i in range(2)]
        x1t = [T(es, "x1t%d" % i, [128, D]) for i in range(3)]; sq_ = T(es, "dsq", [128, D]); ss_ = [T(es, "dss%d" % i, [128, 4]) for i in range(2)]
        pgt = PS(es, "pgt", [128, 512]); pa = [PS(es, "pa%d" % i, [128, 512]) for i in range(2)]
        def EG_(j):
            i = j % 2; xi = j % 3
            tk = slice(j * 128, (j + 1) * 128)
            S.dma('sp', x1t[xi][:], x1_d[tk, :], writes=['x1t%d' % xi], sem='x1t%d' % xi)
            for k in range(4):
                kk = i * 4 + k
                S.idma(out=yk[kk][:, :], in_=Y_d[:, :], idx=idx_all[:, j * 4 + k:j * 4 + k + 1], scatter=False, bound=NSLOT - 1,
                       reads=['idx_all'], writes=['yk%d' % kk], sem='yk%d' % kk)

        def EA_(j):
            i = j % 2; xi = j % 3
            tr(pgt[0:NE, 0:128], G_all[:, j, :], identf[:], ['G_all', 'identf'], ['pgt'])
            cp('act', GT[:], pgt[0:NE, 0:128], ['pgt'], ['GT'])
            for half in range(2):
                hc = slice(half * 512, (half + 1) * 512)
                mm(pa[half][:, :], GT[:, :], b2s[:, hc], True, True, ['GT', 'b2s'], ['pa%d' % half])
                S.op('dve', lambda: V.scalar_tensor_tensor(out=acc[i][:, hc], in0=yk[i * 4][:, hc], scalar=gates_all[:, j, 0:1], in1=pa[half][:, :],
                                                            op0=ALU.mult, op1=ALU.add), ['yk%d' % (i * 4), 'gates_all', 'pa%d' % half], ['acc%d_%d' % (i, half)])
                for k in range(1, 4):
                    S.op('dve', lambda: V.scalar_tensor_tensor(out=acc[i][:, hc], in0=yk[i * 4 + k][:, hc], scalar=gates_all[:, j, k:k + 1], in1=acc[i][:, hc],
                                                                op0=ALU.mult, op1=ALU.add), ['yk%d' % (i * 4 + k), 'gates_all', 'acc%d_%d' % (i, half)], ['acc%d_%d' % (i, half)])
            ak = ['acc%d_0' % i, 'acc%d_1' % i]
            tt('pool', acc[i][:], acc[i][:], g_bc[:, 1, :], ALU.mult, ak + ['g_bc'], ak)
            tt('pool', x1t[xi][:], x1t[xi][:], acc[i][:], ALU.add, ['x1t%d' % xi] + ak, ['x1t%d' % xi])

        def EB_(j):
            i = j % 2; xi = j % 3
            tk = slice(j * 128, (j + 1) * 128)
            act(sq_[:], x1t[xi][:], AF.Square, ['x1t%d' % xi], ['dsq'])
            S.op('dve', lambda: V.reduce_sum(out=ss_[i][:, 0:1], in_=sq_[:], axis=AX.X), ['dsq'], ['dss%d' % i])
            act(ss_[i][:, 1:2], ss_[i][:, 0:1], AF.Ln, ['dss%d' % i], ['dss%d' % i], bias=EPS, scale=1.0 / D)
            act(ss_[i][:, 2:3], ss_[i][:, 1:2], AF.Exp, ['dss%d' % i], ['dss%d' % i], scale=-0.5)
            S.op('dve', lambda: V.scalar_tensor_tensor(out=x1t[xi][:], in0=x1t[xi][:], scalar=ss_[i][:, 2:3], in1=fnw_bc[:], op0=ALU.mult, op1=ALU.mult),
                 ['x1t%d' % xi, 'dss%d' % i, 'fnw_bc'], ['x1t%d' % xi])
            S.dma('sp', out_d[tk, :], x1t[xi][:], reads=['x1t%d' % xi])

        EG_(0)
        if NT > 1:
            EG_(1)
        EA_(0)
        for j in range(NT):
            if j + 2 < NT:
                EG_(j + 2)
            if j + 1 < NT:
                EA_(j + 1)
            EB_(j)
        S.barrier()
    es0.close()
    return nc


def _consts():
    I = np.eye(128, dtype=np.float32)
    k = np.arange(128)
    U = (k[:, None] <= k[None, :]).astype(np.float32)
    Lo = (k[:, None] >= k[None, :]).astype(np.float32)
    mask = np.zeros((128, 2, 128), np.float32)
    mask[:, 0, :] = np.where(k[None, :] >= k[:, None], 0.0, NEG)
    mask[:, 1, :] = np.where(k[None, :] <= k[:, None], 0.0, NEG)
    sel = np.zeros((6, 6, 128), np.float32)
    for h in range(6):
        sel[h, h, :] = 1.0
    AT = np.zeros((128, 4, 128), np.float32)
    t = np.arange(64)
    for g, w in enumerate((2, 4, 8, 16)):
        lo = np.clip(t - w // 2, 0, 64); hi = np.clip(t + w - w // 2, 0, 64)
        Am = np.zeros((64, 64), np.float32)
        for ti in range(64):
            Am[ti, lo[ti]:hi[ti]] = 1.0 / float(hi[ti] - lo[ti])
        Am -= np.eye(64, dtype=np.float32)
        for r in range(2):
            AT[r * 64:(r + 1) * 64, g, r * 64:(r + 1) * 64] = Am.T
    SU = (k[:, None] < k[None, :]).astype(np.float32)
    iota1 = (np.arange(NE, dtype=np.float32) + 1.0).reshape(1, NE)
    jv = (np.arange(NBLK, dtype=np.float32) * float(BS)).reshape(1, NBLK)
    pidx = np.arange(128, dtype=np.float32).reshape(128, 1)
    return dict(cI=I, cU=U, cLo=Lo, cMask=mask, cSel=sel, cAT=AT, cSU=SU, cIota=iota1, cJv=jv, cPidx=pidx)


_NC_CACHE = {}


def kernel(x, c, ctx, c_ctx, w_mod, b_mod, norm1_w, norm2_w, w_in, conv_w, conv_b, dt_bias, a_log, d_skip,
           ssd_norm_w, pool_w, pool_scale, w_out, router_w, router_b, w1, b1, w2, b2, final_norm_w, _dbg=False, _upto='ALL', _cores=8):
    f = lambda a: np.ascontiguousarray(np.asarray(a, dtype=np.float32))
    x = f(x); c = f(c); ctx = f(ctx); c_ctx = f(c_ctx)
    shared = dict(_consts())
    shared["w_mod"] = f(w_mod[0])
    shared["bmodT"] = f(b_mod[0].reshape(48, 128).T)
    shared["bmodr"] = f(b_mod[0].reshape(1, -1))
    shared["n1T"] = f(norm1_w[0].reshape(8, 128).T); shared["n2T"] = f(norm2_w[0].reshape(8, 128).T)
    shared["fnw"] = f(final_norm_w.reshape(1, -1))
    shared["w_in"] = f(w_in[0])
    shared["convw"] = f(np.asarray(conv_w[0]).T.reshape(20, 128, 5).transpose(1, 0, 2))
    shared["convb"] = f(np.asarray(conv_b[0]).reshape(20, 128).T)
    shared["dtb"] = f(np.asarray(dt_bias[0]).reshape(1, 48)); shared["alog"] = f(np.asarray(a_log[0]).reshape(1, 48))
    shared["dvec"] = f(np.repeat(np.asarray(d_skip[0]), 64).reshape(1, -1))
    shared["ynw"] = f(np.asarray(ssd_norm_w[0]).reshape(12, 128).T)
    shared["poolw"] = f(np.asarray(pool_w[0]).transpose(1, 0, 2))
    shared["pscale"] = f(np.asarray(pool_scale[0]).reshape(4, 128).T)
    shared["w_out"] = f(w_out[0])
    shared["rw"] = f(np.asarray(router_w[0]).reshape(8, 128, NE).transpose(1, 0, 2))
    shared["rb"] = f(np.asarray(router_b[0]).reshape(1, NE))
    w1a = np.asarray(w1[0])

    def ptile(w):
        return f(w.reshape(NE, 8, 128, -1).transpose(0, 2, 1, 3).reshape(NE * 128, -1))
    shared["W1G"] = ptile(w1a[:, :, 0::2]); shared["W1U"] = ptile(w1a[:, :, 1::2])
    shared["W2"] = ptile(np.asarray(w2[0]))
    b1a = np.asarray(b1[0])
    b1g_ = b1a[:, 0::2].reshape(NE, 8, 128).transpose(0, 2, 1)
    b1u_ = b1a[:, 1::2].reshape(NE, 8, 128).transpose(0, 2, 1)
    shared["B1"] = f(np.concatenate([b1g_, b1u_], axis=2).reshape(NE * 128, 16))
    shared["b2"] = f(b2[0])
    shared["n2r"] = f(norm2_w[0].reshape(1, -1))
    if (_dbg, _upto) not in _NC_CACHE:
        _NC_CACHE[(_dbg, _upto)] = build(dbg=_dbg, upto=_upto)
    nc = _NC_CACHE[(_dbg, _upto)]
    in_maps = []
    for b in range(_cores):
        m = dict(shared)
        m["x"] = x[b]; m["ctx"] = ctx[b]
        cv = np.stack([c[b], c_ctx], axis=-1)
        m["cvec"] = f(cv.reshape(8, 128, 2).transpose(1, 0, 2))
        in_maps.append(m)
    res = run_bass_kernel_spmd(nc, in_maps, core_ids=list(range(_cores)))
    if _dbg:
        return res
    return np.stack([r["out"] for r in res.results], axis=0).astype(np.float32)
```

```python
import numpy as np
from contextlib import ExitStack
import concourse.bass as bass
import concourse.mybir as mybir
from concourse.bass_utils import run_bass_kernel_spmd

F32 = mybir.dt.float32; BF16 = mybir.dt.bfloat16; I32 = mybir.dt.int32
AF = mybir.ActivationFunctionType; ALU = mybir.AluOpType; AX = mybir.AxisListType

D = 1024; L = 4096; CTXL = 256; NT = 32; NCH = 34
INC = 4656; CONVCH = 2560; SSDW = 1536
RAWW = 4360
NEG = -30000.0
NE = 32
BS = 256
NBLK = 16384 // BS + NE
NSLOT = NBLK * BS
EPS = 1e-6


class Sched:
    STRICT = True

    def __init__(self, nc, es):
        self.nc = nc; self.es = es
        self.eng = {'pe': nc.tensor, 'act': nc.scalar, 'dve': nc.vector, 'pool': nc.gpsimd, 'sp': nc.sync}
        self.sem = {}; self.cnt = {}
        for e in self.eng:
            self.sem[e] = es.enter_context(nc.semaphore("s_" + e)); self.cnt[e] = 0
        self.seen = {e: {} for e in self.eng}
        self.w = {}; self.r = {}
        self.dsem = {}; self.dcnt = {}; self.free_sems = []; self.free_sw = []; self.dsw = {}; self.nsem = 0; self.bregs = {}

    def _dsem(self, name, sw=False):
        if name not in self.dsem:
            pool = self.free_sw if sw else self.free_sems
            if pool:
                h, c = pool.pop()
                self.dsem[name] = h; self.dcnt[name] = c
            else:
                self.nsem += 1
                self.dsem[name] = self.es.enter_context(self.nc.semaphore("d_%d" % self.nsem)); self.dcnt[name] = 0
            self.dsw[name] = sw
        assert self.dsw[name] == sw, name
        return self.dsem[name]

    def _wait(self, e, tok):
        if tok is None:
            return
        kind, name, val = tok
        if kind == 'e' and name == e and (e == 'pe' or not self.STRICT):
            return
        skey = (kind, name)
        if self.seen[e].get(skey, 0) >= val:
            return
        self.seen[e][skey] = val
        s = self.sem[name] if kind == 'e' else self.dsem[name]
        self.eng[e].wait_ge(s, val)

    def deps(self, e, reads, writes):
        for k in reads:
            self._wait(e, self.w.get(k))
        for k in writes:
            self._wait(e, self.w.get(k))
            for tok in list(self.r.get(k, {}).values()):
                self._wait(e, tok)

    def record(self, tok, reads, writes):
        for k in reads:
            self.r.setdefault(k, {})[(tok[0], tok[1])] = tok
        for k in writes:
            self.w[k] = tok; self.r[k] = {}

    def op(self, e, fn, reads=(), writes=(), inc=True):
        self.deps(e, reads, writes)
        ins = fn()
        tok = ('e', e, self.cnt[e] + 1)
        self.record(tok, reads, writes)
        if inc:
            ins.then_inc(self.sem[e], 1); self.cnt[e] += 1
        return ins

    def dma(self, q, out, in_, reads=(), writes=(), sem=None):
        if sem is None:
            sem = ('L_' + writes[0]) if writes else ('S_' + reads[0])
        if q == 'pool':
            sem = 'sw_' + sem
        s = self._dsem(sem, sw=(q == 'pool'))
        self.deps(q, reads, writes)
        ins = self.eng[q].dma_start(out=out, in_=in_)
        ins.then_inc(s, 16); self.dcnt[sem] += 16
        tok = ('d', sem, self.dcnt[sem])
        self.record(tok, reads, writes)
        return ins

    def idma(self, out, in_, idx, scatter, bound, reads=(), writes=(), sem=None):
        sem = 'sw_' + sem
        s = self._dsem(sem, sw=True)
        self.deps('pool', reads, writes)
        if bound not in self.bregs:
            r = self.nc.gpsimd.alloc_register("bc%d" % bound)
            self.nc.gpsimd.reg_mov(r, bound)
            self.bregs[bound] = r
        bound = self.bregs[bound]
        off = bass.IndirectOffsetOnAxis(ap=idx, axis=0)
        if scatter:
            ins = self.nc.gpsimd.indirect_dma_start(out=out, out_offset=off, in_=in_, in_offset=None, bounds_check=bound, oob_is_err=False)
        else:
            ins = self.nc.gpsimd.indirect_dma_start(out=out, out_offset=None, in_=in_, in_offset=off, bounds_check=bound, oob_is_err=False)
        ins.then_inc(s, 16); self.dcnt[sem] += 16
        tok = ('d', sem, self.dcnt[sem])
        self.record(tok, reads, writes)
        return ins

    def regroup(self, sem, keys):
        for k in keys:
            self.w[k] = ('d', sem, self.dcnt[sem])

    def barrier(self):
        for e in self.eng:
            for f in self.eng:
                if f != e and self.cnt[f] > 0:
                    self._wait(e, ('e', f, self.cnt[f]))
            for name in self.dsem:
                if self.dcnt[name] > 0:
                    self._wait(e, ('d', name, self.dcnt[name]))
        for name in list(self.dsem):
            (self.free_sw if self.dsw[name] else self.free_sems).append((self.dsem[name], self.dcnt[name]))
            for e in self.eng:
                self.seen[e].pop(('d', name), None)
        self.dsem = {}; self.dcnt = {}; self.dsw = {}
        for k in list(self.w):
            if self.w[k][0] == 'd':
                del self.w[k]
        for k in list(self.r):
            for kk in [kk for kk in self.r[k] if kk[0] == 'd']:
                del self.r[k][kk]


class Ring:
    def __init__(self, name, n):
        self.name = name; self.n = n; self.i = -1

    def next(self):
        self.i += 1
        return self.i % self.n


class _Stop(Exception):
    pass


def build(dbg=False, upto='ALL'):
    try:
        return _build(dbg, upto)
    except _Stop as e:
        return e.args[0]


def _build(dbg=False, upto='ALL'):
    nc = bass.Bass("TRN2", target_bir_lowering=False)
    es0 = ExitStack()
    S = Sched(nc, es0)
    V = nc.vector; A = nc.scalar; G = nc.gpsimd; PE = nc.tensor

    def din(name, shape, dt=F32):
        return nc.dram_tensor(name, list(shape), dt, kind="ExternalInput").ap()

    def dscr(name, shape, dt):
        return nc.dram_tensor(name, list(shape), dt, kind=("ExternalOutput" if dbg else "Internal")).ap()

    x_d = din("x", [L, D]); ctx_d = din("ctx", [CTXL, D]); cvec_d = din("cvec", [128, 8, 2])
    wmod_d = din("w_mod", [D, 6 * D]); bmodT_d = din("bmodT", [128, 48]); bmodr_d = din("bmodr", [1, 6 * D])
    n1T_d = din("n1T", [128, 8]); n2T_d = din("n2T", [128, 8]); fnw_d = din("fnw", [1, D])
    win_d = din("w_in", [D, INC]); convw_d = din("convw", [128, 20, 5]); convb_d = din("convb", [128, 20])
    dtb_d = din("dtb", [1, 48]); alog_d = din("alog", [1, 48]); dvec_d = din("dvec", [1, SSDW])
    ynw_d = din("ynw", [128, 12]); poolw_d = din("poolw", [128, 4, 128]); pscale_d = din("pscale", [128, 4])
    wout_d = din("w_out", [2 * D, D]); rw_d = din("rw", [128, 8, NE]); rb_d = din("rb", [1, NE])
    W1G_d = din("W1G", [NE * 128, 8 * D]); W1U_d = din("W1U", [NE * 128, 8 * D]); W2_d = din("W2", [NE * 128, 8 * D])
    B1_d = din("B1", [NE * 128, 16]); b2_d = din("b2", [NE, D]); n2r_d = din("n2r", [1, D])
    cSU_d = din("cSU", [128, 128]); cIota_d = din("cIota", [1, NE]); cJv_d = din("cJv", [1, NBLK]); cPidx_d = din("cPidx", [128, 1])
    cI_d = din("cI", [128, 128]); cU_d = din("cU", [128, 128]); cLo_d = din("cLo", [128, 128])
    cMask_d = din("cMask", [128, 2, 128]); cSel_d = din("cSel", [6, 6, 128]); cAT_d = din("cAT", [128, 4, 128])
    out_d = nc.dram_tensor("out", [L, D], F32, kind="ExternalOutput").ap()
    rawT_d = dscr("rawT", [CONVCH, RAWW], BF16); zt_d = dscr("zt", [L, SSDW], BF16)
    ypT_d = dscr("ypT", [512, L], BF16); yzT_d = dscr("yzT", [SSDW, L], BF16)
    x1_d = dscr("x1", [L, D], F32); h2tok_d = dscr("h2tok", [L, D], BF16)
    Xg_d = nc.dram_tensor("Xg", [NSLOT, D], BF16, kind="Internal").ap(); Y_d = nc.dram_tensor("Y", [NSLOT, D], F32, kind="Internal").ap()

    def stop(tag):
        if upto == tag:
            S.barrier()
            raise _Stop(nc)

    def T(es, name, shape, dt=F32):
        return es.enter_context(nc.sbuf_tensor("t_" + name, list(shape), dt))

    def PS(es, name, shape, dt=F32):
        return es.enter_context(nc.psum_tensor("p_" + name, list(shape), dt))

    def mm(out, lhsT, rhs, start, stop, reads, writes, inc=True, sgc=False):
        return S.op('pe', lambda: PE.matmul(out, lhsT=lhsT, rhs=rhs, start=start, stop=stop, skip_group_check=sgc), reads, writes, inc)

    def tr(out, in_, ident, reads, writes, inc=True):
        return S.op('pe', lambda: PE.transpose(out=out, in_=in_, identity=ident), reads, writes, inc)

    def act(out, in_, func, reads, writes, bias=None, scale=None):
        kw = {}
        if bias is not None:
            kw['bias'] = bias
        if scale is not None:
            kw['scale'] = scale
        return S.op('act', lambda: A.activation(out=out, in_=in_, func=func, **kw), reads, writes)

    def tt(e, out, in0, in1, op, reads, writes):
        eng = V if e == 'dve' else G
        return S.op(e, lambda: eng.tensor_tensor(out=out, in0=in0, in1=in1, op=op), reads, writes)

    def ts(e, out, in0, s1, op0, reads, writes, s2=None, op1=None):
        eng = V if e == 'dve' else G
        if op1 is None:
            return S.op(e, lambda: eng.tensor_scalar(out=out, in0=in0, scalar1=s1, scalar2=None, op0=op0), reads, writes)
        return S.op(e, lambda: eng.tensor_scalar(out=out, in0=in0, scalar1=s1, scalar2=s2, op0=op0, op1=op1), reads, writes)

    def cp(e, out, in_, reads, writes):
        if e == 'act':
            return S.op('act', lambda: A.activation(out=out, in_=in_, func=AF.Copy), reads, writes)
        eng = V if e == 'dve' else G
        return S.op(e, lambda: eng.tensor_copy(out=out, in_=in_), reads, writes)

    P0 = es0
    identf = T(P0, "identf", [128, 128]); identb = T(P0, "identb", [128, 128], BF16)
    g_bc = T(P0, "g_bc", [128, 2, D]); fnw_bc = T(P0, "fnw_bc", [128, D])
    s1 = T(P0, "s1", [128, 8]); sh1 = T(P0, "sh1", [128, 8]); cs1 = T(P0, "cs1", [128, 8]); csh1 = T(P0, "csh1", [128, 8])
    s2 = T(P0, "s2", [128, 8]); sh2 = T(P0, "sh2", [128, 8])
    G_all = T(P0, "G_all", [128, NT, NE]); ssq = T(P0, "ssq", [128, 4, NT])
    modrow_d = nc.dram_tensor("modrow", [2, 128, D], F32, kind="Internal").ap()
    S.dma('sp', identf[:], cI_d[:, :], writes=['identf'], sem='c0')
    S.dma('sp', fnw_bc[:], fnw_d.partition_broadcast(128), writes=['fnw_bc'], sem='c0')
    S.regroup('c0', ['identf', 'fnw_bc'])
    cp('dve', identb[:], identf[:], ['identf'], ['identb'])

    esAB = ExitStack()
    dtr_all = T(esAB, "dtr_all", [128, NCH, 48])
    esW = ExitStack()
    win = T(esW, "win", [128, 8, INC], BF16)
    for k in range(8):
        S.dma('pool', win[:, k, :], win_d[k * 128:(k + 1) * 128, :], writes=['win'], sem='win')
    with ExitStack() as es:
        silu_c = T(es, "silu_c", [128, 8, 2]); cbc = T(es, "cbc", [128, 8, 128]); ones = T(es, "ones1", [128, 128])
        wm2 = [T(es, "wm%d" % i, [128, 8, D]) for i in range(2)]; modT = T(es, "modT", [128, 48, 2]); bmodT = T(es, "bmodT_s", [128, 48])
        bm_bc = T(es, "bm_bc", [128, D]); n1T = T(es, "n1T_s", [128, 8]); n2T = T(es, "n2T_s", [128, 8])
        tmp8 = T(es, "tmp8", [128, 8]); n2r_bc = T(es, "n2r_bc", [128, D])
        s2_bc = T(es, "s2_bc1", [128, D]); sh2_bc = T(es, "sh2_bc1", [128, D])
        S.dma('sp', n2r_bc[:], n2r_d.partition_broadcast(128), writes=['n2r_bc'])
        pm = PS(es, "pm", [128, 512]); pg = [PS(es, "pg0", [128, 512]), PS(es, "pg1", [128, 512])]
        S.dma('sp', silu_c[:], cvec_d[:, :, :], writes=['silu_c'], sem='c1')
        S.dma('sp', bmodT[:], bmodT_d[:, :], writes=['bmodT'], sem='c1')
        S.dma('sp', n1T[:], n1T_d[:, :], writes=['n1T'], sem='c1')
        S.dma('sp', n2T[:], n2T_d[:, :], writes=['n2T'], sem='c1')
        S.regroup('c1', ['silu_c', 'bmodT', 'n1T', 'n2T'])
        act(silu_c[:], silu_c[:], AF.Silu, ['silu_c'], ['silu_c'])
        S.op('dve', lambda: V.memset(ones[:], 1.0), writes=['ones1'])
        for k in range(8):
            ts('dve', cbc[:, k, :], ones[:], silu_c[:, k, 0:1], ALU.mult, ['ones1', 'silu_c'], ['cbc'])
        wm_v = wmod_d.rearrange("(k p) c -> p k c", p=128)
        S.dma('sp', wm2[0][:], wm_v[:, :, 0:D], writes=['wm0'], sem='wm0')
        for s in range(6):
            wm = wm2[s % 2]; wk = 'wm%d' % (s % 2)
            if s + 1 < 6:
                S.dma('sp', wm2[(s + 1) % 2][:], wm_v[:, :, (s + 1) * D:(s + 2) * D], writes=['wm%d' % ((s + 1) % 2)], sem='wm%d' % ((s + 1) % 2))
            if s in (2, 3, 4, 5):
                S.dma('sp', bm_bc[:], bmodr_d[:, s * D:(s + 1) * D].partition_broadcast(128), writes=['bm_bc'])
                for half in range(2):
                    hc = slice(half * 512, (half + 1) * 512)
                    for k in range(8):
                        mm(pg[half][:, :], cbc[:, k, :], wm[:, k, hc], k == 0, k == 7,
                           ['cbc', wk], ['pg%d' % half], inc=(k == 7))
                    dst = {2: g_bc[:, 0, hc], 5: g_bc[:, 1, hc], 3: sh2_bc[:, hc], 4: s2_bc[:, hc]}[s]
                    dk = 'g_bc' if s in (2, 5) else 's2_bc'
                    tt('dve', dst, pg[half][:, :], bm_bc[:, hc], ALU.add, ['pg%d' % half, 'bm_bc'], [dk])
                    if s == 4:
                        ts('dve', dst, dst, 1.0, ALU.add, [dk], [dk])
                        tt('dve', dst, dst, n2r_bc[:, hc], ALU.mult, [dk, 'n2r_bc'], [dk])
            if s not in (2, 5):
                for j in range(8):
                    for k in range(8):
                        mm(pm[:, j * 2:j * 2 + 2], wm[:, k, j * 128:(j + 1) * 128], silu_c[:, k, :], k == 0, k == 7,
                           [wk, 'silu_c'], ['pm'], inc=(k == 7))
                tt('dve', modT[:, s * 8:(s + 1) * 8, :], pm[:, 0:16].rearrange("p (j t) -> p j t", t=2),
                   bmodT[:, s * 8:(s + 1) * 8].unsqueeze(2).to_broadcast([128, 8, 2]), ALU.add, ['pm', 'bmodT'], ['modT'])
        for (dst, nT, sc_lo, col) in ((s1, n1T, 8, 0), (cs1, n1T, 8, 1), (s2, n2T, 32, 0)):
            ts('dve', tmp8[:], modT[:, sc_lo:sc_lo + 8, col], 1.0, ALU.add, ['modT'], ['tmp8'])
            tt('dve', dst[:], tmp8[:], nT[:], ALU.mult, ['tmp8', 'n1T', 'n2T'], ['modv'])
        cp('dve', sh1[:], modT[:, 0:8, 0], ['modT'], ['modv'])
        cp('dve', csh1[:], modT[:, 0:8, 1], ['modT'], ['modv'])
        cp('dve', sh2[:], modT[:, 24:32, 0], ['modT'], ['modv'])
        S.dma('sp', modrow_d[0], s2_bc[:], reads=['s2_bc'])
        S.dma('sp', modrow_d[1], sh2_bc[:], reads=['s2_bc'], sem='S_s2_bc')
        S.barrier()

    if upto == '1':
        esW.close(); esAB.close(); es0.close(); return nc

    def rms_to_hT(es_tiles, src_ap, s_vec, sh_vec, dst3, ps_T, names):
        xt, sq, ss, xn, tmp = es_tiles
        (kx, ksq, kss, kxn, ktmp, kps, kdst) = names
        S.dma('sp', xt, src_ap, writes=[kx], sem=kx)
        act(sq, xt, AF.Square, [kx], [ksq])
        S.op('dve', lambda: V.reduce_sum(out=ss[:, 0:1], in_=sq, axis=AX.X), [ksq], [kss])
        act(ss[:, 1:2], ss[:, 0:1], AF.Ln, [kss], [kss], bias=EPS, scale=1.0 / D)
        act(ss[:, 2:3], ss[:, 1:2], AF.Exp, [kss], [kss], scale=-0.5)
        ts('dve', xn, xt, ss[:, 2:3], ALU.mult, [kx, kss], [kxn])
        for k in range(8):
            tr(ps_T[:, k * 128:(k + 1) * 128], xn[:, k * 128:(k + 1) * 128], identf[:], [kxn, 'identf'], [kps], inc=(k == 7))
        tt('dve', tmp, ps_T[:, :].rearrange("p (k t) -> p k t", t=128), s_vec[:, :].unsqueeze(2).to_broadcast([128, 8, 128]),
           ALU.mult, [kps, 'modv'], [ktmp])
        tt('pool', dst3, tmp, sh_vec[:, :].unsqueeze(2).to_broadcast([128, 8, 128]), ALU.add, [ktmp, 'modv'], [kdst])

    with ExitStack() as es:
        dtb_bc = T(es, "dtb_bc", [128, 48]); AT = T(es, "AT", [128, 4, 128], BF16); ATf = T(es, "ATf", [128, 4, 128])
        poolw = T(es, "poolw", [128, 4, 128], BF16); poolwf = T(es, "poolwf", [128, 4, 128]); pscale = T(es, "pscale", [128, 4])
        zpad = T(es, "zpad", [128, 20, 4], BF16)
        S.dma('sp', dtb_bc[:], dtb_d.partition_broadcast(128), writes=['dtb_bc'], sem='c2')
        S.dma('sp', ATf[:], cAT_d[:, :, :], writes=['ATf'], sem='c2')
        S.dma('sp', poolwf[:], poolw_d[:, :, :], writes=['poolwf'], sem='c2')
        S.dma('sp', pscale[:], pscale_d[:, :], writes=['pscale'], sem='c2')
        S.regroup('c2', ['dtb_bc', 'ATf', 'poolwf', 'pscale'])
        cp('dve', AT[:], ATf[:], ['ATf'], ['AT']); cp('dve', poolw[:], poolwf[:], ['poolwf'], ['poolw'])
        S.op('dve', lambda: V.memset(zpad[:], 0.0), writes=['zpad'])
        zrow = T(es, "zrow", [128, 4, D], BF16)
        S.op('pool', lambda: G.memset(zrow[:], 0.0), writes=['zrow'])
        Xg_v0 = Xg_d.rearrange("(b j p) d -> b p j d", p=128, j=4)
        for blk in range(NSLOT // 512):
            S.dma('act', Xg_v0[blk], zrow[:], reads=['zrow'], sem='xgz')
        raw_v = rawT_d.rearrange("(t p) w -> p t w", p=128)
        S.dma('sp', raw_v[:, :, 0:2], zpad[:, :, 0:2], reads=['zpad'])
        S.dma('sp', raw_v[:, :, 4098:4102], zpad[:, :, 0:4], reads=['zpad'])
        S.dma('sp', raw_v[:, :, 4358:4360], zpad[:, :, 0:2], reads=['zpad'])
        xt_ = [T(es, "xt%d" % i, [128, D]) for i in range(2)]; sq_ = T(es, "sq", [128, D]); ss_ = [T(es, "ss%d" % i, [128, 4]) for i in range(2)]
        xn_ = [T(es, "xn%d" % i, [128, D]) for i in range(2)]; tmpm = T(es, "tmpm", [128, 8, 128])
        hT = [T(es, "hT%d" % i, [128, 8, 512], BF16) for i in range(2)]
        rawblk = [T(es, "rawblk0", [128, 20, 512], BF16)] * 2
        zsb = [T(es, "zsb%d" % i, [128, SSDW], BF16) for i in range(2)]
        usb = [T(es, "usb%d" % i, [128, 512], BF16) for i in range(2)]
        plsb = [T(es, "plsb%d" % i, [128, 4, 128], BF16) for i in range(2)]
        ypsb = [T(es, "ypsb%d" % i, [128, 4, 128], BF16) for i in range(2)]
        pT = PS(es, "pT", [128, 1024]); pfm = [PS(es, "pfm%d" % i, [128, 512]) for i in range(2)]
        ptm = [PS(es, "ptm%d" % i, [128, 512]) for i in range(2)]; ppl = PS(es, "ppl", [128, 512]); pyp = PS(es, "pyp", [128, 512])
        rx = Ring('x', 2); rfm = Ring('fm', 2); rtm = Ring('tm', 2); rz = Ring('z', 2)
        ypT_v = ypT_d.rearrange("(g o) t -> o g t", o=128)
        def RMS_(blk):
            ntile = 4 if blk < 8 else 2
            hs = blk % 2
            for j in range(ntile):
                i = rx.next()
                src = x_d[blk * 512 + j * 128: blk * 512 + (j + 1) * 128, :] if blk < 8 else ctx_d[j * 128:(j + 1) * 128, :]
                rms_to_hT((xt_[i][:], sq_[:], ss_[i], xn_[i][:], tmpm[:]), src,
                          s1 if blk < 8 else cs1, sh1 if blk < 8 else csh1, hT[hs][:, :, j * 128:(j + 1) * 128], pT,
                          ('xt%d' % i, 'sq', 'ss%d' % i, 'xn%d' % i, 'tmpm', 'pT', 'hT%d' % hs))

        RMS_(0)
        for blk in range(9):
            ntile = 4 if blk < 8 else 2
            ntok = ntile * 128
            hs = blk % 2
            if blk + 1 < 9:
                RMS_(blk + 1)
            for t in range(20):
                b = rfm.next()
                for k in range(8):
                    mm(pfm[b][:, 0:ntok], win[:, k, t * 128:(t + 1) * 128], hT[hs][:, k, 0:ntok], k == 0, k == 7,
                       ['win', 'hT%d' % hs], ['pfm%d' % b], inc=(k == 7))
                cp('act' if t % 2 == 0 else 'dve', rawblk[hs][:, t, 0:ntok], pfm[b][:, 0:ntok], ['pfm%d' % b], ['rawblk0'])
            off = 2 + 512 * blk if blk < 8 else 4102
            S.dma('sp', raw_v[:, :, off:off + ntok], rawblk[hs][:, :, 0:ntok], reads=['rawblk0'])
            for j in range(ntile):
                chunk = blk * 4 + j
                lt = hT[hs][:, :, j * 128:(j + 1) * 128]
                b = rtm.next()
                for k in range(8):
                    mm(ptm[b][:, 0:48], lt[:, k, :], win[:, k, CONVCH:CONVCH + 48], k == 0, k == 7, ['win', 'hT%d' % hs], ['ptm%d' % b], inc=(k == 7))
                tt('dve', dtr_all[:, chunk, :], ptm[b][:, 0:48], dtb_bc[:], ALU.add, ['ptm%d' % b, 'dtb_bc'], ['dtr_all'])
                if blk == 8:
                    continue
                zi = rz.next()
                for q in range(3):
                    b = rtm.next()
                    c0 = 2608 + q * 512
                    for k in range(8):
                        mm(ptm[b][:, :], lt[:, k, :], win[:, k, c0:c0 + 512], k == 0, k == 7, ['win', 'hT%d' % hs], ['ptm%d' % b], inc=(k == 7))
                    act(zsb[zi][:, q * 512:(q + 1) * 512], ptm[b][:, :], AF.Silu, ['ptm%d' % b], ['zsb%d' % zi])
                S.dma('sp', zt_d[chunk * 128:(chunk + 1) * 128, :], zsb[zi][:], reads=['zsb%d' % zi])
                b = rtm.next()
                for k in range(8):
                    mm(ptm[b][:, :], lt[:, k, :], win[:, k, 4144:4656], k == 0, k == 7, ['win', 'hT%d' % hs], ['ptm%d' % b], inc=(k == 7))
                cp('act', usb[zi][:], ptm[b][:, :], ['ptm%d' % b], ['usb%d' % zi])
                for g in range(4):
                    mm(ppl[:, g * 128:(g + 1) * 128], usb[zi][:, g * 128:(g + 1) * 128], AT[:, g, :], True, True, ['usb%d' % zi, 'AT'], ['ppl'], inc=(g == 3))
                cp('dve', plsb[zi][:], ppl[:, :].rearrange("p (g t) -> p g t", t=128), ['ppl'], ['plsb%d' % zi])
                for g in range(4):
                    mm(pyp[:, g * 128:(g + 1) * 128], poolw[:, g, :], plsb[zi][:, g, :], True, True, ['poolw', 'plsb%d' % zi], ['pyp'], inc=(g == 3))
                tt('dve', ypsb[zi][:], pyp[:, :].rearrange("p (g t) -> p g t", t=128), pscale[:, :].unsqueeze(2).to_broadcast([128, 4, 128]),
                   ALU.mult, ['pyp', 'pscale'], ['ypsb%d' % zi])
                S.dma('sp', ypT_v[:, :, chunk * 128:(chunk + 1) * 128], ypsb[zi][:], reads=['ypsb%d' % zi])
        S.barrier()

    esW.close()
    if upto == 'A':
        esAB.close(); es0.close(); return nc
    with ExitStack() as es:
        convw = T(es, "convw", [128, 20, 5]); convb = T(es, "convb", [128, 20]); Dvec = T(es, "Dvec", [128, SSDW])
        alog_bc = T(es, "alog_bc", [128, 48]); negA = T(es, "negA", [128, 48]); ynw = T(es, "ynw", [128, 12])
        cU = T(es, "cU", [128, 128]); cLo = T(es, "cLo", [128, 128]); ones = T(es, "ones2", [128, 128])
        cMask = T(es, "cMask", [128, 2, 128]); cSel = T(es, "cSel", [6, 6, 128])
        S.dma('sp', convw[:], convw_d[:, :, :], writes=['convw'], sem='c3')
        S.dma('sp', convb[:], convb_d[:, :], writes=['convb'], sem='c3')
        S.dma('sp', Dvec[:], dvec_d.partition_broadcast(128), writes=['Dvec'], sem='c3')
        S.dma('sp', alog_bc[:], alog_d.partition_broadcast(128), writes=['alog'], sem='c3')
        S.dma('sp', ynw[:], ynw_d[:, :], writes=['ynw'], sem='c3')
        S.dma('sp', cU[:], cU_d[:, :], writes=['cU'], sem='c3')
        S.dma('sp', cLo[:], cLo_d[:, :], writes=['cLo'], sem='c3')
        S.dma('sp', cMask[:], cMask_d[:, :, :], writes=['cMask'], sem='c3')
        S.dma('sp', cSel[:], cSel_d[:, :, :], writes=['cSel'], sem='c3')
        S.regroup('c3', ['convw', 'convb', 'Dvec', 'alog', 'ynw', 'cU', 'cLo', 'cMask', 'cSel'])
        S.op('dve', lambda: V.memset(ones[:], 1.0), writes=['ones2'])
        cMaskb = T(es, "cMaskb", [128, 2, 128], BF16); cSelb = T(es, "cSelb", [6, 6, 128], BF16)
        cp('dve', cMaskb[:], cMask[:], ['cMask'], ['cMaskb']); cp('dve', cSelb[:], cSel[:], ['cSel'], ['cSelb'])
        act(negA[:], alog_bc[:], AF.Exp, ['alog'], ['negA'])
        ts('dve', negA[:], negA[:], -1.0, ALU.mult, ['negA'], ['negA'])
        rawg = T(es, "rawg", [128, 5, RAWW], BF16)
        BT = T(es, "BT", [128, NCH * 128], BF16); CT = T(es, "CT", [128, NCH * 128], BF16)
        x_tok = T(es, "x_tok", [128, NCH, 384], BF16); B_tok = T(es, "B_tok", [128, NCH, 128], BF16)
        Sb_all = T(es, "Sb_all", [128, NT, 384], BF16)
        dg = [T(es, "dg%d" % i, [128, 5, 128], BF16) for i in range(2)]
        xc = [T(es, "xc%d" % i, [128, 512], BF16) for i in range(2)]
        dtg = T(es, "dtg", [128, NCH, 12]); av = T(es, "av", [128, NCH, 12]); lndt = T(es, "lndt", [128, NCH, 12])
        cs_all = T(es, "cs_all", [128, NCH, 12]); tot_all = T(es, "tot_all", [128, NCH, 12]); nb = T(es, "nb", [128, NCH, 12])
        eoff = T(es, "eoff", [128, NCH, 12]); wst = T(es, "wst", [128, NCH, 12]); dch = T(es, "dch", [128, NCH, 12])
        Srun = [T(es, "Srun%d" % i, [128, 384]) for i in range(3)]
        Sfb = [T(es, "Sfb%d" % i, [128, 384], BF16) for i in range(2)]
        xd = [T(es, "xd%d" % i, [128, 384], BF16) for i in range(4)]
        CBt = [T(es, "CBt%d" % i, [128, 128], BF16) for i in range(2)]
        csTh = [T(es, "csTh%d" % i, [6, 2, 128], BF16) for i in range(2)]; csTl = [T(es, "csTl%d" % i, [6, 2, 128], BF16) for i in range(2)]
        Lm = [T(es, "Lm%d" % i, [128, 128], BF16) for i in range(8)]
        Mt = [T(es, "Mt%d" % i, [128, 128], BF16) for i in range(8)]
        t1 = [T(es, "t1_%d" % i, [128, 384]) for i in range(2)]; t2 = T(es, "t2", [128, 384]); t3 = T(es, "t3", [128, 384])
        zg = [T(es, "zg%d" % i, [128, 384], BF16) for i in range(2)]; sz = T(es, "sz", [128, 384]); sqj = T(es, "sqj", [128, 384])
        yzb = [T(es, "yzb%d" % i, [128, 384], BF16) for i in range(2)]; yzTs = [T(es, "yzTs%d" % i, [128, 3, 128], BF16) for i in range(2)]
        pf = [PS(es, "pf%d" % i, [128, 512]) for i in range(7)]; pb = PS(es, "pb", [128, 1024], BF16)
        raw_rows = rawT_d.rearrange("(t p) w -> t p w", p=128)
        yzT_v = yzT_d.rearrange("(i p) t -> p i t", p=128)
        rdg = Ring('dg', 2); rcv = Ring('cv', 2); rxc = Ring('xc', 2)
        for g in range(4):
            tiles = [3 * g, 3 * g + 1, 3 * g + 2, 12 + g, 16 + g]
            if g == 0:
                for ti, Tt in enumerate(tiles):
                    S.dma('sp', rawg[:, ti, :], raw_rows[Tt, :, :], writes=['rawg%d' % ti], sem='rawg')
                S.regroup('rawg', ['rawg%d' % ti for ti in range(5)])
            units = [(ti, Tt, blk) for ti, Tt in enumerate(tiles) for blk in range(9)]
            ustate = {}

            def BP_(u):
                ti, Tt, blk = units[u]
                if blk == 0:
                    di = rdg.next()
                    for j in range(5):
                        ts('dve', dg[di][:, j, :], identb[:], convw[:, Tt, j:j + 1], ALU.mult, ['identb', 'convw'], ['dg%d' % di])
                    ustate['di'] = di
                di = ustate['di']
                n = 512 if blk < 8 else 256
                off = (2 + 512 * blk) if blk < 8 else 4102
                tok0 = 512 * blk
                b = rcv.next()
                for j in range(5):
                    mm(pf[b][:, 0:n], dg[di][:, j, :], rawg[:, ti, off - 2 + j: off - 2 + j + n], j == 0, j == 4,
                       ['dg%d' % di, 'rawg%d' % ti], ['pf%d' % b], inc=(j == 4))
                if ti < 3:
                    xi = rxc.next()
                    dst = xc[xi][:, 0:n]; dkey = 'xc%d' % xi
                elif ti == 3:
                    dst = BT[:, tok0:tok0 + n]; dkey = 'BT%d' % blk
                else:
                    dst = CT[:, tok0:tok0 + n]; dkey = 'CT%d' % blk
                act(dst, pf[b][:, 0:n], AF.Silu, ['pf%d' % b, 'convb'], [dkey], bias=convb[:, Tt:Tt + 1])
                ustate[u] = (dst, dkey, n)

            def BQ_(u):
                ti, Tt, blk = units[u]
                dst, dkey, n = ustate.pop(u)
                if ti <= 3:
                    nj = n // 128
                    for jj in range(nj):
                        tr(pb[:, jj * 128:(jj + 1) * 128], dst[:, jj * 128:(jj + 1) * 128], identb[:], [dkey, 'identb'], ['pb'], inc=(jj == nj - 1))
                    src3 = pb[:, 0:n].rearrange("p (j c) -> p j c", c=128)
                    if ti < 3:
                        cp('dve', x_tok[:, 4 * blk:4 * blk + nj, ti * 128:(ti + 1) * 128], src3, ['pb'], ['x_tok'])
                    else:
                        cp('dve', B_tok[:, 4 * blk:4 * blk + nj, :], src3, ['pb'], ['B_tok'])

            BP_(0)
            for u in range(len(units)):
                if u + 1 < len(units):
                    BP_(u + 1)
                BQ_(u)
            S.barrier()
            stop('B1')
            if g + 1 < 4:
                for ti, Tt in enumerate([3 * (g + 1), 3 * (g + 1) + 1, 3 * (g + 1) + 2, 12 + g + 1, 16 + g + 1]):
                    S.dma('sp', rawg[:, ti, :], raw_rows[Tt, :, :], writes=['rawg%d' % ti], sem='rawg')
                S.regroup('rawg', ['rawg%d' % ti for ti in range(5)])
            for d in range(2):
                cp('dve', dtg[:, :, d * 6:(d + 1) * 6], dtr_all[:, :, d * 24 + g * 6: d * 24 + g * 6 + 6], ['dtr_all'], ['dtg'])
            act(dtg[:], dtg[:], AF.Exp, ['dtg'], ['dtg'])
            act(dtg[:], dtg[:], AF.Ln, ['dtg'], ['dtg'], bias=1.0, scale=1.0)
            act(lndt[:], dtg[:], AF.Ln, ['dtg'], ['lndt'])
            for d in range(2):
                tt('dve', av[:, :, d * 6:(d + 1) * 6], dtg[:, :, d * 6:(d + 1) * 6],
                   negA[:, d * 24 + g * 6: d * 24 + g * 6 + 6].unsqueeze(1).to_broadcast([128, NCH, 6]), ALU.mult, ['dtg', 'negA'], ['av'])
            for c in range(NCH):
                last = (c == NCH - 1)
                mm(pf[4][:, c * 12:c * 12 + 6], cU[:], av[:, c, 0:6], True, True, ['cU', 'av'], ['pf4'], inc=False)
                mm(pf[4][:, c * 12 + 6:c * 12 + 12], cLo[:], av[:, c, 6:12], True, True, ['cLo', 'av'], ['pf4'], inc=False)
                mm(pf[5][:, c * 12:c * 12 + 12], ones[:], av[:, c, :], True, True, ['ones2', 'av'], ['pf5'], inc=last)
            cp('dve', cs_all[:], pf[4][:, 0:NCH * 12].rearrange("p (c h) -> p c h", h=12), ['pf4'], ['cs_all'])
            cp('dve', tot_all[:], pf[5][:, 0:NCH * 12].rearrange("p (c h) -> p c h", h=12), ['pf5'], ['tot_all'])
            tt('dve', nb[:], lndt[:], cs_all[:], ALU.subtract, ['lndt', 'cs_all'], ['nb'])
            act(eoff[:], cs_all[:], AF.Exp, ['cs_all'], ['eoff'])
            tt('dve', wst[:], tot_all[:], nb[:], ALU.add, ['tot_all', 'nb'], ['wst'])
            act(wst[:], wst[:], AF.Exp, ['wst'], ['wst'])
            act(dch[:], tot_all[:], AF.Exp, ['tot_all'], ['dch'])

            stop('B2')
            rxd = Ring('xd', 4)

            def chunk_state(c, d, bank=6):
                i = rxd.next()
                tt('dve', xd[i][:].rearrange("p (h q) -> p h q", q=64), x_tok[:, c, :].rearrange("p (h q) -> p h q", q=64),
                   wst[:, c, d * 6:(d + 1) * 6].unsqueeze(2).to_broadcast([128, 6, 64]), ALU.mult, ['x_tok', 'wst'], ['xd%d' % i])
                mm(pf[bank][:, 0:384], B_tok[:, c, :], xd[i][:], True, True, ['B_tok', 'xd%d' % i], ['pf%d' % bank])

            def dec_bc(c, d):
                return dch[:, c, d * 6:(d + 1) * 6].unsqueeze(2).to_broadcast([128, 6, 64])

            def v3(ap):
                return ap.rearrange("p (h q) -> p h q", q=64)

            for d, (ca, cb_) in enumerate(((32, 33), (33, 32))):
                chunk_state(ca, d)
                cp('dve', Srun[d][:], pf[6][:, 0:384], ['pf6'], ['Srun%d' % d])
                tt('dve', v3(Srun[d][:]), v3(Srun[d][:]), dec_bc(cb_, d), ALU.mult, ['Srun%d' % d, 'dch'], ['Srun%d' % d])
                chunk_state(cb_, d)
                tt('dve', Srun[d][:], Srun[d][:], pf[6][:, 0:384], ALU.add, ['Srun%d' % d, 'pf6'], ['Srun%d' % d])
            stop('B3')
            order = list(range(NT - 1, -1, -1))
            banks = [3, 4, 5, 6]
            AHEAD = 3
            for n in range(min(AHEAD, NT)):
                chunk_state(order[n], 1, banks[n % 4])
            cur = 1
            for n, c in enumerate(order):
                if n + AHEAD < NT:
                    chunk_state(order[n + AHEAD], 1, banks[(n + AHEAD) % 4])
                nxt = 2 if cur == 1 else 1
                cp('act', Sb_all[:, c, :], Srun[cur][:], ['Srun%d' % cur], ['Sb_all%d' % c])
                tt('dve', v3(Srun[nxt][:]), v3(Srun[cur][:]), dec_bc(c, 1), ALU.mult, ['Srun%d' % cur, 'dch'], ['Srun%d' % nxt])
                bk = banks[n % 4]
                tt('dve', Srun[nxt][:], Srun[nxt][:], pf[bk][:, 0:384], ALU.add, ['Srun%d' % nxt, 'pf%d' % bk], ['Srun%d' % nxt])
                cur = nxt
            S.barrier()
            stop('B4')
            rL = Ring('L', 2); rq = Ring('q', 8)
            pairs = [(d, h) for d in range(2) for h in range(6)]

            def H_(c, ci):
                tk = slice(c * 128, (c + 1) * 128)
                cp('act', Sfb[ci][:], Srun[0][:], ['Srun0'], ['Sfb%d' % ci])
                mm(pf[0][:, 0:128], BT[:, tk], CT[:, tk], True, True, ['BT%d' % (c // 4), 'CT%d' % (c // 4)], ['pf0'], inc=False)
                mm(pf[0][0:6, 128:256], av[:, c, 0:6], cU[:], True, True, ['av', 'cU'], ['pf0'], inc=False)
                mm(pf[0][0:6, 256:384], av[:, c, 6:12], cLo[:], True, True, ['av', 'cLo'], ['pf0'])
                cp('dve', CBt[ci][:], pf[0][:, 0:128], ['pf0'], ['CBt%d' % ci])
                src = pf[0][0:6, 128:384].rearrange("p (d l) -> p d l", l=128)
                cp('dve', csTh[ci][:], src, ['pf0'], ['csTh%d' % ci])
                tt('dve', csTl[ci][:], src, csTh[ci][:], ALU.subtract, ['pf0', 'csTh%d' % ci], ['csTl%d' % ci])

            def L_(c, ci, bt):
                lb = 1 + rL.next()
                bk = 'pf%d' % lb
                for q in range(4):
                    d, h = pairs[bt * 4 + q]
                    reg = pf[lb][:, q * 128:(q + 1) * 128]
                    mm(reg, cSelb[0:6, h, :], csTh[ci][0:6, d, :], True, False, ['cSelb', 'csTh%d' % ci], [bk], inc=False)
                    mm(reg, cSelb[0:6, h, :], csTl[ci][0:6, d, :], False, False, ['cSelb', 'csTl%d' % ci], [bk], inc=False)
                    mm(reg, identb[:], cMaskb[:, d, :], False, True, ['identb', 'cMaskb'], [bk], inc=(q == 3))
                return lb

            def E_(c, ci, bt, lb):
                bk = 'pf%d' % lb
                qis = []
                for q in range(4):
                    d, h = pairs[bt * 4 + q]
                    reg = pf[lb][:, q * 128:(q + 1) * 128]
                    qi = rq.next()
                    act(Lm[qi][:], reg, AF.Exp, [bk, 'nb'], ['Lm%d' % qi], bias=nb[:, c, d * 6 + h: d * 6 + h + 1])
                    tt('dve', Mt[qi][:], Lm[qi][:], CBt[ci][:], ALU.mult, ['Lm%d' % qi, 'CBt%d' % ci], ['Mt%d' % qi])
                    qis.append(qi)
                return qis

            def Y_(c, bt, qis):
                for q in range(4):
                    d, h = pairs[bt * 4 + q]
                    qi = qis[q]
                    mm(pf[3][:, h * 64:(h + 1) * 64], Mt[qi][:], x_tok[:, c, h * 64:(h + 1) * 64], (bt == 0 and q == 0), (bt == 2 and q == 3),
                       ['Mt%d' % qi, 'x_tok'], ['pf3'], inc=(q == 3), sgc=True)

            def O_(c, ci):
                tk = slice(c * 128, (c + 1) * 128)
                mm(pf[4][:, 0:384], CT[:, tk], Sfb[ci][:], True, True, ['CT%d' % (c // 4), 'Sfb%d' % ci], ['pf4'])
                mm(pf[5][:, 0:384], CT[:, tk], Sb_all[:, c, :], True, True, ['CT%d' % (c // 4), 'Sb_all%d' % c], ['pf5'])

            def U_(c):
                chunk_state(c, 0)
                tt('dve', v3(Srun[0][:]), v3(Srun[0][:]), dec_bc(c, 0), ALU.mult, ['Srun0', 'dch'], ['Srun0'])
                tt('dve', Srun[0][:], Srun[0][:], pf[6][:, 0:384], ALU.add, ['Srun0', 'pf6'], ['Srun0'])

            def F1_(c, ci):
                k1 = 't1_%d' % ci
                S.dma('sp', zg[ci][:], zt_d[c * 128:(c + 1) * 128, g * 384:(g + 1) * 384], writes=['zg%d' % ci], sem='zg%d' % ci)
                tt('dve', v3(t1[ci][:]), v3(pf[4][:, 0:384]), eoff[:, c, 0:6].unsqueeze(2).to_broadcast([128, 6, 64]), ALU.mult, ['pf4', 'eoff'], [k1])
                tt('dve', v3(t2[:]), v3(pf[5][:, 0:384]), eoff[:, c, 6:12].unsqueeze(2).to_broadcast([128, 6, 64]), ALU.mult, ['pf5', 'eoff'], ['t2'])
                tt('dve', t1[ci][:], pf[3][:, 0:384], t1[ci][:], ALU.add, ['pf3', k1], [k1])
                tt('pool', t3[:], x_tok[:, c, :], Dvec[:, g * 384:(g + 1) * 384], ALU.mult, ['x_tok', 'Dvec'], ['t3'])
                tt('pool', t2[:], t2[:], t3[:], ALU.add, ['t2', 't3'], ['t2'])
                tt('pool', t1[ci][:], t1[ci][:], t2[:], ALU.add, [k1, 't2'], [k1])

            def F2_(c, ci):
                k1 = 't1_%d' % ci
                tt('dve', t1[ci][:], t1[ci][:], zg[ci][:], ALU.mult, [k1, 'zg%d' % ci], [k1])
                tt('pool', sqj[:], t1[ci][:], t1[ci][:], ALU.mult, [k1], ['sqj'])

            def F3_(c, ci):
                k1 = 't1_%d' % ci
                S.op('dve', lambda: V.reduce_sum(out=ssq[:, g, c:c + 1], in_=sqj[:], axis=AX.X), ['sqj'], ['ssq'])
                cp('act', yzb[ci][:], t1[ci][:], [k1], ['yzb%d' % ci])

            def T_(c, ci):
                tk = slice(c * 128, (c + 1) * 128)
                for i3 in range(3):
                    tr(pb[:, 512 + i3 * 128: 512 + (i3 + 1) * 128], yzb[ci][:, i3 * 128:(i3 + 1) * 128], identb[:], ['yzb%d' % ci, 'identb'], ['pbz'], inc=(i3 == 2))
                tt('dve', yzTs[ci][:], pb[:, 512:896].rearrange("p (i t) -> p i t", t=128),
                   ynw[:, g * 3:(g + 1) * 3].unsqueeze(2).to_broadcast([128, 3, 128]), ALU.mult, ['pbz', 'ynw'], ['yzTs%d' % ci])
                S.dma('sp', yzT_v[:, g * 3:(g + 1) * 3, tk], yzTs[ci][:], reads=['yzTs%d' % ci])

            for c in range(NT):
                ci = c % 2
                H_(c, ci)
                lb0 = L_(c, ci, 0); q0 = E_(c, ci, 0, lb0)
                if c > 0:
                    F2_(c - 1, (c - 1) % 2)
                lb1 = L_(c, ci, 1); q1 = E_(c, ci, 1, lb1)
                if c > 0:
                    F3_(c - 1, (c - 1) % 2)
                Y_(c, 0, q0)
                lb2 = L_(c, ci, 2); q2 = E_(c, ci, 2, lb2)
                Y_(c, 1, q1)
                if c > 0:
                    T_(c - 1, (c - 1) % 2)
                Y_(c, 2, q2)
                O_(c, ci)
                U_(c)
                F1_(c, ci)
            F2_(NT - 1, (NT - 1) % 2)
            F3_(NT - 1, (NT - 1) % 2)
            T_(NT - 1, (NT - 1) % 2)
            S.barrier()
    esAB.close()
    if upto == 'B':
        es0.close(); return nc

    mask_all = T(P0, "mask_all", [128, NT, NE]); gates_all = T(P0, "gates_all", [128, NT, 4])
    idx_all = T(P0, "idx_all", [128, NT * 4], I32); widx = T(P0, "widx", [128, NBLK], I32)
    esC = ExitStack()
    s2_bc = T(esC, "s2_bc", [128, D]); sh2_bc = T(esC, "sh2_bc", [128, D])
    S.dma('sp', s2_bc[:], modrow_d[0], writes=['s2_bc'], sem='L_s2bc')
    S.dma('sp', sh2_bc[:], modrow_d[1], writes=['s2_bc'], sem='L_s2bc')
    with ExitStack() as es:
        wout = T(es, "wout", [128, 16, D], BF16)
        for k in range(16):
            S.dma('pool', wout[:, k, :], wout_d[k * 128:(k + 1) * 128, :], writes=['wout'], sem='wout')
        rw = T(es, "rw", [128, 8, NE]); rb_bc = T(es, "rb_bc", [128, NE])
        S.dma('sp', rw[:], rw_d[:, :, :], writes=['rw'], sem='c4')
        S.dma('sp', rb_bc[:], rb_d.partition_broadcast(128), writes=['rb_bc'], sem='c4')
        S.regroup('c4', ['rw', 'rb_bc'])
        rs_ssd = T(es, "rs_ssd", [128, NT]); tq = T(es, "tq", [128, NT])
        tt('dve', tq[:], ssq[:, 0, :], ssq[:, 1, :], ALU.add, ['ssq'], ['tq'])
        tt('dve', tq[:], tq[:], ssq[:, 2, :], ALU.add, ['tq', 'ssq'], ['tq'])
        tt('dve', tq[:], tq[:], ssq[:, 3, :], ALU.add, ['tq', 'ssq'], ['tq'])
        act(tq[:], tq[:], AF.Ln, ['tq'], ['tq'], bias=EPS, scale=1.0 / SSDW)
        act(rs_ssd[:], tq[:], AF.Exp, ['tq'], ['rs_ssd'], scale=-0.5)
        yzt = [T(es, "yzt%d" % i, [128, 12, 128], BF16) for i in range(2)]; ypt = [T(es, "ypt%d" % i, [128, 4, 128], BF16) for i in range(2)]
        xt_ = [T(es, "cxt%d" % i, [128, D]) for i in range(2)]; m_ = T(es, "cm", [128, D]); x1s = [T(es, "x1s%d" % i, [128, D]) for i in range(2)]
        sq_ = T(es, "csq", [128, D]); ss_ = [T(es, "css%d" % i, [128, 4]) for i in range(2)]; xn_ = [T(es, "cxn%d" % i, [128, D]) for i in range(2)]
        tmpm = T(es, "ctmpm", [128, 8, 128]); h2f = T(es, "h2f", [128, 8, 128]); htk = T(es, "htk", [128, D]); htb = [T(es, "htb%d" % i, [128, D], BF16) for i in range(2)]
        lg = T(es, "lg", [128, NE]); m8 = T(es, "m8", [128, 8]); ex = T(es, "ex", [128, NE]); sm = T(es, "sm", [128, 4])
        ps_s = [PS(es, "ps_s%d" % i, [128, 512]) for i in range(2)]; ps_p = [PS(es, "ps_p%d" % i, [128, 512]) for i in range(2)]
        pT = PS(es, "pT2", [128, 1024]); pr = PS(es, "pr", [128, 512])
        yzT_v = yzT_d.rearrange("(i p) t -> p i t", p=128); ypT_v = ypT_d.rearrange("(g o) t -> o g t", o=128)
        def CX_(j):
            i = j % 2
            tk = slice(j * 128, (j + 1) * 128)
            S.dma('sp', yzt[i][:], yzT_v[:, :, tk], writes=['yzt%d' % i], sem='yzt%d' % i)
            S.dma('sp', ypt[i][:], ypT_v[:, :, tk], writes=['ypt%d' % i], sem='ypt%d' % i)
            S.dma('sp', xt_[i][:], x_d[tk, :], writes=['cxt%d' % i], sem='cxt%d' % i)
            for half in range(2):
                hc = slice(half * 512, (half + 1) * 512)
                for k in range(12):
                    mm(ps_s[half][:, :], yzt[i][:, k, :], wout[:, k, hc], k == 0, k == 11, ['yzt%d' % i, 'wout'], ['ps_s%d' % half], inc=(k == 11))
                for k in range(4):
                    mm(ps_p[half][:, :], ypt[i][:, k, :], wout[:, 12 + k, hc], k == 0, k == 3, ['ypt%d' % i, 'wout'], ['ps_p%d' % half], inc=(k == 3))
                ts('dve', m_[:, hc], ps_s[half][:, :], rs_ssd[:, j:j + 1], ALU.mult, ['ps_s%d' % half, 'rs_ssd'], ['cm%d' % half])
                tt('dve', m_[:, hc], m_[:, hc], ps_p[half][:, :], ALU.add, ['cm%d' % half, 'ps_p%d' % half], ['cm%d' % half])
                tt('pool', m_[:, hc], m_[:, hc], g_bc[:, 0, hc], ALU.mult, ['cm%d' % half, 'g_bc'], ['cm%d' % half])
                tt('pool', x1s[i][:, hc], m_[:, hc], xt_[i][:, hc], ALU.add, ['cm%d' % half, 'cxt%d' % i], ['x1s%d' % i])
            S.dma('sp', x1_d[tk, :], x1s[i][:], reads=['x1s%d' % i])
            xt = x1s[i][:]
            act(sq_[:], xt, AF.Square, ['x1s%d' % i], ['csq'])
            S.op('dve', lambda: V.reduce_sum(out=ss_[i][:, 0:1], in_=sq_[:], axis=AX.X), ['csq'], ['css%d' % i])
            act(ss_[i][:, 1:2], ss_[i][:, 0:1], AF.Ln, ['css%d' % i], ['css%d' % i], bias=EPS, scale=1.0 / D)
            act(ss_[i][:, 2:3], ss_[i][:, 1:2], AF.Exp, ['css%d' % i], ['css%d' % i], scale=-0.5)
            ts('dve', xn_[i][:], xt, ss_[i][:, 2:3], ALU.mult, ['x1s%d' % i, 'css%d' % i], ['cxn%d' % i])
        def CY_(j):
            i = j % 2
            tk = slice(j * 128, (j + 1) * 128)
            for k in range(8):
                tr(pT[:, k * 128:(k + 1) * 128], xn_[i][:, k * 128:(k + 1) * 128], identf[:], ['cxn%d' % i, 'identf'], ['pT2'], inc=(k == 7))
            tt('dve', tmpm[:], pT[:, :].rearrange("p (k t) -> p k t", t=128), s2[:, :].unsqueeze(2).to_broadcast([128, 8, 128]),
               ALU.mult, ['pT2', 'modv'], ['ctmpm'])
            tt('pool', h2f[:], tmpm[:], sh2[:, :].unsqueeze(2).to_broadcast([128, 8, 128]), ALU.add, ['ctmpm', 'modv'], ['h2f'])
            tt('pool', htk[:], xn_[i][:], s2_bc[:], ALU.mult, ['cxn%d' % i, 's2_bc'], ['htk'])
            tt('pool', htb[i][:], htk[:], sh2_bc[:], ALU.add, ['htk', 's2_bc'], ['htb%d' % i])
            S.dma('sp', h2tok_d[tk, :], htb[i][:], reads=['htb%d' % i])
            for k in range(8):
                mm(pr[:, 0:NE], h2f[:, k, :], rw[:, k, :], k == 0, k == 7, ['h2f', 'rw'], ['pr'], inc=(k == 7))
            tt('dve', lg[:], pr[:, 0:NE], rb_bc[:], ALU.add, ['pr', 'rb_bc'], ['lg'])
            S.op('dve', lambda: V.max(out=m8[:], in_=lg[:]), ['lg'], ['m8'])
            ts('dve', mask_all[:, j, :], lg[:], m8[:, 3:4], ALU.is_ge, ['lg', 'm8'], ['mask_all'])
            ts('dve', sm[:, 0:1], m8[:, 0:1], -1.0, ALU.mult, ['m8'], ['sm'])
            act(ex[:], lg[:], AF.Exp, ['lg', 'sm'], ['ex'], bias=sm[:, 0:1])
            tt('dve', ex[:], ex[:], mask_all[:, j, :], ALU.mult, ['ex', 'mask_all'], ['ex'])
            S.op('dve', lambda: V.reduce_sum(out=sm[:, 1:2], in_=ex[:], axis=AX.X), ['ex'], ['sm'])
            S.op('dve', lambda: V.reciprocal(out=sm[:, 2:3], in_=sm[:, 1:2]), ['sm'], ['sm'])
            ts('dve', G_all[:, j, :], ex[:], sm[:, 2:3], ALU.mult, ['ex', 'sm'], ['G_all'])
        CX_(0)
        for j in range(NT):
            if j + 1 < NT:
                CX_(j + 1)
            CY_(j)
        S.barrier()
    esC.close()
    stop('C')

    with ExitStack() as es:
        SU = T(es, "SU", [128, 128], BF16); SUf = T(es, "SUf", [128, 128]); onesb = T(es, "onesb", [128, 128], BF16)
        mask_bf = T(es, "mask_bf", [128, NT, NE], BF16); P_all = T(es, "P_all", [128, NT + 1, NE], BF16)
        rank_all = T(es, "rank_all", [128, NT, NE]); counts = T(es, "counts", [128, NE]); padded = T(es, "padded", [128, NE])
        tmpc = T(es, "tmpc", [128, NE]); pad_end = T(es, "pad_end", [128, NE]); pad_start = T(es, "pad_start", [128, NE])
        onesf = T(es, "onesf", [128, NE]); Dm = T(es, "Dm", [128, NT, NE]); Em = T(es, "Em", [128, NT, NE])
        iota1 = T(es, "iota1", [128, NE]); jv = T(es, "jv", [128, NBLK]); pidx = T(es, "pidx", [128, 1])
        d8 = T(es, "d8", [128, 8]); e8 = T(es, "e8", [128, 8]); d4 = T(es, "d4", [128, NT, 4]); oh = T(es, "oh", [128, NE])
        cmp3 = T(es, "cmp3", [128, NBLK, NE]); bexp = T(es, "bexp", [128, NBLK])
        rows = [T(es, "rows%d" % i, [128, D], BF16) for i in range(2)]
        pk = [PS(es, "pk%d" % i, [128, 512]) for i in range(2)]; pc_ = PS(es, "pc_", [128, 512])
        S.dma('sp', SUf[:], cSU_d[:, :], writes=['SUf'], sem='c6')
        S.dma('sp', iota1[:], cIota_d.partition_broadcast(128), writes=['iota1'], sem='c6')
        S.dma('sp', jv[:], cJv_d.partition_broadcast(128), writes=['jv'], sem='c6')
        S.dma('sp', pidx[:], cPidx_d[:, :], writes=['pidx'], sem='c6')
        S.regroup('c6', ['SUf', 'iota1', 'jv', 'pidx'])
        cp('dve', SU[:], SUf[:], ['SUf'], ['SU'])
        S.op('dve', lambda: V.memset(onesb[:], 1.0), writes=['onesb'])
        S.op('dve', lambda: V.memset(onesf[:], 1.0), writes=['onesf'])
        cp('dve', mask_bf[:], mask_all[:], ['mask_all'], ['mask_bf'])
        S.op('dve', lambda: V.memset(P_all[:, 0, :], 0.0), writes=['P_all'])
        for j in range(NT):
            tt('dve', P_all[:, j + 1, :], P_all[:, j, :], mask_all[:, j, :], ALU.add, ['P_all', 'mask_all'], ['P_all'])
        for j in range(NT):
            b = j // 16
            reg = pk[b][:, (j % 16) * NE:(j % 16 + 1) * NE]
            mm(reg, SU[:], mask_bf[:, j, :], True, False, ['SU', 'mask_bf'], ['pk%d' % b], inc=False)
            mm(reg, onesb[:], P_all[:, j, :], False, True, ['onesb', 'P_all'], ['pk%d' % b], inc=(j % 16 == 15))
        for b in range(2):
            cp('dve', rank_all[:, b * 16:(b + 1) * 16, :], pk[b][:, :].rearrange("p (j e) -> p j e", e=NE), ['pk%d' % b], ['rank_all'])
        mm(pc_[:, 0:NE], onesb[:], P_all[:, NT, :], True, True, ['onesb', 'P_all'], ['pc_'])
        cp('dve', counts[:], pc_[:, 0:NE], ['pc_'], ['counts'])
        S.op('dve', lambda: V.memset(padded[:], 0.0), writes=['padded'])
        for m in range(4096 // BS):
            ts('dve', tmpc[:], counts[:], float(BS * m), ALU.is_gt, ['counts'], ['tmpc'], s2=float(BS), op1=ALU.mult)
            tt('dve', padded[:], padded[:], tmpc[:], ALU.add, ['padded', 'tmpc'], ['padded'])
        S.op('dve', lambda: V.tensor_tensor_scan(out=pad_end[:], data0=onesf[:], data1=padded[:], initial=0.0, op0=ALU.mult, op1=ALU.add),
             ['onesf', 'padded'], ['pad_end'])
        tt('dve', pad_start[:], pad_end[:], padded[:], ALU.subtract, ['pad_end', 'padded'], ['pad_start'])
        tt('dve', Dm[:], rank_all[:], pad_start[:, :].unsqueeze(1).to_broadcast([128, NT, NE]), ALU.add, ['rank_all', 'pad_start'], ['Dm'])
        ts('dve', Dm[:], Dm[:], 1.0, ALU.add, ['Dm'], ['Dm'])
        tt('dve', Dm[:], Dm[:], mask_all[:], ALU.mult, ['Dm', 'mask_all'], ['Dm'])
        tt('dve', Em[:], mask_all[:], iota1[:, :].unsqueeze(1).to_broadcast([128, NT, NE]), ALU.mult, ['mask_all', 'iota1'], ['Em'])
        for j in range(NT):
            S.op('dve', lambda: V.max(out=d8[:], in_=Dm[:, j, :]), ['Dm'], ['d8'])
            ts('dve', d4[:, j, :], d8[:, 0:4], -1.0, ALU.add, ['d8'], ['d4'])
        cp('dve', idx_all[:].rearrange("p (j k) -> p j k", k=4), d4[:], ['d4'], ['idx_all'])
        tt('dve', cmp3[:], pad_end[:, :].unsqueeze(1).to_broadcast([128, NBLK, NE]), jv[:, :].unsqueeze(2).to_broadcast([128, NBLK, NE]),
           ALU.is_le, ['pad_end', 'jv'], ['cmp3'])
        S.op('dve', lambda: V.reduce_sum(out=bexp[:], in_=cmp3[:], axis=AX.X), ['cmp3'], ['bexp'])
        ts('dve', bexp[:], bexp[:], float(NE - 1), ALU.min, ['bexp'], ['bexp'], s2=128.0, op1=ALU.mult)
        skp = T(es, "skp", [128, NBLK])
        S.op('dve', lambda: V.memset(skp[:], 0.0), writes=['skp'])
        tt('dve', skp[:, 2:NBLK], bexp[:, 2:NBLK], bexp[:, 0:NBLK - 2], ALU.is_equal, ['bexp', 'skp'], ['skp'])
        ts('dve', skp[:], skp[:], 1.0e6, ALU.mult, ['skp'], ['skp'])
        ts('dve', bexp[:], bexp[:], pidx[:, 0:1], ALU.add, ['bexp', 'pidx'], ['bexp'])
        tt('dve', bexp[:], bexp[:], skp[:], ALU.add, ['bexp', 'skp'], ['bexp'])
        cp('dve', widx[:], bexp[:], ['bexp'], ['widx'])
        S.barrier()
        stop('C2')
        for j in range(NT):
            i = j % 2
            S.dma('sp', rows[i][:], h2tok_d[j * 128:(j + 1) * 128, :], writes=['rows%d' % i], sem='rows%d' % i)
            for k in range(4):
                S.idma(out=Xg_d[:, :], in_=rows[i][:, :], idx=idx_all[:, j * 4 + k:j * 4 + k + 1], scatter=True, bound=NSLOT - 1,
                       reads=['rows%d' % i, 'idx_all'], sem='sc%d' % i)
        for j in range(NT):
            S.op('dve', lambda: V.max(out=e8[:], in_=Em[:, j, :]), ['Em'], ['e8'])
            for k in range(4):
                ts('dve', oh[:], iota1[:], e8[:, k:k + 1], ALU.is_equal, ['iota1', 'e8'], ['oh'])
                tt('dve', oh[:], oh[:], G_all[:, j, :], ALU.mult, ['oh', 'G_all'], ['oh'])
                S.op('dve', lambda: V.reduce_sum(out=gates_all[:, j, k:k + 1], in_=oh[:], axis=AX.X), ['oh'], ['gates_all'])
        S.barrier()
    stop('C3')

    with ExitStack() as es:
        wg_ = [T(es, "wg%d" % i, [128, 8, D], BF16) for i in range(2)]; wu_ = [T(es, "wu%d" % i, [128, 8, D], BF16) for i in range(2)]
        w2_ = [T(es, "w2_%d" % i, [128, 8, D], BF16) for i in range(2)]; b1t = [T(es, "b1t%d" % i, [128, 16]) for i in range(2)]
        NJ = BS // 128
        b1p = [T(es, "b1p%d" % i, [128, 8]) for i in range(2)]
        xgs = [T(es, "xgs%d" % i, [128, NJ, D], BF16) for i in range(3)]; xgT = [T(es, "xgT%d" % i, [128, 8, BS], BF16) for i in range(2)]
        actT = [T(es, "actT%d" % i, [128, 8, BS], BF16) for i in range(2)]
        gt3 = [T(es, "gt3_%d" % i, [128, BS]) for i in range(3)]; sg3 = [T(es, "sg3_%d" % i, [128, BS], BF16) for i in range(3)]
        ut3 = [T(es, "ut3_%d" % i, [128, BS]) for i in range(3)]
        ysb = [T(es, "ysb%d" % i, [128, NJ, D]) for i in range(2)]
        pg_ = [PS(es, "mpg%d" % i, [128, 512]) for i in range(2)]; pu_ = [PS(es, "mpu%d" % i, [128, 512]) for i in range(2)]
        py_ = [PS(es, "mpy%d" % i, [128, 512]) for i in range(2)]; pb_ = [PS(es, "mpb%d" % i, [128, 1024], BF16) for i in range(2)]
        Xg_v = Xg_d.rearrange("(b j p) d -> b p j d", p=128, j=NJ); Y_v = Y_d.rearrange("(b j p) o -> b p j o", p=128, j=NJ)

        def wload(blk, i):
            ix = widx[:, blk:blk + 1]
            for (dst, src, nm) in ((wg_[i], W1G_d, 'wg%d' % i), (wu_[i], W1U_d, 'wu%d' % i), (w2_[i], W2_d, 'w2_%d' % i)):
                S.idma(out=dst[:].rearrange("p k c -> p (k c)"), in_=src[:, :], idx=ix, scatter=False, bound=NE * 128 - 1,
                       reads=['widx'], writes=[nm], sem=nm)
            S.idma(out=b1t[i][:, :], in_=B1_d[:, :], idx=ix, scatter=False, bound=NE * 128 - 1, reads=['widx'], writes=['b1t%d' % i], sem='b1t%d' % i)

        rgu = Ring('gu', 2); ry = Ring('y', 2); rpb = Ring('pb', 2); r3 = Ring('r3', 3)
        KB = 1024 // BS

        def XL_(blk):
            xi = blk % 3
            S.dma('sp', xgs[xi][:], Xg_v[blk], writes=['xgs%d' % xi], sem='xgs%d' % xi)

        def TR_(blk):
            wi = blk % 2
            xi = blk % 3
            for k0 in range(0, 8, KB):
                pi = rpb.next()
                for kk in range(KB):
                    k = k0 + kk
                    for jj in range(NJ):
                        tr(pb_[pi][:, kk * BS + jj * 128: kk * BS + (jj + 1) * 128], xgs[xi][:, jj, k * 128:(k + 1) * 128], identb[:],
                           ['xgs%d' % xi, 'identb'], ['mpb%d' % pi], inc=(kk == KB - 1 and jj == NJ - 1))
                cp('act' if (k0 // KB) % 2 == 0 else 'dve', xgT[wi][:, k0:k0 + KB, :], pb_[pi][:, :].rearrange("p (k s) -> p k s", s=BS),
                   ['mpb%d' % pi], ['xgT%d' % wi])

        def FL_(blk):
            wi = blk % 2
            ts('pool', b1p[wi][:], b1t[wi][:, 8:16], 1.0, ALU.add, ['b1t%d' % wi], ['b1p%d' % wi])
            prev = None

            def fin(pv):
                ri, fp = pv
                S.op('dve', lambda: V.scalar_tensor_tensor(out=actT[wi][:, fp, :], in0=ut3[ri][:], scalar=-6.0, in1=gt3[ri][:], op0=ALU.max, op1=ALU.mult),
                     ['ut3_%d' % ri, 'gt3_%d' % ri], ['actT%d' % wi])
            for f in range(8):
                b = rgu.next(); ri = r3.next()
                fs = slice(f * 128, (f + 1) * 128)
                for k in range(8):
                    mm(pg_[b][:, 0:BS], wg_[wi][:, k, fs], xgT[wi][:, k, :], k == 0, k == 7, ['wg%d' % wi, 'xgT%d' % wi], ['mpg%d' % b], inc=(k == 7))
                for k in range(8):
                    mm(pu_[b][:, 0:BS], wu_[wi][:, k, fs], xgT[wi][:, k, :], k == 0, k == 7, ['wu%d' % wi, 'xgT%d' % wi], ['mpu%d' % b], inc=(k == 7))
                ts('dve', gt3[ri][:], pg_[b][:, 0:BS], b1t[wi][:, f:f + 1], ALU.add, ['mpg%d' % b, 'b1t%d' % wi], ['gt3_%d' % ri], s2=7.0, op1=ALU.min)
                act(sg3[ri][:], gt3[ri][:], AF.Sigmoid, ['gt3_%d' % ri], ['sg3_%d' % ri], scale=1.702)
                ts('dve', ut3[ri][:], pu_[b][:, 0:BS], b1p[wi][:, f:f + 1], ALU.add, ['mpu%d' % b, 'b1p%d' % wi], ['ut3_%d' % ri], s2=8.0, op1=ALU.min)
                tt('pool', gt3[ri][:], gt3[ri][:], sg3[ri][:], ALU.mult, ['gt3_%d' % ri, 'sg3_%d' % ri], ['gt3_%d' % ri])
                if prev is not None:
                    fin(prev)
                prev = (ri, f)
            fin(prev)

        def W2_(blk):
            wi = blk % 2
            for jj in range(NJ):
                for half in range(2):
                    b = ry.next()
                    for k in range(8):
                        mm(py_[b][:, :], actT[wi][:, k, jj * 128:(jj + 1) * 128], w2_[wi][:, k, half * 512:(half + 1) * 512], k == 0, k == 7,
                           ['actT%d' % wi, 'w2_%d' % wi], ['mpy%d' % b], inc=(k == 7))
                    cp('act' if half == 0 else 'dve', ysb[wi][:, jj, half * 512:(half + 1) * 512], py_[b][:, :], ['mpy%d' % b], ['ysb%d' % wi])
            S.dma('sp', Y_v[blk], ysb[wi][:], reads=['ysb%d' % wi])

        XL_(0)
        XL_(1)
        wload(0, 0)
        TR_(0)
        for blk in range(NBLK):
            if blk + 2 < NBLK:
                XL_(blk + 2)
            if blk + 1 < NBLK:
                wload(blk + 1, (blk + 1) % 2)
            FL_(blk)
            if blk + 1 < NBLK:
                TR_(blk + 1)
            W2_(blk)
        S.barrier()
    stop('D')

    with ExitStack() as es:
        b2s = T(es, "b2s", [NE, D]); GT = T(es, "GT", [NE, 128])
        S.dma('sp', b2s[:], b2_d[:, :], writes=['b2s'], sem='b2s')
        yk = [T(es, "yk%d" % i, [128, D]) for i in range(8)]; acc = [T(es, "acc%d" % i, [128, D]) for i in range(2)]
        x1t = [T(es, "x1t%d" % i, [128, D]) for i in range(3)]; sq_ = T(es, "dsq", [128, D]); ss_ = [T(es, "dss%d" % i, [128, 4]) for i in range(2)]
        pgt = PS(es, "pgt", [128, 512]); pa = [PS(es, "pa%d" % i, [128, 512]) for i in range(2)]
        def EG_(j):
            i = j % 2; xi = j % 3
            tk = slice(j * 128, (j + 1) * 128)
            S.dma('sp', x1t[xi][:], x1_d[tk, :], writes=['x1t%d' % xi], sem='x1t%d' % xi)
            for k in range(4):
                kk = i * 4 + k
                S.idma(out=yk[kk][:, :], in_=Y_d[:, :], idx=idx_all[:, j * 4 + k:j * 4 + k + 1], scatter=False, bound=NSLOT - 1,
                       reads=['idx_all'], writes=['yk%d' % kk], sem='yk%d' % kk)

        def EA_(j):
            i = j % 2; xi = j % 3
            tr(pgt[0:NE, 0:128], G_all[:, j, :], identf[:], ['G_all', 'identf'], ['pgt'])
            cp('act', GT[:], pgt[0:NE, 0:128], ['pgt'], ['GT'])
            for half in range(2):
                hc = slice(half * 512, (half + 1) * 512)
                mm(pa[half][:, :], GT[:, :], b2s[:, hc], True, True, ['GT', 'b2s'], ['pa%d' % half])
                S.op('dve', lambda: V.scalar_tensor_tensor(out=acc[i][:, hc], in0=yk[i * 4][:, hc], scalar=gates_all[:, j, 0:1], in1=pa[half][:, :],
                                                            op0=ALU.mult, op1=ALU.add), ['yk%d' % (i * 4), 'gates_all', 'pa%d' % half], ['acc%d_%d' % (i, half)])
                for k in range(1, 4):
                    S.op('dve', lambda: V.scalar_tensor_tensor(out=acc[i][:, hc], in0=yk[i * 4 + k][:, hc], scalar=gates_all[:, j, k:k + 1], in1=acc[i][:, hc],
                                                                op0=ALU.mult, op1=ALU.add), ['yk%d' % (i * 4 + k), 'gates_all', 'acc%d_%d' % (i, half)], ['acc%d_%d' % (i, half)])
            ak = ['acc%d_0' % i, 'acc%d_1' % i]
            tt('pool', acc[i][:], acc[i][:], g_bc[:, 1, :], ALU.mult, ak + ['g_bc'], ak)
            tt('pool', x1t[xi][:], x1t[xi][:], acc[i][:], ALU.add, ['x1t%d' % xi] + ak, ['x1t%d' % xi])

        def EB_(j):
            i = j % 2; xi = j % 3
            tk = slice(j * 128, (j + 1) * 128)
            act(sq_[:], x1t[xi][:], AF.Square, ['x1t%d' % xi], ['dsq'])
            S.op('dve', lambda: V.reduce_sum(out=ss_[i][:, 0:1], in_=sq_[:], axis=AX.X), ['dsq'], ['dss%d' % i])
            act(ss_[i][:, 1:2], ss_[i][:, 0:1], AF.Ln, ['dss%d' % i], ['dss%d' % i], bias=EPS, scale=1.0 / D)
            act(ss_[i][:, 2:3], ss_[i][:, 1:2], AF.Exp, ['dss%d' % i], ['dss%d' % i], scale=-0.5)
            S.op('dve', lambda: V.scalar_tensor_tensor(out=x1t[xi][:], in0=x1t[xi][:], scalar=ss_[i][:, 2:3], in1=fnw_bc[:], op0=ALU.mult, op1=ALU.mult),
                 ['x1t%d' % xi, 'dss%d' % i, 'fnw_bc'], ['x1t%d' % xi])
            S.dma('sp', out_d[tk, :], x1t[xi][:], reads=['x1t%d' % xi])

        EG_(0)
        if NT > 1:
            EG_(1)
        EA_(0)
        for j in range(NT):
            if j + 2 < NT:
                EG_(j + 2)
            if j + 1 < NT:
                EA_(j + 1)
            EB_(j)
        S.barrier()
    es0.close()
    return nc


def _consts():
    I = np.eye(128, dtype=np.float32)
    k = np.arange(128)
    U = (k[:, None] <= k[None, :]).astype(np.float32)
    Lo = (k[:, None] >= k[None, :]).astype(np.float32)
    mask = np.zeros((128, 2, 128), np.float32)
    mask[:, 0, :] = np.where(k[None, :] >= k[:, None], 0.0, NEG)
    mask[:, 1, :] = np.where(k[None, :] <= k[:, None], 0.0, NEG)
    sel = np.zeros((6, 6, 128), np.float32)
    for h in range(6):
        sel[h, h, :] = 1.0
    AT = np.zeros((128, 4, 128), np.float32)
    t = np.arange(64)
    for g, w in enumerate((2, 4, 8, 16)):
        lo = np.clip(t - w // 2, 0, 64); hi = np.clip(t + w - w // 2, 0, 64)
        Am = np.zeros((64, 64), np.float32)
        for ti in range(64):
            Am[ti, lo[ti]:hi[ti]] = 1.0 / float(hi[ti] - lo[ti])
        Am -= np.eye(64, dtype=np.float32)
        for r in range(2):
            AT[r * 64:(r + 1) * 64, g, r * 64:(r + 1) * 64] = Am.T
    SU = (k[:, None] < k[None, :]).astype(np.float32)
    iota1 = (np.arange(NE, dtype=np.float32) + 1.0).reshape(1, NE)
    jv = (np.arange(NBLK, dtype=np.float32) * float(BS)).reshape(1, NBLK)
    pidx = np.arange(128, dtype=np.float32).reshape(128, 1)
    return dict(cI=I, cU=U, cLo=Lo, cMask=mask, cSel=sel, cAT=AT, cSU=SU, cIota=iota1, cJv=jv, cPidx=pidx)


_NC_CACHE = {}


def kernel(x, c, ctx, c_ctx, w_mod, b_mod, norm1_w, norm2_w, w_in, conv_w, conv_b, dt_bias, a_log, d_skip,
           ssd_norm_w, pool_w, pool_scale, w_out, router_w, router_b, w1, b1, w2, b2, final_norm_w, _dbg=False, _upto='ALL', _cores=8):
    f = lambda a: np.ascontiguousarray(np.asarray(a, dtype=np.float32))
    x = f(x); c = f(c); ctx = f(ctx); c_ctx = f(c_ctx)
    shared = dict(_consts())
    shared["w_mod"] = f(w_mod[0])
    shared["bmodT"] = f(b_mod[0].reshape(48, 128).T)
    shared["bmodr"] = f(b_mod[0].reshape(1, -1))
    shared["n1T"] = f(norm1_w[0].reshape(8, 128).T); shared["n2T"] = f(norm2_w[0].reshape(8, 128).T)
    shared["fnw"] = f(final_norm_w.reshape(1, -1))
    shared["w_in"] = f(w_in[0])
    shared["convw"] = f(np.asarray(conv_w[0]).T.reshape(20, 128, 5).transpose(1, 0, 2))
    shared["convb"] = f(np.asarray(conv_b[0]).reshape(20, 128).T)
    shared["dtb"] = f(np.asarray(dt_bias[0]).reshape(1, 48)); shared["alog"] = f(np.asarray(a_log[0]).reshape(1, 48))
    shared["dvec"] = f(np.repeat(np.asarray(d_skip[0]), 64).reshape(1, -1))
    shared["ynw"] = f(np.asarray(ssd_norm_w[0]).reshape(12, 128).T)
    shared["poolw"] = f(np.asarray(pool_w[0]).transpose(1, 0, 2))
    shared["pscale"] = f(np.asarray(pool_scale[0]).reshape(4, 128).T)
    shared["w_out"] = f(w_out[0])
    shared["rw"] = f(np.asarray(router_w[0]).reshape(8, 128, NE).transpose(1, 0, 2))
    shared["rb"] = f(np.asarray(router_b[0]).reshape(1, NE))
    w1a = np.asarray(w1[0])

    def ptile(w):
        return f(w.reshape(NE, 8, 128, -1).transpose(0, 2, 1, 3).reshape(NE * 128, -1))
    shared["W1G"] = ptile(w1a[:, :, 0::2]); shared["W1U"] = ptile(w1a[:, :, 1::2])
    shared["W2"] = ptile(np.asarray(w2[0]))
    b1a = np.asarray(b1[0])
    b1g_ = b1a[:, 0::2].reshape(NE, 8, 128).transpose(0, 2, 1)
    b1u_ = b1a[:, 1::2].reshape(NE, 8, 128).transpose(0, 2, 1)
    shared["B1"] = f(np.concatenate([b1g_, b1u_], axis=2).reshape(NE * 128, 16))
    shared["b2"] = f(b2[0])
    shared["n2r"] = f(norm2_w[0].reshape(1, -1))
    if (_dbg, _upto) not in _NC_CACHE:
        _NC_CACHE[(_dbg, _upto)] = build(dbg=_dbg, upto=_upto)
    nc = _NC_CACHE[(_dbg, _upto)]
    in_maps = []
    for b in range(_cores):
        m = dict(shared)
        m["x"] = x[b]; m["ctx"] = ctx[b]
        cv = np.stack([c[b], c_ctx], axis=-1)
        m["cvec"] = f(cv.reshape(8, 128, 2).transpose(1, 0, 2))
        in_maps.append(m)
    res = run_bass_kernel_spmd(nc, in_maps, core_ids=list(range(_cores)))
    if _dbg:
        return res
    return np.stack([r["out"] for r in res.results], axis=0).astype(np.float32)
```

```python
import numpy as np
from contextlib import ExitStack
import concourse.bass as bass
import concourse.mybir as mybir
from concourse.bass_utils import run_bass_kernel_spmd

F32 = mybir.dt.float32; BF16 = mybir.dt.bfloat16; I32 = mybir.dt.int32
AF = mybir.ActivationFunctionType; ALU = mybir.AluOpType; AX = mybir.AxisListType

D = 1024; L = 4096; CTXL = 256; NT = 32; NCH = 34
INC = 4656; CONVCH = 2560; SSDW = 1536
RAWW = 4360
NEG = -30000.0
NE = 32
BS = 256
NBLK = 16384 // BS + NE
NSLOT = NBLK * BS
EPS = 1e-6


class Sched:
    STRICT = True

    def __init__(self, nc, es):
        self.nc = nc; self.es = es
        self.eng = {'pe': nc.tensor, 'act': nc.scalar, 'dve': nc.vector, 'pool': nc.gpsimd, 'sp': nc.sync}
        self.sem = {}; self.cnt = {}
        for e in self.eng:
            self.sem[e] = es.enter_context(nc.semaphore("s_" + e)); self.cnt[e] = 0
        self.seen = {e: {} for e in self.eng}
        self.w = {}; self.r = {}
        self.dsem = {}; self.dcnt = {}; self.free_sems = []; self.free_sw = []; self.dsw = {}; self.nsem = 0; self.bregs = {}

    def _dsem(self, name, sw=False):
        if name not in self.dsem:
            pool = self.free_sw if sw else self.free_sems
            if pool:
                h, c = pool.pop()
                self.dsem[name] = h; self.dcnt[name] = c
            else:
                self.nsem += 1
                self.dsem[name] = self.es.enter_context(self.nc.semaphore("d_%d" % self.nsem)); self.dcnt[name] = 0
            self.dsw[name] = sw
        assert self.dsw[name] == sw, name
        return self.dsem[name]

    def _wait(self, e, tok):
        if tok is None:
            return
        kind, name, val = tok
        if kind == 'e' and name == e and (e == 'pe' or not self.STRICT):
            return
        skey = (kind, name)
        if self.seen[e].get(skey, 0) >= val:
            return
        self.seen[e][skey] = val
        s = self.sem[name] if kind == 'e' else self.dsem[name]
        self.eng[e].wait_ge(s, val)

    def deps(self, e, reads, writes):
        for k in reads:
            self._wait(e, self.w.get(k))
        for k in writes:
            self._wait(e, self.w.get(k))
            for tok in list(self.r.get(k, {}).values()):
                self._wait(e, tok)

    def record(self, tok, reads, writes):
        for k in reads:
            self.r.setdefault(k, {})[(tok[0], tok[1])] = tok
        for k in writes:
            self.w[k] = tok; self.r[k] = {}

    def op(self, e, fn, reads=(), writes=(), inc=True):
        self.deps(e, reads, writes)
        ins = fn()
        tok = ('e', e, self.cnt[e] + 1)
        self.record(tok, reads, writes)
        if inc:
            ins.then_inc(self.sem[e], 1); self.cnt[e] += 1
        return ins

    def dma(self, q, out, in_, reads=(), writes=(), sem=None):
        if sem is None:
            sem = ('L_' + writes[0]) if writes else ('S_' + reads[0])
        if q == 'pool':
            sem = 'sw_' + sem
        s = self._dsem(sem, sw=(q == 'pool'))
        self.deps(q, reads, writes)
        ins = self.eng[q].dma_start(out=out, in_=in_)
        ins.then_inc(s, 16); self.dcnt[sem] += 16
        tok = ('d', sem, self.dcnt[sem])
        self.record(tok, reads, writes)
        return ins

    def idma(self, out, in_, idx, scatter, bound, reads=(), writes=(), sem=None):
        sem = 'sw_' + sem
        s = self._dsem(sem, sw=True)
        self.deps('pool', reads, writes)
        if bound not in self.bregs:
            r = self.nc.gpsimd.alloc_register("bc%d" % bound)
            self.nc.gpsimd.reg_mov(r, bound)
            self.bregs[bound] = r
        bound = self.bregs[bound]
        off = bass.IndirectOffsetOnAxis(ap=idx, axis=0)
        if scatter:
            ins = self.nc.gpsimd.indirect_dma_start(out=out, out_offset=off, in_=in_, in_offset=None, bounds_check=bound, oob_is_err=False)
        else:
            ins = self.nc.gpsimd.indirect_dma_start(out=out, out_offset=None, in_=in_, in_offset=off, bounds_check=bound, oob_is_err=False)
        ins.then_inc(s, 16); self.dcnt[sem] += 16
        tok = ('d', sem, self.dcnt[sem])
        self.record(tok, reads, writes)
        return ins

    def regroup(self, sem, keys):
        for k in keys:
            self.w[k] = ('d', sem, self.dcnt[sem])

    def barrier(self):
        for e in self.eng:
            for f in self.eng:
                if f != e and self.cnt[f] > 0:
                    self._wait(e, ('e', f, self.cnt[f]))
            for name in self.dsem:
                if self.dcnt[name] > 0:
                    self._wait(e, ('d', name, self.dcnt[name]))
        for name in list(self.dsem):
            (self.free_sw if self.dsw[name] else self.free_sems).append((self.dsem[name], self.dcnt[name]))
            for e in self.eng:
                self.seen[e].pop(('d', name), None)
        self.dsem = {}; self.dcnt = {}; self.dsw = {}
        for k in list(self.w):
            if self.w[k][0] == 'd':
                del self.w[k]
        for k in list(self.r):
            for kk in [kk for kk in self.r[k] if kk[0] == 'd']:
                del self.r[k][kk]


class Ring:
    def __init__(self, name, n):
        self.name = name; self.n = n; self.i = -1

    def next(self):
        self.i += 1
        return self.i % self.n


class _Stop(Exception):
    pass


def build(dbg=False, upto='ALL'):
    try:
        return _build(dbg, upto)
    except _Stop as e:
        return e.args[0]


def _build(dbg=False, upto='ALL'):
    nc = bass.Bass("TRN2", target_bir_lowering=False)
    es0 = ExitStack()
    S = Sched(nc, es0)
    V = nc.vector; A = nc.scalar; G = nc.gpsimd; PE = nc.tensor

    def din(name, shape, dt=F32):
        return nc.dram_tensor(name, list(shape), dt, kind="ExternalInput").ap()

    def dscr(name, shape, dt):
        return nc.dram_tensor(name, list(shape), dt, kind=("ExternalOutput" if dbg else "Internal")).ap()

    x_d = din("x", [L, D]); ctx_d = din("ctx", [CTXL, D]); cvec_d = din("cvec", [128, 8, 2])
    wmod_d = din("w_mod", [D, 6 * D]); bmodT_d = din("bmodT", [128, 48]); bmodr_d = din("bmodr", [1, 6 * D])
    n1T_d = din("n1T", [128, 8]); n2T_d = din("n2T", [128, 8]); fnw_d = din("fnw", [1, D])
    win_d = din("w_in", [D, INC]); convw_d = din("convw", [128, 20, 5]); convb_d = din("convb", [128, 20])
    dtb_d = din("dtb", [1, 48]); alog_d = din("alog", [1, 48]); dvec_d = din("dvec", [1, SSDW])
    ynw_d = din("ynw", [128, 12]); poolw_d = din("poolw", [128, 4, 128]); pscale_d = din("pscale", [128, 4])
    wout_d = din("w_out", [2 * D, D]); rw_d = din("rw", [128, 8, NE]); rb_d = din("rb", [1, NE])
    W1G_d = din("W1G", [NE * 128, 8 * D]); W1U_d = din("W1U", [NE * 128, 8 * D]); W2_d = din("W2", [NE * 128, 8 * D])
    B1_d = din("B1", [NE * 128, 16]); b2_d = din("b2", [NE, D]); n2r_d = din("n2r", [1, D])
    cSU_d = din("cSU", [128, 128]); cIota_d = din("cIota", [1, NE]); cJv_d = din("cJv", [1, NBLK]); cPidx_d = din("cPidx", [128, 1])
    cI_d = din("cI", [128, 128]); cU_d = din("cU", [128, 128]); cLo_d = din("cLo", [128, 128])
    cMask_d = din("cMask", [128, 2, 128]); cSel_d = din("cSel", [6, 6, 128]); cAT_d = din("cAT", [128, 4, 128])
    out_d = nc.dram_tensor("out", [L, D], F32, kind="ExternalOutput").ap()
    rawT_d = dscr("rawT", [CONVCH, RAWW], BF16); zt_d = dscr("zt", [L, SSDW], BF16)
    ypT_d = dscr("ypT", [512, L], BF16); yzT_d = dscr("yzT", [SSDW, L], BF16)
    x1_d = dscr("x1", [L, D], F32); h2tok_d = dscr("h2tok", [L, D], BF16)
    Xg_d = nc.dram_tensor("Xg", [NSLOT, D], BF16, kind="Internal").ap(); Y_d = nc.dram_tensor("Y", [NSLOT, D], F32, kind="Internal").ap()

    def stop(tag):
        if upto == tag:
            S.barrier()
            raise _Stop(nc)

    def T(es, name, shape, dt=F32):
        return es.enter_context(nc.sbuf_tensor("t_" + name, list(shape), dt))

    def PS(es, name, shape, dt=F32):
        return es.enter_context(nc.psum_tensor("p_" + name, list(shape), dt))

    def mm(out, lhsT, rhs, start, stop, reads, writes, inc=True, sgc=False):
        return S.op('pe', lambda: PE.matmul(out, lhsT=lhsT, rhs=rhs, start=start, stop=stop, skip_group_check=sgc), reads, writes, inc)

    def tr(out, in_, ident, reads, writes, inc=True):
        return S.op('pe', lambda: PE.transpose(out=out, in_=in_, identity=ident), reads, writes, inc)

    def act(out, in_, func, reads, writes, bias=None, scale=None):
        kw = {}
        if bias is not None:
            kw['bias'] = bias
        if scale is not None:
            kw['scale'] = scale
        return S.op('act', lambda: A.activation(out=out, in_=in_, func=func, **kw), reads, writes)

    def tt(e, out, in0, in1, op, reads, writes):
        eng = V if e == 'dve' else G
        return S.op(e, lambda: eng.tensor_tensor(out=out, in0=in0, in1=in1, op=op), reads, writes)

    def ts(e, out, in0, s1, op0, reads, writes, s2=None, op1=None):
        eng = V if e == 'dve' else G
        if op1 is None:
            return S.op(e, lambda: eng.tensor_scalar(out=out, in0=in0, scalar1=s1, scalar2=None, op0=op0), reads, writes)
        return S.op(e, lambda: eng.tensor_scalar(out=out, in0=in0, scalar1=s1, scalar2=s2, op0=op0, op1=op1), reads, writes)

    def cp(e, out, in_, reads, writes):
        if e == 'act':
            return S.op('act', lambda: A.activation(out=out, in_=in_, func=AF.Copy), reads, writes)
        eng = V if e == 'dve' else G
        return S.op(e, lambda: eng.tensor_copy(out=out, in_=in_), reads, writes)

    P0 = es0
    identf = T(P0, "identf", [128, 128]); identb = T(P0, "identb", [128, 128], BF16)
    g_bc = T(P0, "g_bc", [128, 2, D]); fnw_bc = T(P0, "fnw_bc", [128, D])
    s1 = T(P0, "s1", [128, 8]); sh1 = T(P0, "sh1", [128, 8]); cs1 = T(P0, "cs1", [128, 8]); csh1 = T(P0, "csh1", [128, 8])
    s2 = T(P0, "s2", [128, 8]); sh2 = T(P0, "sh2", [128, 8])
    G_all = T(P0, "G_all", [128, NT, NE]); ssq = T(P0, "ssq", [128, 4, NT])
    modrow_d = nc.dram_tensor("modrow", [2, 128, D], F32, kind="Internal").ap()
    S.dma('sp', identf[:], cI_d[:, :], writes=['identf'], sem='c0')
    S.dma('sp', fnw_bc[:], fnw_d.partition_broadcast(128), writes=['fnw_bc'], sem='c0')
    S.regroup('c0', ['identf', 'fnw_bc'])
    cp('dve', identb[:], identf[:], ['identf'], ['identb'])

    esAB = ExitStack()
    dtr_all = T(esAB, "dtr_all", [128, NCH, 48])
    esW = ExitStack()
    win = T(esW, "win", [128, 8, INC], BF16)
    for k in range(8):
        S.dma('pool', win[:, k, :], win_d[k * 128:(k + 1) * 128, :], writes=['win'], sem='win')
    with ExitStack() as es:
        silu_c = T(es, "silu_c", [128, 8, 2]); cbc = T(es, "cbc", [128, 8, 128]); ones = T(es, "ones1", [128, 128])
        wm2 = [T(es, "wm%d" % i, [128, 8, D]) for i in range(2)]; modT = T(es, "modT", [128, 48, 2]); bmodT = T(es, "bmodT_s", [128, 48])
        bm_bc = T(es, "bm_bc", [128, D]); n1T = T(es, "n1T_s", [128, 8]); n2T = T(es, "n2T_s", [128, 8])
        tmp8 = T(es, "tmp8", [128, 8]); n2r_bc = T(es, "n2r_bc", [128, D])
        s2_bc = T(es, "s2_bc1", [128, D]); sh2_bc = T(es, "sh2_bc1", [128, D])
        S.dma('sp', n2r_bc[:], n2r_d.partition_broadcast(128), writes=['n2r_bc'])
        pm = PS(es, "pm", [128, 512]); pg = [PS(es, "pg0", [128, 512]), PS(es, "pg1", [128, 512])]
        S.dma('sp', silu_c[:], cvec_d[:, :, :], writes=['silu_c'], sem='c1')
        S.dma('sp', bmodT[:], bmodT_d[:, :], writes=['bmodT'], sem='c1')
        S.dma('sp', n1T[:], n1T_d[:, :], writes=['n1T'], sem='c1')
        S.dma('sp', n2T[:], n2T_d[:, :], writes=['n2T'], sem='c1')
        S.regroup('c1', ['silu_c', 'bmodT', 'n1T', 'n2T'])
        act(silu_c[:], silu_c[:], AF.Silu, ['silu_c'], ['silu_c'])
        S.op('dve', lambda: V.memset(ones[:], 1.0), writes=['ones1'])
        for k in range(8):
            ts('dve', cbc[:, k, :], ones[:], silu_c[:, k, 0:1], ALU.mult, ['ones1', 'silu_c'], ['cbc'])
        wm_v = wmod_d.rearrange("(k p) c -> p k c", p=128)
        S.dma('sp', wm2[0][:], wm_v[:, :, 0:D], writes=['wm0'], sem='wm0')
        for s in range(6):
            wm = wm2[s % 2]; wk = 'wm%d' % (s % 2)
            if s + 1 < 6:
                S.dma('sp', wm2[(s + 1) % 2][:], wm_v[:, :, (s + 1) * D:(s + 2) * D], writes=['wm%d' % ((s + 1) % 2)], sem='wm%d' % ((s + 1) % 2))
            if s in (2, 3, 4, 5):
                S.dma('sp', bm_bc[:], bmodr_d[:, s * D:(s + 1) * D].partition_broadcast(128), writes=['bm_bc'])
                for half in range(2):
                    hc = slice(half * 512, (half + 1) * 512)
                    for k in range(8):
                        mm(pg[half][:, :], cbc[:, k, :], wm[:, k, hc], k == 0, k == 7,
                           ['cbc', wk], ['pg%d' % half], inc=(k == 7))
                    dst = {2: g_bc[:, 0, hc], 5: g_bc[:, 1, hc], 3: sh2_bc[:, hc], 4: s2_bc[:, hc]}[s]
                    dk = 'g_bc' if s in (2, 5) else 's2_bc'
                    tt('dve', dst, pg[half][:, :], bm_bc[:, hc], ALU.add, ['pg%d' % half, 'bm_bc'], [dk])
                    if s == 4:
                        ts('dve', dst, dst, 1.0, ALU.add, [dk], [dk])
                        tt('dve', dst, dst, n2r_bc[:, hc], ALU.mult, [dk, 'n2r_bc'], [dk])
            if s not in (2, 5):
                for j in range(8):
                    for k in range(8):
                        mm(pm[:, j * 2:j * 2 + 2], wm[:, k, j * 128:(j + 1) * 128], silu_c[:, k, :], k == 0, k == 7,
                           [wk, 'silu_c'], ['pm'], inc=(k == 7))
                tt('dve', modT[:, s * 8:(s + 1) * 8, :], pm[:, 0:16].rearrange("p (j t) -> p j t", t=2),
                   bmodT[:, s * 8:(s + 1) * 8].unsqueeze(2).to_broadcast([128, 8, 2]), ALU.add, ['pm', 'bmodT'], ['modT'])
        for (dst, nT, sc_lo, col) in ((s1, n1T, 8, 0), (cs1, n1T, 8, 1), (s2, n2T, 32, 0)):
            ts('dve', tmp8[:], modT[:, sc_lo:sc_lo + 8, col], 1.0, ALU.add, ['modT'], ['tmp8'])
            tt('dve', dst[:], tmp8[:], nT[:], ALU.mult, ['tmp8', 'n1T', 'n2T'], ['modv'])
        cp('dve', sh1[:], modT[:, 0:8, 0], ['modT'], ['modv'])
        cp('dve', csh1[:], modT[:, 0:8, 1], ['modT'], ['modv'])
        cp('dve', sh2[:], modT[:, 24:32, 0], ['modT'], ['modv'])
        S.dma('sp', modrow_d[0], s2_bc[:], reads=['s2_bc'])
        S.dma('sp', modrow_d[1], sh2_bc[:], reads=['s2_bc'], sem='S_s2_bc')
        S.barrier()

    if upto == '1':
        esW.close(); esAB.close(); es0.close(); return nc

    def rms_to_hT(es_tiles, src_ap, s_vec, sh_vec, dst3, ps_T, names):
        xt, sq, ss, xn, tmp = es_tiles
        (kx, ksq, kss, kxn, ktmp, kps, kdst) = names
        S.dma('sp', xt, src_ap, writes=[kx], sem=kx)
        act(sq, xt, AF.Square, [kx], [ksq])
        S.op('dve', lambda: V.reduce_sum(out=ss[:, 0:1], in_=sq, axis=AX.X), [ksq], [kss])
        act(ss[:, 1:2], ss[:, 0:1], AF.Ln, [kss], [kss], bias=EPS, scale=1.0 / D)
        act(ss[:, 2:3], ss[:, 1:2], AF.Exp, [kss], [kss], scale=-0.5)
        ts('dve', xn, xt, ss[:, 2:3], ALU.mult, [kx, kss], [kxn])
        for k in range(8):
            tr(ps_T[:, k * 128:(k + 1) * 128], xn[:, k * 128:(k + 1) * 128], identf[:], [kxn, 'identf'], [kps], inc=(k == 7))
        tt('dve', tmp, ps_T[:, :].rearrange("p (k t) -> p k t", t=128), s_vec[:, :].unsqueeze(2).to_broadcast([128, 8, 128]),
           ALU.mult, [kps, 'modv'], [ktmp])
        tt('pool', dst3, tmp, sh_vec[:, :].unsqueeze(2).to_broadcast([128, 8, 128]), ALU.add, [ktmp, 'modv'], [kdst])

    with ExitStack() as es:
        dtb_bc = T(es, "dtb_bc", [128, 48]); AT = T(es, "AT", [128, 4, 128], BF16); ATf = T(es, "ATf", [128, 4, 128])
        poolw = T(es, "poolw", [128, 4, 128], BF16); poolwf = T(es, "poolwf", [128, 4, 128]); pscale = T(es, "pscale", [128, 4])
        zpad = T(es, "zpad", [128, 20, 4], BF16)
        S.dma('sp', dtb_bc[:], dtb_d.partition_broadcast(128), writes=['dtb_bc'], sem='c2')
        S.dma('sp', ATf[:], cAT_d[:, :, :], writes=['ATf'], sem='c2')
        S.dma('sp', poolwf[:], poolw_d[:, :, :], writes=['poolwf'], sem='c2')
        S.dma('sp', pscale[:], pscale_d[:, :], writes=['pscale'], sem='c2')
        S.regroup('c2', ['dtb_bc', 'ATf', 'poolwf', 'pscale'])
        cp('dve', AT[:], ATf[:], ['ATf'], ['AT']); cp('dve', poolw[:], poolwf[:], ['poolwf'], ['poolw'])
        S.op('dve', lambda: V.memset(zpad[:], 0.0), writes=['zpad'])
        zrow = T(es, "zrow", [128, 4, D], BF16)
        S.op('pool', lambda: G.memset(zrow[:], 0.0), writes=['zrow'])
        Xg_v0 = Xg_d.rearrange("(b j p) d -> b p j d", p=128, j=4)
        for blk in range(NSLOT // 512):
            S.dma('act', Xg_v0[blk], zrow[:], reads=['zrow'], sem='xgz')
        raw_v = rawT_d.rearrange("(t p) w -> p t w", p=128)
        S.dma('sp', raw_v[:, :, 0:2], zpad[:, :, 0:2], reads=['zpad'])
        S.dma('sp', raw_v[:, :, 4098:4102], zpad[:, :, 0:4], reads=['zpad'])
        S.dma('sp', raw_v[:, :, 4358:4360], zpad[:, :, 0:2], reads=['zpad'])
        xt_ = [T(es, "xt%d" % i, [128, D]) for i in range(2)]; sq_ = T(es, "sq", [128, D]); ss_ = [T(es, "ss%d" % i, [128, 4]) for i in range(2)]
        xn_ = [T(es, "xn%d" % i, [128, D]) for i in range(2)]; tmpm = T(es, "tmpm", [128, 8, 128])
        hT = [T(es, "hT%d" % i, [128, 8, 512], BF16) for i in range(2)]
        rawblk = [T(es, "rawblk0", [128, 20, 512], BF16)] * 2
        zsb = [T(es, "zsb%d" % i, [128, SSDW], BF16) for i in range(2)]
        usb = [T(es, "usb%d" % i, [128, 512], BF16) for i in range(2)]
        plsb = [T(es, "plsb%d" % i, [128, 4, 128], BF16) for i in range(2)]
        ypsb = [T(es, "ypsb%d" % i, [128, 4, 128], BF16) for i in range(2)]
        pT = PS(es, "pT", [128, 1024]); pfm = [PS(es, "pfm%d" % i, [128, 512]) for i in range(2)]
        ptm = [PS(es, "ptm%d" % i, [128, 512]) for i in range(2)]; ppl = PS(es, "ppl", [128, 512]); pyp = PS(es, "pyp", [128, 512])
        rx = Ring('x', 2); rfm = Ring('fm', 2); rtm = Ring('tm', 2); rz = Ring('z', 2)
        ypT_v = ypT_d.rearrange("(g o) t -> o g t", o=128)
        def RMS_(blk):
            ntile = 4 if blk < 8 else 2
            hs = blk % 2
            for j in range(ntile):
                i = rx.next()
                src = x_d[blk * 512 + j * 128: blk * 512 + (j + 1) * 128, :] if blk < 8 else ctx_d[j * 128:(j + 1) * 128, :]
                rms_to_hT((xt_[i][:], sq_[:], ss_[i], xn_[i][:], tmpm[:]), src,
                          s1 if blk < 8 else cs1, sh1 if blk < 8 else csh1, hT[hs][:, :, j * 128:(j + 1) * 128], pT,
                          ('xt%d' % i, 'sq', 'ss%d' % i, 'xn%d' % i, 'tmpm', 'pT', 'hT%d' % hs))

        RMS_(0)
        for blk in range(9):
            ntile = 4 if blk < 8 else 2
            ntok = ntile * 128
            hs = blk % 2
            if blk + 1 < 9:
                RMS_(blk + 1)
            for t in range(20):
                b = rfm.next()
                for k in range(8):
                    mm(pfm[b][:, 0:ntok], win[:, k, t * 128:(t + 1) * 128], hT[hs][:, k, 0:ntok], k == 0, k == 7,
                       ['win', 'hT%d' % hs], ['pfm%d' % b], inc=(k == 7))
                cp('act' if t % 2 == 0 else 'dve', rawblk[hs][:, t, 0:ntok], pfm[b][:, 0:ntok], ['pfm%d' % b], ['rawblk0'])
            off = 2 + 512 * blk if blk < 8 else 4102
            S.dma('sp', raw_v[:, :, off:off + ntok], rawblk[hs][:, :, 0:ntok], reads=['rawblk0'])
            for j in range(ntile):
                chunk = blk * 4 + j
                lt = hT[hs][:, :, j * 128:(j + 1) * 128]
                b = rtm.next()
                for k in range(8):
                    mm(ptm[b][:, 0:48], lt[:, k, :], win[:, k, CONVCH:CONVCH + 48], k == 0, k == 7, ['win', 'hT%d' % hs], ['ptm%d' % b], inc=(k == 7))
                tt('dve', dtr_all[:, chunk, :], ptm[b][:, 0:48], dtb_bc[:], ALU.add, ['ptm%d' % b, 'dtb_bc'], ['dtr_all'])
                if blk == 8:
                    continue
                zi = rz.next()
                for q in range(3):
                    b = rtm.next()
                    c0 = 2608 + q * 512
                    for k in range(8):
                        mm(ptm[b][:, :], lt[:, k, :], win[:, k, c0:c0 + 512], k == 0, k == 7, ['win', 'hT%d' % hs], ['ptm%d' % b], inc=(k == 7))
                    act(zsb[zi][:, q * 512:(q + 1) * 512], ptm[b][:, :], AF.Silu, ['ptm%d' % b], ['zsb%d' % zi])
                S.dma('sp', zt_d[chunk * 128:(chunk + 1) * 128, :], zsb[zi][:], reads=['zsb%d' % zi])
                b = rtm.next()
                for k in range(8):
                    mm(ptm[b][:, :], lt[:, k, :], win[:, k, 4144:4656], k == 0, k == 7, ['win', 'hT%d' % hs], ['ptm%d' % b], inc=(k == 7))
                cp('act', usb[zi][:], ptm[b][:, :], ['ptm%d' % b], ['usb%d' % zi])
                for g in range(4):
                    mm(ppl[:, g * 128:(g + 1) * 128], usb[zi][:, g * 128:(g + 1) * 128], AT[:, g, :], True, True, ['usb%d' % zi, 'AT'], ['ppl'], inc=(g == 3))
                cp('dve', plsb[zi][:], ppl[:, :].rearrange("p (g t) -> p g t", t=128), ['ppl'], ['plsb%d' % zi])
                for g in range(4):
                    mm(pyp[:, g * 128:(g + 1) * 128], poolw[:, g, :], plsb[zi][:, g, :], True, True, ['poolw', 'plsb%d' % zi], ['pyp'], inc=(g == 3))
                tt('dve', ypsb[zi][:], pyp[:, :].rearrange("p (g t) -> p g t", t=128), pscale[:, :].unsqueeze(2).to_broadcast([128, 4, 128]),
                   ALU.mult, ['pyp', 'pscale'], ['ypsb%d' % zi])
                S.dma('sp', ypT_v[:, :, chunk * 128:(chunk + 1) * 128], ypsb[zi][:], reads=['ypsb%d' % zi])
        S.barrier()

    esW.close()
    if upto == 'A':
        esAB.close(); es0.close(); return nc
    with ExitStack() as es:
        convw = T(es, "convw", [128, 20, 5]); convb = T(es, "convb", [128, 20]); Dvec = T(es, "Dvec", [128, SSDW])
        alog_bc = T(es, "alog_bc", [128, 48]); negA = T(es, "negA", [128, 48]); ynw = T(es, "ynw", [128, 12])
        cU = T(es, "cU", [128, 128]); cLo = T(es, "cLo", [128, 128]); ones = T(es, "ones2", [128, 128])
        cMask = T(es, "cMask", [128, 2, 128]); cSel = T(es, "cSel", [6, 6, 128])
        S.dma('sp', convw[:], convw_d[:, :, :], writes=['convw'], sem='c3')
        S.dma('sp', convb[:], convb_d[:, :], writes=['convb'], sem='c3')
        S.dma('sp', Dvec[:], dvec_d.partition_broadcast(128), writes=['Dvec'], sem='c3')
        S.dma('sp', alog_bc[:], alog_d.partition_broadcast(128), writes=['alog'], sem='c3')
        S.dma('sp', ynw[:], ynw_d[:, :], writes=['ynw'], sem='c3')
        S.dma('sp', cU[:], cU_d[:, :], writes=['cU'], sem='c3')
        S.dma('sp', cLo[:], cLo_d[:, :], writes=['cLo'], sem='c3')
        S.dma('sp', cMask[:], cMask_d[:, :, :], writes=['cMask'], sem='c3')
        S.dma('sp', cSel[:], cSel_d[:, :, :], writes=['cSel'], sem='c3')
        S.regroup('c3', ['convw', 'convb', 'Dvec', 'alog', 'ynw', 'cU', 'cLo', 'cMask', 'cSel'])
        S.op('dve', lambda: V.memset(ones[:], 1.0), writes=['ones2'])
        cMaskb = T(es, "cMaskb", [128, 2, 128], BF16); cSelb = T(es, "cSelb", [6, 6, 128], BF16)
        cp('dve', cMaskb[:], cMask[:], ['cMask'], ['cMaskb']); cp('dve', cSelb[:], cSel[:], ['cSel'], ['cSelb'])
        act(negA[:], alog_bc[:], AF.Exp, ['alog'], ['negA'])
        ts('dve', negA[:], negA[:], -1.0, ALU.mult, ['negA'], ['negA'])
        rawg = T(es, "rawg", [128, 5, RAWW], BF16)
        BT = T(es, "BT", [128, NCH * 128], BF16); CT = T(es, "CT", [128, NCH * 128], BF16)
        x_tok = T(es, "x_tok", [128, NCH, 384], BF16); B_tok = T(es, "B_tok", [128, NCH, 128], BF16)
        Sb_all = T(es, "Sb_all", [128, NT, 384], BF16)
        dg = [T(es, "dg%d" % i, [128, 5, 128], BF16) for i in range(2)]
        xc = [T(es, "xc%d" % i, [128, 512], BF16) for i in range(2)]
        dtg = T(es, "dtg", [128, NCH, 12]); av = T(es, "av", [128, NCH, 12]); lndt = T(es, "lndt", [128, NCH, 12])
        cs_all = T(es, "cs_all", [128, NCH, 12]); tot_all = T(es, "tot_all", [128, NCH, 12]); nb = T(es, "nb", [128, NCH, 12])
        eoff = T(es, "eoff", [128, NCH, 12]); wst = T(es, "wst", [128, NCH, 12]); dch = T(es, "dch", [128, NCH, 12])
        Srun = [T(es, "Srun%d" % i, [128, 384]) for i in range(3)]
        Sfb = [T(es, "Sfb%d" % i, [128, 384], BF16) for i in range(2)]
        xd = [T(es, "xd%d" % i, [128, 384], BF16) for i in range(4)]
        CBt = [T(es, "CBt%d" % i, [128, 128], BF16) for i in range(2)]
        csTh = [T(es, "csTh%d" % i, [6, 2, 128], BF16) for i in range(2)]; csTl = [T(es, "csTl%d" % i, [6, 2, 128], BF16) for i in range(2)]
        Lm = [T(es, "Lm%d" % i, [128, 128], BF16) for i in range(8)]
        Mt = [T(es, "Mt%d" % i, [128, 128], BF16) for i in range(8)]
        t1 = [T(es, "t1_%d" % i, [128, 384]) for i in range(2)]; t2 = T(es, "t2", [128, 384]); t3 = T(es, "t3", [128, 384])
        zg = [T(es, "zg%d" % i, [128, 384], BF16) for i in range(2)]; sz = T(es, "sz", [128, 384]); sqj = T(es, "sqj", [128, 384])
        yzb = [T(es, "yzb%d" % i, [128, 384], BF16) for i in range(2)]; yzTs = [T(es, "yzTs%d" % i, [128, 3, 128], BF16) for i in range(2)]
        pf = [PS(es, "pf%d" % i, [128, 512]) for i in range(7)]; pb = PS(es, "pb", [128, 1024], BF16)
        raw_rows = rawT_d.rearrange("(t p) w -> t p w", p=128)
        yzT_v = yzT_d.rearrange("(i p) t -> p i t", p=128)
        rdg = Ring('dg', 2); rcv = Ring('cv', 2); rxc = Ring('xc', 2)
        for g in range(4):
            tiles = [3 * g, 3 * g + 1, 3 * g + 2, 12 + g, 16 + g]
            if g == 0:
                for ti, Tt in enumerate(tiles):
                    S.dma('sp', rawg[:, ti, :], raw_rows[Tt, :, :], writes=['rawg%d' % ti], sem='rawg')
                S.regroup('rawg', ['rawg%d' % ti for ti in range(5)])
            units = [(ti, Tt, blk) for ti, Tt in enumerate(tiles) for blk in range(9)]
            ustate = {}

            def BP_(u):
                ti, Tt, blk = units[u]
                if blk == 0:
                    di = rdg.next()
                    for j in range(5):
                        ts('dve', dg[di][:, j, :], identb[:], convw[:, Tt, j:j + 1], ALU.mult, ['identb', 'convw'], ['dg%d' % di])
                    ustate['di'] = di
                di = ustate['di']
                n = 512 if blk < 8 else 256
                off = (2 + 512 * blk) if blk < 8 else 4102
                tok0 = 512 * blk
                b = rcv.next()
                for j in range(5):
                    mm(pf[b][:, 0:n], dg[di][:, j, :], rawg[:, ti, off - 2 + j: off - 2 + j + n], j == 0, j == 4,
                       ['dg%d' % di, 'rawg%d' % ti], ['pf%d' % b], inc=(j == 4))
                if ti < 3:
                    xi = rxc.next()
                    dst = xc[xi][:, 0:n]; dkey = 'xc%d' % xi
                elif ti == 3:
                    dst = BT[:, tok0:tok0 + n]; dkey = 'BT%d' % blk
                else:
                    dst = CT[:, tok0:tok0 + n]; dkey = 'CT%d' % blk
                act(dst, pf[b][:, 0:n], AF.Silu, ['pf%d' % b, 'convb'], [dkey], bias=convb[:, Tt:Tt + 1])
                ustate[u] = (dst, dkey, n)

            def BQ_(u):
                ti, Tt, blk = units[u]
                dst, dkey, n = ustate.pop(u)
                if ti <= 3:
                    nj = n // 128
                    for jj in range(nj):
                        tr(pb[:, jj * 128:(jj + 1) * 128], dst[:, jj * 128:(jj + 1) * 128], identb[:], [dkey, 'identb'], ['pb'], inc=(jj == nj - 1))
                    src3 = pb[:, 0:n].rearrange("p (j c) -> p j c", c=128)
                    if ti < 3:
                        cp('dve', x_tok[:, 4 * blk:4 * blk + nj, ti * 128:(ti + 1) * 128], src3, ['pb'], ['x_tok'])
                    else:
                        cp('dve', B_tok[:, 4 * blk:4 * blk + nj, :], src3, ['pb'], ['B_tok'])

            BP_(0)
            for u in range(len(units)):
                if u + 1 < len(units):
                    BP_(u + 1)
                BQ_(u)
            S.barrier()
            stop('B1')
            if g + 1 < 4:
                for ti, Tt in enumerate([3 * (g + 1), 3 * (g + 1) + 1, 3 * (g + 1) + 2, 12 + g + 1, 16 + g + 1]):
                    S.dma('sp', rawg[:, ti, :], raw_rows[Tt, :, :], writes=['rawg%d' % ti], sem='rawg')
                S.regroup('rawg', ['rawg%d' % ti for ti in range(5)])
            for d in range(2):
                cp('dve', dtg[:, :, d * 6:(d + 1) * 6], dtr_all[:, :, d * 24 + g * 6: d * 24 + g * 6 + 6], ['dtr_all'], ['dtg'])
            act(dtg[:], dtg[:], AF.Exp, ['dtg'], ['dtg'])
            act(dtg[:], dtg[:], AF.Ln, ['dtg'], ['dtg'], bias=1.0, scale=1.0)
            act(lndt[:], dtg[:], AF.Ln, ['dtg'], ['lndt'])
            for d in range(2):
                tt('dve', av[:, :, d * 6:(d + 1) * 6], dtg[:, :, d * 6:(d + 1) * 6],
                   negA[:, d * 24 + g * 6: d * 24 + g * 6 + 6].unsqueeze(1).to_broadcast([128, NCH, 6]), ALU.mult, ['dtg', 'negA'], ['av'])
            for c in range(NCH):
                last = (c == NCH - 1)
                mm(pf[4][:, c * 12:c * 12 + 6], cU[:], av[:, c, 0:6], True, True, ['cU', 'av'], ['pf4'], inc=False)
                mm(pf[4][:, c * 12 + 6:c * 12 + 12], cLo[:], av[:, c, 6:12], True, True, ['cLo', 'av'], ['pf4'], inc=False)
                mm(pf[5][:, c * 12:c * 12 + 12], ones[:], av[:, c, :], True, True, ['ones2', 'av'], ['pf5'], inc=last)
            cp('dve', cs_all[:], pf[4][:, 0:NCH * 12].rearrange("p (c h) -> p c h", h=12), ['pf4'], ['cs_all'])
            cp('dve', tot_all[:], pf[5][:, 0:NCH * 12].rearrange("p (c h) -> p c h", h=12), ['pf5'], ['tot_all'])
            tt('dve', nb[:], lndt[:], cs_all[:], ALU.subtract, ['lndt', 'cs_all'], ['nb'])
            act(eoff[:], cs_all[:], AF.Exp, ['cs_all'], ['eoff'])
            tt('dve', wst[:], tot_all[:], nb[:], ALU.add, ['tot_all', 'nb'], ['wst'])
            act(wst[:], wst[:], AF.Exp, ['wst'], ['wst'])
            act(dch[:], tot_all[:], AF.Exp, ['tot_all'], ['dch'])

            stop('B2')
            rxd = Ring('xd', 4)

            def chunk_state(c, d, bank=6):
                i = rxd.next()
                tt('dve', xd[i][:].rearrange("p (h q) -> p h q", q=64), x_tok[:, c, :].rearrange("p (h q) -> p h q", q=64),
                   wst[:, c, d * 6:(d + 1) * 6].unsqueeze(2).to_broadcast([128, 6, 64]), ALU.mult, ['x_tok', 'wst'], ['xd%d' % i])
                mm(pf[bank][:, 0:384], B_tok[:, c, :], xd[i][:], True, True, ['B_tok', 'xd%d' % i], ['pf%d' % bank])

            def dec_bc(c, d):
                return dch[:, c, d * 6:(d + 1) * 6].unsqueeze(2).to_broadcast([128, 6, 64])

            def v3(ap):
                return ap.rearrange("p (h q) -> p h q", q=64)

            for d, (ca, cb_) in enumerate(((32, 33), (33, 32))):
                chunk_state(ca, d)
                cp('dve', Srun[d][:], pf[6][:, 0:384], ['pf6'], ['Srun%d' % d])
                tt('dve', v3(Srun[d][:]), v3(Srun[d][:]), dec_bc(cb_, d), ALU.mult, ['Srun%d' % d, 'dch'], ['Srun%d' % d])
                chunk_state(cb_, d)
                tt('dve', Srun[d][:], Srun[d][:], pf[6][:, 0:384], ALU.add, ['Srun%d' % d, 'pf6'], ['Srun%d' % d])
            stop('B3')
            order = list(range(NT - 1, -1, -1))
            banks = [3, 4, 5, 6]
            AHEAD = 3
            for n in range(min(AHEAD, NT)):
                chunk_state(order[n], 1, banks[n % 4])
            cur = 1
            for n, c in enumerate(order):
                if n + AHEAD < NT:
                    chunk_state(order[n + AHEAD], 1, banks[(n + AHEAD) % 4])
                nxt = 2 if cur == 1 else 1
                cp('act', Sb_all[:, c, :], Srun[cur][:], ['Srun%d' % cur], ['Sb_all%d' % c])
                tt('dve', v3(Srun[nxt][:]), v3(Srun[cur][:]), dec_bc(c, 1), ALU.mult, ['Srun%d' % cur, 'dch'], ['Srun%d' % nxt])
                bk = banks[n % 4]
                tt('dve', Srun[nxt][:], Srun[nxt][:], pf[bk][:, 0:384], ALU.add, ['Srun%d' % nxt, 'pf%d' % bk], ['Srun%d' % nxt])
                cur = nxt
            S.barrier()
            stop('B4')
            rL = Ring('L', 2); rq = Ring('q', 8)
            pairs = [(d, h) for d in range(2) for h in range(6)]

            def H_(c, ci):
                tk = slice(c * 128, (c + 1) * 128)
                cp('act', Sfb[ci][:], Srun[0][:], ['Srun0'], ['Sfb%d' % ci])
                mm(pf[0][:, 0:128], BT[:, tk], CT[:, tk], True, True, ['BT%d' % (c // 4), 'CT%d' % (c // 4)], ['pf0'], inc=False)
                mm(pf[0][0:6, 128:256], av[:, c, 0:6], cU[:], True, True, ['av', 'cU'], ['pf0'], inc=False)
                mm(pf[0][0:6, 256:384], av[:, c, 6:12], cLo[:], True, True, ['av', 'cLo'], ['pf0'])
                cp('dve', CBt[ci][:], pf[0][:, 0:128], ['pf0'], ['CBt%d' % ci])
                src = pf[0][0:6, 128:384].rearrange("p (d l) -> p d l", l=128)
                cp('dve', csTh[ci][:], src, ['pf0'], ['csTh%d' % ci])
                tt('dve', csTl[ci][:], src, csTh[ci][:], ALU.subtract, ['pf0', 'csTh%d' % ci], ['csTl%d' % ci])

            def L_(c, ci, bt):
                lb = 1 + rL.next()
                bk = 'pf%d' % lb
                for q in range(4):
                    d, h = pairs[bt * 4 + q]
                    reg = pf[lb][:, q * 128:(q + 1) * 128]
                    mm(reg, cSelb[0:6, h, :], csTh[ci][0:6, d, :], True, False, ['cSelb', 'csTh%d' % ci], [bk], inc=False)
                    mm(reg, cSelb[0:6, h, :], csTl[ci][0:6, d, :], False, False, ['cSelb', 'csTl%d' % ci], [bk], inc=False)
                    mm(reg, identb[:], cMaskb[:, d, :], False, True, ['identb', 'cMaskb'], [bk], inc=(q == 3))
                return lb

            def E_(c, ci, bt, lb):
                bk = 'pf%d' % lb
                qis = []
                for q in range(4):
                    d, h = pairs[bt * 4 + q]
                    reg = pf[lb][:, q * 128:(q + 1) * 128]
                    qi = rq.next()
                    act(Lm[qi][:], reg, AF.Exp, [bk, 'nb'], ['Lm%d' % qi], bias=nb[:, c, d * 6 + h: d * 6 + h + 1])
                    tt('dve', Mt[qi][:], Lm[qi][:], CBt[ci][:], ALU.mult, ['Lm%d' % qi, 'CBt%d' % ci], ['Mt%d' % qi])
                    qis.append(qi)
                return qis

            def Y_(c, bt, qis):
                for q in range(4):
                    d, h = pairs[bt * 4 + q]
                    qi = qis[q]
                    mm(pf[3][:, h * 64:(h + 1) * 64], Mt[qi][:], x_tok[:, c, h * 64:(h + 1) * 64], (bt == 0 and q == 0), (bt == 2 and q == 3),
                       ['Mt%d' % qi, 'x_tok'], ['pf3'], inc=(q == 3), sgc=True)

            def O_(c, ci):
                tk = slice(c * 128, (c + 1) * 128)
                mm(pf[4][:, 0:384], CT[:, tk], Sfb[ci][:], True, True, ['CT%d' % (c // 4), 'Sfb%d' % ci], ['pf4'])
                mm(pf[5][:, 0:384], CT[:, tk], Sb_all[:, c, :], True, True, ['CT%d' % (c // 4), 'Sb_all%d' % c], ['pf5'])

            def U_(c):
                chunk_state(c, 0)
                tt('dve', v3(Srun[0][:]), v3(Srun[0][:]), dec_bc(c, 0), ALU.mult, ['Srun0', 'dch'], ['Srun0'])
                tt('dve', Srun[0][:], Srun[0][:], pf[6][:, 0:384], ALU.add, ['Srun0', 'pf6'], ['Srun0'])

            def F1_(c, ci):
                k1 = 't1_%d' % ci
                S.dma('sp', zg[ci][:], zt_d[c * 128:(c + 1) * 128, g * 384:(g + 1) * 384], writes=['zg%d' % ci], sem='zg%d' % ci)
                tt('dve', v3(t1[ci][:]), v3(pf[4][:, 0:384]), eoff[:, c, 0:6].unsqueeze(2).to_broadcast([128, 6, 64]), ALU.mult, ['pf4', 'eoff'], [k1])
                tt('dve', v3(t2[:]), v3(pf[5][:, 0:384]), eoff[:, c, 6:12].unsqueeze(2).to_broadcast([128, 6, 64]), ALU.mult, ['pf5', 'eoff'], ['t2'])
                tt('dve', t1[ci][:], pf[3][:, 0:384], t1[ci][:], ALU.add, ['pf3', k1], [k1])
                tt('pool', t3[:], x_tok[:, c, :], Dvec[:, g * 384:(g + 1) * 384], ALU.mult, ['x_tok', 'Dvec'], ['t3'])
                tt('pool', t2[:], t2[:], t3[:], ALU.add, ['t2', 't3'], ['t2'])
                tt('pool', t1[ci][:], t1[ci][:], t2[:], ALU.add, [k1, 't2'], [k1])

            def F2_(c, ci):
                k1 = 't1_%d' % ci
                tt('dve', t1[ci][:], t1[ci][:], zg[ci][:], ALU.mult, [k1, 'zg%d' % ci], [k1])
                tt('pool', sqj[:], t1[ci][:], t1[ci][:], ALU.mult, [k1], ['sqj'])

            def F3_(c, ci):
                k1 = 't1_%d' % ci
                S.op('dve', lambda: V.reduce_sum(out=ssq[:, g, c:c + 1], in_=sqj[:], axis=AX.X), ['sqj'], ['ssq'])
                cp('act', yzb[ci][:], t1[ci][:], [k1], ['yzb%d' % ci])

            def T_(c, ci):
                tk = slice(c * 128, (c + 1) * 128)
                for i3 in range(3):
                    tr(pb[:, 512 + i3 * 128: 512 + (i3 + 1) * 128], yzb[ci][:, i3 * 128:(i3 + 1) * 128], identb[:], ['yzb%d' % ci, 'identb'], ['pbz'], inc=(i3 == 2))
                tt('dve', yzTs[ci][:], pb[:, 512:896].rearrange("p (i t) -> p i t", t=128),
                   ynw[:, g * 3:(g + 1) * 3].unsqueeze(2).to_broadcast([128, 3, 128]), ALU.mult, ['pbz', 'ynw'], ['yzTs%d' % ci])
                S.dma('sp', yzT_v[:, g * 3:(g + 1) * 3, tk], yzTs[ci][:], reads=['yzTs%d' % ci])

            for c in range(NT):
                ci = c % 2
                H_(c, ci)
                lb0 = L_(c, ci, 0); q0 = E_(c, ci, 0, lb0)
                if c > 0:
                    F2_(c - 1, (c - 1) % 2)
                lb1 = L_(c, ci, 1); q1 = E_(c, ci, 1, lb1)
                if c > 0:
                    F3_(c - 1, (c - 1) % 2)
                Y_(c, 0, q0)
                lb2 = L_(c, ci, 2); q2 = E_(c, ci, 2, lb2)
                Y_(c, 1, q1)
                if c > 0:
                    T_(c - 1, (c - 1) % 2)
                Y_(c, 2, q2)
                O_(c, ci)
                U_(c)
                F1_(c, ci)
            F2_(NT - 1, (NT - 1) % 2)
            F3_(NT - 1, (NT - 1) % 2)
            T_(NT - 1, (NT - 1) % 2)
            S.barrier()
    esAB.close()
    if upto == 'B':
        es0.close(); return nc

    mask_all = T(P0, "mask_all", [128, NT, NE]); gates_all = T(P0, "gates_all", [128, NT, 4])
    idx_all = T(P0, "idx_all", [128, NT * 4], I32); widx = T(P0, "widx", [128, NBLK], I32)
    esC = ExitStack()
    s2_bc = T(esC, "s2_bc", [128, D]); sh2_bc = T(esC, "sh2_bc", [128, D])
    S.dma('sp', s2_bc[:], modrow_d[0], writes=['s2_bc'], sem='L_s2bc')
    S.dma('sp', sh2_bc[:], modrow_d[1], writes=['s2_bc'], sem='L_s2bc')
    with ExitStack() as es:
        wout = T(es, "wout", [128, 16, D], BF16)
        for k in range(16):
            S.dma('pool', wout[:, k, :], wout_d[k * 128:(k + 1) * 128, :], writes=['wout'], sem='wout')
        rw = T(es, "rw", [128, 8, NE]); rb_bc = T(es, "rb_bc", [128, NE])
        S.dma('sp', rw[:], rw_d[:, :, :], writes=['rw'], sem='c4')
        S.dma('sp', rb_bc[:], rb_d.partition_broadcast(128), writes=['rb_bc'], sem='c4')
        S.regroup('c4', ['rw', 'rb_bc'])
        rs_ssd = T(es, "rs_ssd", [128, NT]); tq = T(es, "tq", [128, NT])
        tt('dve', tq[:], ssq[:, 0, :], ssq[:, 1, :], ALU.add, ['ssq'], ['tq'])
        tt('dve', tq[:], tq[:], ssq[:, 2, :], ALU.add, ['tq', 'ssq'], ['tq'])
        tt('dve', tq[:], tq[:], ssq[:, 3, :], ALU.add, ['tq', 'ssq'], ['tq'])
        act(tq[:], tq[:], AF.Ln, ['tq'], ['tq'], bias=EPS, scale=1.0 / SSDW)
        act(rs_ssd[:], tq[:], AF.Exp, ['tq'], ['rs_ssd'], scale=-0.5)
        yzt = [T(es, "yzt%d" % i, [128, 12, 128], BF16) for i in range(2)]; ypt = [T(es, "ypt%d" % i, [128, 4, 128], BF16) for i in range(2)]
        xt_ = [T(es, "cxt%d" % i, [128, D]) for i in range(2)]; m_ = T(es, "cm", [128, D]); x1s = [T(es, "x1s%d" % i, [128, D]) for i in range(2)]
        sq_ = T(es, "csq", [128, D]); ss_ = [T(es, "css%d" % i, [128, 4]) for i in range(2)]; xn_ = [T(es, "cxn%d" % i, [128, D]) for i in range(2)]
        tmpm = T(es, "ctmpm", [128, 8, 128]); h2f = T(es, "h2f", [128, 8, 128]); htk = T(es, "htk", [128, D]); htb = [T(es, "htb%d" % i, [128, D], BF16) for i in range(2)]
        lg = T(es, "lg", [128, NE]); m8 = T(es, "m8", [128, 8]); ex = T(es, "ex", [128, NE]); sm = T(es, "sm", [128, 4])
        ps_s = [PS(es, "ps_s%d" % i, [128, 512]) for i in range(2)]; ps_p = [PS(es, "ps_p%d" % i, [128, 512]) for i in range(2)]
        pT = PS(es, "pT2", [128, 1024]); pr = PS(es, "pr", [128, 512])
        yzT_v = yzT_d.rearrange("(i p) t -> p i t", p=128); ypT_v = ypT_d.rearrange("(g o) t -> o g t", o=128)
        def CX_(j):
            i = j % 2
            tk = slice(j * 128, (j + 1) * 128)
            S.dma('sp', yzt[i][:], yzT_v[:, :, tk], writes=['yzt%d' % i], sem='yzt%d' % i)
            S.dma('sp', ypt[i][:], ypT_v[:, :, tk], writes=['ypt%d' % i], sem='ypt%d' % i)
            S.dma('sp', xt_[i][:], x_d[tk, :], writes=['cxt%d' % i], sem='cxt%d' % i)
            for half in range(2):
                hc = slice(half * 512, (half + 1) * 512)
                for k in range(12):
                    mm(ps_s[half][:, :], yzt[i][:, k, :], wout[:, k, hc], k == 0, k == 11, ['yzt%d' % i, 'wout'], ['ps_s%d' % half], inc=(k == 11))
                for k in range(4):
                    mm(ps_p[half][:, :], ypt[i][:, k, :], wout[:, 12 + k, hc], k == 0, k == 3, ['ypt%d' % i, 'wout'], ['ps_p%d' % half], inc=(k == 3))
                ts('dve', m_[:, hc], ps_s[half][:, :], rs_ssd[:, j:j + 1], ALU.mult, ['ps_s%d' % half, 'rs_ssd'], ['cm%d' % half])
                tt('dve', m_[:, hc], m_[:, hc], ps_p[half][:, :], ALU.add, ['cm%d' % half, 'ps_p%d' % half], ['cm%d' % half])
                tt('pool', m_[:, hc], m_[:, hc], g_bc[:, 0, hc], ALU.mult, ['cm%d' % half, 'g_bc'], ['cm%d' % half])
                tt('pool', x1s[i][:, hc], m_[:, hc], xt_[i][:, hc], ALU.add, ['cm%d' % half, 'cxt%d' % i], ['x1s%d' % i])
            S.dma('sp', x1_d[tk, :], x1s[i][:], reads=['x1s%d' % i])
            xt = x1s[i][:]
            act(sq_[:], xt, AF.Square, ['x1s%d' % i], ['csq'])
            S.op('dve', lambda: V.reduce_sum(out=ss_[i][:, 0:1], in_=sq_[:], axis=AX.X), ['csq'], ['css%d' % i])
            act(ss_[i][:, 1:2], ss_[i][:, 0:1], AF.Ln, ['css%d' % i], ['css%d' % i], bias=EPS, scale=1.0 / D)
            act(ss_[i][:, 2:3], ss_[i][:, 1:2], AF.Exp, ['css%d' % i], ['css%d' % i], scale=-0.5)
            ts('dve', xn_[i][:], xt, ss_[i][:, 2:3], ALU.mult, ['x1s%d' % i, 'css%d' % i], ['cxn%d' % i])
        def CY_(j):
            i = j % 2
            tk = slice(j * 128, (j + 1) * 128)
            for k in range(8):
                tr(pT[:, k * 128:(k + 1) * 128], xn_[i][:, k * 128:(k + 1) * 128], identf[:], ['cxn%d' % i, 'identf'], ['pT2'], inc=(k == 7))
            tt('dve', tmpm[:], pT[:, :].rearrange("p (k t) -> p k t", t=128), s2[:, :].unsqueeze(2).to_broadcast([128, 8, 128]),
               ALU.mult, ['pT2', 'modv'], ['ctmpm'])
            tt('pool', h2f[:], tmpm[:], sh2[:, :].unsqueeze(2).to_broadcast([128, 8, 128]), ALU.add, ['ctmpm', 'modv'], ['h2f'])
            tt('pool', htk[:], xn_[i][:], s2_bc[:], ALU.mult, ['cxn%d' % i, 's2_bc'], ['htk'])
            tt('pool', htb[i][:], htk[:], sh2_bc[:], ALU.add, ['htk', 's2_bc'], ['htb%d' % i])
            S.dma('sp', h2tok_d[tk, :], htb[i][:], reads=['htb%d' % i])
            for k in range(8):
                mm(pr[:, 0:NE], h2f[:, k, :], rw[:, k, :], k == 0, k == 7, ['h2f', 'rw'], ['pr'], inc=(k == 7))
            tt('dve', lg[:], pr[:, 0:NE], rb_bc[:], ALU.add, ['pr', 'rb_bc'], ['lg'])
            S.op('dve', lambda: V.max(out=m8[:], in_=lg[:]), ['lg'], ['m8'])
            ts('dve', mask_all[:, j, :], lg[:], m8[:, 3:4], ALU.is_ge, ['lg', 'm8'], ['mask_all'])
            ts('dve', sm[:, 0:1], m8[:, 0:1], -1.0, ALU.mult, ['m8'], ['sm'])
            act(ex[:], lg[:], AF.Exp, ['lg', 'sm'], ['ex'], bias=sm[:, 0:1])
            tt('dve', ex[:], ex[:], mask_all[:, j, :], ALU.mult, ['ex', 'mask_all'], ['ex'])
            S.op('dve', lambda: V.reduce_sum(out=sm[:, 1:2], in_=ex[:], axis=AX.X), ['ex'], ['sm'])
            S.op('dve', lambda: V.reciprocal(out=sm[:, 2:3], in_=sm[:, 1:2]), ['sm'], ['sm'])
            ts('dve', G_all[:, j, :], ex[:], sm[:, 2:3], ALU.mult, ['ex', 'sm'], ['G_all'])
        CX_(0)
        for j in range(NT):
            if j + 1 < NT:
                CX_(j + 1)
            CY_(j)
        S.barrier()
    esC.close()
    stop('C')

    with ExitStack() as es:
        SU = T(es, "SU", [128, 128], BF16); SUf = T(es, "SUf", [128, 128]); onesb = T(es, "onesb", [128, 128], BF16)
        mask_bf = T(es, "mask_bf", [128, NT, NE], BF16); P_all = T(es, "P_all", [128, NT + 1, NE], BF16)
        rank_all = T(es, "rank_all", [128, NT, NE]); counts = T(es, "counts", [128, NE]); padded = T(es, "padded", [128, NE])
        tmpc = T(es, "tmpc", [128, NE]); pad_end = T(es, "pad_end", [128, NE]); pad_start = T(es, "pad_start", [128, NE])
        onesf = T(es, "onesf", [128, NE]); Dm = T(es, "Dm", [128, NT, NE]); Em = T(es, "Em", [128, NT, NE])
        iota1 = T(es, "iota1", [128, NE]); jv = T(es, "jv", [128, NBLK]); pidx = T(es, "pidx", [128, 1])
        d8 = T(es, "d8", [128, 8]); e8 = T(es, "e8", [128, 8]); d4 = T(es, "d4", [128, NT, 4]); oh = T(es, "oh", [128, NE])
        cmp3 = T(es, "cmp3", [128, NBLK, NE]); bexp = T(es, "bexp", [128, NBLK])
        rows = [T(es, "rows%d" % i, [128, D], BF16) for i in range(2)]
        pk = [PS(es, "pk%d" % i, [128, 512]) for i in range(2)]; pc_ = PS(es, "pc_", [128, 512])
        S.dma('sp', SUf[:], cSU_d[:, :], writes=['SUf'], sem='c6')
        S.dma('sp', iota1[:], cIota_d.partition_broadcast(128), writes=['iota1'], sem='c6')
        S.dma('sp', jv[:], cJv_d.partition_broadcast(128), writes=['jv'], sem='c6')
        S.dma('sp', pidx[:], cPidx_d[:, :], writes=['pidx'], sem='c6')
        S.regroup('c6', ['SUf', 'iota1', 'jv', 'pidx'])
        cp('dve', SU[:], SUf[:], ['SUf'], ['SU'])
        S.op('dve', lambda: V.memset(onesb[:], 1.0), writes=['onesb'])
        S.op('dve', lambda: V.memset(onesf[:], 1.0), writes=['onesf'])
        cp('dve', mask_bf[:], mask_all[:], ['mask_all'], ['mask_bf'])
        S.op('dve', lambda: V.memset(P_all[:, 0, :], 0.0), writes=['P_all'])
        for j in range(NT):
            tt('dve', P_all[:, j + 1, :], P_all[:, j, :], mask_all[:, j, :], ALU.add, ['P_all', 'mask_all'], ['P_all'])
        for j in range(NT):
            b = j // 16
            reg = pk[b][:, (j % 16) * NE:(j % 16 + 1) * NE]
            mm(reg, SU[:], mask_bf[:, j, :], True, False, ['SU', 'mask_bf'], ['pk%d' % b], inc=False)
            mm(reg, onesb[:], P_all[:, j, :], False, True, ['onesb', 'P_all'], ['pk%d' % b], inc=(j % 16 == 15))
        for b in range(2):
            cp('dve', rank_all[:, b * 16:(b + 1) * 16, :], pk[b][:, :].rearrange("p (j e) -> p j e", e=NE), ['pk%d' % b], ['rank_all'])
        mm(pc_[:, 0:NE], onesb[:], P_all[:, NT, :], True, True, ['onesb', 'P_all'], ['pc_'])
        cp('dve', counts[:], pc_[:, 0:NE], ['pc_'], ['counts'])
        S.op('dve', lambda: V.memset(padded[:], 0.0), writes=['padded'])
        for m in range(4096 // BS):
            ts('dve', tmpc[:], counts[:], float(BS * m), ALU.is_gt, ['counts'], ['tmpc'], s2=float(BS), op1=ALU.mult)
            tt('dve', padded[:], padded[:], tmpc[:], ALU.add, ['padded', 'tmpc'], ['padded'])
        S.op('dve', lambda: V.tensor_tensor_scan(out=pad_end[:], data0=onesf[:], data1=padded[:], initial=0.0, op0=ALU.mult, op1=ALU.add),
             ['onesf', 'padded'], ['pad_end'])
        tt('dve', pad_start[:], pad_end[:], padded[:], ALU.subtract, ['pad_end', 'padded'], ['pad_start'])
        tt('dve', Dm[:], rank_all[:], pad_start[:, :].unsqueeze(1).to_broadcast([128, NT, NE]), ALU.add, ['rank_all', 'pad_start'], ['Dm'])
        ts('dve', Dm[:], Dm[:], 1.0, ALU.add, ['Dm'], ['Dm'])
        tt('dve', Dm[:], Dm[:], mask_all[:], ALU.mult, ['Dm', 'mask_all'], ['Dm'])
        tt('dve', Em[:], mask_all[:], iota1[:, :].unsqueeze(1).to_broadcast([128, NT, NE]), ALU.mult, ['mask_all', 'iota1'], ['Em'])
        for j in range(NT):
            S.op('dve', lambda: V.max(out=d8[:], in_=Dm[:, j, :]), ['Dm'], ['d8'])
            ts('dve', d4[:, j, :], d8[:, 0:4], -1.0, ALU.add, ['d8'], ['d4'])
        cp('dve', idx_all[:].rearrange("p (j k) -> p j k", k=4), d4[:], ['d4'], ['idx_all'])
        tt('dve', cmp3[:], pad_end[:, :].unsqueeze(1).to_broadcast([128, NBLK, NE]), jv[:, :].unsqueeze(2).to_broadcast([128, NBLK, NE]),
           ALU.is_le, ['pad_end', 'jv'], ['cmp3'])
        S.op('dve', lambda: V.reduce_sum(out=bexp[:], in_=cmp3[:], axis=AX.X), ['cmp3'], ['bexp'])
        ts('dve', bexp[:], bexp[:], float(NE - 1), ALU.min, ['bexp'], ['bexp'], s2=128.0, op1=ALU.mult)
        skp = T(es, "skp", [128, NBLK])
        S.op('dve', lambda: V.memset(skp[:], 0.0), writes=['skp'])
        tt('dve', skp[:, 2:NBLK], bexp[:, 2:NBLK], bexp[:, 0:NBLK - 2], ALU.is_equal, ['bexp', 'skp'], ['skp'])
        ts('dve', skp[:], skp[:], 1.0e6, ALU.mult, ['skp'], ['skp'])
        ts('dve', bexp[:], bexp[:], pidx[:, 0:1], ALU.add, ['bexp', 'pidx'], ['bexp'])
        tt('dve', bexp[:], bexp[:], skp[:], ALU.add, ['bexp', 'skp'], ['bexp'])
        cp('dve', widx[:], bexp[:], ['bexp'], ['widx'])
        S.barrier()
        stop('C2')
        for j in range(NT):
            i = j % 2
            S.dma('sp', rows[i][:], h2tok_d[j * 128:(j + 1) * 128, :], writes=['rows%d' % i], sem='rows%d' % i)
            for k in range(4):
                S.idma(out=Xg_d[:, :], in_=rows[i][:, :], idx=idx_all[:, j * 4 + k:j * 4 + k + 1], scatter=True, bound=NSLOT - 1,
                       reads=['rows%d' % i, 'idx_all'], sem='sc%d' % i)
        for j in range(NT):
            S.op('dve', lambda: V.max(out=e8[:], in_=Em[:, j, :]), ['Em'], ['e8'])
            for k in range(4):
                ts('dve', oh[:], iota1[:], e8[:, k:k + 1], ALU.is_equal, ['iota1', 'e8'], ['oh'])
                tt('dve', oh[:], oh[:], G_all[:, j, :], ALU.mult, ['oh', 'G_all'], ['oh'])
                S.op('dve', lambda: V.reduce_sum(out=gates_all[:, j, k:k + 1], in_=oh[:], axis=AX.X), ['oh'], ['gates_all'])
        S.barrier()
    stop('C3')

    with ExitStack() as es:
        wg_ = [T(es, "wg%d" % i, [128, 8, D], BF16) for i in range(2)]; wu_ = [T(es, "wu%d" % i, [128, 8, D], BF16) for i in range(2)]
        w2_ = [T(es, "w2_%d" % i, [128, 8, D], BF16) for i in range(2)]; b1t = [T(es, "b1t%d" % i, [128, 16]) for i in range(2)]
        NJ = BS // 128
        b1p = [T(es, "b1p%d" % i, [128, 8]) for i in range(2)]
        xgs = [T(es, "xgs%d" % i, [128, NJ, D], BF16) for i in range(3)]; xgT = [T(es, "xgT%d" % i, [128, 8, BS], BF16) for i in range(2)]
        actT = [T(es, "actT%d" % i, [128, 8, BS], BF16) for i in range(2)]
        gt3 = [T(es, "gt3_%d" % i, [128, BS]) for i in range(3)]; sg3 = [T(es, "sg3_%d" % i, [128, BS], BF16) for i in range(3)]
        ut3 = [T(es, "ut3_%d" % i, [128, BS]) for i in range(3)]
        ysb = [T(es, "ysb%d" % i, [128, NJ, D]) for i in range(2)]
        pg_ = [PS(es, "mpg%d" % i, [128, 512]) for i in range(2)]; pu_ = [PS(es, "mpu%d" % i, [128, 512]) for i in range(2)]
        py_ = [PS(es, "mpy%d" % i, [128, 512]) for i in range(2)]; pb_ = [PS(es, "mpb%d" % i, [128, 1024], BF16) for i in range(2)]
        Xg_v = Xg_d.rearrange("(b j p) d -> b p j d", p=128, j=NJ); Y_v = Y_d.rearrange("(b j p) o -> b p j o", p=128, j=NJ)

        def wload(blk, i):
            ix = widx[:, blk:blk + 1]
            for (dst, src, nm) in ((wg_[i], W1G_d, 'wg%d' % i), (wu_[i], W1U_d, 'wu%d' % i), (w2_[i], W2_d, 'w2_%d' % i)):
                S.idma(out=dst[:].rearrange("p k c -> p (k c)"), in_=src[:, :], idx=ix, scatter=False, bound=NE * 128 - 1,
                       reads=['widx'], writes=[nm], sem=nm)
            S.idma(out=b1t[i][:, :], in_=B1_d[:, :], idx=ix, scatter=False, bound=NE * 128 - 1, reads=['widx'], writes=['b1t%d' % i], sem='b1t%d' % i)

        rgu = Ring('gu', 2); ry = Ring('y', 2); rpb = Ring('pb', 2); r3 = Ring('r3', 3)
        KB = 1024 // BS

        def XL_(blk):
            xi = blk % 3
            S.dma('sp', xgs[xi][:], Xg_v[blk], writes=['xgs%d' % xi], sem='xgs%d' % xi)

        def TR_(blk):
            wi = blk % 2
            xi = blk % 3
            for k0 in range(0, 8, KB):
                pi = rpb.next()
                for kk in range(KB):
                    k = k0 + kk
                    for jj in range(NJ):
                        tr(pb_[pi][:, kk * BS + jj * 128: kk * BS + (jj + 1) * 128], xgs[xi][:, jj, k * 128:(k + 1) * 128], identb[:],
                           ['xgs%d' % xi, 'identb'], ['mpb%d' % pi], inc=(kk == KB - 1 and jj == NJ - 1))
                cp('act' if (k0 // KB) % 2 == 0 else 'dve', xgT[wi][:, k0:k0 + KB, :], pb_[pi][:, :].rearrange("p (k s) -> p k s", s=BS),
                   ['mpb%d' % pi], ['xgT%d' % wi])

        def FL_(blk):
            wi = blk % 2
            ts('pool', b1p[wi][:], b1t[wi][:, 8:16], 1.0, ALU.add, ['b1t%d' % wi], ['b1p%d' % wi])
            prev = None

            def fin(pv):
                ri, fp = pv
                S.op('dve', lambda: V.scalar_tensor_tensor(out=actT[wi][:, fp, :], in0=ut3[ri][:], scalar=-6.0, in1=gt3[ri][:], op0=ALU.max, op1=ALU.mult),
                     ['ut3_%d' % ri, 'gt3_%d' % ri], ['actT%d' % wi])
            for f in range(8):
                b = rgu.next(); ri = r3.next()
                fs = slice(f * 128, (f + 1) * 128)
                for k in range(8):
                    mm(pg_[b][:, 0:BS], wg_[wi][:, k, fs], xgT[wi][:, k, :], k == 0, k == 7, ['wg%d' % wi, 'xgT%d' % wi], ['mpg%d' % b], inc=(k == 7))
                for k in range(8):
                    mm(pu_[b][:, 0:BS], wu_[wi][:, k, fs], xgT[wi][:, k, :], k == 0, k == 7, ['wu%d' % wi, 'xgT%d' % wi], ['mpu%d' % b], inc=(k == 7))
                ts('dve', gt3[ri][:], pg_[b][:, 0:BS], b1t[wi][:, f:f + 1], ALU.add, ['mpg%d' % b, 'b1t%d' % wi], ['gt3_%d' % ri], s2=7.0, op1=ALU.min)
                act(sg3[ri][:], gt3[ri][:], AF.Sigmoid, ['gt3_%d' % ri], ['sg3_%d' % ri], scale=1.702)
                ts('dve', ut3[ri][:], pu_[b][:, 0:BS], b1p[wi][:, f:f + 1], ALU.add, ['mpu%d' % b, 'b1p%d' % wi], ['ut3_%d' % ri], s2=8.0, op1=ALU.min)
                tt('pool', gt3[ri][:], gt3[ri][:], sg3[ri][:], ALU.mult, ['gt3_%d' % ri, 'sg3_%d' % ri], ['gt3_%d' % ri])
                if prev is not None:
                    fin(prev)
                prev = (ri, f)
            fin(prev)

        def W2_(blk):
            wi = blk % 2
            for jj in range(NJ):
                for half in range(2):
                    b = ry.next()
                    for k in range(8):
                        mm(py_[b][:, :], actT[wi][:, k, jj * 128:(jj + 1) * 128], w2_[wi][:, k, half * 512:(half + 1) * 512], k == 0, k == 7,
                           ['actT%d' % wi, 'w2_%d' % wi], ['mpy%d' % b], inc=(k == 7))
                    cp('act', ysb[wi][:, jj, half * 512:(half + 1) * 512], py_[b][:, :], ['mpy%d' % b], ['ysb%d' % wi])
            S.dma('sp', Y_v[blk], ysb[wi][:], reads=['ysb%d' % wi])

        XL_(0)
        XL_(1)
        wload(0, 0)
        TR_(0)
        for blk in range(NBLK):
            if blk + 2 < NBLK:
                XL_(blk + 2)
            if blk + 1 < NBLK:
                wload(blk + 1, (blk + 1) % 2)
            FL_(blk)
            if blk + 1 < NBLK:
                TR_(blk + 1)
            W2_(blk)
        S.barrier()
    stop('D')

    with ExitStack() as es:
        b2s = T(es, "b2s", [NE, D]); GT = T(es, "GT", [NE, 128])
        S.dma('sp', b2s[:], b2_d[:, :], writes=['b2s'], sem='b2s')
        yk = [T(es, "yk%d" % i, [128, D]) for i in range(8)]; acc = [T(es, "acc%d" % i, [128, D]) for i in range(2)]
        x1t = [T(es, "x1t%d" % i, [128, D]) for i in range(3)]; sq_ = T(es, "dsq", [128, D]); ss_ = [T(es, "dss%d" % i, [128, 4]) for i in range(2)]
        pgt = PS(es, "pgt", [128, 512]); pa = [PS(es, "pa%d" % i, [128, 512]) for i in range(2)]
        def EG_(j):
            i = j % 2; xi = j % 3
            tk = slice(j * 128, (j + 1) * 128)
            S.dma('sp', x1t[xi][:], x1_d[tk, :], writes=['x1t%d' % xi], sem='x1t%d' % xi)
            for k in range(4):
                kk = i * 4 + k
                S.idma(out=yk[kk][:, :], in_=Y_d[:, :], idx=idx_all[:, j * 4 + k:j * 4 + k + 1], scatter=False, bound=NSLOT - 1,
                       reads=['idx_all'], writes=['yk%d' % kk], sem='yk%d' % kk)

        def EA_(j):
            i = j % 2; xi = j % 3
            tr(pgt[0:NE, 0:128], G_all[:, j, :], identf[:], ['G_all', 'identf'], ['pgt'])
            cp('act', GT[:], pgt[0:NE, 0:128], ['pgt'], ['GT'])
            for half in range(2):
                hc = slice(half * 512, (half + 1) * 512)
                mm(pa[half][:, :], GT[:, :], b2s[:, hc], True, True, ['GT', 'b2s'], ['pa%d' % half])
                S.op('dve', lambda: V.scalar_tensor_tensor(out=acc[i][:, hc], in0=yk[i * 4][:, hc], scalar=gates_all[:, j, 0:1], in1=pa[half][:, :],
                                                            op0=ALU.mult, op1=ALU.add), ['yk%d' % (i * 4), 'gates_all', 'pa%d' % half], ['acc%d_%d' % (i, half)])
                for k in range(1, 4):
                    S.op('dve', lambda: V.scalar_tensor_tensor(out=acc[i][:, hc], in0=yk[i * 4 + k][:, hc], scalar=gates_all[:, j, k:k + 1], in1=acc[i][:, hc],
                                                                op0=ALU.mult, op1=ALU.add), ['yk%d' % (i * 4 + k), 'gates_all', 'acc%d_%d' % (i, half)], ['acc%d_%d' % (i, half)])
            ak = ['acc%d_0' % i, 'acc%d_1' % i]
            tt('pool', acc[i][:], acc[i][:], g_bc[:, 1, :], ALU.mult, ak + ['g_bc'], ak)
            tt('pool', x1t[xi][:], x1t[xi][:], acc[i][:], ALU.add, ['x1t%d' % xi] + ak, ['x1t%d' % xi])

        def EB_(j):
            i = j % 2; xi = j % 3
            tk = slice(j * 128, (j + 1) * 128)
            act(sq_[:], x1t[xi][:], AF.Square, ['x1t%d' % xi], ['dsq'])
            S.op('dve', lambda: V.reduce_sum(out=ss_[i][:, 0:1], in_=sq_[:], axis=AX.X), ['dsq'], ['dss%d' % i])
            act(ss_[i][:, 1:2], ss_[i][:, 0:1], AF.Ln, ['dss%d' % i], ['dss%d' % i], bias=EPS, scale=1.0 / D)
            act(ss_[i][:, 2:3], ss_[i][:, 1:2], AF.Exp, ['dss%d' % i], ['dss%d' % i], scale=-0.5)
            S.op('dve', lambda: V.scalar_tensor_tensor(out=x1t[xi][:], in0=x1t[xi][:], scalar=ss_[i][:, 2:3], in1=fnw_bc[:], op0=ALU.mult, op1=ALU.mult),
                 ['x1t%d' % xi, 'dss%d' % i, 'fnw_bc'], ['x1t%d' % xi])
            S.dma('sp', out_d[tk, :], x1t[xi][:], reads=['x1t%d' % xi])

        EG_(0)
        if NT > 1:
            EG_(1)
        EA_(0)
        for j in range(NT):
            if j + 2 < NT:
                EG_(j + 2)
            if j + 1 < NT:
                EA_(j + 1)
            EB_(j)
        S.barrier()
    es0.close()
    return nc


def _consts():
    I = np.eye(128, dtype=np.float32)
    k = np.arange(128)
    U = (k[:, None] <= k[None, :]).astype(np.float32)
    Lo = (k[:, None] >= k[None, :]).astype(np.float32)
    mask = np.zeros((128, 2, 128), np.float32)
    mask[:, 0, :] = np.where(k[None, :] >= k[:, None], 0.0, NEG)
    mask[:, 1, :] = np.where(k[None, :] <= k[:, None], 0.0, NEG)
    sel = np.zeros((6, 6, 128), np.float32)
    for h in range(6):
        sel[h, h, :] = 1.0
    AT = np.zeros((128, 4, 128), np.float32)
    t = np.arange(64)
    for g, w in enumerate((2, 4, 8, 16)):
        lo = np.clip(t - w // 2, 0, 64); hi = np.clip(t + w - w // 2, 0, 64)
        Am = np.zeros((64, 64), np.float32)
        for ti in range(64):
            Am[ti, lo[ti]:hi[ti]] = 1.0 / float(hi[ti] - lo[ti])
        Am -= np.eye(64, dtype=np.float32)
        for r in range(2):
            AT[r * 64:(r + 1) * 64, g, r * 64:(r + 1) * 64] = Am.T
    SU = (k[:, None] < k[None, :]).astype(np.float32)
    iota1 = (np.arange(NE, dtype=np.float32) + 1.0).reshape(1, NE)
    jv = (np.arange(NBLK, dtype=np.float32) * float(BS)).reshape(1, NBLK)
    pidx = np.arange(128, dtype=np.float32).reshape(128, 1)
    return dict(cI=I, cU=U, cLo=Lo, cMask=mask, cSel=sel, cAT=AT, cSU=SU, cIota=iota1, cJv=jv, cPidx=pidx)


_NC_CACHE = {}


def kernel(x, c, ctx, c_ctx, w_mod, b_mod, norm1_w, norm2_w, w_in, conv_w, conv_b, dt_bias, a_log, d_skip,
           ssd_norm_w, pool_w, pool_scale, w_out, router_w, router_b, w1, b1, w2, b2, final_norm_w, _dbg=False, _upto='ALL', _cores=8):
    f = lambda a: np.ascontiguousarray(np.asarray(a, dtype=np.float32))
    x = f(x); c = f(c); ctx = f(ctx); c_ctx = f(c_ctx)
    shared = dict(_consts())
    shared["w_mod"] = f(w_mod[0])
    shared["bmodT"] = f(b_mod[0].reshape(48, 128).T)
    shared["bmodr"] = f(b_mod[0].reshape(1, -1))
    shared["n1T"] = f(norm1_w[0].reshape(8, 128).T); shared["n2T"] = f(norm2_w[0].reshape(8, 128).T)
    shared["fnw"] = f(final_norm_w.reshape(1, -1))
    shared["w_in"] = f(w_in[0])
    shared["convw"] = f(np.asarray(conv_w[0]).T.reshape(20, 128, 5).transpose(1, 0, 2))
    shared["convb"] = f(np.asarray(conv_b[0]).reshape(20, 128).T)
    shared["dtb"] = f(np.asarray(dt_bias[0]).reshape(1, 48)); shared["alog"] = f(np.asarray(a_log[0]).reshape(1, 48))
    shared["dvec"] = f(np.repeat(np.asarray(d_skip[0]), 64).reshape(1, -1))
    shared["ynw"] = f(np.asarray(ssd_norm_w[0]).reshape(12, 128).T)
    shared["poolw"] = f(np.asarray(pool_w[0]).transpose(1, 0, 2))
    shared["pscale"] = f(np.asarray(pool_scale[0]).reshape(4, 128).T)
    shared["w_out"] = f(w_out[0])
    shared["rw"] = f(np.asarray(router_w[0]).reshape(8, 128, NE).transpose(1, 0, 2))
    shared["rb"] = f(np.asarray(router_b[0]).reshape(1, NE))
    w1a = np.asarray(w1[0])

    def ptile(w):
        return f(w.reshape(NE, 8, 128, -1).transpose(0, 2, 1, 3).reshape(NE * 128, -1))
    shared["W1G"] = ptile(w1a[:, :, 0::2]); shared["W1U"] = ptile(w1a[:, :, 1::2])
    shared["W2"] = ptile(np.asarray(w2[0]))
    b1a = np.asarray(b1[0])
    b1g_ = b1a[:, 0::2].reshape(NE, 8, 128).transpose(0, 2, 1)
    b1u_ = b1a[:, 1::2].reshape(NE, 8, 128).transpose(0, 2, 1)
    shared["B1"] = f(np.concatenate([b1g_, b1u_], axis=2).reshape(NE * 128, 16))
    shared["b2"] = f(b2[0])
    shared["n2r"] = f(norm2_w[0].reshape(1, -1))
    if (_dbg, _upto) not in _NC_CACHE:
        _NC_CACHE[(_dbg, _upto)] = build(dbg=_dbg, upto=_upto)
    nc = _NC_CACHE[(_dbg, _upto)]
    in_maps = []
    for b in range(_cores):
        m = dict(shared)
        m["x"] = x[b]; m["ctx"] = ctx[b]
        cv = np.stack([c[b], c_ctx], axis=-1)
        m["cvec"] = f(cv.reshape(8, 128, 2).transpose(1, 0, 2))
        in_maps.append(m)
    res = run_bass_kernel_spmd(nc, in_maps, core_ids=list(range(_cores)))
    if _dbg:
        return res
    return np.stack([r["out"] for r in res.results], axis=0).astype(np.float32)
```
